# Optimizing a Trainium2 kernel written in Bass

```python
import math
import jax
import jax.numpy as jnp
from jax import lax
import numpy as np

D_MODEL = 1024
BATCH = 8
SEQ = 4096
DEPTH = 4

CTX_LEN = 256
GRID_W = 64
MIX_WIDTH = D_MODEL
ATTN_WIDTH = MIX_WIDTH // 2
SGU_WIDTH = MIX_WIDTH - ATTN_WIDTH
DIFF_HEAD_DIM = 64
DIFF_HEADS = ATTN_WIDTH // (2 * DIFF_HEAD_DIM)
SGU_HEADS = 8
SGU_HEAD_DIM = SGU_WIDTH // SGU_HEADS
CHUNK = 128
Q_BLOCK = 128
N_EXPERTS = 16
CAPACITY_FACTOR = 2
EXPERT_FF = 2 * D_MODEL
ROPE_THETA = 10000.0
EPS = 1e-6
PROJ_WIDTH = 3 * ATTN_WIDTH + 2 * SGU_WIDTH
SPLITS = (ATTN_WIDTH, 2 * ATTN_WIDTH, 3 * ATTN_WIDTH, 3 * ATTN_WIDTH + SGU_WIDTH)

kernel_name = 'hybrid_diffattn_sgu_ecmoe_dit'


def rmsnorm(x, g):
    xf = x.astype(jnp.float32)
    y = xf * lax.rsqrt(jnp.mean(xf * xf, axis=-1, keepdims=True) + EPS)
    return (y * g.astype(jnp.float32)).astype(x.dtype)


def modulate(h, shift, scale):
    return h * (1 + scale) + shift


def axial_rope_tables(n_tokens):
    rows = n_tokens // GRID_W
    row = jnp.repeat(jnp.arange(rows), GRID_W).astype(jnp.float32)
    col = jnp.tile(jnp.arange(GRID_W), rows).astype(jnp.float32)
    n_freq = DIFF_HEAD_DIM // 4
    freqs = ROPE_THETA ** (-jnp.arange(n_freq, dtype=jnp.float32) / n_freq)
    ang_r = row[:, None] * freqs
    ang_c = col[:, None] * freqs
    ang = jnp.concatenate([ang_r, ang_r, ang_c, ang_c], axis=-1)
    return jnp.cos(ang), jnp.sin(ang)


def apply_axial_rope(t, cos, sin):
    tr = t.reshape(t.shape[:-1] + (2, 2, DIFF_HEAD_DIM // 4))
    rot = jnp.concatenate([-tr[..., 1:2, :], tr[..., 0:1, :]], axis=-2).reshape(t.shape)
    cos = cos.astype(t.dtype)[:, None, None, :]
    sin = sin.astype(t.dtype)[:, None, None, :]
    return t * cos + rot * sin


def diff_attn_core(q, k, v, lam):
    s = jnp.einsum('bqchd,bkchd->bchqk', q, k).astype(jnp.float32)
    p = jax.nn.softmax(s, axis=-1)
    a = p[:, 0] - lam * p[:, 1]
    return jnp.einsum('bhqk,bkhe->bqhe', a.astype(v.dtype), v)


def spatial_gating(u, gv, norm_g, w_s, b_s):
    b_, n, _ = u.shape
    u = jax.nn.gelu(u, approximate=False)
    gv = jax.nn.gelu(gv, approximate=False).reshape(b_, n, SGU_HEADS, SGU_HEAD_DIM)
    gv = rmsnorm(gv, norm_g.reshape(SGU_HEADS, SGU_HEAD_DIM))
    gv = gv.reshape(b_, n // CHUNK, CHUNK, SGU_HEADS, SGU_HEAD_DIM)
    s = jnp.einsum('hpq,bnqhe->bnphe', w_s, gv) + b_s.T[:, :, None]
    return u * s.reshape(b_, n, SGU_WIDTH)


def ec_moe(h, w_router, w_gate, w_up, w_down):
    b_, n, d = h.shape
    cap = CAPACITY_FACTOR * n // N_EXPERTS
    aff = jax.nn.softmax(jnp.einsum('bnd,de->bne', h, w_router).astype(jnp.float32), axis=-1)
    g, idx = lax.top_k(jnp.swapaxes(aff, 1, 2), cap)
    xs = jax.vmap(lambda hb, ib: hb[ib])(h, idx)
    hid = jax.nn.silu(jnp.einsum('becd,edf->becf', xs, w_gate)) * jnp.einsum('becd,edf->becf', xs, w_up)
    y = jnp.einsum('becf,efd->becd', hid, w_down) * g[..., None].astype(h.dtype)
    return jax.vmap(lambda yb, ib: jnp.zeros((n, d), yb.dtype).at[ib.reshape(-1)].add(yb.reshape(-1, d)))(y, idx)


def hybrid_mixer(h_lat, h_ctx, w_in, w_out, lq1, lk1, lq2, lk2, subln_g, sgu_norm_g, sgu_w, sgu_b, cos, sin, lam_init, ctx_out):
    b_, n, _ = h_lat.shape
    m = h_ctx.shape[1]
    hd = DIFF_HEAD_DIM
    scale = hd ** -0.5
    lam = (jnp.exp(jnp.sum(lq1 * lk1).astype(jnp.float32))
           - jnp.exp(jnp.sum(lq2 * lk2).astype(jnp.float32)) + lam_init)
    q, k, v, u, gv = jnp.split(h_lat @ w_in, SPLITS, axis=-1)
    q = apply_axial_rope(q.reshape(b_, n, 2, DIFF_HEADS, hd), cos, sin) * scale
    k = apply_axial_rope(k.reshape(b_, n, 2, DIFF_HEADS, hd), cos, sin)
    v = v.reshape(b_, n, DIFF_HEADS, 2 * hd)
    if ctx_out:
        qc, kc, vc, uc, gvc = jnp.split(h_ctx @ w_in, SPLITS, axis=-1)
    else:
        kc, vc = jnp.split(h_ctx @ w_in[:, ATTN_WIDTH:3 * ATTN_WIDTH], 2, axis=-1)
    kc = kc.reshape(b_, m, 2, DIFF_HEADS, hd)
    vc = vc.reshape(b_, m, DIFF_HEADS, 2 * hd)
    k_all = jnp.concatenate([kc, k], axis=1)
    v_all = jnp.concatenate([vc, v], axis=1)
    qb = q.reshape(b_, n // Q_BLOCK, Q_BLOCK, 2, DIFF_HEADS, hd).transpose(1, 0, 2, 3, 4, 5)
    o = lax.map(lambda qblk: diff_attn_core(qblk, k_all, v_all, lam), qb)
    o = o.transpose(1, 0, 2, 3, 4).reshape(b_, n, DIFF_HEADS, 2 * hd)
    o = rmsnorm(o, subln_g) * (1 - lam_init)
    y_lat = jnp.concatenate([o.reshape(b_, n, ATTN_WIDTH),
                             spatial_gating(u, gv, sgu_norm_g, sgu_w, sgu_b)], axis=-1) @ w_out
    if not ctx_out:
        return y_lat, None
    qc = qc.reshape(b_, m, 2, DIFF_HEADS, hd) * scale
    oc = rmsnorm(diff_attn_core(qc, kc, vc, lam), subln_g) * (1 - lam_init)
    y_ctx = jnp.concatenate([oc.reshape(b_, m, ATTN_WIDTH),
                             spatial_gating(uc, gvc, sgu_norm_g, sgu_w, sgu_b)], axis=-1) @ w_out
    return y_lat, y_ctx


def setup_inputs(seed: int = 0) -> dict:
    key = jax.random.key(seed)
    ks = jax.random.split(key, 24)
    f32 = jnp.float32
    d = D_MODEL

    def nrm(k, shape, s):
        return jax.random.normal(k, shape, f32) * s

    return {
        'x': nrm(ks[0], (BATCH, SEQ, d), 1.0),
        'c': nrm(ks[1], (BATCH, d), 1.0),
        'ctx': nrm(ks[2], (BATCH, CTX_LEN, d), 1.0),
        'c_ctx': nrm(ks[3], (d,), 1.0),
        'w_ada': nrm(ks[4], (DEPTH, d, 6 * d), 0.5 * d ** -0.5),
        'b_ada': nrm(ks[5], (DEPTH, 6 * d), 0.02),
        'norm1_g': 1.0 + nrm(ks[6], (DEPTH, d), 0.02),
        'norm2_g': 1.0 + nrm(ks[7], (DEPTH, d), 0.02),
        'w_in': nrm(ks[8], (DEPTH, d, PROJ_WIDTH), d ** -0.5),
        'w_out': nrm(ks[9], (DEPTH, MIX_WIDTH, d), MIX_WIDTH ** -0.5),
        'lambda_q1': nrm(ks[10], (DEPTH, DIFF_HEAD_DIM), 0.1),
        'lambda_k1': nrm(ks[11], (DEPTH, DIFF_HEAD_DIM), 0.1),
        'lambda_q2': nrm(ks[12], (DEPTH, DIFF_HEAD_DIM), 0.1),
        'lambda_k2': nrm(ks[13], (DEPTH, DIFF_HEAD_DIM), 0.1),
        'subln_g': 1.0 + nrm(ks[14], (DEPTH, 2 * DIFF_HEAD_DIM), 0.02),
        'sgu_norm_g': 1.0 + nrm(ks[15], (DEPTH, SGU_WIDTH), 0.02),
        'sgu_w': nrm(ks[16], (DEPTH, SGU_HEADS, CHUNK, CHUNK), CHUNK ** -0.5),
        'sgu_b': 1.0 + nrm(ks[17], (DEPTH, SGU_HEADS, CHUNK), 0.02),
        'w_router': nrm(ks[18], (DEPTH, d, N_EXPERTS), d ** -0.5),
        'w_gate': nrm(ks[19], (DEPTH, N_EXPERTS, d, EXPERT_FF), d ** -0.5),
        'w_up': nrm(ks[20], (DEPTH, N_EXPERTS, d, EXPERT_FF), d ** -0.5),
        'w_down': nrm(ks[21], (DEPTH, N_EXPERTS, EXPERT_FF, d), EXPERT_FF ** -0.5),
        'norm_f_g': 1.0 + nrm(ks[22], (d,), 0.02),
    }


def reference(x, c, ctx, c_ctx, w_ada, b_ada, norm1_g, norm2_g, w_in, w_out, lambda_q1, lambda_k1, lambda_q2, lambda_k2, subln_g, sgu_norm_g, sgu_w, sgu_b, w_router, w_gate, w_up, w_down, norm_f_g):
    n = x.shape[1]
    cos, sin = axial_rope_tables(n)
    silu_c = jax.nn.silu(c)
    silu_cc = jax.nn.silu(c_ctx)
    cx = ctx
    for l in range(DEPTH):
        last = l == DEPTH - 1
        lam_init = 0.8 - 0.6 * math.exp(-0.3 * l)
        mod = silu_c @ w_ada[l] + b_ada[l]
        sh1, sc1, g1, sh2, sc2, g2 = [t[:, None, :] for t in jnp.split(mod, 6, axis=-1)]
        cmod = silu_cc @ w_ada[l] + b_ada[l]
        csh1, csc1, cg1, csh2, csc2, cg2 = jnp.split(cmod, 6, axis=-1)
        h_lat = modulate(rmsnorm(x, norm1_g[l]), sh1, sc1)
        h_ctx = modulate(rmsnorm(cx, norm1_g[l]), csh1, csc1)
        y_lat, y_ctx = hybrid_mixer(h_lat, h_ctx, w_in[l], w_out[l], lambda_q1[l], lambda_k1[l],
                                    lambda_q2[l], lambda_k2[l], subln_g[l], sgu_norm_g[l], sgu_w[l],
                                    sgu_b[l], cos, sin, lam_init, not last)
        x = x + g1 * y_lat
        x = x + g2 * ec_moe(modulate(rmsnorm(x, norm2_g[l]), sh2, sc2),
                            w_router[l], w_gate[l], w_up[l], w_down[l])
        if not last:
            cx = cx + cg1 * y_ctx
            cx = cx + cg2 * ec_moe(modulate(rmsnorm(cx, norm2_g[l]), csh2, csc2),
                                   w_router[l], w_gate[l], w_up[l], w_down[l])
    return rmsnorm(x, norm_f_g)
```

```python
import math
import numpy as np
from contextlib import ExitStack
import concourse.bass as bass
import concourse.mybir as mybir
from concourse.bass_utils import run_bass_kernel_spmd

F32 = mybir.dt.float32; BF16 = mybir.dt.bfloat16; I32 = mybir.dt.int32; U32 = mybir.dt.uint32
AF = mybir.ActivationFunctionType; ALU = mybir.AluOpType; AX = mybir.AxisListType

D = 1024; SEQ = 4096; CTX = 256; T = SEQ + CTX; NT = T // 128; NLT = SEQ // 128
DEPTH = 4; NE = 16; CAP = 512; CCAP = 32; FF = 2048
EPS = 1e-6


class Sem:
    def __init__(self, h, name):
        self.h = h; self.name = name; self.val = 0


class Buf:
    def __init__(self, name, sem=None):
        self.name = name; self.w = None; self.r = {}; self.sem = sem


class Call:
    def __init__(self, name, a, k):
        self.name = name; self.a = a; self.k = k

    def run(self, e):
        return getattr(e, self.name)(*self.a, **self.k)


class Rec:
    def __getattr__(self, name):
        return lambda *a, **k: Call(name, a, k)


REC = Rec()


class Eng:
    def __init__(self, prog, name, sem):
        self.prog = prog; self.name = name; self.sem = sem; self.q = []; self.waited = {}

    def wait_tok(self, sem, val):
        if self.name == 'pe' and sem is self.sem:
            return
        if self.waited.get(sem, 0) >= val:
            return
        self.waited[sem] = val
        h = sem.h
        self.q.append(lambda e, h=h, val=val: e.wait_ge(h, val))

    def deps(self, reads, writes, pe_accum=False):
        for b in reads:
            if b.w is not None:
                self.wait_tok(*b.w)
        for b in writes:
            if pe_accum:
                continue
            if b.w is not None:
                self.wait_tok(*b.w)
            for s, v in b.r.items():
                self.wait_tok(s, v)

    def mark(self, tok, reads, writes):
        s, v = tok
        for b in reads:
            if b.r.get(s, 0) < v:
                b.r[s] = v
        for b in writes:
            b.w = tok; b.r = {}

    def op(self, fn, reads=(), writes=(), pe_accum=False):
        self.deps(reads, writes, pe_accum)
        self.sem.val += 1
        tok = (self.sem, self.sem.val)
        h = self.sem.h
        call = fn(REC)
        self.q.append(lambda e, call=call, h=h: call.run(e).then_inc(h, 1))
        self.mark(tok, reads, writes)
        return tok

    def dma(self, fn, sem, reads=(), writes=()):
        fns = fn if isinstance(fn, (list, tuple)) else [fn]
        self.deps(reads, writes)
        h = sem.h
        for f in fns:
            sem.val += 16
            call = f(REC)
            self.q.append(lambda e, call=call, h=h: call.run(e).then_inc(h, 16))
        tok = (sem, sem.val)
        self.mark(tok, reads, writes)
        return tok


class Prog:
    def __init__(self, nc):
        self.nc = nc; self.es = ExitStack(); self.sems = []; self.semcache = {}
        self.sp = Eng(self, 'sp', self.new_sem('e_sp'))
        self.act = Eng(self, 'act', self.new_sem('e_act'))
        self.pool = Eng(self, 'pool', self.new_sem('e_pool'))
        self.dve = Eng(self, 'dve', self.new_sem('e_dve'))
        self.pe = Eng(self, 'pe', self.new_sem('e_pe'))
        self.engs = [self.sp, self.act, self.pool, self.dve, self.pe]

    def new_sem(self, name):
        s = Sem(self.es.enter_context(self.nc.semaphore(name)), name)
        self.sems.append(s)
        return s

    def sbuf(self, name, shape, dt):
        return self.es.enter_context(self.nc.sbuf_tensor(name, shape, dt))

    def psum(self, name, shape, dt):
        return self.es.enter_context(self.nc.psum_tensor(name, shape, dt))

    def buf(self, name, dma=False):
        if not dma:
            return Buf(name, None)
        if name not in self.semcache:
            self.semcache[name] = self.new_sem('b_' + name)
        return Buf(name, self.semcache[name])

    def barrier(self):
        for e in self.engs:
            for s in self.sems:
                if s.val > 0:
                    e.wait_tok(s, s.val)

    def finish(self):
        self.barrier()
        with self.nc.Block() as block:
            @block.sync
            def _(e):
                for f in self.sp.q: f(e)
            @block.scalar
            def _(e):
                for f in self.act.q: f(e)
            @block.gpsimd
            def _(e):
                for f in self.pool.q: f(e)
            @block.vector
            def _(e):
                for f in self.dve.q: f(e)
            @block.tensor
            def _(e):
                for f in self.pe.q: f(e)
        self.es.close()


class Arena:
    def __init__(self, P, name, ncols, dt):
        self.t = P.sbuf(name, [128, ncols], dt); self.off = 0; self.n = ncols; self.name = name

    def alloc(self, cols):
        a = self.off; self.off += cols
        assert self.off <= self.n, (self.name, self.off, self.n)
        return self.t[:, a:a + cols]

    def reset(self):
        self.off = 0


def v3(ap, a):
    return ap.rearrange("p (a b) -> p a b", a=a)


def build(n_layers=DEPTH, final_norm=True):
    nc = bass.Bass("TRN2", target_bir_lowering=False)
    dram = lambda name, shape, dt, kind="ExternalInput": nc.dram_tensor(name, shape, dt, kind=kind).ap()
    x_in = dram("x", [T, D], F32)
    cc_d = dram("cc", [128, 16], F32)
    rope_d = dram("rope", [T, 128], F32)
    ident_d = dram("ident", [128, 128], F32)
    w_ada = dram("w_ada", [DEPTH, D, 6 * D], F32)
    b_ada_col = dram("b_ada_col", [DEPTH, 128, 48], F32)
    b_ada = dram("b_ada", [DEPTH, 6 * D], F32)
    n1col = dram("n1col", [DEPTH, 128, 8], F32)
    n2col = dram("n2col", [DEPTH, 128, 8], F32)
    w_in = dram("w_in", [DEPTH, D, 2560], F32)
    w_out = dram("w_out", [DEPTH, D, D], F32)
    lamv = dram("lamv", [DEPTH, 256], F32)
    subln_g = dram("subln_g", [DEPTH, 128], F32)
    sgu_norm_g = dram("sgu_norm_g", [DEPTH, 512], F32)
    sgu_w = dram("sgu_w", [DEPTH, 8, 128, 128], F32)
    sgu_bT = dram("sgu_bT", [DEPTH, 128, 8], F32)
    w_router = dram("w_router", [DEPTH, D, NE], F32)
    w_gate = dram("w_gate", [DEPTH, NE, D, FF], F32)
    w_up = dram("w_up", [DEPTH, NE, D, FF], F32)
    w_down = dram("w_down", [DEPTH, NE, FF, D], F32)
    norm_f_g = dram("norm_f_g", [1, D], F32)
    out_d = dram("out", [SEQ, D], F32, kind="ExternalOutput")
    xr = dram("xr", [T, D], F32, kind="Internal")
    qd = dram("qd", [T, 512], BF16, kind="Internal")
    sgd = dram("sgd", [T, 512], BF16, kind="Internal")
    h2d = dram("h2d", [T, D], BF16, kind="Internal")

    P = Prog(nc)
    sp, act, pool, dve, pe = P.sp, P.act, P.pool, P.dve, P.pe

    ident = P.sbuf("ident_sb", [128, 128], F32); b_ident = P.buf("ident", True)
    identb = P.sbuf("identb_sb", [128, 128], BF16); b_identb = P.buf("identb")
    grow = P.sbuf("grow", [128, 4 * D], F32); b_grow = P.buf("grow")
    siluc = P.sbuf("siluc", [128, 16], F32); b_siluc = P.buf("siluc", True)
    modT = P.sbuf("modT", [128, 96], F32); b_modT = P.buf("modT")
    bcol = P.sbuf("bcol", [128, 48], F32); b_bcol = P.buf("bcol", True)
    ncol = P.sbuf("ncol", [128, 16], F32); b_ncol = P.buf("ncol", True)
    AB = P.sbuf("ABcols", [128, 32], F32); b_AB = P.buf("ABc")
    lamt = P.sbuf("lamt", [128, 8], F32); b_lamt = P.buf("lamt")
    lv = P.sbuf("lv", [128, 256], F32); b_lv = P.buf("lv", True)
    lprod = P.sbuf("lprod", [128, 128], F32); b_lprod = P.buf("lprod")
    gsub = P.sbuf("gsub", [128, 128], F32); b_gsub = P.buf("gsub", True)
    sgng = P.sbuf("sgng", [128, 512], F32); b_sgng = P.buf("sgng", True)
    sbT = P.sbuf("sbT", [128, 8], F32); b_sbT = P.buf("sbT", True)
    bfull = P.sbuf("bfull", [128, 512], F32); b_bfull = P.buf("bfull")
    wsT = P.sbuf("wsT", [128, 8 * 128], BF16); b_wsT = P.buf("wsT")
    wrt = P.sbuf("wrt", [128, 8 * NE], F32); b_wrt = P.buf("wrt", True)
    small = P.sbuf("small", [128, 64], F32)
    epsc = P.sbuf("epsc", [128, 1], F32); b_epsc = P.buf("epsc")
    dve.op(lambda e: e.memset(epsc[:], EPS), writes=[b_epsc])

    def rstd_op(dst, src, scale, bufs):
        act.op(lambda e: e.activation(out=dst, in_=src, func=AF.Ln, scale=float(scale), bias=epsc[:, 0:1]), reads=bufs + [b_epsc], writes=bufs)
        act.op(lambda e: e.activation(out=dst, in_=dst, func=AF.Exp, scale=-0.5), reads=bufs, writes=bufs)

    AFa = Arena(P, "arenaF", 12928, F32)
    ABa = Arena(P, "arenaB", 58880, BF16)

    pb = [P.psum("pb%d" % i, [128, 512], F32) for i in range(5)]
    pbh = P.psum("pbh", [128, 1024], BF16)
    pbb = P.psum("pbb", [128, 1024], F32)
    b_pb = [P.buf("pb%d" % i) for i in range(5)]
    b_pbh = P.buf("pbh"); b_pbb = P.buf("pbb")

    sp.dma(lambda e: e.dma_start(out=ident[:], in_=ident_d), b_ident.sem, writes=[b_ident])
    dve.op(lambda e: e.tensor_copy(out=identb[:], in_=ident[:]), reads=[b_ident], writes=[b_identb])
    sp.dma(lambda e: e.dma_start(out=siluc[:], in_=cc_d), b_siluc.sem, writes=[b_siluc])
    act.op(lambda e: e.activation(out=siluc[:], in_=siluc[:], func=AF.Silu), reads=[b_siluc], writes=[b_siluc])
    b_xr = P.buf("xr", True)
    sp.dma([lambda e, i=i: e.dma_start(out=xr[i * (T // 4):(i + 1) * (T // 4), :], in_=x_in[i * (T // 4):(i + 1) * (T // 4), :]) for i in range(4)],
           b_xr.sem, writes=[b_xr])
    P.barrier()

    for l in range(n_layers):
        last = (l == DEPTH - 1)
        lam_init = 0.8 - 0.6 * math.exp(-0.3 * l)
        NTq = NLT if last else NT
        AFa.reset(); ABa.reset()
        wa = [AFa.alloc(8 * 512) for _ in range(2)]; b_wa = [P.buf("wa_%d" % i, True) for i in range(2)]
        brow = AFa.alloc(512); b_brow = P.buf("brow", True)
        swl = AFa.alloc(8 * 128); b_swl = P.buf("swl", True)
        srep = AFa.alloc(16 * 128); b_srep = P.buf("srep%d" % l)
        for ks in range(16):
            dve.op(lambda e, ks=ks: e.tensor_copy(out=srep[:, ks * 128:(ks + 1) * 128], in_=siluc[:, ks:ks + 1].to_broadcast([128, 128])),
                   reads=[b_siluc], writes=[b_srep])
        sp.dma(lambda e: e.dma_start(out=bcol[:], in_=b_ada_col[l]), b_bcol.sem, writes=[b_bcol])
        sp.dma([lambda e: e.dma_start(out=ncol[:, 0:8], in_=n1col[l]), lambda e: e.dma_start(out=ncol[:, 8:16], in_=n2col[l])], b_ncol.sem, writes=[b_ncol])
        sp.dma(lambda e: e.dma_start(out=lv[:], in_=lamv[l:l + 1, :].to_broadcast([128, 256])), b_lv.sem, writes=[b_lv])
        sp.dma(lambda e: e.dma_start(out=gsub[:], in_=subln_g[l:l + 1, :].to_broadcast([128, 128])), b_gsub.sem, writes=[b_gsub])
        sp.dma(lambda e: e.dma_start(out=sgng[:], in_=sgu_norm_g[l:l + 1, :].to_broadcast([128, 512])), b_sgng.sem, writes=[b_sgng])
        sp.dma(lambda e: e.dma_start(out=sbT[:], in_=sgu_bT[l]), b_sbT.sem, writes=[b_sbT])
        sp.dma(lambda e: e.dma_start(out=wrt[:].rearrange("p (k n) -> p k n", k=8), in_=w_router[l].rearrange("(k p) n -> p k n", p=128)),
               b_wrt.sem, writes=[b_wrt])
        dve.op(lambda e: e.tensor_tensor(out=v3(lprod[:], 2), in0=v3(lv[:], 2)[:, :, 0:64], in1=v3(lv[:], 2)[:, :, 64:128], op=ALU.mult),
               reads=[b_lv], writes=[b_lprod])
        dve.op(lambda e: e.tensor_reduce(out=lamt[:, 0:2], in_=v3(lprod[:], 2), axis=AX.X, op=ALU.add), reads=[b_lprod], writes=[b_lamt])
        act.op(lambda e: e.activation(out=lamt[:, 0:2], in_=lamt[:, 0:2], func=AF.Exp), reads=[b_lamt], writes=[b_lamt])
        dve.op(lambda e: e.tensor_tensor(out=lamt[:, 2:3], in0=lamt[:, 0:1], in1=lamt[:, 1:2], op=ALU.subtract), reads=[b_lamt], writes=[b_lamt])
        dve.op(lambda e: e.tensor_scalar(out=lamt[:, 2:3], in0=lamt[:, 2:3], scalar1=float(lam_init), scalar2=None, op0=ALU.add), reads=[b_lamt], writes=[b_lamt])
        dve.op(lambda e: e.tensor_scalar(out=lamt[:, 3:4], in0=lamt[:, 2:3], scalar1=-1.0, scalar2=None, op0=ALU.mult), reads=[b_lamt], writes=[b_lamt])
        dve.op(lambda e: e.tensor_scalar(out=gsub[:], in0=gsub[:], scalar1=float(1.0 - lam_init), scalar2=None, op0=ALU.mult), reads=[b_gsub], writes=[b_gsub])
        dve.op(lambda e: e.tensor_copy(out=v3(bfull[:], 8), in_=sbT[:].unsqueeze(2).to_broadcast([128, 8, 64])), reads=[b_sbT], writes=[b_bfull])
        for hh in range(2):
            sp.dma(lambda e, hh=hh: e.dma_start(out=v3(swl, 8)[:, 0:4, :], in_=sgu_w[l, hh * 4:(hh + 1) * 4].rearrange("h p q -> p h q")),
                   b_swl.sem, writes=[b_swl])
            for h4 in range(4):
                pe.op(lambda e, h4=h4: e.transpose(out=pbb[:, h4 * 128:(h4 + 1) * 128], in_=v3(swl, 8)[:, h4, :], identity=ident[:]),
                      reads=[b_swl, b_ident], writes=[b_pbb], pe_accum=(h4 > 0))
            act.op(lambda e, hh=hh: e.activation(out=wsT[:, hh * 512:(hh + 1) * 512], in_=pbb[:, 0:512], func=AF.Copy), reads=[b_pbb], writes=[b_wsT])
        for hg in range(12):
            g = hg // 2; half = hg % 2
            wb_ = wa[hg % 2]; bw = b_wa[hg % 2]
            sp.dma(lambda e, wb_=wb_, hg=hg: e.dma_start(out=v3(wb_, 8), in_=w_ada[l].rearrange("(k p) n -> p k n", p=128)[:, :, hg * 512:(hg + 1) * 512]),
                   bw.sem, writes=[bw])
            for nn in range(4):
                n = hg * 4 + nn
                for k in range(8):
                    pe.op(lambda e, wb_=wb_, nn=nn, k=k, n=n: e.matmul(pb[3][:, 2 * n:2 * n + 2], lhsT=v3(wb_, 8)[:, k, nn * 128:(nn + 1) * 128],
                                                                      rhs=siluc[:, 2 * k:2 * k + 2], start=(k == 0), stop=(k == 7)),
                          reads=[bw, b_siluc], writes=[b_pb[3]], pe_accum=not (hg == 0 and nn == 0 and k == 0))
            if g in (2, 5):
                which = 0 if g == 2 else 1
                sp.dma(lambda e, g=g, half=half: e.dma_start(out=brow, in_=b_ada[l:l + 1, g * 1024 + half * 512: g * 1024 + (half + 1) * 512].to_broadcast([128, 512])),
                       b_brow.sem, writes=[b_brow])
                for s in range(2):
                    for k in range(8):
                        pe.op(lambda e, wb_=wb_, s=s, k=k: e.matmul(pb[s][:, :], lhsT=srep[:, (2 * k + s) * 128:(2 * k + s + 1) * 128], rhs=v3(wb_, 8)[:, k, :],
                                                                    start=(k == 0), stop=(k == 7)),
                              reads=[bw, b_srep], writes=[b_pb[s]], pe_accum=(k > 0))
                    c0 = (s * 2 + which) * D + half * 512
                    dve.op(lambda e, s=s, c0=c0: e.tensor_tensor(out=grow[:, c0:c0 + 512], in0=pb[s][:, :], in1=brow, op=ALU.add),
                           reads=[b_pb[s], b_brow], writes=[b_grow])
        dve.op(lambda e: e.tensor_tensor(out=v3(modT[:], 48), in0=v3(pb[3][:, 0:96], 48), in1=bcol[:].unsqueeze(2).to_broadcast([128, 48, 2]), op=ALU.add),
               reads=[b_pb[3], b_bcol], writes=[b_modT])
        mT = v3(modT[:], 48)
        for w_ in range(2):
            for s in range(2):
                sc = mT[:, (1 + 3 * w_) * 8:(2 + 3 * w_) * 8, s]
                o = AB[:, (w_ * 2 + s) * 8:(w_ * 2 + s + 1) * 8]
                dve.op(lambda e, sc=sc, o=o, w_=w_: e.scalar_tensor_tensor(out=o, in0=sc, scalar=1.0, in1=ncol[:, w_ * 8:(w_ + 1) * 8], op0=ALU.add, op1=ALU.mult),
                       reads=[b_modT, b_ncol], writes=[b_AB])

        def Acol(w_, s, k): return AB[:, (w_ * 2 + s) * 8 + k:(w_ * 2 + s) * 8 + k + 1]
        def Bcol(w_, s, k): return modT[:, ((3 * w_) * 8 + k) * 2 + s:((3 * w_) * 8 + k) * 2 + s + 1]
        def growv(s, which): return grow[:, (s * 2 + which) * D:(s * 2 + which + 1) * D]
        P.barrier()

        AFa.reset(); ABa.reset()
        kT = ABa.alloc(4 * T); b_kT = P.buf("kT%d" % l)
        vaug = ABa.alloc(NT * 4 * 130); b_vaug = P.buf("vaug%d" % l)
        ab_mark = ABa.off
        w_in_sb = ABa.alloc(8 * 2560); b_win = P.buf("win", True)
        hT = ABa.alloc(8 * 128); b_hT = P.buf("hT%d" % l)
        qr = ABa.alloc(512); b_qr = P.buf("qr", True)
        kr = ABa.alloc(512); b_kr = P.buf("kr%d" % l)
        gvn = ABa.alloc(512); b_gvn = P.buf("gvn%d" % l)
        sgo = ABa.alloc(512); b_sgo = P.buf("sgo", True)
        xt = [AFa.alloc(D) for _ in range(2)]; b_xt = [P.buf("xt_%d" % i, True) for i in range(2)]
        rp = [AFa.alloc(128) for _ in range(2)]; b_rp = [P.buf("rp_%d" % i, True) for i in range(2)]
        xn = AFa.alloc(D); b_xn = P.buf("xn%d" % l)
        junk = AFa.alloc(D); b_junk = P.buf("junk%d" % l)
        t1 = AFa.alloc(512); b_t1 = P.buf("t1%d" % l)
        t2 = AFa.alloc(512); b_t2 = P.buf("t2%d" % l)
        u_sb = AFa.alloc(512); b_u = P.buf("u%d" % l)
        gvg = AFa.alloc(512); b_gvg = P.buf("gvg%d" % l)
        sq = AFa.alloc(512); b_sq = P.buf("sq%d" % l)
        st = small[:, 0:32]; b_st = P.buf("st%d" % l)
        pool.dma([lambda e, i=i: e.dma_start(out=v3(w_in_sb, 8)[:, 2 * i:2 * i + 2, :], in_=w_in[l].rearrange("(k p) n -> p k n", p=128)[:, 2 * i:2 * i + 2, :]) for i in range(4)],
                 b_win.sem, writes=[b_win])
        va4 = vaug.rearrange("p (t h c) -> p t h c", t=NT, h=4)
        pool.op(lambda e: e.memset(vaug, 1.0), writes=[b_vaug])

        def load_tile(t):
            i = t % 2
            sp.dma(lambda e, t=t, i=i: e.dma_start(out=xt[i], in_=xr[t * 128:(t + 1) * 128, :]), b_xt[i].sem, writes=[b_xt[i]])
            sp.dma(lambda e, t=t, i=i: e.dma_start(out=rp[i], in_=rope_d[t * 128:(t + 1) * 128, :]), b_rp[i].sem, writes=[b_rp[i]])

        def rmsnorm_T(xsrc, b_xsrc, w_, s, dst, b_dst, dst_is_f32=False):
            pool.op(lambda e: e.memset(st[:, 0:1], 0.0), writes=[b_st])
            act.op(lambda e: e.activation(out=junk, in_=xsrc, func=AF.Square, accum_out=st[:, 0:1]), reads=[b_xsrc, b_st], writes=[b_junk, b_st])
            rstd_op(st[:, 1:2], st[:, 0:1], 1.0 / D, [b_st])
            dve.op(lambda e: e.tensor_scalar(out=xn, in0=xsrc, scalar1=st[:, 1:2], scalar2=None, op0=ALU.mult), reads=[b_xsrc, b_st], writes=[b_xn])
            for k in range(8):
                pe.op(lambda e, k=k: e.transpose(out=pbb[:, k * 128:(k + 1) * 128], in_=xn[:, k * 128:(k + 1) * 128], identity=ident[:]),
                      reads=[b_xn, b_ident], writes=[b_pbb], pe_accum=(k > 0))
            for k in range(8):
                act.op(lambda e, k=k: e.activation(out=dst[:, k * 128:(k + 1) * 128], in_=pbb[:, k * 128:(k + 1) * 128], func=AF.Identity,
                                                   scale=Acol(w_, s, k), bias=Bcol(w_, s, k)),
                       reads=[b_pbb, b_AB, b_modT], writes=[b_dst])

        def rope(src_ps, b_src, rp_, b_rp_, dst, b_dst):
            s5 = src_ps.rearrange("p (g a r f) -> p g a r f", g=8, a=2, r=2)
            t5 = t2.rearrange("p (g a r f) -> p g a r f", g=8, a=2, r=2)
            sn = rp_[:, 64:128].rearrange("p (a r f) -> p a r f", a=2, r=2)
            dve.op(lambda e: e.tensor_tensor(out=v3(t1, 8), in0=v3(src_ps, 8), in1=rp_[:, 0:64].unsqueeze(1).to_broadcast([128, 8, 64]), op=ALU.mult),
                   reads=[b_src, b_rp_], writes=[b_t1])
            for a in range(2):
                for r in range(2):
                    dve.op(lambda e, a=a, r=r: e.tensor_tensor(out=t5[:, :, a, r, :], in0=s5[:, :, a, 1 - r, :],
                                                               in1=sn[:, a, r, :].unsqueeze(1).to_broadcast([128, 8, 16]), op=ALU.mult),
                           reads=[b_src, b_rp_], writes=[b_t2])
            dve.op(lambda e: e.tensor_tensor(out=dst, in0=t1, in1=t2, op=ALU.add), reads=[b_t1, b_t2], writes=[b_dst])

        load_tile(0)
        for t in range(NT):
            s = 0 if t < NLT else 1
            i = t % 2
            if t + 1 < NT:
                load_tile(t + 1)
            need_q = t < NTq
            rmsnorm_T(xt[i], b_xt[i], 0, s, hT, b_hT)
            groups = [0, 1, 2, 3, 4] if need_q else [1, 2]
            for gi, g in enumerate(groups):
                pbk = pb[gi % 2]; bpbk = b_pb[gi % 2]
                for k in range(8):
                    pe.op(lambda e, k=k, g=g, pbk=pbk: e.matmul(pbk[:, :], lhsT=hT[:, k * 128:(k + 1) * 128], rhs=v3(w_in_sb, 8)[:, k, g * 512:(g + 1) * 512],
                                                                start=(k == 0), stop=(k == 7)),
                          reads=[b_hT, b_win], writes=[bpbk], pe_accum=(k > 0))
                if g == 0:
                    rope(pbk[:, :], bpbk, rp[i], b_rp[i], qr, b_qr)
                    sp.dma(lambda e, t=t: e.dma_start(out=qd[t * 128:(t + 1) * 128, :], in_=qr), b_qr.sem, reads=[b_qr])
                elif g == 1:
                    rope(pbk[:, :], bpbk, rp[i], b_rp[i], kr, b_kr)
                    for j in range(4):
                        pe.op(lambda e, j=j: e.transpose(out=pbh[:, j * 128:(j + 1) * 128], in_=kr[:, j * 128:(j + 1) * 128], identity=identb[:]),
                              reads=[b_kr, b_identb], writes=[b_pbh], pe_accum=(j > 0))
                    act.op(lambda e, t=t: e.activation(out=v3(kT, 4)[:, :, t * 128:(t + 1) * 128], in_=v3(pbh[:, 0:512], 4), func=AF.Copy),
                           reads=[b_pbh], writes=[b_kT])
                elif g == 2:
                    act.op(lambda e, t=t, pbk=pbk: e.activation(out=va4[:, t, :, 0:128], in_=v3(pbk[:, :], 4), func=AF.Copy), reads=[bpbk], writes=[b_vaug])
                elif g == 3:
                    act.op(lambda e, pbk=pbk: e.activation(out=u_sb, in_=pbk[:, :], func=AF.Gelu), reads=[bpbk], writes=[b_u])
                elif g == 4:
                    act.op(lambda e, pbk=pbk: e.activation(out=gvg, in_=pbk[:, :], func=AF.Gelu), reads=[bpbk], writes=[b_gvg])
                    dve.op(lambda e: e.tensor_tensor(out=sq, in0=gvg, in1=gvg, op=ALU.mult), reads=[b_gvg], writes=[b_sq])
                    dve.op(lambda e: e.tensor_reduce(out=st[:, 8:16], in_=v3(sq, 8), axis=AX.X, op=ALU.add), reads=[b_sq], writes=[b_st])
                    rstd_op(st[:, 16:24], st[:, 8:16], 1.0 / 64, [b_st])
                    dve.op(lambda e: e.tensor_tensor(out=v3(sq, 8), in0=v3(gvg, 8), in1=st[:, 16:24].unsqueeze(2).to_broadcast([128, 8, 64]), op=ALU.mult),
                           reads=[b_gvg, b_st], writes=[b_sq])
                    dve.op(lambda e: e.tensor_tensor(out=gvn, in0=sq, in1=sgng[:], op=ALU.mult), reads=[b_sq, b_sgng], writes=[b_gvn])
                    for h in range(8):
                        pe.op(lambda e, h=h: e.matmul(pb[2][:, h * 64:(h + 1) * 64], lhsT=wsT[:, h * 128:(h + 1) * 128], rhs=gvn[:, h * 64:(h + 1) * 64],
                                                      start=True, stop=True),
                              reads=[b_wsT, b_gvn], writes=[b_pb[2]], pe_accum=(h > 0))
                    dve.op(lambda e: e.tensor_tensor(out=sq, in0=pb[2][:, :], in1=bfull[:], op=ALU.add), reads=[b_pb[2], b_bfull], writes=[b_sq])
                    dve.op(lambda e: e.tensor_tensor(out=sgo, in0=sq, in1=u_sb, op=ALU.mult), reads=[b_sq, b_u], writes=[b_sgo])
                    sp.dma(lambda e, t=t: e.dma_start(out=sgd[t * 128:(t + 1) * 128, :], in_=sgo), b_sgo.sem, reads=[b_sgo])
        P.barrier()

        AFa.reset(); ABa.off = ab_mark
        w_out_sb = ABa.alloc(8 * D); b_wout = P.buf("wout", True)
        qg = ABa.alloc(4 * 512); b_qg = P.buf("qg", True)
        qTm = ABa.alloc(8 * 512); b_qTm = P.buf("qTm%d" % l)
        pT = [ABa.alloc(512) for _ in range(3)]; b_pT = [P.buf("pT%d_%d" % (l, i)) for i in range(3)]
        mixo = [ABa.alloc(512) for _ in range(4)]; b_mixo = [P.buf("mixo%d_%d" % (l, i)) for i in range(4)]
        mixT = ABa.alloc(8 * 128); b_mixT = P.buf("mixT%d" % l)
        sgi = [ABa.alloc(512) for _ in range(2)]; b_sgi = [P.buf("sgi_%d" % i, True) for i in range(2)]
        h2 = ABa.alloc(D); b_h2 = P.buf("h2", True)
        affT = AFa.alloc(T); b_affT = P.buf("affT%d" % l)
        af_mark = AFa.off
        xt = [AFa.alloc(D) for _ in range(2)]; b_xt = [P.buf("xt_%d" % i, True) for i in range(2)]
        x1 = AFa.alloc(D); b_x1 = P.buf("x1", True)
        xn = AFa.alloc(D); b_xn = P.buf("xnc%d" % l)
        junk = AFa.alloc(D); b_junk = P.buf("junkc%d" % l)
        h2T = AFa.alloc(8 * 128); b_h2T = P.buf("h2T%d" % l)
        accS = AFa.alloc(8 * 130); b_accS = P.buf("accS%d" % l)
        t1 = AFa.alloc(128); b_t1 = P.buf("t1c%d" % l)
        osb = AFa.alloc(128); b_osb = P.buf("osb%d" % l)
        af = AFa.alloc(NE); b_af = P.buf("af%d" % l)
        st = small[:, 0:32]; b_st = P.buf("stc%d" % l)
        pool.dma(lambda e: e.dma_start(out=v3(w_out_sb, 8), in_=w_out[l].rearrange("(k p) n -> p k n", p=128)), b_wout.sem, writes=[b_wout])
        pool.op(lambda e: e.memset(qTm, 0.0), writes=[b_qTm])

        def acc_ap(a):
            bank = 2 + a // 3; off = (a % 3) * 160
            return pb[bank][:, off:off + 130], b_pb[bank]

        pti = [0]
        qgroups = [(G * 4, 4) for G in range(8)] + ([] if last else [(NLT, 2)])
        for (t0, nq) in qgroups:
            s = 0 if t0 < NLT else 1
            NQ = nq * 128
            kts = list(range(NT)) if s == 0 else [NLT, NLT + 1]
            sp.dma(lambda e, t0=t0, nq=nq: e.dma_start(out=v3(qg, 4)[:, 0:nq, :], in_=qd[t0 * 128:(t0 + nq) * 128, :].rearrange("(a p) n -> p a n", p=128)),
                   b_qg.sem, writes=[b_qg])
            for qi in range(nq):
                for j in range(4):
                    pe.op(lambda e, qi=qi, j=j: e.transpose(out=pbh[:, j * 128:(j + 1) * 128], in_=v3(qg, 4)[:, qi, j * 128:(j + 1) * 128], identity=identb[:]),
                          reads=[b_qg, b_identb], writes=[b_pbh], pe_accum=(j > 0))
                q8 = v3(qTm, 8)
                for hf in range(2):
                    dst = qTm.rearrange("p (j f q) -> p j f q", j=4, f=2)[hf * 64:(hf + 1) * 64, :, hf, qi * 128:(qi + 1) * 128]
                    src = v3(pbh[:, 0:512], 4)[hf * 64:(hf + 1) * 64, :, :]
                    act.op(lambda e, dst=dst, src=src: e.activation(out=dst, in_=src, func=AF.Copy), reads=[b_pbh], writes=[b_qTm])
            for h in range(4):
                for c in range(2):
                    i8 = c * 4 + h
                    j = i8 // 2
                    for ki, kt in enumerate(kts):
                        sb = ki % 2
                        pe.op(lambda e, j=j, kt=kt, i8=i8, sb=sb, NQ=NQ: e.matmul(pb[sb][:, 0:NQ], lhsT=v3(kT, 4)[:, j, kt * 128:(kt + 1) * 128],
                                                                               rhs=v3(qTm, 8)[:, i8, 0:NQ], start=True, stop=True),
                              reads=[b_kT, b_qTm], writes=[b_pb[sb]])
                        pi = pti[0] % 3; pti[0] += 1
                        act.op(lambda e, sb=sb, pi=pi, NQ=NQ: e.activation(out=pT[pi][:, 0:NQ], in_=pb[sb][:, 0:NQ], func=AF.Exp, scale=0.125),
                               reads=[b_pb[sb]], writes=[b_pT[pi]])
                        for qi in range(nq):
                            aap, bacc = acc_ap(c * 4 + qi)
                            pe.op(lambda e, aap=aap, pi=pi, qi=qi, kt=kt, h=h, ki=ki: e.matmul(aap, lhsT=pT[pi][:, qi * 128:(qi + 1) * 128], rhs=va4[:, kt, h, :],
                                                                                         start=(ki == 0), stop=(ki == len(kts) - 1)),
                                  reads=[b_pT[pi], b_vaug], writes=[bacc], pe_accum=(ki > 0))
                a8 = v3(accS, 8)
                for bank in range(3):
                    na = 3 if bank < 2 else 2
                    src = pb[2 + bank][:, 0:na * 160].rearrange("p (a c) -> p a c", a=na)[:, :, 0:130]
                    dve.op(lambda e, bank=bank, na=na, src=src: e.tensor_copy(out=a8[:, bank * 3:bank * 3 + na, :], in_=src), reads=[b_pb[2 + bank]], writes=[b_accS])
                for qi in range(nq):
                    a0 = a8[:, qi, :]; a1 = a8[:, 4 + qi, :]
                    dve.op(lambda e, a0=a0: e.reciprocal(out=st[:, 2:3], in_=a0[:, 128:129]), reads=[b_accS], writes=[b_st])
                    dve.op(lambda e, a1=a1: e.reciprocal(out=st[:, 3:4], in_=a1[:, 128:129]), reads=[b_accS], writes=[b_st])
                    dve.op(lambda e: e.tensor_tensor(out=st[:, 3:4], in0=st[:, 3:4], in1=lamt[:, 3:4], op=ALU.mult), reads=[b_st, b_lamt], writes=[b_st])
                    dve.op(lambda e, a1=a1: e.tensor_scalar(out=t1, in0=a1[:, 0:128], scalar1=st[:, 3:4], scalar2=None, op0=ALU.mult), reads=[b_accS, b_st], writes=[b_t1])
                    dve.op(lambda e, a0=a0: e.scalar_tensor_tensor(out=osb, in0=a0[:, 0:128], scalar=st[:, 2:3], in1=t1, op0=ALU.mult, op1=ALU.add),
                           reads=[b_accS, b_st, b_t1], writes=[b_osb])
                    dve.op(lambda e: e.tensor_tensor(out=t1, in0=osb, in1=osb, op=ALU.mult), reads=[b_osb], writes=[b_t1])
                    dve.op(lambda e: e.tensor_reduce(out=st[:, 4:5], in_=t1, axis=AX.X, op=ALU.add), reads=[b_t1], writes=[b_st])
                    rstd_op(st[:, 4:5], st[:, 4:5], 1.0 / 128, [b_st])
                    dve.op(lambda e, qi=qi, h=h: e.scalar_tensor_tensor(out=mixo[qi][:, h * 128:(h + 1) * 128], in0=osb, scalar=st[:, 4:5], in1=gsub[:], op0=ALU.mult, op1=ALU.mult),
                           reads=[b_osb, b_st, b_gsub], writes=[b_mixo[qi]])
            for qi in range(nq):
                t = t0 + qi
                ii = t % 2
                sp.dma(lambda e, t=t, ii=ii: e.dma_start(out=sgi[ii], in_=sgd[t * 128:(t + 1) * 128, :]), b_sgi[ii].sem, writes=[b_sgi[ii]])
                sp.dma(lambda e, t=t, ii=ii: e.dma_start(out=xt[ii], in_=xr[t * 128:(t + 1) * 128, :]), b_xt[ii].sem, writes=[b_xt[ii]])
                for half, (srcb, bsrc) in enumerate([(mixo[qi], b_mixo[qi]), (sgi[ii], b_sgi[ii])]):
                    for j in range(4):
                        pe.op(lambda e, j=j, srcb=srcb: e.transpose(out=pbh[:, j * 128:(j + 1) * 128], in_=srcb[:, j * 128:(j + 1) * 128], identity=identb[:]),
                              reads=[bsrc, b_identb], writes=[b_pbh], pe_accum=(j > 0))
                    act.op(lambda e, half=half: e.activation(out=mixT[:, half * 512:(half + 1) * 512], in_=pbh[:, 0:512], func=AF.Copy), reads=[b_pbh], writes=[b_mixT])
                for hv in range(2):
                    for f in range(8):
                        pe.op(lambda e, hv=hv, f=f: e.matmul(pbb[:, hv * 512:(hv + 1) * 512], lhsT=mixT[:, f * 128:(f + 1) * 128],
                                                             rhs=v3(w_out_sb, 8)[:, f, hv * 512:(hv + 1) * 512], start=(f == 0), stop=(f == 7)),
                              reads=[b_mixT, b_wout], writes=[b_pbb], pe_accum=not (hv == 0 and f == 0))
                dve.op(lambda e, s=s: e.tensor_tensor(out=x1, in0=pbb[:, :], in1=growv(s, 0), op=ALU.mult), reads=[b_pbb, b_grow], writes=[b_x1])
                dve.op(lambda e, ii=ii: e.tensor_tensor(out=x1, in0=x1, in1=xt[ii], op=ALU.add), reads=[b_x1, b_xt[ii]], writes=[b_x1])
                sp.dma(lambda e, t=t: e.dma_start(out=xr[t * 128:(t + 1) * 128, :], in_=x1), b_x1.sem, reads=[b_x1])
                pool.op(lambda e: e.memset(st[:, 0:1], 0.0), writes=[b_st])
                act.op(lambda e: e.activation(out=junk, in_=x1, func=AF.Square, accum_out=st[:, 0:1]), reads=[b_x1, b_st], writes=[b_junk, b_st])
                rstd_op(st[:, 1:2], st[:, 0:1], 1.0 / D, [b_st])
                dve.op(lambda e: e.tensor_scalar(out=xn, in0=x1, scalar1=st[:, 1:2], scalar2=None, op0=ALU.mult), reads=[b_x1, b_st], writes=[b_xn])
                for k in range(8):
                    pe.op(lambda e, k=k: e.transpose(out=pbb[:, k * 128:(k + 1) * 128], in_=xn[:, k * 128:(k + 1) * 128], identity=ident[:]),
                          reads=[b_xn, b_ident], writes=[b_pbb], pe_accum=(k > 0))
                for k in range(8):
                    act.op(lambda e, k=k, s=s: e.activation(out=h2T[:, k * 128:(k + 1) * 128], in_=pbb[:, k * 128:(k + 1) * 128], func=AF.Identity,
                                                            scale=Acol(1, s, k), bias=Bcol(1, s, k)),
                           reads=[b_pbb, b_AB, b_modT], writes=[b_h2T])
                for k in range(8):
                    pe.op(lambda e, k=k: e.matmul(pb[4][:, 320:336], lhsT=h2T[:, k * 128:(k + 1) * 128], rhs=wrt[:, k * NE:(k + 1) * NE], start=(k == 0), stop=(k == 7)),
                          reads=[b_h2T, b_wrt], writes=[b_pb[4]], pe_accum=(k > 0))
                for k in range(8):
                    pe.op(lambda e, k=k: e.transpose(out=pbb[:, k * 128:(k + 1) * 128], in_=h2T[:, k * 128:(k + 1) * 128], identity=ident[:]),
                          reads=[b_h2T, b_ident], writes=[b_pbb], pe_accum=(k > 0))
                act.op(lambda e: e.activation(out=h2, in_=pbb[:, :], func=AF.Copy), reads=[b_pbb], writes=[b_h2])
                sp.dma(lambda e, t=t: e.dma_start(out=h2d[t * 128:(t + 1) * 128, :], in_=h2), b_h2.sem, reads=[b_h2])
                dve.op(lambda e: e.tensor_reduce(out=st[:, 5:6], in_=pb[4][:, 320:336], axis=AX.X, op=ALU.max), reads=[b_pb[4]], writes=[b_st])
                dve.op(lambda e: e.tensor_scalar(out=st[:, 5:6], in0=st[:, 5:6], scalar1=-1.0, scalar2=None, op0=ALU.mult), reads=[b_st], writes=[b_st])
                pool.op(lambda e: e.memset(st[:, 6:7], 0.0), writes=[b_st])
                act.op(lambda e: e.activation(out=af, in_=pb[4][:, 320:336], func=AF.Exp, bias=st[:, 5:6], scale=1.0, accum_out=st[:, 6:7]),
                       reads=[b_pb[4], b_st], writes=[b_af, b_st])
                dve.op(lambda e: e.reciprocal(out=st[:, 6:7], in_=st[:, 6:7]), reads=[b_st], writes=[b_st])
                dve.op(lambda e: e.tensor_scalar(out=af, in0=af, scalar1=st[:, 6:7], scalar2=None, op0=ALU.mult), reads=[b_af, b_st], writes=[b_af])
                pe.op(lambda e: e.transpose(out=pb[4][0:NE, 352:480], in_=af, identity=ident[:]), reads=[b_af, b_ident], writes=[b_pb[4]])
                dve.op(lambda e, t=t: e.tensor_copy(out=affT[0:NE, t * 128:(t + 1) * 128], in_=pb[4][0:NE, 352:480]), reads=[b_pb[4]], writes=[b_affT])
        P.barrier()

        AFa.off = af_mark; ABa.reset()
        NTOK = CAP if last else CAP + CCAP
        NJ = 4 if last else 5
        work = affT[:, 0:SEQ]; b_work = b_affT
        workc = affT[:, SEQ:T]
        vals = AFa.alloc(NTOK); b_vals = P.buf("vals%d" % l)
        idxu = AFa.alloc(NTOK).bitcast(U32); b_idxu = P.buf("idxu%d" % l)
        idxf = AFa.alloc(NTOK); b_idxf = P.buf("idxf%d" % l)
        gT = AFa.alloc(5 * NE); b_gT = P.buf("gT%d" % l)
        idxT = AFa.alloc(5 * NE).bitcast(I32); b_idxT = P.buf("idxT%d" % l)
        sgs = [AFa.alloc(544) for _ in range(2)]; b_sgs = [P.buf("sgs%d_%d" % (l, i)) for i in range(2)]
        ysc = AFa.alloc(5 * D); b_ysc = P.buf("ysc%d" % l)
        ring = [ABa.alloc(8192) for _ in range(3)]; b_ring = [P.buf("ring_%d" % i, True) for i in range(3)]
        xs = [ABa.alloc(5 * D) for _ in range(2)]; b_xs = [P.buf("xs_%d" % i, True) for i in range(2)]
        xsT = ABa.alloc(8 * 544); b_xsT = P.buf("xsT%d" % l)
        hidT = ABa.alloc(16 * 544); b_hidT = P.buf("hidT%d" % l)
        for r in range(CAP // 8):
            dve.op(lambda e, r=r: e.max(out=vals[0:NE, r * 8:(r + 1) * 8], in_=work[0:NE, :]), reads=[b_work], writes=[b_vals])
            dve.op(lambda e, r=r: e.max_index(out=idxu[0:NE, r * 8:(r + 1) * 8], in_max=vals[0:NE, r * 8:(r + 1) * 8], in_values=work[0:NE, :]),
                   reads=[b_work, b_vals], writes=[b_idxu])
            if r < CAP // 8 - 1:
                dve.op(lambda e, r=r: e.match_replace(out=work[0:NE, :], in_to_replace=vals[0:NE, r * 8:(r + 1) * 8], in_values=work[0:NE, :], imm_value=-1.0),
                       reads=[b_vals, b_work], writes=[b_work])
        if not last:
            for r in range(CCAP // 8):
                c0 = CAP + r * 8
                dve.op(lambda e, c0=c0: e.max(out=vals[0:NE, c0:c0 + 8], in_=workc[0:NE, :]), reads=[b_work], writes=[b_vals])
                dve.op(lambda e, c0=c0: e.max_index(out=idxu[0:NE, c0:c0 + 8], in_max=vals[0:NE, c0:c0 + 8], in_values=workc[0:NE, :]),
                       reads=[b_work, b_vals], writes=[b_idxu])
                if r < CCAP // 8 - 1:
                    dve.op(lambda e, c0=c0: e.match_replace(out=workc[0:NE, :], in_to_replace=vals[0:NE, c0:c0 + 8], in_values=workc[0:NE, :], imm_value=-1.0),
                           reads=[b_vals, b_work], writes=[b_work])
        dve.op(lambda e: e.tensor_copy(out=idxf[0:NE, :], in_=idxu[0:NE, :]), reads=[b_idxu], writes=[b_idxf])
        if not last:
            dve.op(lambda e: e.tensor_scalar(out=idxf[0:NE, CAP:NTOK], in0=idxf[0:NE, CAP:NTOK], scalar1=float(SEQ), scalar2=None, op0=ALU.add),
                   reads=[b_idxf], writes=[b_idxf])
        for (srcv, bsrc, dstv, bdst) in ((idxf, b_idxf, idxT, b_idxT), (vals, b_vals, gT, b_gT)):
            for jc in range(NJ):
                nj = 128 if jc < 4 else CCAP
                pe.op(lambda e, jc=jc, nj=nj, srcv=srcv: e.transpose(out=pb[4][0:nj, jc * NE:(jc + 1) * NE], in_=srcv[0:NE, jc * 128:jc * 128 + nj], identity=ident[0:NE, 0:NE]),
                      reads=[bsrc, b_ident], writes=[b_pb[4]], pe_accum=(jc > 0))
            dve.op(lambda e, dstv=dstv: e.tensor_copy(out=dstv[:, 0:4 * NE], in_=pb[4][:, 0:4 * NE]), reads=[b_pb[4]], writes=[bdst])
            if not last:
                dve.op(lambda e, dstv=dstv: e.tensor_copy(out=dstv[0:CCAP, 4 * NE:5 * NE], in_=pb[4][0:CCAP, 4 * NE:5 * NE]), reads=[b_pb[4]], writes=[bdst])

        def gather(e_):
            xb = xs[e_ % 2]; bx = b_xs[e_ % 2]
            NJs = [(jc, 128 if jc < 4 else CCAP) for jc in range(NJ)]
            pool.dma([lambda e, jc=jc, nj=nj: e.indirect_dma_start(
                out=xb[0:nj, jc * D:(jc + 1) * D], out_offset=None, in_=h2d[:, :],
                in_offset=bass.IndirectOffsetOnAxis(ap=idxT[0:nj, jc * NE + e_: jc * NE + e_ + 1], axis=0)) for (jc, nj) in NJs],
                bx.sem, reads=[b_idxT], writes=[bx])

        pieces = [(e_, p_) for e_ in range(NE) for p_ in range(6)]

        def load_piece(pi_):
            e_, p_ = pieces[pi_]
            rb = ring[pi_ % 3]; brb = b_ring[pi_ % 3]
            if p_ < 4:
                pool.dma([lambda e: e.dma_start(out=v3(rb[:, 0:4096], 8), in_=w_gate[l, e_].rearrange("(k p) n -> p k n", p=128)[:, :, p_ * 512:(p_ + 1) * 512]),
                          lambda e: e.dma_start(out=v3(rb[:, 4096:8192], 8), in_=w_up[l, e_].rearrange("(k p) n -> p k n", p=128)[:, :, p_ * 512:(p_ + 1) * 512])],
                         brb.sem, writes=[brb])
            else:
                hv = p_ - 4
                pool.dma([lambda e, q_=q_: e.dma_start(
                    out=v3(rb[:, q_ * 4096:(q_ + 1) * 4096], 8),
                    in_=w_down[l, e_].rearrange("(f p) n -> p f n", p=128)[:, q_ * 8:(q_ + 1) * 8, hv * 512:(hv + 1) * 512]) for q_ in range(2)],
                    brb.sem, writes=[brb])

        b_xrs = P.buf("xrs", True)
        gather(0)
        load_piece(0); load_piece(1)
        sgi_ = [0]
        for e_ in range(NE):
            xb = xs[e_ % 2]; bx = b_xs[e_ % 2]
            if e_ + 1 < NE:
                gather(e_ + 1)
            for jc in range(NJ):
                nj = 128 if jc < 4 else CCAP
                for k in range(8):
                    pe.op(lambda e, jc=jc, nj=nj, k=k, xb=xb: e.transpose(out=pbh[:, k * 128:k * 128 + nj], in_=xb[0:nj, jc * D + k * 128: jc * D + (k + 1) * 128],
                                                                          identity=identb[0:nj, 0:nj]),
                          reads=[bx, b_identb], writes=[b_pbh], pe_accum=(k > 0))
                act.op(lambda e, jc=jc, nj=nj: e.activation(out=v3(xsT, 8)[:, :, jc * 128:jc * 128 + nj], in_=v3(pbh[:, :], 8)[:, :, 0:nj], func=AF.Copy),
                       reads=[b_pbh], writes=[b_xsT])
            for p_ in range(6):
                pi_ = e_ * 6 + p_
                if pi_ + 2 < len(pieces):
                    load_piece(pi_ + 2)
                rb = ring[pi_ % 3]; brb = b_ring[pi_ % 3]
                if p_ < 4:
                    for fi in range(4):
                        f = p_ * 4 + fi
                        gb = fi % 2
                        for wi, (woff, pbt, bpbt) in enumerate(((0, pb[gb], b_pb[gb]), (4096, pb[2 + gb], b_pb[2 + gb]))):
                            for k in range(8):
                                pe.op(lambda e, rb=rb, woff=woff, k=k, fi=fi, pbt=pbt: e.matmul(pbt[:, 0:CAP], lhsT=v3(rb[:, woff:woff + 4096], 8)[:, k, fi * 128:(fi + 1) * 128],
                                                                                              rhs=v3(xsT, 8)[:, k, 0:CAP], start=(k == 0), stop=(k == 7)),
                                      reads=[brb, b_xsT], writes=[bpbt], pe_accum=(k > 0))
                            if not last:
                                co = gb * 64 + wi * 32
                                for k in range(8):
                                    pe.op(lambda e, rb=rb, woff=woff, k=k, fi=fi, co=co: e.matmul(pb[4][:, co:co + CCAP], lhsT=v3(rb[:, woff:woff + 4096], 8)[:, k, fi * 128:(fi + 1) * 128],
                                                                                                rhs=v3(xsT, 8)[:, k, CAP:NTOK], start=(k == 0), stop=(k == 7)),
                                          reads=[brb, b_xsT], writes=[b_pb[4]], pe_accum=(k > 0))
                        sg_ = sgs[sgi_[0] % 2]; bsg = b_sgs[sgi_[0] % 2]; sgi_[0] += 1
                        act.op(lambda e, gb=gb, sg_=sg_: e.activation(out=sg_[:, 0:CAP], in_=pb[gb][:, 0:CAP], func=AF.Silu), reads=[b_pb[gb]], writes=[bsg])
                        dve.op(lambda e, gb=gb, sg_=sg_, f=f: e.tensor_tensor(out=v3(hidT, 16)[:, f, 0:CAP], in0=sg_[:, 0:CAP], in1=pb[2 + gb][:, 0:CAP], op=ALU.mult),
                               reads=[bsg, b_pb[2 + gb]], writes=[b_hidT])
                        if not last:
                            co = gb * 64
                            act.op(lambda e, co=co, sg_=sg_: e.activation(out=sg_[:, CAP:NTOK], in_=pb[4][:, co:co + CCAP], func=AF.Silu), reads=[b_pb[4]], writes=[bsg])
                            dve.op(lambda e, co=co, sg_=sg_, f=f: e.tensor_tensor(out=v3(hidT, 16)[:, f, CAP:NTOK], in0=sg_[:, CAP:NTOK], in1=pb[4][:, co + 32:co + 32 + CCAP], op=ALU.mult),
                                   reads=[bsg, b_pb[4]], writes=[b_hidT])
                else:
                    hv = p_ - 4
                    for jc in range(NJ):
                        nj = 128 if jc < 4 else CCAP
                        yb = jc % 2
                        for f in range(16):
                            pe.op(lambda e, rb=rb, f=f, jc=jc, nj=nj, yb=yb: e.matmul(pbb[0:nj, yb * 512:(yb + 1) * 512], lhsT=v3(hidT, 16)[:, f, jc * 128:jc * 128 + nj],
                                                                                    rhs=v3(rb[:, (f // 8) * 4096:(f // 8 + 1) * 4096], 8)[:, f % 8, :], start=(f == 0), stop=(f == 15)),
                                  reads=[b_hidT, brb], writes=[b_pbb], pe_accum=(f > 0))
                        s = 0 if jc < 4 else 1
                        dve.op(lambda e, jc=jc, nj=nj, yb=yb, hv=hv, s=s, e_=e_: e.scalar_tensor_tensor(
                            out=ysc[0:nj, jc * D + hv * 512: jc * D + (hv + 1) * 512], in0=pbb[0:nj, yb * 512:(yb + 1) * 512],
                            scalar=gT[0:nj, jc * NE + e_: jc * NE + e_ + 1], in1=growv(s, 1)[0:nj, hv * 512:(hv + 1) * 512], op0=ALU.mult, op1=ALU.mult),
                            reads=[b_pbb, b_gT, b_grow], writes=[b_ysc])
            NJs = [(jc, 128 if jc < 4 else CCAP) for jc in range(NJ)]
            pool.dma([lambda e, jc=jc, nj=nj: e.indirect_dma_start(
                out=xr[:, :], out_offset=bass.IndirectOffsetOnAxis(ap=idxT[0:nj, jc * NE + e_: jc * NE + e_ + 1], axis=0),
                in_=ysc[0:nj, jc * D:(jc + 1) * D], in_offset=None, compute_op=ALU.add) for (jc, nj) in NJs],
                b_xrs.sem, reads=[b_ysc, b_idxT, b_xrs], writes=[b_xrs])
        P.barrier()

    AFa.reset(); ABa.reset()
    gf = AFa.alloc(D); b_gf = P.buf("gf", True)
    xt = [AFa.alloc(D) for _ in range(2)]; b_xt = [P.buf("xt_%d" % i, True) for i in range(2)]
    yo = [AFa.alloc(D) for _ in range(2)]; b_yo = [P.buf("yo%d" % i, True) for i in range(2)]
    junk = AFa.alloc(D); b_junk = P.buf("junkf")
    st = small[:, 32:40]; b_st = P.buf("stf")
    sp.dma(lambda e: e.dma_start(out=gf, in_=norm_f_g[0:1, :].to_broadcast([128, D])), b_gf.sem, writes=[b_gf])
    for t in range(NLT):
        i = t % 2
        sp.dma(lambda e, t=t, i=i: e.dma_start(out=xt[i], in_=xr[t * 128:(t + 1) * 128, :]), b_xt[i].sem, writes=[b_xt[i]])
        if final_norm:
            pool.op(lambda e: e.memset(st[:, 0:1], 0.0), writes=[b_st])
            act.op(lambda e, i=i: e.activation(out=junk, in_=xt[i], func=AF.Square, accum_out=st[:, 0:1]), reads=[b_xt[i], b_st], writes=[b_junk, b_st])
            rstd_op(st[:, 1:2], st[:, 0:1], 1.0 / D, [b_st])
            dve.op(lambda e, i=i: e.scalar_tensor_tensor(out=yo[i], in0=xt[i], scalar=st[:, 1:2], in1=gf, op0=ALU.mult, op1=ALU.mult),
                   reads=[b_xt[i], b_st, b_gf], writes=[b_yo[i]])
        else:
            dve.op(lambda e, i=i: e.tensor_copy(out=yo[i], in_=xt[i]), reads=[b_xt[i]], writes=[b_yo[i]])
        sp.dma(lambda e, t=t, i=i: e.dma_start(out=out_d[t * 128:(t + 1) * 128, :], in_=yo[i]), b_yo[i].sem, reads=[b_yo[i]])
    P.finish()
    return nc


def _rope_table():
    rows = SEQ // 64
    row = np.repeat(np.arange(rows), 64).astype(np.float32)
    col = np.tile(np.arange(64), rows).astype(np.float32)
    n_freq = 16
    freqs = (np.float32(10000.0) ** (-np.arange(n_freq, dtype=np.float32) / np.float32(n_freq))).astype(np.float32)
    ang_r = (row[:, None] * freqs).astype(np.float32)
    ang_c = (col[:, None] * freqs).astype(np.float32)
    ang = np.concatenate([ang_r, ang_r, ang_c, ang_c], axis=-1)
    cos = np.cos(ang).astype(np.float32); sin = np.sin(ang).astype(np.float32)
    tab = np.zeros((T, 128), np.float32)
    tab[:SEQ, 0:64] = cos
    sgn = np.ones((2, 2, 16), np.float32); sgn[:, 0, :] = -1.0
    tab[:SEQ, 64:128] = sin * sgn.reshape(64)
    tab[SEQ:, 0:64] = 1.0
    return tab


def prep_shared(inp):
    f = lambda a: np.ascontiguousarray(np.asarray(a, dtype=np.float32))
    sh = {
        "rope": _rope_table(), "ident": np.eye(128, dtype=np.float32),
        "w_ada": f(inp["w_ada"]), "b_ada": f(inp["b_ada"]),
        "b_ada_col": f(np.asarray(inp["b_ada"]).reshape(DEPTH, 48, 128).transpose(0, 2, 1)),
        "n1col": f(np.asarray(inp["norm1_g"]).reshape(DEPTH, 8, 128).transpose(0, 2, 1)),
        "n2col": f(np.asarray(inp["norm2_g"]).reshape(DEPTH, 8, 128).transpose(0, 2, 1)),
        "w_in": f(inp["w_in"]), "w_out": f(inp["w_out"]),
        "lamv": f(np.concatenate([np.asarray(inp["lambda_q1"]), np.asarray(inp["lambda_k1"]), np.asarray(inp["lambda_q2"]), np.asarray(inp["lambda_k2"])], axis=1)),
        "subln_g": f(inp["subln_g"]), "sgu_norm_g": f(inp["sgu_norm_g"]), "sgu_w": f(inp["sgu_w"]),
        "sgu_bT": f(np.asarray(inp["sgu_b"]).transpose(0, 2, 1)),
        "w_router": f(inp["w_router"]), "w_gate": f(inp["w_gate"]), "w_up": f(inp["w_up"]), "w_down": f(inp["w_down"]),
        "norm_f_g": f(np.asarray(inp["norm_f_g"]).reshape(1, D)),
    }
    return sh


def prep_core(inp, b):
    x = np.asarray(inp["x"], dtype=np.float32)[b]; cx = np.asarray(inp["ctx"], dtype=np.float32)[b]
    c = np.asarray(inp["c"], dtype=np.float32)[b]; cctx = np.asarray(inp["c_ctx"], dtype=np.float32)
    cc = np.stack([c.reshape(8, 128).T, cctx.reshape(8, 128).T], axis=-1).reshape(128, 16)
    return {"x": np.ascontiguousarray(np.concatenate([x, cx], axis=0)), "cc": np.ascontiguousarray(cc)}


def kernel(**inputs):
    sh = prep_shared(inputs)
    nc = build()
    in_maps = []
    for b in range(8):
        m = dict(sh); m.update(prep_core(inputs, b)); in_maps.append(m)
    res = run_bass_kernel_spmd(nc, in_maps, core_ids=list(range(8)))
    return np.stack([np.asarray(r["out"], dtype=np.float32) for r in res.results], axis=0)
```

```python
import math
import numpy as np
from contextlib import ExitStack
import concourse.bass as bass
import concourse.mybir as mybir
from concourse.bass_utils import run_bass_kernel_spmd

F32 = mybir.dt.float32; BF16 = mybir.dt.bfloat16; I32 = mybir.dt.int32; U32 = mybir.dt.uint32
AF = mybir.ActivationFunctionType; ALU = mybir.AluOpType; AX = mybir.AxisListType

D = 1024; SEQ = 4096; CTX = 256; T = SEQ + CTX; NT = T // 128; NLT = SEQ // 128
DEPTH = 4; NE = 16; CAP = 512; CCAP = 32; FF = 2048
EPS = 1e-6


class Sem:
    def __init__(self, h, name):
        self.h = h; self.name = name; self.val = 0


class Buf:
    def __init__(self, name, sem=None, excl=False):
        self.name = name; self.w = None; self.r = {}; self.sem = sem; self.excl = excl


class Call:
    def __init__(self, name, a, k):
        self.name = name; self.a = a; self.k = k

    def run(self, e):
        return getattr(e, self.name)(*self.a, **self.k)


class Rec:
    def __getattr__(self, name):
        return lambda *a, **k: Call(name, a, k)


REC = Rec()


class Eng:
    def __init__(self, prog, name, sem):
        self.prog = prog; self.name = name; self.sem = sem; self.q = []; self.waited = {}

    def wait_tok(self, sem, val):
        if self.name == 'pe' and sem is self.sem:
            return
        if self.waited.get(sem, 0) >= val:
            return
        self.waited[sem] = val
        h = sem.h
        self.q.append(lambda e, h=h, val=val: e.wait_ge(h, val))

    def deps(self, reads, writes, pe_accum=False):
        for b in reads:
            if b.w is not None:
                self.wait_tok(*b.w)
            if b.excl:
                for s, v in b.r.items():
                    if s is not self.sem:
                        self.wait_tok(s, v)
        for b in writes:
            if pe_accum:
                continue
            if b.w is not None:
                self.wait_tok(*b.w)
            for s, v in b.r.items():
                self.wait_tok(s, v)

    def mark(self, tok, reads, writes):
        s, v = tok
        for b in reads:
            if b.r.get(s, 0) < v:
                b.r[s] = v
        for b in writes:
            b.w = tok; b.r = {}

    def op(self, fn, reads=(), writes=(), pe_accum=False):
        self.deps(reads, writes, pe_accum)
        self.sem.val += 1
        tok = (self.sem, self.sem.val)
        h = self.sem.h
        call = fn(REC)
        self.q.append(lambda e, call=call, h=h: call.run(e).then_inc(h, 1))
        self.mark(tok, reads, writes)
        return tok

    def dma(self, fn, sem, reads=(), writes=()):
        fns = fn if isinstance(fn, (list, tuple)) else [fn]
        self.deps(reads, writes)
        h = sem.h
        for f in fns:
            sem.val += 16
            call = f(REC)
            self.q.append(lambda e, call=call, h=h: call.run(e).then_inc(h, 16))
        tok = (sem, sem.val)
        self.mark(tok, reads, writes)
        return tok


class Prog:
    def __init__(self, nc):
        self.nc = nc; self.es = ExitStack(); self.sems = []; self.semcache = {}
        self.sp = Eng(self, 'sp', self.new_sem('e_sp'))
        self.act = Eng(self, 'act', self.new_sem('e_act'))
        self.pool = Eng(self, 'pool', self.new_sem('e_pool'))
        self.dve = Eng(self, 'dve', self.new_sem('e_dve'))
        self.pe = Eng(self, 'pe', self.new_sem('e_pe'))
        self.engs = [self.sp, self.act, self.pool, self.dve, self.pe]

    def new_sem(self, name):
        s = Sem(self.es.enter_context(self.nc.semaphore(name)), name)
        self.sems.append(s)
        return s

    def sbuf(self, name, shape, dt):
        return self.es.enter_context(self.nc.sbuf_tensor(name, shape, dt))

    def psum(self, name, shape, dt):
        return self.es.enter_context(self.nc.psum_tensor(name, shape, dt))

    def buf(self, name, dma=False):
        if not dma:
            return Buf(name, None)
        if name not in self.semcache:
            self.semcache[name] = self.new_sem('b_' + name)
        return Buf(name, self.semcache[name])

    def barrier(self):
        for e in self.engs:
            for s in self.sems:
                if s.val > 0:
                    e.wait_tok(s, s.val)

    def finish(self):
        self.barrier()
        with self.nc.Block() as block:
            @block.sync
            def _(e):
                for f in self.sp.q: f(e)
            @block.scalar
            def _(e):
                for f in self.act.q: f(e)
            @block.gpsimd
            def _(e):
                for f in self.pool.q: f(e)
            @block.vector
            def _(e):
                for f in self.dve.q: f(e)
            @block.tensor
            def _(e):
                for f in self.pe.q: f(e)
        self.es.close()


class Arena:
    def __init__(self, P, name, ncols, dt):
        self.t = P.sbuf(name, [128, ncols], dt); self.off = 0; self.n = ncols; self.name = name

    def alloc(self, cols):
        a = self.off; self.off += cols
        assert self.off <= self.n, (self.name, self.off, self.n)
        return self.t[:, a:a + cols]

    def reset(self):
        self.off = 0


def v3(ap, a):
    return ap.rearrange("p (a b) -> p a b", a=a)


def build(n_layers=DEPTH, final_norm=True):
    nc = bass.Bass("TRN2", target_bir_lowering=False)
    dram = lambda name, shape, dt, kind="ExternalInput": nc.dram_tensor(name, shape, dt, kind=kind).ap()
    x_in = dram("x", [T, D], F32)
    cc_d = dram("cc", [128, 16], F32)
    rope_d = dram("rope", [T, 128], F32)
    ident_d = dram("ident", [128, 128], F32)
    w_ada = dram("w_ada", [DEPTH, D, 6 * D], F32)
    b_ada_col = dram("b_ada_col", [DEPTH, 128, 48], F32)
    b_ada = dram("b_ada", [DEPTH, 6 * D], F32)
    n1col = dram("n1col", [DEPTH, 128, 8], F32)
    n2col = dram("n2col", [DEPTH, 128, 8], F32)
    w_in = dram("w_in", [DEPTH, D, 2560], F32)
    w_out = dram("w_out", [DEPTH, D, D], F32)
    lamv = dram("lamv", [DEPTH, 256], F32)
    subln_g = dram("subln_g", [DEPTH, 128], F32)
    sgu_norm_g = dram("sgu_norm_g", [DEPTH, 512], F32)
    sgu_w = dram("sgu_w", [DEPTH, 8, 128, 128], F32)
    sgu_bT = dram("sgu_bT", [DEPTH, 128, 8], F32)
    w_router = dram("w_router", [DEPTH, D, NE], F32)
    w_gate = dram("w_gate", [DEPTH, NE, D, FF], F32)
    w_up = dram("w_up", [DEPTH, NE, D, FF], F32)
    w_down = dram("w_down", [DEPTH, NE, FF, D], F32)
    norm_f_g = dram("norm_f_g", [1, D], F32)
    out_d = dram("out", [SEQ, D], F32, kind="ExternalOutput")
    xr = dram("xr", [T, D], F32, kind="Internal")
    qd = dram("qd", [T, 512], BF16, kind="Internal")
    sgd = dram("sgd", [T, 512], BF16, kind="Internal")
    h2d = dram("h2d", [T, D], BF16, kind="Internal")

    P = Prog(nc)
    sp, act, pool, dve, pe = P.sp, P.act, P.pool, P.dve, P.pe

    ident = P.sbuf("ident_sb", [128, 128], F32); b_ident = P.buf("ident", True)
    identb = P.sbuf("identb_sb", [128, 128], BF16); b_identb = P.buf("identb")
    grow = P.sbuf("grow", [128, 4 * D], F32); b_grow = P.buf("grow")
    siluc = P.sbuf("siluc", [128, 16], F32); b_siluc = P.buf("siluc", True)
    modT = P.sbuf("modT", [128, 96], F32); b_modT = P.buf("modT")
    bcol = P.sbuf("bcol", [128, 48], F32); b_bcol = P.buf("bcol", True)
    ncol = P.sbuf("ncol", [128, 16], F32); b_ncol = P.buf("ncol", True)
    AB = P.sbuf("ABcols", [128, 32], F32); b_AB = P.buf("ABc")
    lamt = P.sbuf("lamt", [128, 8], F32); b_lamt = P.buf("lamt")
    lv = P.sbuf("lv", [128, 256], F32); b_lv = P.buf("lv", True)
    lprod = P.sbuf("lprod", [128, 128], F32); b_lprod = P.buf("lprod")
    gsub = P.sbuf("gsub", [128, 128], F32); b_gsub = P.buf("gsub", True)
    sgng = P.sbuf("sgng", [128, 512], F32); b_sgng = P.buf("sgng", True)
    sbT = P.sbuf("sbT", [128, 8], F32); b_sbT = P.buf("sbT", True)
    bfull = P.sbuf("bfull", [128, 512], F32); b_bfull = P.buf("bfull")
    wsT = P.sbuf("wsT", [128, 8 * 128], BF16); b_wsT = P.buf("wsT")
    wrt = P.sbuf("wrt", [128, 8 * NE], F32); b_wrt = P.buf("wrt", True)
    small = P.sbuf("small", [128, 64], F32)
    epsc = P.sbuf("epsc", [128, 1], F32); b_epsc = P.buf("epsc")
    dve.op(lambda e: e.memset(epsc[:], EPS), writes=[b_epsc])

    def rstd_op(dst, src, scale, bufs):
        act.op(lambda e: e.activation(out=dst, in_=src, func=AF.Ln, scale=float(scale), bias=epsc[:, 0:1]), reads=bufs + [b_epsc], writes=bufs)
        act.op(lambda e: e.activation(out=dst, in_=dst, func=AF.Exp, scale=-0.5), reads=bufs, writes=bufs)

    AFa = Arena(P, "arenaF", 12928, F32)
    ABa = Arena(P, "arenaB", 62464, BF16)

    pb = [P.psum("pb%d" % i, [128, 512], F32) for i in range(5)]
    pbhr = P.psum("pbhr", [128, 512], F32)
    pbh = pbhr[:, 0:256].bitcast(BF16)
    pbr = pbhr[:, 256:512]
    pbb = P.psum("pbb", [128, 1024], F32)
    b_pb = [Buf("pb%d" % i, excl=True) for i in range(5)]
    b_pbh = Buf("pbh", excl=True); b_pbb = Buf("pbb", excl=True)

    sp.dma(lambda e: e.dma_start(out=ident[:], in_=ident_d), b_ident.sem, writes=[b_ident])
    dve.op(lambda e: e.tensor_copy(out=identb[:], in_=ident[:]), reads=[b_ident], writes=[b_identb])
    sp.dma(lambda e: e.dma_start(out=siluc[:], in_=cc_d), b_siluc.sem, writes=[b_siluc])
    act.op(lambda e: e.activation(out=siluc[:], in_=siluc[:], func=AF.Silu), reads=[b_siluc], writes=[b_siluc])
    b_xr = P.buf("xr", True)
    sp.dma([lambda e, i=i: e.dma_start(out=xr[i * (T // 4):(i + 1) * (T // 4), :], in_=x_in[i * (T // 4):(i + 1) * (T // 4), :]) for i in range(4)],
           b_xr.sem, writes=[b_xr])
    P.barrier()

    for l in range(n_layers):
        last = (l == DEPTH - 1)
        lam_init = 0.8 - 0.6 * math.exp(-0.3 * l)
        NTq = NLT if last else NT
        AFa.reset(); ABa.reset()
        wa = [AFa.alloc(8 * 512) for _ in range(2)]; b_wa = [P.buf("wa_%d" % i, True) for i in range(2)]
        brow = AFa.alloc(512); b_brow = P.buf("brow", True)
        swl = AFa.alloc(8 * 128); b_swl = P.buf("swl", True)
        srep = AFa.alloc(16 * 128); b_srep = P.buf("srep%d" % l)
        for ks in range(16):
            dve.op(lambda e, ks=ks: e.tensor_copy(out=srep[:, ks * 128:(ks + 1) * 128], in_=siluc[:, ks:ks + 1].to_broadcast([128, 128])),
                   reads=[b_siluc], writes=[b_srep])
        sp.dma(lambda e: e.dma_start(out=bcol[:], in_=b_ada_col[l]), b_bcol.sem, writes=[b_bcol])
        sp.dma([lambda e: e.dma_start(out=ncol[:, 0:8], in_=n1col[l]), lambda e: e.dma_start(out=ncol[:, 8:16], in_=n2col[l])], b_ncol.sem, writes=[b_ncol])
        sp.dma(lambda e: e.dma_start(out=lv[:], in_=lamv[l:l + 1, :].to_broadcast([128, 256])), b_lv.sem, writes=[b_lv])
        sp.dma(lambda e: e.dma_start(out=gsub[:], in_=subln_g[l:l + 1, :].to_broadcast([128, 128])), b_gsub.sem, writes=[b_gsub])
        sp.dma(lambda e: e.dma_start(out=sgng[:], in_=sgu_norm_g[l:l + 1, :].to_broadcast([128, 512])), b_sgng.sem, writes=[b_sgng])
        sp.dma(lambda e: e.dma_start(out=sbT[:], in_=sgu_bT[l]), b_sbT.sem, writes=[b_sbT])
        sp.dma(lambda e: e.dma_start(out=wrt[:].rearrange("p (k n) -> p k n", k=8), in_=w_router[l].rearrange("(k p) n -> p k n", p=128)),
               b_wrt.sem, writes=[b_wrt])
        dve.op(lambda e: e.tensor_tensor(out=v3(lprod[:], 2), in0=v3(lv[:], 2)[:, :, 0:64], in1=v3(lv[:], 2)[:, :, 64:128], op=ALU.mult),
               reads=[b_lv], writes=[b_lprod])
        dve.op(lambda e: e.tensor_reduce(out=lamt[:, 0:2], in_=v3(lprod[:], 2), axis=AX.X, op=ALU.add), reads=[b_lprod], writes=[b_lamt])
        act.op(lambda e: e.activation(out=lamt[:, 0:2], in_=lamt[:, 0:2], func=AF.Exp), reads=[b_lamt], writes=[b_lamt])
        dve.op(lambda e: e.tensor_tensor(out=lamt[:, 2:3], in0=lamt[:, 0:1], in1=lamt[:, 1:2], op=ALU.subtract), reads=[b_lamt], writes=[b_lamt])
        dve.op(lambda e: e.tensor_scalar(out=lamt[:, 2:3], in0=lamt[:, 2:3], scalar1=float(lam_init), scalar2=None, op0=ALU.add), reads=[b_lamt], writes=[b_lamt])
        dve.op(lambda e: e.tensor_scalar(out=lamt[:, 3:4], in0=lamt[:, 2:3], scalar1=-1.0, scalar2=None, op0=ALU.mult), reads=[b_lamt], writes=[b_lamt])
        dve.op(lambda e: e.tensor_scalar(out=gsub[:], in0=gsub[:], scalar1=float(1.0 - lam_init), scalar2=None, op0=ALU.mult), reads=[b_gsub], writes=[b_gsub])
        dve.op(lambda e: e.tensor_copy(out=v3(bfull[:], 8), in_=sbT[:].unsqueeze(2).to_broadcast([128, 8, 64])), reads=[b_sbT], writes=[b_bfull])
        for hh in range(2):
            sp.dma(lambda e, hh=hh: e.dma_start(out=v3(swl, 8)[:, 0:4, :], in_=sgu_w[l, hh * 4:(hh + 1) * 4].rearrange("h p q -> p h q")),
                   b_swl.sem, writes=[b_swl])
            for h4 in range(4):
                pe.op(lambda e, h4=h4: e.transpose(out=pbb[:, h4 * 128:(h4 + 1) * 128], in_=v3(swl, 8)[:, h4, :], identity=ident[:]),
                      reads=[b_swl, b_ident], writes=[b_pbb], pe_accum=(h4 > 0))
            act.op(lambda e, hh=hh: e.activation(out=wsT[:, hh * 512:(hh + 1) * 512], in_=pbb[:, 0:512], func=AF.Copy), reads=[b_pbb], writes=[b_wsT])
        for hg in range(12):
            g = hg // 2; half = hg % 2
            wb_ = wa[hg % 2]; bw = b_wa[hg % 2]
            sp.dma(lambda e, wb_=wb_, hg=hg: e.dma_start(out=v3(wb_, 8), in_=w_ada[l].rearrange("(k p) n -> p k n", p=128)[:, :, hg * 512:(hg + 1) * 512]),
                   bw.sem, writes=[bw])
            for nn in range(4):
                n = hg * 4 + nn
                for k in range(8):
                    pe.op(lambda e, wb_=wb_, nn=nn, k=k, n=n: e.matmul(pb[3][:, 2 * n:2 * n + 2], lhsT=v3(wb_, 8)[:, k, nn * 128:(nn + 1) * 128],
                                                                      rhs=siluc[:, 2 * k:2 * k + 2], start=(k == 0), stop=(k == 7)),
                          reads=[bw, b_siluc], writes=[b_pb[3]], pe_accum=not (hg == 0 and nn == 0 and k == 0))
            if g in (2, 5):
                which = 0 if g == 2 else 1
                sp.dma(lambda e, g=g, half=half: e.dma_start(out=brow, in_=b_ada[l:l + 1, g * 1024 + half * 512: g * 1024 + (half + 1) * 512].to_broadcast([128, 512])),
                       b_brow.sem, writes=[b_brow])
                for s in range(2):
                    for k in range(8):
                        pe.op(lambda e, wb_=wb_, s=s, k=k: e.matmul(pb[s][:, :], lhsT=srep[:, (2 * k + s) * 128:(2 * k + s + 1) * 128], rhs=v3(wb_, 8)[:, k, :],
                                                                    start=(k == 0), stop=(k == 7)),
                              reads=[bw, b_srep], writes=[b_pb[s]], pe_accum=(k > 0))
                    c0 = (s * 2 + which) * D + half * 512
                    dve.op(lambda e, s=s, c0=c0: e.tensor_tensor(out=grow[:, c0:c0 + 512], in0=pb[s][:, :], in1=brow, op=ALU.add),
                           reads=[b_pb[s], b_brow], writes=[b_grow])
        dve.op(lambda e: e.tensor_tensor(out=v3(modT[:], 48), in0=v3(pb[3][:, 0:96], 48), in1=bcol[:].unsqueeze(2).to_broadcast([128, 48, 2]), op=ALU.add),
               reads=[b_pb[3], b_bcol], writes=[b_modT])
        mT = v3(modT[:], 48)
        for w_ in range(2):
            for s in range(2):
                sc = mT[:, (1 + 3 * w_) * 8:(2 + 3 * w_) * 8, s]
                o = AB[:, (w_ * 2 + s) * 8:(w_ * 2 + s + 1) * 8]
                dve.op(lambda e, sc=sc, o=o, w_=w_: e.scalar_tensor_tensor(out=o, in0=sc, scalar=1.0, in1=ncol[:, w_ * 8:(w_ + 1) * 8], op0=ALU.add, op1=ALU.mult),
                       reads=[b_modT, b_ncol], writes=[b_AB])

        def Acol(w_, s, k): return AB[:, (w_ * 2 + s) * 8 + k:(w_ * 2 + s) * 8 + k + 1]
        def Bcol(w_, s, k): return modT[:, ((3 * w_) * 8 + k) * 2 + s:((3 * w_) * 8 + k) * 2 + s + 1]
        def growv(s, which): return grow[:, (s * 2 + which) * D:(s * 2 + which + 1) * D]
        P.barrier()

        AFa.reset(); ABa.reset()
        kT = ABa.alloc(4 * T); b_kT = P.buf("kT%d" % l)
        vaug = ABa.alloc(NT * 4 * 130); b_vaug = P.buf("vaug%d" % l)
        ab_mark = ABa.off
        w_in_sb = ABa.alloc(8 * 2560); b_win = P.buf("win", True)
        hT = [ABa.alloc(8 * 128) for _ in range(2)]; b_hT = [P.buf("hT%d_%d" % (l, i)) for i in range(2)]
        qr = ABa.alloc(512); b_qr = P.buf("qr", True)
        kr = ABa.alloc(512); b_kr = P.buf("kr%d" % l)
        gvn = ABa.alloc(512); b_gvn = P.buf("gvn%d" % l)
        sgo = ABa.alloc(512); b_sgo = P.buf("sgo", True)
        xt = [AFa.alloc(D) for _ in range(2)]; b_xt = [P.buf("xt_%d" % i, True) for i in range(2)]
        rp = [AFa.alloc(128) for _ in range(3)]; b_rp = [P.buf("rp_%d" % i, True) for i in range(3)]
        xn = AFa.alloc(D); b_xn = P.buf("xn%d" % l)
        junk = AFa.alloc(D); b_junk = P.buf("junk%d" % l)
        t1 = AFa.alloc(512); b_t1 = P.buf("t1%d" % l)
        t2 = AFa.alloc(512); b_t2 = P.buf("t2%d" % l)
        u_sb = AFa.alloc(512); b_u = P.buf("u%d" % l)
        gvg = AFa.alloc(512); b_gvg = P.buf("gvg%d" % l)
        sq = AFa.alloc(512); b_sq = P.buf("sq%d" % l)
        st = small[:, 0:32]; b_stN = P.buf("stN%d" % l); b_stP = P.buf("stP%d" % l)
        pool.dma([lambda e, i=i: e.dma_start(out=v3(w_in_sb, 8)[:, 2 * i:2 * i + 2, :], in_=w_in[l].rearrange("(k p) n -> p k n", p=128)[:, 2 * i:2 * i + 2, :]) for i in range(4)],
                 b_win.sem, writes=[b_win])
        va4 = vaug.rearrange("p (t h c) -> p t h c", t=NT, h=4)
        pool.op(lambda e: e.memset(vaug, 1.0), writes=[b_vaug])

        def load_tile(t):
            i = t % 2; i3 = t % 3
            sp.dma(lambda e: e.dma_start(out=xt[i], in_=xr[t * 128:(t + 1) * 128, :]), b_xt[i].sem, writes=[b_xt[i]])
            sp.dma(lambda e: e.dma_start(out=rp[i3], in_=rope_d[t * 128:(t + 1) * 128, :]), b_rp[i3].sem, writes=[b_rp[i3]])

        def N1(t):
            i = t % 2
            pool.op(lambda e: e.memset(st[:, 0:1], 0.0), writes=[b_stN])
            act.op(lambda e: e.activation(out=junk, in_=xt[i], func=AF.Square, accum_out=st[:, 0:1]), reads=[b_xt[i], b_stN], writes=[b_junk, b_stN])
            rstd_op(st[:, 1:2], st[:, 0:1], 1.0 / D, [b_stN])

        def N2(t):
            i = t % 2
            dve.op(lambda e: e.tensor_scalar(out=xn, in0=xt[i], scalar1=st[:, 1:2], scalar2=None, op0=ALU.mult), reads=[b_xt[i], b_stN], writes=[b_xn])

        def N3(t):
            for k in range(8):
                pe.op(lambda e: e.transpose(out=pbb[:, k * 128:(k + 1) * 128], in_=xn[:, k * 128:(k + 1) * 128], identity=ident[:]),
                      reads=[b_xn, b_ident], writes=[b_pbb], pe_accum=(k > 0))

        def N4(t):
            s_ = 0 if t < NLT else 1
            dst = hT[t % 2]
            for k in range(8):
                act.op(lambda e: e.activation(out=dst[:, k * 128:(k + 1) * 128], in_=pbb[:, k * 128:(k + 1) * 128], func=AF.Identity,
                                              scale=Acol(0, s_, k), bias=Bcol(0, s_, k)),
                       reads=[b_pbb, b_AB, b_modT], writes=[b_hT[t % 2]])

        def rope(src_ps, b_src, rp_, b_rp_, dst, b_dst):
            s5 = src_ps.rearrange("p (g a r f) -> p g a r f", g=8, a=2, r=2)
            t5 = t2.rearrange("p (g a r f) -> p g a r f", g=8, a=2, r=2)
            sn = rp_[:, 64:128].rearrange("p (a r f) -> p a r f", a=2, r=2)
            dve.op(lambda e: e.tensor_tensor(out=v3(t1, 8), in0=v3(src_ps, 8), in1=rp_[:, 0:64].unsqueeze(1).to_broadcast([128, 8, 64]), op=ALU.mult),
                   reads=[b_src, b_rp_], writes=[b_t1])
            for a in range(2):
                for r in range(2):
                    dve.op(lambda e: e.tensor_tensor(out=t5[:, :, a, r, :], in0=s5[:, :, a, 1 - r, :],
                                                     in1=sn[:, a, r, :].unsqueeze(1).to_broadcast([128, 8, 16]), op=ALU.mult),
                           reads=[b_src, b_rp_], writes=[b_t2])
            dve.op(lambda e: e.tensor_tensor(out=dst, in0=t1, in1=t2, op=ALU.add), reads=[b_t1, b_t2], writes=[b_dst])

        prot = [0, 1, 3, 4]; pcnt = [0]

        def proj(t, g):
            bi = prot[pcnt[0] % 4]; pcnt[0] += 1
            hsrc = hT[t % 2]
            for k in range(8):
                pe.op(lambda e: e.matmul(pb[bi][:, :], lhsT=hsrc[:, k * 128:(k + 1) * 128], rhs=v3(w_in_sb, 8)[:, k, g * 512:(g + 1) * 512],
                                         start=(k == 0), stop=(k == 7)),
                      reads=[b_hT[t % 2], b_win], writes=[b_pb[bi]], pe_accum=(k > 0))
            return pb[bi], b_pb[bi]

        load_tile(0); load_tile(1)
        N1(0); N2(0); N3(0); N4(0)
        for t in range(NT):
            nxt = t + 1 < NT
            i3 = t % 3
            need_q = t < NTq
            if t + 2 < NT:
                load_tile(t + 2)
            if nxt: N1(t + 1)
            if need_q:
                pbk, bpbk = proj(t, 0)
                rope(pbk[:, :], bpbk, rp[i3], b_rp[i3], qr, b_qr)
                sp.dma(lambda e: e.dma_start(out=qd[t * 128:(t + 1) * 128, :], in_=qr), b_qr.sem, reads=[b_qr])
            if nxt: N2(t + 1)
            pbk, bpbk = proj(t, 1)
            rope(pbk[:, :], bpbk, rp[i3], b_rp[i3], kr, b_kr)
            for j in range(4):
                pe.op(lambda e: e.transpose(out=pbh[:, j * 128:(j + 1) * 128], in_=kr[:, j * 128:(j + 1) * 128], identity=identb[:]),
                      reads=[b_kr, b_identb], writes=[b_pbh], pe_accum=(j > 0))
            act.op(lambda e: e.activation(out=v3(kT, 4)[:, :, t * 128:(t + 1) * 128], in_=v3(pbh[:, 0:512], 4), func=AF.Copy),
                   reads=[b_pbh], writes=[b_kT])
            if nxt: N3(t + 1)
            pbk, bpbk = proj(t, 2)
            act.op(lambda e: e.activation(out=va4[:, t, :, 0:128], in_=v3(pbk[:, :], 4), func=AF.Copy), reads=[bpbk], writes=[b_vaug])
            if nxt: N4(t + 1)
            if need_q:
                pbk, bpbk = proj(t, 3)
                act.op(lambda e: e.activation(out=u_sb, in_=pbk[:, :], func=AF.Gelu), reads=[bpbk], writes=[b_u])
                pbk, bpbk = proj(t, 4)
                act.op(lambda e: e.activation(out=gvg, in_=pbk[:, :], func=AF.Gelu), reads=[bpbk], writes=[b_gvg])
                dve.op(lambda e: e.tensor_tensor(out=sq, in0=gvg, in1=gvg, op=ALU.mult), reads=[b_gvg], writes=[b_sq])
                dve.op(lambda e: e.tensor_reduce(out=st[:, 8:16], in_=v3(sq, 8), axis=AX.X, op=ALU.add), reads=[b_sq], writes=[b_stP])
                rstd_op(st[:, 16:24], st[:, 8:16], 1.0 / 64, [b_stP])
                dve.op(lambda e: e.tensor_tensor(out=v3(sq, 8), in0=v3(gvg, 8), in1=st[:, 16:24].unsqueeze(2).to_broadcast([128, 8, 64]), op=ALU.mult),
                       reads=[b_gvg, b_stP], writes=[b_sq])
                dve.op(lambda e: e.tensor_tensor(out=gvn, in0=sq, in1=sgng[:], op=ALU.mult), reads=[b_sq, b_sgng], writes=[b_gvn])
                for h in range(8):
                    pe.op(lambda e: e.matmul(pb[2][:, h * 64:(h + 1) * 64], lhsT=wsT[:, h * 128:(h + 1) * 128], rhs=gvn[:, h * 64:(h + 1) * 64],
                                             start=True, stop=True),
                          reads=[b_wsT, b_gvn], writes=[b_pb[2]], pe_accum=(h > 0))
                dve.op(lambda e: e.tensor_tensor(out=sq, in0=pb[2][:, :], in1=bfull[:], op=ALU.add), reads=[b_pb[2], b_bfull], writes=[b_sq])
                dve.op(lambda e: e.tensor_tensor(out=sgo, in0=sq, in1=u_sb, op=ALU.mult), reads=[b_sq, b_u], writes=[b_sgo])
                sp.dma(lambda e: e.dma_start(out=sgd[t * 128:(t + 1) * 128, :], in_=sgo), b_sgo.sem, reads=[b_sgo])
        P.barrier()

        AFa.reset(); ABa.off = ab_mark
        w_out_sb = ABa.alloc(8 * D); b_wout = P.buf("wout", True)
        qg = ABa.alloc(4 * 512); b_qg = P.buf("qg", True)
        qTm = [ABa.alloc(8 * 512) for _ in range(2)]; b_qTm = [P.buf("qTm%d_%d" % (l, i)) for i in range(2)]
        pT = [ABa.alloc(512) for _ in range(3)]; b_pT = [P.buf("pT%d_%d" % (l, i)) for i in range(3)]
        mixo = [[ABa.alloc(512) for _ in range(4)] for _ in range(2)]
        b_mixo = [[P.buf("mixo%d_%d_%d" % (l, pp, i)) for i in range(4)] for pp in range(2)]
        mixT = ABa.alloc(8 * 128); b_mixT = P.buf("mixT%d" % l)
        sgi = [ABa.alloc(512) for _ in range(2)]; b_sgi = [P.buf("sgi_%d" % i, True) for i in range(2)]
        h2 = ABa.alloc(D); b_h2 = P.buf("h2", True)
        affT = AFa.alloc(T); b_affT = P.buf("affT%d" % l)
        af_mark = AFa.off
        xt = [AFa.alloc(D) for _ in range(2)]; b_xt = [P.buf("xt_%d" % i, True) for i in range(2)]
        x1 = AFa.alloc(D); b_x1 = P.buf("x1", True)
        xn = AFa.alloc(D); b_xn = P.buf("xnc%d" % l)
        junk = AFa.alloc(D); b_junk = P.buf("junkc%d" % l)
        h2T = AFa.alloc(8 * 128); b_h2T = P.buf("h2T%d" % l)
        accS = AFa.alloc(8 * 130); b_accS = P.buf("accS%d" % l)
        t1 = AFa.alloc(128); b_t1 = P.buf("t1c%d" % l)
        osb = [AFa.alloc(128) for _ in range(4)]; b_osb = [P.buf("osb%d_%d" % (l, i)) for i in range(4)]
        af = AFa.alloc(NE); b_af = P.buf("af%d" % l)
        st = small[:, 0:32]
        b_stT = P.buf("stT%d" % l); b_stE = P.buf("stE%d" % l); b_stE2 = P.buf("stE2%d" % l)
        b_pbR = b_pbh; b_pbAf = b_pbh
        pool.dma(lambda e: e.dma_start(out=v3(w_out_sb, 8), in_=w_out[l].rearrange("(k p) n -> p k n", p=128)), b_wout.sem, writes=[b_wout])
        for pp in range(2):
            pool.op(lambda e: e.memset(qTm[pp], 0.0), writes=[b_qTm[pp]])

        def acc_ap(a):
            bank = 2 + a // 3; off = (a % 3) * 160
            return pb[bank][:, off:off + 130], b_pb[bank]

        qgroups = [(G * 4, 4) for G in range(8)] + ([] if last else [(NLT, 2)])

        def build_q(gi):
            t0, nq = qgroups[gi]; par = gi % 2
            ths = []

            def ld():
                sp.dma(lambda e: e.dma_start(out=v3(qg, 4)[:, 0:nq, :], in_=qd[t0 * 128:(t0 + nq) * 128, :].rearrange("(a p) n -> p a n", p=128)),
                       b_qg.sem, writes=[b_qg])
            ths.append(ld)
            for qi in range(nq):
                def f(qi=qi):
                    for j in range(4):
                        pe.op(lambda e: e.transpose(out=pbh[:, j * 128:(j + 1) * 128], in_=v3(qg, 4)[:, qi, j * 128:(j + 1) * 128], identity=identb[:]),
                              reads=[b_qg, b_identb], writes=[b_pbh], pe_accum=(j > 0))
                    for hf in range(2):
                        dst = qTm[par].rearrange("p (j f q) -> p j f q", j=4, f=2)[hf * 64:(hf + 1) * 64, :, hf, qi * 128:(qi + 1) * 128]
                        src = v3(pbh[:, 0:512], 4)[hf * 64:(hf + 1) * 64, :, :]
                        act.op(lambda e: e.activation(out=dst, in_=src, func=AF.Copy), reads=[b_pbh], writes=[b_qTm[par]])
                ths.append(f)
            return ths

        def tile_thunks(gi, qi):
            t0, nq = qgroups[gi]; par = gi % 2
            t = t0 + qi; ii = t % 2
            s_ = 0 if t0 < NLT else 1
            mo = mixo[par][qi]; bmo = b_mixo[par][qi]
            ths = []

            def s1():
                sp.dma(lambda e: e.dma_start(out=sgi[ii], in_=sgd[t * 128:(t + 1) * 128, :]), b_sgi[ii].sem, writes=[b_sgi[ii]])
                sp.dma(lambda e: e.dma_start(out=xt[ii], in_=xr[t * 128:(t + 1) * 128, :]), b_xt[ii].sem, writes=[b_xt[ii]])
                for half, (srcb, bsrc) in enumerate([(mo, bmo), (sgi[ii], b_sgi[ii])]):
                    for j in range(4):
                        pe.op(lambda e: e.transpose(out=pbh[:, j * 128:(j + 1) * 128], in_=srcb[:, j * 128:(j + 1) * 128], identity=identb[:]),
                              reads=[bsrc, b_identb], writes=[b_pbh], pe_accum=(j > 0))
                    act.op(lambda e: e.activation(out=mixT[:, half * 512:(half + 1) * 512], in_=pbh[:, 0:512], func=AF.Copy), reads=[b_pbh], writes=[b_mixT])
            ths.append(s1)

            def s2():
                for hv in range(2):
                    for f in range(8):
                        pe.op(lambda e: e.matmul(pbb[:, hv * 512:(hv + 1) * 512], lhsT=mixT[:, f * 128:(f + 1) * 128],
                                                 rhs=v3(w_out_sb, 8)[:, f, hv * 512:(hv + 1) * 512], start=(f == 0), stop=(f == 7)),
                              reads=[b_mixT, b_wout], writes=[b_pbb], pe_accum=not (hv == 0 and f == 0))
            ths.append(s2)

            def s3():
                dve.op(lambda e: e.tensor_tensor(out=x1, in0=pbb[:, :], in1=growv(s_, 0), op=ALU.mult), reads=[b_pbb, b_grow], writes=[b_x1])
                dve.op(lambda e: e.tensor_tensor(out=x1, in0=x1, in1=xt[ii], op=ALU.add), reads=[b_x1, b_xt[ii]], writes=[b_x1])
                sp.dma(lambda e: e.dma_start(out=xr[t * 128:(t + 1) * 128, :], in_=x1), b_x1.sem, reads=[b_x1])
                pool.op(lambda e: e.memset(st[:, 0:1], 0.0), writes=[b_stT])
                act.op(lambda e: e.activation(out=junk, in_=x1, func=AF.Square, accum_out=st[:, 0:1]), reads=[b_x1, b_stT], writes=[b_junk, b_stT])
            ths.append(s3)

            def s4():
                rstd_op(st[:, 1:2], st[:, 0:1], 1.0 / D, [b_stT])
            ths.append(s4)

            def s5():
                dve.op(lambda e: e.tensor_scalar(out=xn, in0=x1, scalar1=st[:, 1:2], scalar2=None, op0=ALU.mult), reads=[b_x1, b_stT], writes=[b_xn])
                for k in range(8):
                    pe.op(lambda e: e.transpose(out=pbb[:, k * 128:(k + 1) * 128], in_=xn[:, k * 128:(k + 1) * 128], identity=ident[:]),
                          reads=[b_xn, b_ident], writes=[b_pbb], pe_accum=(k > 0))
            ths.append(s5)

            def s6():
                for k in range(8):
                    act.op(lambda e: e.activation(out=h2T[:, k * 128:(k + 1) * 128], in_=pbb[:, k * 128:(k + 1) * 128], func=AF.Identity,
                                                  scale=Acol(1, s_, k), bias=Bcol(1, s_, k)),
                           reads=[b_pbb, b_AB, b_modT], writes=[b_h2T])
            ths.append(s6)

            def s7():
                for k in range(8):
                    pe.op(lambda e: e.matmul(pbr[:, 0:NE], lhsT=h2T[:, k * 128:(k + 1) * 128], rhs=wrt[:, k * NE:(k + 1) * NE], start=(k == 0), stop=(k == 7)),
                          reads=[b_h2T, b_wrt], writes=[b_pbR], pe_accum=(k > 0))
                for k in range(8):
                    pe.op(lambda e: e.transpose(out=pbb[:, k * 128:(k + 1) * 128], in_=h2T[:, k * 128:(k + 1) * 128], identity=ident[:]),
                          reads=[b_h2T, b_ident], writes=[b_pbb], pe_accum=(k > 0))
            ths.append(s7)

            def s8():
                act.op(lambda e: e.activation(out=h2, in_=pbb[:, :], func=AF.Copy), reads=[b_pbb], writes=[b_h2])
                sp.dma(lambda e: e.dma_start(out=h2d[t * 128:(t + 1) * 128, :], in_=h2), b_h2.sem, reads=[b_h2])
                dve.op(lambda e: e.tensor_reduce(out=st[:, 5:6], in_=pbr[:, 0:NE], axis=AX.X, op=ALU.max), reads=[b_pbR], writes=[b_stT])
                dve.op(lambda e: e.tensor_scalar(out=st[:, 5:6], in0=st[:, 5:6], scalar1=-1.0, scalar2=None, op0=ALU.mult), reads=[b_stT], writes=[b_stT])
                pool.op(lambda e: e.memset(st[:, 6:7], 0.0), writes=[b_stT])
                act.op(lambda e: e.activation(out=af, in_=pbr[:, 0:NE], func=AF.Exp, bias=st[:, 5:6], scale=1.0, accum_out=st[:, 6:7]),
                       reads=[b_pbR, b_stT], writes=[b_af, b_stT])
            ths.append(s8)

            def s9():
                dve.op(lambda e: e.reciprocal(out=st[:, 6:7], in_=st[:, 6:7]), reads=[b_stT], writes=[b_stT])
                dve.op(lambda e: e.tensor_scalar(out=af, in0=af, scalar1=st[:, 6:7], scalar2=None, op0=ALU.mult), reads=[b_af, b_stT], writes=[b_af])
                pe.op(lambda e: e.transpose(out=pbr[0:NE, 128:256], in_=af, identity=ident[:]), reads=[b_af, b_ident], writes=[b_pbAf])
            ths.append(s9)

            def s10():
                dve.op(lambda e: e.tensor_copy(out=affT[0:NE, t * 128:(t + 1) * 128], in_=pbr[0:NE, 128:256]), reads=[b_pbAf], writes=[b_affT])
            ths.append(s10)
            return ths

        from collections import deque
        bg = deque()
        for th in build_q(0):
            th()
        a8 = v3(accS, 8)
        pti = 0
        for gi, (t0, nq) in enumerate(qgroups):
            par = gi % 2
            s = 0 if t0 < NLT else 1
            NQ = nq * 128
            kts = list(range(NT)) if s == 0 else [NLT, NLT + 1]
            if gi + 1 < len(qgroups):
                bg.extendleft(reversed(build_q(gi + 1)))
            iters = [(h, c, ki, kt) for h in range(4) for c in range(2) for ki, kt in enumerate(kts)]
            timed = []

            def emit_S(idx):
                h, c, ki, kt = iters[idx]; sb = idx % 2
                i8 = c * 4 + h; j = i8 // 2
                pe.op(lambda e: e.matmul(pb[sb][:, 0:NQ], lhsT=v3(kT, 4)[:, j, kt * 128:(kt + 1) * 128], rhs=v3(qTm[par], 8)[:, i8, 0:NQ], start=True, stop=True),
                      reads=[b_kT, b_qTm[par]], writes=[b_pb[sb]])

            def E1(h):
                for bank in range(3):
                    na = 3 if bank < 2 else 2
                    src = pb[2 + bank][:, 0:na * 160].rearrange("p (a c) -> p a c", a=na)[:, :, 0:130]
                    dve.op(lambda e: e.tensor_copy(out=a8[:, bank * 3:bank * 3 + na, :], in_=src), reads=[b_pb[2 + bank]], writes=[b_accS])
                for qi in range(nq):
                    a0 = a8[:, qi, :]; a1 = a8[:, 4 + qi, :]
                    dve.op(lambda e: e.reciprocal(out=st[:, 2:3], in_=a0[:, 128:129]), reads=[b_accS], writes=[b_stE])
                    dve.op(lambda e: e.reciprocal(out=st[:, 3:4], in_=a1[:, 128:129]), reads=[b_accS], writes=[b_stE])
                    dve.op(lambda e: e.tensor_tensor(out=st[:, 3:4], in0=st[:, 3:4], in1=lamt[:, 3:4], op=ALU.mult), reads=[b_stE, b_lamt], writes=[b_stE])
                    dve.op(lambda e: e.tensor_scalar(out=t1, in0=a1[:, 0:128], scalar1=st[:, 3:4], scalar2=None, op0=ALU.mult), reads=[b_accS, b_stE], writes=[b_t1])
                    dve.op(lambda e: e.scalar_tensor_tensor(out=osb[qi], in0=a0[:, 0:128], scalar=st[:, 2:3], in1=t1, op0=ALU.mult, op1=ALU.add),
                           reads=[b_accS, b_stE, b_t1], writes=[b_osb[qi]])
                    dve.op(lambda e: e.tensor_tensor(out=t1, in0=osb[qi], in1=osb[qi], op=ALU.mult), reads=[b_osb[qi]], writes=[b_t1])
                    dve.op(lambda e: e.tensor_reduce(out=st[:, 8 + qi:9 + qi], in_=t1, axis=AX.X, op=ALU.add), reads=[b_t1], writes=[b_stE2])

            def E2(h):
                rstd_op(st[:, 8:8 + nq], st[:, 8:8 + nq], 1.0 / 128, [b_stE2])

            def E3(h):
                for qi in range(nq):
                    dve.op(lambda e: e.scalar_tensor_tensor(out=mixo[par][qi][:, h * 128:(h + 1) * 128], in0=osb[qi], scalar=st[:, 8 + qi:9 + qi], in1=gsub[:], op0=ALU.mult, op1=ALU.mult),
                           reads=[b_osb[qi], b_stE2, b_gsub], writes=[b_mixo[par][qi]])

            import os as _os
            NOLOOK = _os.environ.get("KNOLOOK") == "1"; NOBG = _os.environ.get("KNOBG") == "1"
            if not NOLOOK:
                emit_S(0)
            for idx in range(len(iters)):
                if NOLOOK:
                    emit_S(idx)
                elif idx + 1 < len(iters):
                    emit_S(idx + 1)
                h, c, ki, kt = iters[idx]; sb = idx % 2
                pi = pti % 3; pti += 1
                act.op(lambda e: e.activation(out=pT[pi][:, 0:NQ], in_=pb[sb][:, 0:NQ], func=AF.Exp, scale=0.125),
                       reads=[b_pb[sb]], writes=[b_pT[pi]])
                banks_seen = set()
                for qi in range(nq):
                    a_ = c * 4 + qi
                    aap, bacc = acc_ap(a_)
                    first_in_bank = (a_ // 3) not in banks_seen
                    banks_seen.add(a_ // 3)
                    pe.op(lambda e: e.matmul(aap, lhsT=pT[pi][:, qi * 128:(qi + 1) * 128], rhs=va4[:, kt, h, :],
                                             start=(ki == 0 and first_in_bank), stop=(ki == len(kts) - 1), skip_group_check=True),
                          reads=[b_pT[pi], b_vaug], writes=[bacc], pe_accum=(ki > 0))
                if c == 1 and ki == len(kts) - 1:
                    E1(h)
                    timed.append((idx + 6, lambda h=h: E2(h)))
                    timed.append((idx + 12, lambda h=h: E3(h)))
                while timed and timed[0][0] <= idx:
                    timed.pop(0)[1]()
                if idx % 3 == 2 and bg and not NOBG:
                    bg.popleft()()
            while timed:
                timed.pop(0)[1]()
            while NOBG and bg:
                bg.popleft()()
            for qi in range(nq):
                bg.extend(tile_thunks(gi, qi))
        while bg:
            bg.popleft()()
        P.barrier()

        AFa.off = af_mark; ABa.reset()
        NTOK = CAP if last else CAP + CCAP
        NJ = 4 if last else 5
        work = affT[:, 0:SEQ]; b_work = b_affT
        workc = affT[:, SEQ:T]
        vals = AFa.alloc(NTOK); b_vals = P.buf("vals%d" % l)
        idxu = AFa.alloc(NTOK).bitcast(U32); b_idxu = P.buf("idxu%d" % l)
        idxf = AFa.alloc(NTOK); b_idxf = P.buf("idxf%d" % l)
        gT = AFa.alloc(5 * NE); b_gT = P.buf("gT%d" % l)
        idxT = AFa.alloc(5 * NE).bitcast(I32); b_idxT = P.buf("idxT%d" % l)
        sgs = [AFa.alloc(544) for _ in range(2)]; b_sgs = [P.buf("sgs%d_%d" % (l, i)) for i in range(2)]
        ysc = AFa.alloc(5 * D); b_ysc = P.buf("ysc%d" % l)
        ring = [ABa.alloc(8192) for _ in range(3)]; b_ring = [P.buf("ring_%d" % i, True) for i in range(3)]
        xs = [ABa.alloc(5 * D) for _ in range(2)]; b_xs = [P.buf("xs_%d" % i, True) for i in range(2)]
        xsT = ABa.alloc(8 * 544); b_xsT = P.buf("xsT%d" % l)
        hidT = ABa.alloc(16 * 544); b_hidT = P.buf("hidT%d" % l)
        for r in range(CAP // 8):
            dve.op(lambda e, r=r: e.max(out=vals[0:NE, r * 8:(r + 1) * 8], in_=work[0:NE, :]), reads=[b_work], writes=[b_vals])
            dve.op(lambda e, r=r: e.max_index(out=idxu[0:NE, r * 8:(r + 1) * 8], in_max=vals[0:NE, r * 8:(r + 1) * 8], in_values=work[0:NE, :]),
                   reads=[b_work, b_vals], writes=[b_idxu])
            if r < CAP // 8 - 1:
                dve.op(lambda e, r=r: e.match_replace(out=work[0:NE, :], in_to_replace=vals[0:NE, r * 8:(r + 1) * 8], in_values=work[0:NE, :], imm_value=-1.0),
                       reads=[b_vals, b_work], writes=[b_work])
        if not last:
            for r in range(CCAP // 8):
                c0 = CAP + r * 8
                dve.op(lambda e, c0=c0: e.max(out=vals[0:NE, c0:c0 + 8], in_=workc[0:NE, :]), reads=[b_work], writes=[b_vals])
                dve.op(lambda e, c0=c0: e.max_index(out=idxu[0:NE, c0:c0 + 8], in_max=vals[0:NE, c0:c0 + 8], in_values=workc[0:NE, :]),
                       reads=[b_work, b_vals], writes=[b_idxu])
                if r < CCAP // 8 - 1:
                    dve.op(lambda e, c0=c0: e.match_replace(out=workc[0:NE, :], in_to_replace=vals[0:NE, c0:c0 + 8], in_values=workc[0:NE, :], imm_value=-1.0),
                           reads=[b_vals, b_work], writes=[b_work])
        dve.op(lambda e: e.tensor_copy(out=idxf[0:NE, :], in_=idxu[0:NE, :]), reads=[b_idxu], writes=[b_idxf])
        if not last:
            dve.op(lambda e: e.tensor_scalar(out=idxf[0:NE, CAP:NTOK], in0=idxf[0:NE, CAP:NTOK], scalar1=float(SEQ), scalar2=None, op0=ALU.add),
                   reads=[b_idxf], writes=[b_idxf])
        for (srcv, bsrc, dstv, bdst) in ((idxf, b_idxf, idxT, b_idxT), (vals, b_vals, gT, b_gT)):
            for jc in range(NJ):
                nj = 128 if jc < 4 else CCAP
                pe.op(lambda e, jc=jc, nj=nj, srcv=srcv: e.transpose(out=pb[4][0:nj, jc * NE:(jc + 1) * NE], in_=srcv[0:NE, jc * 128:jc * 128 + nj], identity=ident[0:NE, 0:NE]),
                      reads=[bsrc, b_ident], writes=[b_pb[4]], pe_accum=(jc > 0))
            dve.op(lambda e, dstv=dstv: e.tensor_copy(out=dstv[:, 0:4 * NE], in_=pb[4][:, 0:4 * NE]), reads=[b_pb[4]], writes=[bdst])
            if not last:
                dve.op(lambda e, dstv=dstv: e.tensor_copy(out=dstv[0:CCAP, 4 * NE:5 * NE], in_=pb[4][0:CCAP, 4 * NE:5 * NE]), reads=[b_pb[4]], writes=[bdst])

        def gather(e_):
            xb = xs[e_ % 2]; bx = b_xs[e_ % 2]
            NJs = [(jc, 128 if jc < 4 else CCAP) for jc in range(NJ)]
            pool.dma([lambda e, jc=jc, nj=nj: e.indirect_dma_start(
                out=xb[0:nj, jc * D:(jc + 1) * D], out_offset=None, in_=h2d[:, :],
                in_offset=bass.IndirectOffsetOnAxis(ap=idxT[0:nj, jc * NE + e_: jc * NE + e_ + 1], axis=0)) for (jc, nj) in NJs],
                bx.sem, reads=[b_idxT], writes=[bx])

        pieces = [(e_, p_) for e_ in range(NE) for p_ in range(6)]

        def load_piece(pi_):
            e_, p_ = pieces[pi_]
            rb = ring[pi_ % 3]; brb = b_ring[pi_ % 3]
            if p_ < 4:
                pool.dma([lambda e: e.dma_start(out=v3(rb[:, 0:4096], 8), in_=w_gate[l, e_].rearrange("(k p) n -> p k n", p=128)[:, :, p_ * 512:(p_ + 1) * 512]),
                          lambda e: e.dma_start(out=v3(rb[:, 4096:8192], 8), in_=w_up[l, e_].rearrange("(k p) n -> p k n", p=128)[:, :, p_ * 512:(p_ + 1) * 512])],
                         brb.sem, writes=[brb])
            else:
                hv = p_ - 4
                pool.dma([lambda e, q_=q_: e.dma_start(
                    out=v3(rb[:, q_ * 4096:(q_ + 1) * 4096], 8),
                    in_=w_down[l, e_].rearrange("(f p) n -> p f n", p=128)[:, q_ * 8:(q_ + 1) * 8, hv * 512:(hv + 1) * 512]) for q_ in range(2)],
                    brb.sem, writes=[brb])

        b_xrs = P.buf("xrs", True)
        gather(0)
        load_piece(0); load_piece(1)
        sgi_ = [0]
        for e_ in range(NE):
            xb = xs[e_ % 2]; bx = b_xs[e_ % 2]
            if e_ + 1 < NE:
                gather(e_ + 1)
            for jc in range(NJ):
                nj = 128 if jc < 4 else CCAP
                for kh in range(2):
                    for k4 in range(4):
                        k = kh * 4 + k4
                        pe.op(lambda e: e.transpose(out=pbh[:, k4 * 128:k4 * 128 + nj], in_=xb[0:nj, jc * D + k * 128: jc * D + (k + 1) * 128],
                                                    identity=identb[0:nj, 0:nj]),
                              reads=[bx, b_identb], writes=[b_pbh], pe_accum=(k4 > 0))
                    act.op(lambda e: e.activation(out=v3(xsT, 8)[:, kh * 4:(kh + 1) * 4, jc * 128:jc * 128 + nj], in_=v3(pbh[:, :], 4)[:, :, 0:nj], func=AF.Copy),
                           reads=[b_pbh], writes=[b_xsT])
            for p_ in range(6):
                pi_ = e_ * 6 + p_
                if pi_ + 2 < len(pieces):
                    load_piece(pi_ + 2)
                rb = ring[pi_ % 3]; brb = b_ring[pi_ % 3]
                if p_ < 4:
                    for fi in range(4):
                        f = p_ * 4 + fi
                        gb = fi % 2
                        for wi, (woff, pbt, bpbt) in enumerate(((0, pb[gb], b_pb[gb]), (4096, pb[2 + gb], b_pb[2 + gb]))):
                            for k in range(8):
                                pe.op(lambda e, rb=rb, woff=woff, k=k, fi=fi, pbt=pbt: e.matmul(pbt[:, 0:CAP], lhsT=v3(rb[:, woff:woff + 4096], 8)[:, k, fi * 128:(fi + 1) * 128],
                                                                                              rhs=v3(xsT, 8)[:, k, 0:CAP], start=(k == 0), stop=(k == 7)),
                                      reads=[brb, b_xsT], writes=[bpbt], pe_accum=(k > 0))
                            if not last:
                                co = gb * 64 + wi * 32
                                for k in range(8):
                                    pe.op(lambda e, rb=rb, woff=woff, k=k, fi=fi, co=co: e.matmul(pb[4][:, co:co + CCAP], lhsT=v3(rb[:, woff:woff + 4096], 8)[:, k, fi * 128:(fi + 1) * 128],
                                                                                                rhs=v3(xsT, 8)[:, k, CAP:NTOK], start=(k == 0), stop=(k == 7)),
                                          reads=[brb, b_xsT], writes=[b_pb[4]], pe_accum=(k > 0))
                        sg_ = sgs[sgi_[0] % 2]; bsg = b_sgs[sgi_[0] % 2]; sgi_[0] += 1
                        act.op(lambda e, gb=gb, sg_=sg_: e.activation(out=sg_[:, 0:CAP], in_=pb[gb][:, 0:CAP], func=AF.Silu), reads=[b_pb[gb]], writes=[bsg])
                        dve.op(lambda e, gb=gb, sg_=sg_, f=f: e.tensor_tensor(out=v3(hidT, 16)[:, f, 0:CAP], in0=sg_[:, 0:CAP], in1=pb[2 + gb][:, 0:CAP], op=ALU.mult),
                               reads=[bsg, b_pb[2 + gb]], writes=[b_hidT])
                        if not last:
                            co = gb * 64
                            act.op(lambda e, co=co, sg_=sg_: e.activation(out=sg_[:, CAP:NTOK], in_=pb[4][:, co:co + CCAP], func=AF.Silu), reads=[b_pb[4]], writes=[bsg])
                            dve.op(lambda e, co=co, sg_=sg_, f=f: e.tensor_tensor(out=v3(hidT, 16)[:, f, CAP:NTOK], in0=sg_[:, CAP:NTOK], in1=pb[4][:, co + 32:co + 32 + CCAP], op=ALU.mult),
                                   reads=[bsg, b_pb[4]], writes=[b_hidT])
                else:
                    hv = p_ - 4
                    for jc in range(NJ):
                        nj = 128 if jc < 4 else CCAP
                        yb = jc % 2
                        for f in range(16):
                            pe.op(lambda e, rb=rb, f=f, jc=jc, nj=nj, yb=yb: e.matmul(pbb[0:nj, yb * 512:(yb + 1) * 512], lhsT=v3(hidT, 16)[:, f, jc * 128:jc * 128 + nj],
                                                                                    rhs=v3(rb[:, (f // 8) * 4096:(f // 8 + 1) * 4096], 8)[:, f % 8, :], start=(f == 0), stop=(f == 15)),
                                  reads=[b_hidT, brb], writes=[b_pbb], pe_accum=(f > 0))
                        s = 0 if jc < 4 else 1
                        dve.op(lambda e, jc=jc, nj=nj, yb=yb, hv=hv, s=s, e_=e_: e.scalar_tensor_tensor(
                            out=ysc[0:nj, jc * D + hv * 512: jc * D + (hv + 1) * 512], in0=pbb[0:nj, yb * 512:(yb + 1) * 512],
                            scalar=gT[0:nj, jc * NE + e_: jc * NE + e_ + 1], in1=growv(s, 1)[0:nj, hv * 512:(hv + 1) * 512], op0=ALU.mult, op1=ALU.mult),
                            reads=[b_pbb, b_gT, b_grow], writes=[b_ysc])
            NJs = [(jc, 128 if jc < 4 else CCAP) for jc in range(NJ)]
            pool.dma([lambda e, jc=jc, nj=nj: e.indirect_dma_start(
                out=xr[:, :], out_offset=bass.IndirectOffsetOnAxis(ap=idxT[0:nj, jc * NE + e_: jc * NE + e_ + 1], axis=0),
                in_=ysc[0:nj, jc * D:(jc + 1) * D], in_offset=None, compute_op=ALU.add) for (jc, nj) in NJs],
                b_xrs.sem, reads=[b_ysc, b_idxT, b_xrs], writes=[b_xrs])
        P.barrier()

    AFa.reset(); ABa.reset()
    gf = AFa.alloc(D); b_gf = P.buf("gf", True)
    xt = [AFa.alloc(D) for _ in range(2)]; b_xt = [P.buf("xt_%d" % i, True) for i in range(2)]
    yo = [AFa.alloc(D) for _ in range(2)]; b_yo = [P.buf("yo%d" % i, True) for i in range(2)]
    junk = AFa.alloc(D); b_junk = P.buf("junkf")
    st = small[:, 32:40]; b_st = P.buf("stf")
    sp.dma(lambda e: e.dma_start(out=gf, in_=norm_f_g[0:1, :].to_broadcast([128, D])), b_gf.sem, writes=[b_gf])
    for t in range(NLT):
        i = t % 2
        sp.dma(lambda e, t=t, i=i: e.dma_start(out=xt[i], in_=xr[t * 128:(t + 1) * 128, :]), b_xt[i].sem, writes=[b_xt[i]])
        if final_norm:
            pool.op(lambda e: e.memset(st[:, 0:1], 0.0), writes=[b_st])
            act.op(lambda e, i=i: e.activation(out=junk, in_=xt[i], func=AF.Square, accum_out=st[:, 0:1]), reads=[b_xt[i], b_st], writes=[b_junk, b_st])
            rstd_op(st[:, 1:2], st[:, 0:1], 1.0 / D, [b_st])
            dve.op(lambda e, i=i: e.scalar_tensor_tensor(out=yo[i], in0=xt[i], scalar=st[:, 1:2], in1=gf, op0=ALU.mult, op1=ALU.mult),
                   reads=[b_xt[i], b_st, b_gf], writes=[b_yo[i]])
        else:
            dve.op(lambda e, i=i: e.tensor_copy(out=yo[i], in_=xt[i]), reads=[b_xt[i]], writes=[b_yo[i]])
        sp.dma(lambda e, t=t, i=i: e.dma_start(out=out_d[t * 128:(t + 1) * 128, :], in_=yo[i]), b_yo[i].sem, reads=[b_yo[i]])
    P.finish()
    return nc


def _rope_table():
    rows = SEQ // 64
    row = np.repeat(np.arange(rows), 64).astype(np.float32)
    col = np.tile(np.arange(64), rows).astype(np.float32)
    n_freq = 16
    freqs = (np.float32(10000.0) ** (-np.arange(n_freq, dtype=np.float32) / np.float32(n_freq))).astype(np.float32)
    ang_r = (row[:, None] * freqs).astype(np.float32)
    ang_c = (col[:, None] * freqs).astype(np.float32)
    ang = np.concatenate([ang_r, ang_r, ang_c, ang_c], axis=-1)
    cos = np.cos(ang).astype(np.float32); sin = np.sin(ang).astype(np.float32)
    tab = np.zeros((T, 128), np.float32)
    tab[:SEQ, 0:64] = cos
    sgn = np.ones((2, 2, 16), np.float32); sgn[:, 0, :] = -1.0
    tab[:SEQ, 64:128] = sin * sgn.reshape(64)
    tab[SEQ:, 0:64] = 1.0
    return tab


def prep_shared(inp):
    f = lambda a: np.ascontiguousarray(np.asarray(a, dtype=np.float32))
    sh = {
        "rope": _rope_table(), "ident": np.eye(128, dtype=np.float32),
        "w_ada": f(inp["w_ada"]), "b_ada": f(inp["b_ada"]),
        "b_ada_col": f(np.asarray(inp["b_ada"]).reshape(DEPTH, 48, 128).transpose(0, 2, 1)),
        "n1col": f(np.asarray(inp["norm1_g"]).reshape(DEPTH, 8, 128).transpose(0, 2, 1)),
        "n2col": f(np.asarray(inp["norm2_g"]).reshape(DEPTH, 8, 128).transpose(0, 2, 1)),
        "w_in": f(inp["w_in"]), "w_out": f(inp["w_out"]),
        "lamv": f(np.concatenate([np.asarray(inp["lambda_q1"]), np.asarray(inp["lambda_k1"]), np.asarray(inp["lambda_q2"]), np.asarray(inp["lambda_k2"])], axis=1)),
        "subln_g": f(inp["subln_g"]), "sgu_norm_g": f(inp["sgu_norm_g"]), "sgu_w": f(inp["sgu_w"]),
        "sgu_bT": f(np.asarray(inp["sgu_b"]).transpose(0, 2, 1)),
        "w_router": f(inp["w_router"]), "w_gate": f(inp["w_gate"]), "w_up": f(inp["w_up"]), "w_down": f(inp["w_down"]),
        "norm_f_g": f(np.asarray(inp["norm_f_g"]).reshape(1, D)),
    }
    return sh


def prep_core(inp, b):
    x = np.asarray(inp["x"], dtype=np.float32)[b]; cx = np.asarray(inp["ctx"], dtype=np.float32)[b]
    c = np.asarray(inp["c"], dtype=np.float32)[b]; cctx = np.asarray(inp["c_ctx"], dtype=np.float32)
    cc = np.stack([c.reshape(8, 128).T, cctx.reshape(8, 128).T], axis=-1).reshape(128, 16)
    return {"x": np.ascontiguousarray(np.concatenate([x, cx], axis=0)), "cc": np.ascontiguousarray(cc)}


def kernel(**inputs):
    sh = prep_shared(inputs)
    nc = build()
    in_maps = []
    for b in range(8):
        m = dict(sh); m.update(prep_core(inputs, b)); in_maps.append(m)
    res = run_bass_kernel_spmd(nc, in_maps, core_ids=list(range(8)))
    return np.stack([np.asarray(r["out"], dtype=np.float32) for r in res.results], axis=0)
```

```python
import math
import numpy as np
from contextlib import ExitStack
import concourse.bass as bass
import concourse.mybir as mybir
from concourse.bass_utils import run_bass_kernel_spmd

F32 = mybir.dt.float32; BF16 = mybir.dt.bfloat16; I32 = mybir.dt.int32; U32 = mybir.dt.uint32
AF = mybir.ActivationFunctionType; ALU = mybir.AluOpType; AX = mybir.AxisListType

D = 1024; SEQ = 4096; CTX = 256; T = SEQ + CTX; NT = T // 128; NLT = SEQ // 128
DEPTH = 4; NE = 16; CAP = 512; CCAP = 32; FF = 2048
EPS = 1e-6


class Sem:
    def __init__(self, h, name):
        self.h = h; self.name = name; self.val = 0


class Buf:
    def __init__(self, name, sem=None, excl=False):
        self.name = name; self.w = None; self.r = {}; self.sem = sem; self.excl = excl


class Call:
    def __init__(self, name, a, k):
        self.name = name; self.a = a; self.k = k

    def run(self, e):
        return getattr(e, self.name)(*self.a, **self.k)


class Rec:
    def __getattr__(self, name):
        return lambda *a, **k: Call(name, a, k)


REC = Rec()


class Eng:
    def __init__(self, prog, name, sem):
        self.prog = prog; self.name = name; self.sem = sem; self.q = []; self.waited = {}

    def wait_tok(self, sem, val):
        if self.name == 'pe' and sem is self.sem:
            return
        if self.waited.get(sem, 0) >= val:
            return
        self.waited[sem] = val
        h = sem.h
        self.q.append(lambda e, h=h, val=val: e.wait_ge(h, val))

    def deps(self, reads, writes, pe_accum=False):
        for b in reads:
            if b.w is not None:
                self.wait_tok(*b.w)
            if b.excl:
                for s, v in b.r.items():
                    if s is not self.sem:
                        self.wait_tok(s, v)
        for b in writes:
            if pe_accum:
                continue
            if b.w is not None:
                self.wait_tok(*b.w)
            for s, v in b.r.items():
                self.wait_tok(s, v)

    def mark(self, tok, reads, writes):
        s, v = tok
        for b in reads:
            if b.r.get(s, 0) < v:
                b.r[s] = v
        for b in writes:
            b.w = tok; b.r = {}

    def op(self, fn, reads=(), writes=(), pe_accum=False):
        self.deps(reads, writes, pe_accum)
        self.sem.val += 1
        tok = (self.sem, self.sem.val)
        h = self.sem.h
        call = fn(REC)
        self.q.append(lambda e, call=call, h=h: call.run(e).then_inc(h, 1))
        self.mark(tok, reads, writes)
        return tok

    def dma(self, fn, sem, reads=(), writes=()):
        fns = fn if isinstance(fn, (list, tuple)) else [fn]
        self.deps(reads, writes)
        h = sem.h
        for f in fns:
            sem.val += 16
            call = f(REC)
            self.q.append(lambda e, call=call, h=h: call.run(e).then_inc(h, 16))
        tok = (sem, sem.val)
        self.mark(tok, reads, writes)
        return tok


class Prog:
    def __init__(self, nc):
        self.nc = nc; self.es = ExitStack(); self.sems = []; self.semcache = {}
        self.sp = Eng(self, 'sp', self.new_sem('e_sp'))
        self.act = Eng(self, 'act', self.new_sem('e_act'))
        self.pool = Eng(self, 'pool', self.new_sem('e_pool'))
        self.dve = Eng(self, 'dve', self.new_sem('e_dve'))
        self.pe = Eng(self, 'pe', self.new_sem('e_pe'))
        self.engs = [self.sp, self.act, self.pool, self.dve, self.pe]

    def new_sem(self, name):
        s = Sem(self.es.enter_context(self.nc.semaphore(name)), name)
        self.sems.append(s)
        return s

    def sbuf(self, name, shape, dt):
        return self.es.enter_context(self.nc.sbuf_tensor(name, shape, dt))

    def psum(self, name, shape, dt):
        return self.es.enter_context(self.nc.psum_tensor(name, shape, dt))

    def buf(self, name, dma=False):
        if not dma:
            return Buf(name, None)
        if name not in self.semcache:
            self.semcache[name] = self.new_sem('b_' + name)
        return Buf(name, self.semcache[name])

    def barrier(self):
        for e in self.engs:
            for s in self.sems:
                if s.val > 0:
                    e.wait_tok(s, s.val)

    def finish(self):
        self.barrier()
        with self.nc.Block() as block:
            @block.sync
            def _(e):
                for f in self.sp.q: f(e)
            @block.scalar
            def _(e):
                for f in self.act.q: f(e)
            @block.gpsimd
            def _(e):
                for f in self.pool.q: f(e)
            @block.vector
            def _(e):
                for f in self.dve.q: f(e)
            @block.tensor
            def _(e):
                for f in self.pe.q: f(e)
        self.es.close()


class Arena:
    def __init__(self, P, name, ncols, dt):
        self.t = P.sbuf(name, [128, ncols], dt); self.off = 0; self.n = ncols; self.name = name

    def alloc(self, cols):
        a = self.off; self.off += cols
        assert self.off <= self.n, (self.name, self.off, self.n)
        return self.t[:, a:a + cols]

    def reset(self):
        self.off = 0


def v3(ap, a):
    return ap.rearrange("p (a b) -> p a b", a=a)


def build(n_layers=DEPTH, final_norm=True):
    nc = bass.Bass("TRN2", target_bir_lowering=False)
    dram = lambda name, shape, dt, kind="ExternalInput": nc.dram_tensor(name, shape, dt, kind=kind).ap()
    x_in = dram("x", [T, D], F32)
    cc_d = dram("cc", [128, 16], F32)
    rope_d = dram("rope", [T, 128], F32)
    ident_d = dram("ident", [128, 128], F32)
    w_ada = dram("w_ada", [DEPTH, D, 6 * D], F32)
    b_ada_col = dram("b_ada_col", [DEPTH, 128, 48], F32)
    b_ada = dram("b_ada", [DEPTH, 6 * D], F32)
    n1col = dram("n1col", [DEPTH, 128, 8], F32)
    n2col = dram("n2col", [DEPTH, 128, 8], F32)
    w_in = dram("w_in", [DEPTH, D, 2560], F32)
    w_out = dram("w_out", [DEPTH, D, D], F32)
    lamv = dram("lamv", [DEPTH, 256], F32)
    subln_g = dram("subln_g", [DEPTH, 128], F32)
    sgu_norm_g = dram("sgu_norm_g", [DEPTH, 512], F32)
    sgu_w = dram("sgu_w", [DEPTH, 8, 128, 128], F32)
    sgu_bT = dram("sgu_bT", [DEPTH, 128, 8], F32)
    w_router = dram("w_router", [DEPTH, D, NE], F32)
    w_gate = dram("w_gate", [DEPTH, NE, D, FF], F32)
    w_up = dram("w_up", [DEPTH, NE, D, FF], F32)
    w_down = dram("w_down", [DEPTH, NE, FF, D], F32)
    norm_f_g = dram("norm_f_g", [1, D], F32)
    out_d = dram("out", [SEQ, D], F32, kind="ExternalOutput")
    xr = dram("xr", [T, D], F32, kind="Internal")
    qd = dram("qd", [T, 512], BF16, kind="Internal")
    sgd = dram("sgd", [T, 512], BF16, kind="Internal")
    h2d = dram("h2d", [T, D], BF16, kind="Internal")

    P = Prog(nc)
    sp, act, pool, dve, pe = P.sp, P.act, P.pool, P.dve, P.pe

    ident = P.sbuf("ident_sb", [128, 128], F32); b_ident = P.buf("ident", True)
    identb = P.sbuf("identb_sb", [128, 128], BF16); b_identb = P.buf("identb")
    grow = P.sbuf("grow", [128, 4 * D], F32); b_grow = P.buf("grow")
    siluc = P.sbuf("siluc", [128, 16], F32); b_siluc = P.buf("siluc", True)
    modT = P.sbuf("modT", [128, 96], F32); b_modT = P.buf("modT")
    bcol = P.sbuf("bcol", [128, 48], F32); b_bcol = P.buf("bcol", True)
    ncol = P.sbuf("ncol", [128, 16], F32); b_ncol = P.buf("ncol", True)
    AB = P.sbuf("ABcols", [128, 32], F32); b_AB = P.buf("ABc")
    lamt = P.sbuf("lamt", [128, 8], F32); b_lamt = P.buf("lamt")
    lv = P.sbuf("lv", [128, 256], F32); b_lv = P.buf("lv", True)
    lprod = P.sbuf("lprod", [128, 128], F32); b_lprod = P.buf("lprod")
    gsub = P.sbuf("gsub", [128, 128], F32); b_gsub = P.buf("gsub", True)
    sgng = P.sbuf("sgng", [128, 512], F32); b_sgng = P.buf("sgng", True)
    sbT = P.sbuf("sbT", [128, 8], F32); b_sbT = P.buf("sbT", True)
    bfull = P.sbuf("bfull", [128, 512], F32); b_bfull = P.buf("bfull")
    wsT = P.sbuf("wsT", [128, 8 * 128], BF16); b_wsT = P.buf("wsT")
    wrt = P.sbuf("wrt", [128, 8 * NE], F32); b_wrt = P.buf("wrt", True)
    small = P.sbuf("small", [128, 64], F32)
    epsc = P.sbuf("epsc", [128, 1], F32); b_epsc = P.buf("epsc")
    dve.op(lambda e: e.memset(epsc[:], EPS), writes=[b_epsc])

    def rstd_op(dst, src, scale, bufs):
        act.op(lambda e: e.activation(out=dst, in_=src, func=AF.Ln, scale=float(scale), bias=epsc[:, 0:1]), reads=bufs + [b_epsc], writes=bufs)
        act.op(lambda e: e.activation(out=dst, in_=dst, func=AF.Exp, scale=-0.5), reads=bufs, writes=bufs)

    AFa = Arena(P, "arenaF", 12928, F32)
    ABa = Arena(P, "arenaB", 62464, BF16)

    pb = [P.psum("pb%d" % i, [128, 512], F32) for i in range(5)]
    pbhr = P.psum("pbhr", [128, 512], F32)
    pbh = pbhr[:, 0:256].bitcast(BF16)
    pbr = pbhr[:, 256:512]
    pbb = P.psum("pbb", [128, 1024], F32)
    b_pb = [Buf("pb%d" % i, excl=True) for i in range(5)]
    b_pbh = Buf("pbh", excl=True); b_pbb = Buf("pbb", excl=True)

    sp.dma(lambda e: e.dma_start(out=ident[:], in_=ident_d), b_ident.sem, writes=[b_ident])
    dve.op(lambda e: e.tensor_copy(out=identb[:], in_=ident[:]), reads=[b_ident], writes=[b_identb])
    sp.dma(lambda e: e.dma_start(out=siluc[:], in_=cc_d), b_siluc.sem, writes=[b_siluc])
    act.op(lambda e: e.activation(out=siluc[:], in_=siluc[:], func=AF.Silu), reads=[b_siluc], writes=[b_siluc])
    b_xr = P.buf("xr", True)
    sp.dma([lambda e, i=i: e.dma_start(out=xr[i * (T // 4):(i + 1) * (T // 4), :], in_=x_in[i * (T // 4):(i + 1) * (T // 4), :]) for i in range(4)],
           b_xr.sem, writes=[b_xr])
    P.barrier()

    for l in range(n_layers):
        last = (l == DEPTH - 1)
        lam_init = 0.8 - 0.6 * math.exp(-0.3 * l)
        NTq = NLT if last else NT
        AFa.reset(); ABa.reset()
        wa = [AFa.alloc(8 * 512) for _ in range(2)]; b_wa = [P.buf("wa_%d" % i, True) for i in range(2)]
        brow = AFa.alloc(512); b_brow = P.buf("brow", True)
        swl = AFa.alloc(8 * 128); b_swl = P.buf("swl", True)
        srep = AFa.alloc(16 * 128); b_srep = P.buf("srep%d" % l)
        for ks in range(16):
            dve.op(lambda e, ks=ks: e.tensor_copy(out=srep[:, ks * 128:(ks + 1) * 128], in_=siluc[:, ks:ks + 1].to_broadcast([128, 128])),
                   reads=[b_siluc], writes=[b_srep])
        sp.dma(lambda e: e.dma_start(out=bcol[:], in_=b_ada_col[l]), b_bcol.sem, writes=[b_bcol])
        sp.dma([lambda e: e.dma_start(out=ncol[:, 0:8], in_=n1col[l]), lambda e: e.dma_start(out=ncol[:, 8:16], in_=n2col[l])], b_ncol.sem, writes=[b_ncol])
        sp.dma(lambda e: e.dma_start(out=lv[:], in_=lamv[l:l + 1, :].to_broadcast([128, 256])), b_lv.sem, writes=[b_lv])
        sp.dma(lambda e: e.dma_start(out=gsub[:], in_=subln_g[l:l + 1, :].to_broadcast([128, 128])), b_gsub.sem, writes=[b_gsub])
        sp.dma(lambda e: e.dma_start(out=sgng[:], in_=sgu_norm_g[l:l + 1, :].to_broadcast([128, 512])), b_sgng.sem, writes=[b_sgng])
        sp.dma(lambda e: e.dma_start(out=sbT[:], in_=sgu_bT[l]), b_sbT.sem, writes=[b_sbT])
        sp.dma(lambda e: e.dma_start(out=wrt[:].rearrange("p (k n) -> p k n", k=8), in_=w_router[l].rearrange("(k p) n -> p k n", p=128)),
               b_wrt.sem, writes=[b_wrt])
        dve.op(lambda e: e.tensor_tensor(out=v3(lprod[:], 2), in0=v3(lv[:], 2)[:, :, 0:64], in1=v3(lv[:], 2)[:, :, 64:128], op=ALU.mult),
               reads=[b_lv], writes=[b_lprod])
        dve.op(lambda e: e.tensor_reduce(out=lamt[:, 0:2], in_=v3(lprod[:], 2), axis=AX.X, op=ALU.add), reads=[b_lprod], writes=[b_lamt])
        act.op(lambda e: e.activation(out=lamt[:, 0:2], in_=lamt[:, 0:2], func=AF.Exp), reads=[b_lamt], writes=[b_lamt])
        dve.op(lambda e: e.tensor_tensor(out=lamt[:, 2:3], in0=lamt[:, 0:1], in1=lamt[:, 1:2], op=ALU.subtract), reads=[b_lamt], writes=[b_lamt])
        dve.op(lambda e: e.tensor_scalar(out=lamt[:, 2:3], in0=lamt[:, 2:3], scalar1=float(lam_init), scalar2=None, op0=ALU.add), reads=[b_lamt], writes=[b_lamt])
        dve.op(lambda e: e.tensor_scalar(out=lamt[:, 3:4], in0=lamt[:, 2:3], scalar1=-1.0, scalar2=None, op0=ALU.mult), reads=[b_lamt], writes=[b_lamt])
        dve.op(lambda e: e.tensor_scalar(out=gsub[:], in0=gsub[:], scalar1=float(1.0 - lam_init), scalar2=None, op0=ALU.mult), reads=[b_gsub], writes=[b_gsub])
        dve.op(lambda e: e.tensor_copy(out=v3(bfull[:], 8), in_=sbT[:].unsqueeze(2).to_broadcast([128, 8, 64])), reads=[b_sbT], writes=[b_bfull])
        for hh in range(2):
            sp.dma(lambda e, hh=hh: e.dma_start(out=v3(swl, 8)[:, 0:4, :], in_=sgu_w[l, hh * 4:(hh + 1) * 4].rearrange("h p q -> p h q")),
                   b_swl.sem, writes=[b_swl])
            for h4 in range(4):
                pe.op(lambda e, h4=h4: e.transpose(out=pbb[:, h4 * 128:(h4 + 1) * 128], in_=v3(swl, 8)[:, h4, :], identity=ident[:]),
                      reads=[b_swl, b_ident], writes=[b_pbb], pe_accum=(h4 > 0))
            act.op(lambda e, hh=hh: e.activation(out=wsT[:, hh * 512:(hh + 1) * 512], in_=pbb[:, 0:512], func=AF.Copy), reads=[b_pbb], writes=[b_wsT])
        for hg in range(12):
            g = hg // 2; half = hg % 2
            wb_ = wa[hg % 2]; bw = b_wa[hg % 2]
            sp.dma(lambda e, wb_=wb_, hg=hg: e.dma_start(out=v3(wb_, 8), in_=w_ada[l].rearrange("(k p) n -> p k n", p=128)[:, :, hg * 512:(hg + 1) * 512]),
                   bw.sem, writes=[bw])
            for nn in range(4):
                n = hg * 4 + nn
                for k in range(8):
                    pe.op(lambda e, wb_=wb_, nn=nn, k=k, n=n: e.matmul(pb[3][:, 2 * n:2 * n + 2], lhsT=v3(wb_, 8)[:, k, nn * 128:(nn + 1) * 128],
                                                                      rhs=siluc[:, 2 * k:2 * k + 2], start=(k == 0), stop=(k == 7)),
                          reads=[bw, b_siluc], writes=[b_pb[3]], pe_accum=not (hg == 0 and nn == 0 and k == 0))
            if g in (2, 5):
                which = 0 if g == 2 else 1
                sp.dma(lambda e, g=g, half=half: e.dma_start(out=brow, in_=b_ada[l:l + 1, g * 1024 + half * 512: g * 1024 + (half + 1) * 512].to_broadcast([128, 512])),
                       b_brow.sem, writes=[b_brow])
                for s in range(2):
                    for k in range(8):
                        pe.op(lambda e, wb_=wb_, s=s, k=k: e.matmul(pb[s][:, :], lhsT=srep[:, (2 * k + s) * 128:(2 * k + s + 1) * 128], rhs=v3(wb_, 8)[:, k, :],
                                                                    start=(k == 0), stop=(k == 7)),
                              reads=[bw, b_srep], writes=[b_pb[s]], pe_accum=(k > 0))
                    c0 = (s * 2 + which) * D + half * 512
                    dve.op(lambda e, s=s, c0=c0: e.tensor_tensor(out=grow[:, c0:c0 + 512], in0=pb[s][:, :], in1=brow, op=ALU.add),
                           reads=[b_pb[s], b_brow], writes=[b_grow])
        dve.op(lambda e: e.tensor_tensor(out=v3(modT[:], 48), in0=v3(pb[3][:, 0:96], 48), in1=bcol[:].unsqueeze(2).to_broadcast([128, 48, 2]), op=ALU.add),
               reads=[b_pb[3], b_bcol], writes=[b_modT])
        mT = v3(modT[:], 48)
        for w_ in range(2):
            for s in range(2):
                sc = mT[:, (1 + 3 * w_) * 8:(2 + 3 * w_) * 8, s]
                o = AB[:, (w_ * 2 + s) * 8:(w_ * 2 + s + 1) * 8]
                dve.op(lambda e, sc=sc, o=o, w_=w_: e.scalar_tensor_tensor(out=o, in0=sc, scalar=1.0, in1=ncol[:, w_ * 8:(w_ + 1) * 8], op0=ALU.add, op1=ALU.mult),
                       reads=[b_modT, b_ncol], writes=[b_AB])

        def Acol(w_, s, k): return AB[:, (w_ * 2 + s) * 8 + k:(w_ * 2 + s) * 8 + k + 1]
        def Bcol(w_, s, k): return modT[:, ((3 * w_) * 8 + k) * 2 + s:((3 * w_) * 8 + k) * 2 + s + 1]
        def growv(s, which): return grow[:, (s * 2 + which) * D:(s * 2 + which + 1) * D]
        P.barrier()

        AFa.reset(); ABa.reset()
        kT = ABa.alloc(4 * T); b_kT = P.buf("kT%d" % l)
        vaug = ABa.alloc(NT * 4 * 130); b_vaug = P.buf("vaug%d" % l)
        ab_mark = ABa.off
        w_in_sb = ABa.alloc(8 * 2560); b_win = P.buf("win", True)
        hT = [ABa.alloc(8 * 128) for _ in range(2)]; b_hT = [P.buf("hT%d_%d" % (l, i)) for i in range(2)]
        qr = ABa.alloc(512); b_qr = P.buf("qr", True)
        kr = ABa.alloc(512); b_kr = P.buf("kr%d" % l)
        gvn = ABa.alloc(512); b_gvn = P.buf("gvn%d" % l)
        sgo = ABa.alloc(512); b_sgo = P.buf("sgo", True)
        xt = [AFa.alloc(D) for _ in range(2)]; b_xt = [P.buf("xt_%d" % i, True) for i in range(2)]
        rp = [AFa.alloc(128) for _ in range(3)]; b_rp = [P.buf("rp_%d" % i, True) for i in range(3)]
        xn = AFa.alloc(D); b_xn = P.buf("xn%d" % l)
        junk = AFa.alloc(D); b_junk = P.buf("junk%d" % l)
        t1 = AFa.alloc(512); b_t1 = P.buf("t1%d" % l)
        t2 = AFa.alloc(512); b_t2 = P.buf("t2%d" % l)
        u_sb = AFa.alloc(512); b_u = P.buf("u%d" % l)
        gvg = AFa.alloc(512); b_gvg = P.buf("gvg%d" % l)
        sq = AFa.alloc(512); b_sq = P.buf("sq%d" % l)
        st = small[:, 0:32]; b_stN = P.buf("stN%d" % l); b_stP = P.buf("stP%d" % l)
        pool.dma([lambda e, i=i: e.dma_start(out=v3(w_in_sb, 8)[:, 2 * i:2 * i + 2, :], in_=w_in[l].rearrange("(k p) n -> p k n", p=128)[:, 2 * i:2 * i + 2, :]) for i in range(4)],
                 b_win.sem, writes=[b_win])
        va4 = vaug.rearrange("p (t h c) -> p t h c", t=NT, h=4)
        pool.op(lambda e: e.memset(vaug, 1.0), writes=[b_vaug])

        def load_tile(t):
            i = t % 2; i3 = t % 3
            sp.dma(lambda e: e.dma_start(out=xt[i], in_=xr[t * 128:(t + 1) * 128, :]), b_xt[i].sem, writes=[b_xt[i]])
            sp.dma(lambda e: e.dma_start(out=rp[i3], in_=rope_d[t * 128:(t + 1) * 128, :]), b_rp[i3].sem, writes=[b_rp[i3]])

        def N1(t):
            i = t % 2
            pool.op(lambda e: e.memset(st[:, 0:1], 0.0), writes=[b_stN])
            act.op(lambda e: e.activation(out=junk, in_=xt[i], func=AF.Square, accum_out=st[:, 0:1]), reads=[b_xt[i], b_stN], writes=[b_junk, b_stN])
            rstd_op(st[:, 1:2], st[:, 0:1], 1.0 / D, [b_stN])

        def N2(t):
            i = t % 2
            dve.op(lambda e: e.tensor_scalar(out=xn, in0=xt[i], scalar1=st[:, 1:2], scalar2=None, op0=ALU.mult), reads=[b_xt[i], b_stN], writes=[b_xn])

        def N3(t):
            for k in range(8):
                pe.op(lambda e: e.transpose(out=pbb[:, k * 128:(k + 1) * 128], in_=xn[:, k * 128:(k + 1) * 128], identity=ident[:]),
                      reads=[b_xn, b_ident], writes=[b_pbb], pe_accum=(k > 0))

        def N4(t):
            s_ = 0 if t < NLT else 1
            dst = hT[t % 2]
            for k in range(8):
                act.op(lambda e: e.activation(out=dst[:, k * 128:(k + 1) * 128], in_=pbb[:, k * 128:(k + 1) * 128], func=AF.Identity,
                                              scale=Acol(0, s_, k), bias=Bcol(0, s_, k)),
                       reads=[b_pbb, b_AB, b_modT], writes=[b_hT[t % 2]])

        def rope(src_ps, b_src, rp_, b_rp_, dst, b_dst):
            s5 = src_ps.rearrange("p (g a r f) -> p g a r f", g=8, a=2, r=2)
            t5 = t2.rearrange("p (g a r f) -> p g a r f", g=8, a=2, r=2)
            sn = rp_[:, 64:128].rearrange("p (a r f) -> p a r f", a=2, r=2)
            dve.op(lambda e: e.tensor_tensor(out=v3(t1, 8), in0=v3(src_ps, 8), in1=rp_[:, 0:64].unsqueeze(1).to_broadcast([128, 8, 64]), op=ALU.mult),
                   reads=[b_src, b_rp_], writes=[b_t1])
            for a in range(2):
                for r in range(2):
                    dve.op(lambda e: e.tensor_tensor(out=t5[:, :, a, r, :], in0=s5[:, :, a, 1 - r, :],
                                                     in1=sn[:, a, r, :].unsqueeze(1).to_broadcast([128, 8, 16]), op=ALU.mult),
                           reads=[b_src, b_rp_], writes=[b_t2])
            dve.op(lambda e: e.tensor_tensor(out=dst, in0=t1, in1=t2, op=ALU.add), reads=[b_t1, b_t2], writes=[b_dst])

        prot = [0, 1, 3, 4]; pcnt = [0]

        def proj(t, g):
            bi = prot[pcnt[0] % 4]; pcnt[0] += 1
            hsrc = hT[t % 2]
            for k in range(8):
                pe.op(lambda e: e.matmul(pb[bi][:, :], lhsT=hsrc[:, k * 128:(k + 1) * 128], rhs=v3(w_in_sb, 8)[:, k, g * 512:(g + 1) * 512],
                                         start=(k == 0), stop=(k == 7)),
                      reads=[b_hT[t % 2], b_win], writes=[b_pb[bi]], pe_accum=(k > 0))
            return pb[bi], b_pb[bi]

        load_tile(0); load_tile(1)
        N1(0); N2(0); N3(0); N4(0)
        for t in range(NT):
            nxt = t + 1 < NT
            i3 = t % 3
            need_q = t < NTq
            if t + 2 < NT:
                load_tile(t + 2)
            if nxt: N1(t + 1)
            if need_q:
                pbk, bpbk = proj(t, 0)
                rope(pbk[:, :], bpbk, rp[i3], b_rp[i3], qr, b_qr)
                sp.dma(lambda e: e.dma_start(out=qd[t * 128:(t + 1) * 128, :], in_=qr), b_qr.sem, reads=[b_qr])
            if nxt: N2(t + 1)
            pbk, bpbk = proj(t, 1)
            rope(pbk[:, :], bpbk, rp[i3], b_rp[i3], kr, b_kr)
            for j in range(4):
                pe.op(lambda e: e.transpose(out=pbh[:, j * 128:(j + 1) * 128], in_=kr[:, j * 128:(j + 1) * 128], identity=identb[:]),
                      reads=[b_kr, b_identb], writes=[b_pbh], pe_accum=(j > 0))
            act.op(lambda e: e.activation(out=v3(kT, 4)[:, :, t * 128:(t + 1) * 128], in_=v3(pbh[:, 0:512], 4), func=AF.Copy),
                   reads=[b_pbh], writes=[b_kT])
            if nxt: N3(t + 1)
            pbk, bpbk = proj(t, 2)
            act.op(lambda e: e.activation(out=va4[:, t, :, 0:128], in_=v3(pbk[:, :], 4), func=AF.Copy), reads=[bpbk], writes=[b_vaug])
            if nxt: N4(t + 1)
            if need_q:
                pbk, bpbk = proj(t, 3)
                act.op(lambda e: e.activation(out=u_sb, in_=pbk[:, :], func=AF.Gelu), reads=[bpbk], writes=[b_u])
                pbk, bpbk = proj(t, 4)
                act.op(lambda e: e.activation(out=gvg, in_=pbk[:, :], func=AF.Gelu), reads=[bpbk], writes=[b_gvg])
                dve.op(lambda e: e.tensor_tensor(out=sq, in0=gvg, in1=gvg, op=ALU.mult), reads=[b_gvg], writes=[b_sq])
                dve.op(lambda e: e.tensor_reduce(out=st[:, 8:16], in_=v3(sq, 8), axis=AX.X, op=ALU.add), reads=[b_sq], writes=[b_stP])
                rstd_op(st[:, 16:24], st[:, 8:16], 1.0 / 64, [b_stP])
                dve.op(lambda e: e.tensor_tensor(out=v3(sq, 8), in0=v3(gvg, 8), in1=st[:, 16:24].unsqueeze(2).to_broadcast([128, 8, 64]), op=ALU.mult),
                       reads=[b_gvg, b_stP], writes=[b_sq])
                dve.op(lambda e: e.tensor_tensor(out=gvn, in0=sq, in1=sgng[:], op=ALU.mult), reads=[b_sq, b_sgng], writes=[b_gvn])
                for h in range(8):
                    pe.op(lambda e: e.matmul(pb[2][:, h * 64:(h + 1) * 64], lhsT=wsT[:, h * 128:(h + 1) * 128], rhs=gvn[:, h * 64:(h + 1) * 64],
                                             start=True, stop=True),
                          reads=[b_wsT, b_gvn], writes=[b_pb[2]], pe_accum=(h > 0))
                dve.op(lambda e: e.tensor_tensor(out=sq, in0=pb[2][:, :], in1=bfull[:], op=ALU.add), reads=[b_pb[2], b_bfull], writes=[b_sq])
                dve.op(lambda e: e.tensor_tensor(out=sgo, in0=sq, in1=u_sb, op=ALU.mult), reads=[b_sq, b_u], writes=[b_sgo])
                sp.dma(lambda e: e.dma_start(out=sgd[t * 128:(t + 1) * 128, :], in_=sgo), b_sgo.sem, reads=[b_sgo])
        P.barrier()

        AFa.reset(); ABa.off = ab_mark
        w_out_sb = ABa.alloc(8 * D); b_wout = P.buf("wout", True)
        qg = ABa.alloc(4 * 512); b_qg = P.buf("qg", True)
        qTm = [ABa.alloc(8 * 512) for _ in range(2)]; b_qTm = [P.buf("qTm%d_%d" % (l, i)) for i in range(2)]
        pT = [ABa.alloc(512) for _ in range(3)]; b_pT = [P.buf("pT%d_%d" % (l, i)) for i in range(3)]
        mixo = [[ABa.alloc(512) for _ in range(4)] for _ in range(2)]
        b_mixo = [[P.buf("mixo%d_%d_%d" % (l, pp, i)) for i in range(4)] for pp in range(2)]
        mixT = ABa.alloc(8 * 128); b_mixT = P.buf("mixT%d" % l)
        sgi = [ABa.alloc(512) for _ in range(2)]; b_sgi = [P.buf("sgi_%d" % i, True) for i in range(2)]
        h2 = ABa.alloc(D); b_h2 = P.buf("h2", True)
        affT = AFa.alloc(T); b_affT = P.buf("affT%d" % l)
        af_mark = AFa.off
        xt = [AFa.alloc(D) for _ in range(2)]; b_xt = [P.buf("xt_%d" % i, True) for i in range(2)]
        x1 = AFa.alloc(D); b_x1 = P.buf("x1", True)
        xn = AFa.alloc(D); b_xn = P.buf("xnc%d" % l)
        junk = AFa.alloc(D); b_junk = P.buf("junkc%d" % l)
        h2T = AFa.alloc(8 * 128); b_h2T = P.buf("h2T%d" % l)
        accS = AFa.alloc(8 * 130); b_accS = P.buf("accS%d" % l)
        t1 = AFa.alloc(128); b_t1 = P.buf("t1c%d" % l)
        osb = [AFa.alloc(128) for _ in range(4)]; b_osb = [P.buf("osb%d_%d" % (l, i)) for i in range(4)]
        af = AFa.alloc(NE); b_af = P.buf("af%d" % l)
        st = small[:, 0:32]
        b_stT = P.buf("stT%d" % l); b_stE = P.buf("stE%d" % l); b_stE2 = P.buf("stE2%d" % l)
        b_pbR = b_pbh; b_pbAf = b_pbh
        pool.dma(lambda e: e.dma_start(out=v3(w_out_sb, 8), in_=w_out[l].rearrange("(k p) n -> p k n", p=128)), b_wout.sem, writes=[b_wout])
        for pp in range(2):
            pool.op(lambda e: e.memset(qTm[pp], 0.0), writes=[b_qTm[pp]])

        def acc_ap(a):
            bank = 2 + a // 3; off = (a % 3) * 160
            return pb[bank][:, off:off + 130], b_pb[bank]

        qgroups = [(G * 4, 4) for G in range(8)] + ([] if last else [(NLT, 2)])

        def build_q(gi):
            t0, nq = qgroups[gi]; par = gi % 2
            ths = []

            def ld():
                sp.dma(lambda e: e.dma_start(out=v3(qg, 4)[:, 0:nq, :], in_=qd[t0 * 128:(t0 + nq) * 128, :].rearrange("(a p) n -> p a n", p=128)),
                       b_qg.sem, writes=[b_qg])
            ths.append(ld)
            for qi in range(nq):
                def f(qi=qi):
                    for j in range(4):
                        pe.op(lambda e: e.transpose(out=pbh[:, j * 128:(j + 1) * 128], in_=v3(qg, 4)[:, qi, j * 128:(j + 1) * 128], identity=identb[:]),
                              reads=[b_qg, b_identb], writes=[b_pbh], pe_accum=(j > 0))
                    for hf in range(2):
                        dst = qTm[par].rearrange("p (j f q) -> p j f q", j=4, f=2)[hf * 64:(hf + 1) * 64, :, hf, qi * 128:(qi + 1) * 128]
                        src = v3(pbh[:, 0:512], 4)[hf * 64:(hf + 1) * 64, :, :]
                        act.op(lambda e: e.activation(out=dst, in_=src, func=AF.Copy), reads=[b_pbh], writes=[b_qTm[par]])
                ths.append(f)
            return ths

        def tile_thunks(gi, qi):
            t0, nq = qgroups[gi]; par = gi % 2
            t = t0 + qi; ii = t % 2
            s_ = 0 if t0 < NLT else 1
            mo = mixo[par][qi]; bmo = b_mixo[par][qi]
            ths = []

            def s1():
                sp.dma(lambda e: e.dma_start(out=sgi[ii], in_=sgd[t * 128:(t + 1) * 128, :]), b_sgi[ii].sem, writes=[b_sgi[ii]])
                sp.dma(lambda e: e.dma_start(out=xt[ii], in_=xr[t * 128:(t + 1) * 128, :]), b_xt[ii].sem, writes=[b_xt[ii]])
                for half, (srcb, bsrc) in enumerate([(mo, bmo), (sgi[ii], b_sgi[ii])]):
                    for j in range(4):
                        pe.op(lambda e: e.transpose(out=pbh[:, j * 128:(j + 1) * 128], in_=srcb[:, j * 128:(j + 1) * 128], identity=identb[:]),
                              reads=[bsrc, b_identb], writes=[b_pbh], pe_accum=(j > 0))
                    act.op(lambda e: e.activation(out=mixT[:, half * 512:(half + 1) * 512], in_=pbh[:, 0:512], func=AF.Copy), reads=[b_pbh], writes=[b_mixT])
            ths.append(s1)

            def s2():
                for hv in range(2):
                    for f in range(8):
                        pe.op(lambda e: e.matmul(pbb[:, hv * 512:(hv + 1) * 512], lhsT=mixT[:, f * 128:(f + 1) * 128],
                                                 rhs=v3(w_out_sb, 8)[:, f, hv * 512:(hv + 1) * 512], start=(f == 0), stop=(f == 7)),
                              reads=[b_mixT, b_wout], writes=[b_pbb], pe_accum=not (hv == 0 and f == 0))
            ths.append(s2)

            def s3():
                dve.op(lambda e: e.tensor_tensor(out=x1, in0=pbb[:, :], in1=growv(s_, 0), op=ALU.mult), reads=[b_pbb, b_grow], writes=[b_x1])
                dve.op(lambda e: e.tensor_tensor(out=x1, in0=x1, in1=xt[ii], op=ALU.add), reads=[b_x1, b_xt[ii]], writes=[b_x1])
                sp.dma(lambda e: e.dma_start(out=xr[t * 128:(t + 1) * 128, :], in_=x1), b_x1.sem, reads=[b_x1])
                pool.op(lambda e: e.memset(st[:, 0:1], 0.0), writes=[b_stT])
                act.op(lambda e: e.activation(out=junk, in_=x1, func=AF.Square, accum_out=st[:, 0:1]), reads=[b_x1, b_stT], writes=[b_junk, b_stT])
            ths.append(s3)

            def s4():
                rstd_op(st[:, 1:2], st[:, 0:1], 1.0 / D, [b_stT])
            ths.append(s4)

            def s5():
                dve.op(lambda e: e.tensor_scalar(out=xn, in0=x1, scalar1=st[:, 1:2], scalar2=None, op0=ALU.mult), reads=[b_x1, b_stT], writes=[b_xn])
                for k in range(8):
                    pe.op(lambda e: e.transpose(out=pbb[:, k * 128:(k + 1) * 128], in_=xn[:, k * 128:(k + 1) * 128], identity=ident[:]),
                          reads=[b_xn, b_ident], writes=[b_pbb], pe_accum=(k > 0))
            ths.append(s5)

            def s6():
                for k in range(8):
                    act.op(lambda e: e.activation(out=h2T[:, k * 128:(k + 1) * 128], in_=pbb[:, k * 128:(k + 1) * 128], func=AF.Identity,
                                                  scale=Acol(1, s_, k), bias=Bcol(1, s_, k)),
                           reads=[b_pbb, b_AB, b_modT], writes=[b_h2T])
            ths.append(s6)

            def s7():
                for k in range(8):
                    pe.op(lambda e: e.matmul(pbr[:, 0:NE], lhsT=h2T[:, k * 128:(k + 1) * 128], rhs=wrt[:, k * NE:(k + 1) * NE], start=(k == 0), stop=(k == 7)),
                          reads=[b_h2T, b_wrt], writes=[b_pbR], pe_accum=(k > 0))
                for k in range(8):
                    pe.op(lambda e: e.transpose(out=pbb[:, k * 128:(k + 1) * 128], in_=h2T[:, k * 128:(k + 1) * 128], identity=ident[:]),
                          reads=[b_h2T, b_ident], writes=[b_pbb], pe_accum=(k > 0))
            ths.append(s7)

            def s8():
                act.op(lambda e: e.activation(out=h2, in_=pbb[:, :], func=AF.Copy), reads=[b_pbb], writes=[b_h2])
                sp.dma(lambda e: e.dma_start(out=h2d[t * 128:(t + 1) * 128, :], in_=h2), b_h2.sem, reads=[b_h2])
                dve.op(lambda e: e.tensor_reduce(out=st[:, 5:6], in_=pbr[:, 0:NE], axis=AX.X, op=ALU.max), reads=[b_pbR], writes=[b_stT])
                dve.op(lambda e: e.tensor_scalar(out=st[:, 5:6], in0=st[:, 5:6], scalar1=-1.0, scalar2=None, op0=ALU.mult), reads=[b_stT], writes=[b_stT])
                pool.op(lambda e: e.memset(st[:, 6:7], 0.0), writes=[b_stT])
                act.op(lambda e: e.activation(out=af, in_=pbr[:, 0:NE], func=AF.Exp, bias=st[:, 5:6], scale=1.0, accum_out=st[:, 6:7]),
                       reads=[b_pbR, b_stT], writes=[b_af, b_stT])
            ths.append(s8)

            def s9():
                dve.op(lambda e: e.reciprocal(out=st[:, 6:7], in_=st[:, 6:7]), reads=[b_stT], writes=[b_stT])
                dve.op(lambda e: e.tensor_scalar(out=af, in0=af, scalar1=st[:, 6:7], scalar2=None, op0=ALU.mult), reads=[b_af, b_stT], writes=[b_af])
                pe.op(lambda e: e.transpose(out=pbr[0:NE, 128:256], in_=af, identity=ident[:]), reads=[b_af, b_ident], writes=[b_pbAf])
            ths.append(s9)

            def s10():
                dve.op(lambda e: e.tensor_copy(out=affT[0:NE, t * 128:(t + 1) * 128], in_=pbr[0:NE, 128:256]), reads=[b_pbAf], writes=[b_affT])
            ths.append(s10)
            return ths

        from collections import deque
        bg = deque()
        for th in build_q(0):
            th()
        a8 = v3(accS, 8)
        pti = 0
        for gi, (t0, nq) in enumerate(qgroups):
            par = gi % 2
            s = 0 if t0 < NLT else 1
            NQ = nq * 128
            kts = list(range(NT)) if s == 0 else [NLT, NLT + 1]
            if gi + 1 < len(qgroups):
                bg.extendleft(reversed(build_q(gi + 1)))
            iters = [(h, c, ki, kt) for h in range(4) for c in range(2) for ki, kt in enumerate(kts)]
            timed = []

            def emit_S(idx):
                h, c, ki, kt = iters[idx]; sb = idx % 2
                i8 = c * 4 + h; j = i8 // 2
                pe.op(lambda e: e.matmul(pb[sb][:, 0:NQ], lhsT=v3(kT, 4)[:, j, kt * 128:(kt + 1) * 128], rhs=v3(qTm[par], 8)[:, i8, 0:NQ], start=True, stop=True),
                      reads=[b_kT, b_qTm[par]], writes=[b_pb[sb]])

            def E1(h):
                for bank in range(3):
                    na = 3 if bank < 2 else 2
                    src = pb[2 + bank][:, 0:na * 160].rearrange("p (a c) -> p a c", a=na)[:, :, 0:130]
                    dve.op(lambda e: e.tensor_copy(out=a8[:, bank * 3:bank * 3 + na, :], in_=src), reads=[b_pb[2 + bank]], writes=[b_accS])
                for qi in range(nq):
                    a0 = a8[:, qi, :]; a1 = a8[:, 4 + qi, :]
                    dve.op(lambda e: e.reciprocal(out=st[:, 2:3], in_=a0[:, 128:129]), reads=[b_accS], writes=[b_stE])
                    dve.op(lambda e: e.reciprocal(out=st[:, 3:4], in_=a1[:, 128:129]), reads=[b_accS], writes=[b_stE])
                    dve.op(lambda e: e.tensor_tensor(out=st[:, 3:4], in0=st[:, 3:4], in1=lamt[:, 3:4], op=ALU.mult), reads=[b_stE, b_lamt], writes=[b_stE])
                    dve.op(lambda e: e.tensor_scalar(out=t1, in0=a1[:, 0:128], scalar1=st[:, 3:4], scalar2=None, op0=ALU.mult), reads=[b_accS, b_stE], writes=[b_t1])
                    dve.op(lambda e: e.scalar_tensor_tensor(out=osb[qi], in0=a0[:, 0:128], scalar=st[:, 2:3], in1=t1, op0=ALU.mult, op1=ALU.add),
                           reads=[b_accS, b_stE, b_t1], writes=[b_osb[qi]])
                    dve.op(lambda e: e.tensor_tensor(out=t1, in0=osb[qi], in1=osb[qi], op=ALU.mult), reads=[b_osb[qi]], writes=[b_t1])
                    dve.op(lambda e: e.tensor_reduce(out=st[:, 8 + qi:9 + qi], in_=t1, axis=AX.X, op=ALU.add), reads=[b_t1], writes=[b_stE2])

            def E2(h):
                rstd_op(st[:, 8:8 + nq], st[:, 8:8 + nq], 1.0 / 128, [b_stE2])

            def E3(h):
                for qi in range(nq):
                    dve.op(lambda e: e.scalar_tensor_tensor(out=mixo[par][qi][:, h * 128:(h + 1) * 128], in0=osb[qi], scalar=st[:, 8 + qi:9 + qi], in1=gsub[:], op0=ALU.mult, op1=ALU.mult),
                           reads=[b_osb[qi], b_stE2, b_gsub], writes=[b_mixo[par][qi]])

            import os as _os
            NOLOOK = _os.environ.get("KNOLOOK") == "1"; NOBG = _os.environ.get("KNOBG") == "1"
            if not NOLOOK:
                emit_S(0)
            for idx in range(len(iters)):
                if NOLOOK:
                    emit_S(idx)
                elif idx + 1 < len(iters):
                    emit_S(idx + 1)
                h, c, ki, kt = iters[idx]; sb = idx % 2
                pi = pti % 3; pti += 1
                act.op(lambda e: e.activation(out=pT[pi][:, 0:NQ], in_=pb[sb][:, 0:NQ], func=AF.Exp, scale=0.125),
                       reads=[b_pb[sb]], writes=[b_pT[pi]])
                banks_seen = set()
                for qi in range(nq):
                    a_ = c * 4 + qi
                    aap, bacc = acc_ap(a_)
                    first_in_bank = (a_ // 3) not in banks_seen
                    banks_seen.add(a_ // 3)
                    pe.op(lambda e: e.matmul(aap, lhsT=pT[pi][:, qi * 128:(qi + 1) * 128], rhs=va4[:, kt, h, :],
                                             start=(ki == 0 and first_in_bank), stop=(ki == len(kts) - 1), skip_group_check=True),
                          reads=[b_pT[pi], b_vaug], writes=[bacc], pe_accum=(ki > 0))
                if c == 1 and ki == len(kts) - 1:
                    while timed:
                        timed.pop(0)[1]()
                    E1(h)
                    timed.append((idx + 6, lambda h=h: E2(h)))
                    timed.append((idx + 12, lambda h=h: E3(h)))
                while timed and timed[0][0] <= idx:
                    timed.pop(0)[1]()
                if idx % 3 == 2 and bg and not NOBG:
                    bg.popleft()()
            while timed:
                timed.pop(0)[1]()
            while NOBG and bg:
                bg.popleft()()
            for qi in range(nq):
                bg.extend(tile_thunks(gi, qi))
        while bg:
            bg.popleft()()
        P.barrier()

        AFa.off = af_mark; ABa.reset()
        NTOK = CAP if last else CAP + CCAP
        NJ = 4 if last else 5
        work = affT[:, 0:SEQ]; b_work = b_affT
        workc = affT[:, SEQ:T]
        vals = AFa.alloc(NTOK); b_vals = P.buf("vals%d" % l)
        idxu = AFa.alloc(NTOK).bitcast(U32); b_idxu = P.buf("idxu%d" % l)
        idxf = AFa.alloc(NTOK); b_idxf = P.buf("idxf%d" % l)
        gT = AFa.alloc(5 * NE); b_gT = P.buf("gT%d" % l)
        idxT = AFa.alloc(5 * NE).bitcast(I32); b_idxT = P.buf("idxT%d" % l)
        sgs = [AFa.alloc(544) for _ in range(2)]; b_sgs = [P.buf("sgs%d_%d" % (l, i)) for i in range(2)]
        ysc = AFa.alloc(5 * D); b_ysc = P.buf("ysc%d" % l)
        ring = [ABa.alloc(8192) for _ in range(3)]; b_ring = [P.buf("ring_%d" % i, True) for i in range(3)]
        xs = [ABa.alloc(5 * D) for _ in range(2)]; b_xs = [P.buf("xs_%d" % i, True) for i in range(2)]
        xsT = ABa.alloc(8 * 544); b_xsT = P.buf("xsT%d" % l)
        hidT = ABa.alloc(16 * 544); b_hidT = P.buf("hidT%d" % l)
        for r in range(CAP // 8):
            dve.op(lambda e, r=r: e.max(out=vals[0:NE, r * 8:(r + 1) * 8], in_=work[0:NE, :]), reads=[b_work], writes=[b_vals])
            dve.op(lambda e, r=r: e.max_index(out=idxu[0:NE, r * 8:(r + 1) * 8], in_max=vals[0:NE, r * 8:(r + 1) * 8], in_values=work[0:NE, :]),
                   reads=[b_work, b_vals], writes=[b_idxu])
            if r < CAP // 8 - 1:
                dve.op(lambda e, r=r: e.match_replace(out=work[0:NE, :], in_to_replace=vals[0:NE, r * 8:(r + 1) * 8], in_values=work[0:NE, :], imm_value=-1.0),
                       reads=[b_vals, b_work], writes=[b_work])
        if not last:
            for r in range(CCAP // 8):
                c0 = CAP + r * 8
                dve.op(lambda e, c0=c0: e.max(out=vals[0:NE, c0:c0 + 8], in_=workc[0:NE, :]), reads=[b_work], writes=[b_vals])
                dve.op(lambda e, c0=c0: e.max_index(out=idxu[0:NE, c0:c0 + 8], in_max=vals[0:NE, c0:c0 + 8], in_values=workc[0:NE, :]),
                       reads=[b_work, b_vals], writes=[b_idxu])
                if r < CCAP // 8 - 1:
                    dve.op(lambda e, c0=c0: e.match_replace(out=workc[0:NE, :], in_to_replace=vals[0:NE, c0:c0 + 8], in_values=workc[0:NE, :], imm_value=-1.0),
                           reads=[b_vals, b_work], writes=[b_work])
        dve.op(lambda e: e.tensor_copy(out=idxf[0:NE, :], in_=idxu[0:NE, :]), reads=[b_idxu], writes=[b_idxf])
        if not last:
            dve.op(lambda e: e.tensor_scalar(out=idxf[0:NE, CAP:NTOK], in0=idxf[0:NE, CAP:NTOK], scalar1=float(SEQ), scalar2=None, op0=ALU.add),
                   reads=[b_idxf], writes=[b_idxf])
        for (srcv, bsrc, dstv, bdst) in ((idxf, b_idxf, idxT, b_idxT), (vals, b_vals, gT, b_gT)):
            for jc in range(NJ):
                nj = 128 if jc < 4 else CCAP
                pe.op(lambda e, jc=jc, nj=nj, srcv=srcv: e.transpose(out=pb[4][0:nj, jc * NE:(jc + 1) * NE], in_=srcv[0:NE, jc * 128:jc * 128 + nj], identity=ident[0:NE, 0:NE]),
                      reads=[bsrc, b_ident], writes=[b_pb[4]], pe_accum=(jc > 0))
            dve.op(lambda e, dstv=dstv: e.tensor_copy(out=dstv[:, 0:4 * NE], in_=pb[4][:, 0:4 * NE]), reads=[b_pb[4]], writes=[bdst])
            if not last:
                dve.op(lambda e, dstv=dstv: e.tensor_copy(out=dstv[0:CCAP, 4 * NE:5 * NE], in_=pb[4][0:CCAP, 4 * NE:5 * NE]), reads=[b_pb[4]], writes=[bdst])

        def gather(e_):
            xb = xs[e_ % 2]; bx = b_xs[e_ % 2]
            NJs = [(jc, 128 if jc < 4 else CCAP) for jc in range(NJ)]
            pool.dma([lambda e, jc=jc, nj=nj: e.indirect_dma_start(
                out=xb[0:nj, jc * D:(jc + 1) * D], out_offset=None, in_=h2d[:, :],
                in_offset=bass.IndirectOffsetOnAxis(ap=idxT[0:nj, jc * NE + e_: jc * NE + e_ + 1], axis=0)) for (jc, nj) in NJs],
                bx.sem, reads=[b_idxT], writes=[bx])

        pieces = [(e_, p_) for e_ in range(NE) for p_ in range(6)]

        def load_piece(pi_):
            e_, p_ = pieces[pi_]
            rb = ring[pi_ % 3]; brb = b_ring[pi_ % 3]
            if p_ < 4:
                pool.dma([lambda e: e.dma_start(out=v3(rb[:, 0:4096], 8), in_=w_gate[l, e_].rearrange("(k p) n -> p k n", p=128)[:, :, p_ * 512:(p_ + 1) * 512]),
                          lambda e: e.dma_start(out=v3(rb[:, 4096:8192], 8), in_=w_up[l, e_].rearrange("(k p) n -> p k n", p=128)[:, :, p_ * 512:(p_ + 1) * 512])],
                         brb.sem, writes=[brb])
            else:
                hv = p_ - 4
                pool.dma([lambda e, q_=q_: e.dma_start(
                    out=v3(rb[:, q_ * 4096:(q_ + 1) * 4096], 8),
                    in_=w_down[l, e_].rearrange("(f p) n -> p f n", p=128)[:, q_ * 8:(q_ + 1) * 8, hv * 512:(hv + 1) * 512]) for q_ in range(2)],
                    brb.sem, writes=[brb])

        b_xrs = P.buf("xrs", True)
        gather(0)
        load_piece(0); load_piece(1)
        sgi_ = [0]
        for e_ in range(NE):
            xb = xs[e_ % 2]; bx = b_xs[e_ % 2]
            if e_ + 1 < NE:
                gather(e_ + 1)
            for jc in range(NJ):
                nj = 128 if jc < 4 else CCAP
                for kh in range(2):
                    for k4 in range(4):
                        k = kh * 4 + k4
                        pe.op(lambda e: e.transpose(out=pbh[:, k4 * 128:k4 * 128 + nj], in_=xb[0:nj, jc * D + k * 128: jc * D + (k + 1) * 128],
                                                    identity=identb[0:nj, 0:nj]),
                              reads=[bx, b_identb], writes=[b_pbh], pe_accum=(k4 > 0))
                    act.op(lambda e: e.activation(out=v3(xsT, 8)[:, kh * 4:(kh + 1) * 4, jc * 128:jc * 128 + nj], in_=v3(pbh[:, :], 4)[:, :, 0:nj], func=AF.Copy),
                           reads=[b_pbh], writes=[b_xsT])
            for p_ in range(6):
                pi_ = e_ * 6 + p_
                if pi_ + 2 < len(pieces):
                    load_piece(pi_ + 2)
                rb = ring[pi_ % 3]; brb = b_ring[pi_ % 3]
                if p_ < 4:
                    for fi in range(4):
                        f = p_ * 4 + fi
                        gb = fi % 2
                        for wi, (woff, pbt, bpbt) in enumerate(((0, pb[gb], b_pb[gb]), (4096, pb[2 + gb], b_pb[2 + gb]))):
                            for k in range(8):
                                pe.op(lambda e, rb=rb, woff=woff, k=k, fi=fi, pbt=pbt: e.matmul(pbt[:, 0:CAP], lhsT=v3(rb[:, woff:woff + 4096], 8)[:, k, fi * 128:(fi + 1) * 128],
                                                                                              rhs=v3(xsT, 8)[:, k, 0:CAP], start=(k == 0), stop=(k == 7)),
                                      reads=[brb, b_xsT], writes=[bpbt], pe_accum=(k > 0))
                            if not last:
                                co = gb * 64 + wi * 32
                                for k in range(8):
                                    pe.op(lambda e, rb=rb, woff=woff, k=k, fi=fi, co=co: e.matmul(pb[4][:, co:co + CCAP], lhsT=v3(rb[:, woff:woff + 4096], 8)[:, k, fi * 128:(fi + 1) * 128],
                                                                                                rhs=v3(xsT, 8)[:, k, CAP:NTOK], start=(k == 0), stop=(k == 7)),
                                          reads=[brb, b_xsT], writes=[b_pb[4]], pe_accum=(k > 0))
                        sg_ = sgs[sgi_[0] % 2]; bsg = b_sgs[sgi_[0] % 2]; sgi_[0] += 1
                        act.op(lambda e, gb=gb, sg_=sg_: e.activation(out=sg_[:, 0:CAP], in_=pb[gb][:, 0:CAP], func=AF.Silu), reads=[b_pb[gb]], writes=[bsg])
                        dve.op(lambda e, gb=gb, sg_=sg_, f=f: e.tensor_tensor(out=v3(hidT, 16)[:, f, 0:CAP], in0=sg_[:, 0:CAP], in1=pb[2 + gb][:, 0:CAP], op=ALU.mult),
                               reads=[bsg, b_pb[2 + gb]], writes=[b_hidT])
                        if not last:
                            co = gb * 64
                            act.op(lambda e, co=co, sg_=sg_: e.activation(out=sg_[:, CAP:NTOK], in_=pb[4][:, co:co + CCAP], func=AF.Silu), reads=[b_pb[4]], writes=[bsg])
                            dve.op(lambda e, co=co, sg_=sg_, f=f: e.tensor_tensor(out=v3(hidT, 16)[:, f, CAP:NTOK], in0=sg_[:, CAP:NTOK], in1=pb[4][:, co + 32:co + 32 + CCAP], op=ALU.mult),
                                   reads=[bsg, b_pb[4]], writes=[b_hidT])
                else:
                    hv = p_ - 4
                    for jc in range(NJ):
                        nj = 128 if jc < 4 else CCAP
                        yb = jc % 2
                        for f in range(16):
                            pe.op(lambda e, rb=rb, f=f, jc=jc, nj=nj, yb=yb: e.matmul(pbb[0:nj, yb * 512:(yb + 1) * 512], lhsT=v3(hidT, 16)[:, f, jc * 128:jc * 128 + nj],
                                                                                    rhs=v3(rb[:, (f // 8) * 4096:(f // 8 + 1) * 4096], 8)[:, f % 8, :], start=(f == 0), stop=(f == 15)),
                                  reads=[b_hidT, brb], writes=[b_pbb], pe_accum=(f > 0))
                        s = 0 if jc < 4 else 1
                        dve.op(lambda e, jc=jc, nj=nj, yb=yb, hv=hv, s=s, e_=e_: e.scalar_tensor_tensor(
                            out=ysc[0:nj, jc * D + hv * 512: jc * D + (hv + 1) * 512], in0=pbb[0:nj, yb * 512:(yb + 1) * 512],
                            scalar=gT[0:nj, jc * NE + e_: jc * NE + e_ + 1], in1=growv(s, 1)[0:nj, hv * 512:(hv + 1) * 512], op0=ALU.mult, op1=ALU.mult),
                            reads=[b_pbb, b_gT, b_grow], writes=[b_ysc])
            NJs = [(jc, 128 if jc < 4 else CCAP) for jc in range(NJ)]
            pool.dma([lambda e, jc=jc, nj=nj: e.indirect_dma_start(
                out=xr[:, :], out_offset=bass.IndirectOffsetOnAxis(ap=idxT[0:nj, jc * NE + e_: jc * NE + e_ + 1], axis=0),
                in_=ysc[0:nj, jc * D:(jc + 1) * D], in_offset=None, compute_op=ALU.add) for (jc, nj) in NJs],
                b_xrs.sem, reads=[b_ysc, b_idxT, b_xrs], writes=[b_xrs])
        P.barrier()

    AFa.reset(); ABa.reset()
    gf = AFa.alloc(D); b_gf = P.buf("gf", True)
    xt = [AFa.alloc(D) for _ in range(2)]; b_xt = [P.buf("xt_%d" % i, True) for i in range(2)]
    yo = [AFa.alloc(D) for _ in range(2)]; b_yo = [P.buf("yo%d" % i, True) for i in range(2)]
    junk = AFa.alloc(D); b_junk = P.buf("junkf")
    st = small[:, 32:40]; b_st = P.buf("stf")
    sp.dma(lambda e: e.dma_start(out=gf, in_=norm_f_g[0:1, :].to_broadcast([128, D])), b_gf.sem, writes=[b_gf])
    for t in range(NLT):
        i = t % 2
        sp.dma(lambda e, t=t, i=i: e.dma_start(out=xt[i], in_=xr[t * 128:(t + 1) * 128, :]), b_xt[i].sem, writes=[b_xt[i]])
        if final_norm:
            pool.op(lambda e: e.memset(st[:, 0:1], 0.0), writes=[b_st])
            act.op(lambda e, i=i: e.activation(out=junk, in_=xt[i], func=AF.Square, accum_out=st[:, 0:1]), reads=[b_xt[i], b_st], writes=[b_junk, b_st])
            rstd_op(st[:, 1:2], st[:, 0:1], 1.0 / D, [b_st])
            dve.op(lambda e, i=i: e.scalar_tensor_tensor(out=yo[i], in0=xt[i], scalar=st[:, 1:2], in1=gf, op0=ALU.mult, op1=ALU.mult),
                   reads=[b_xt[i], b_st, b_gf], writes=[b_yo[i]])
        else:
            dve.op(lambda e, i=i: e.tensor_copy(out=yo[i], in_=xt[i]), reads=[b_xt[i]], writes=[b_yo[i]])
        sp.dma(lambda e, t=t, i=i: e.dma_start(out=out_d[t * 128:(t + 1) * 128, :], in_=yo[i]), b_yo[i].sem, reads=[b_yo[i]])
    P.finish()
    return nc


def _rope_table():
    rows = SEQ // 64
    row = np.repeat(np.arange(rows), 64).astype(np.float32)
    col = np.tile(np.arange(64), rows).astype(np.float32)
    n_freq = 16
    freqs = (np.float32(10000.0) ** (-np.arange(n_freq, dtype=np.float32) / np.float32(n_freq))).astype(np.float32)
    ang_r = (row[:, None] * freqs).astype(np.float32)
    ang_c = (col[:, None] * freqs).astype(np.float32)
    ang = np.concatenate([ang_r, ang_r, ang_c, ang_c], axis=-1)
    cos = np.cos(ang).astype(np.float32); sin = np.sin(ang).astype(np.float32)
    tab = np.zeros((T, 128), np.float32)
    tab[:SEQ, 0:64] = cos
    sgn = np.ones((2, 2, 16), np.float32); sgn[:, 0, :] = -1.0
    tab[:SEQ, 64:128] = sin * sgn.reshape(64)
    tab[SEQ:, 0:64] = 1.0
    return tab


def prep_shared(inp):
    f = lambda a: np.ascontiguousarray(np.asarray(a, dtype=np.float32))
    sh = {
        "rope": _rope_table(), "ident": np.eye(128, dtype=np.float32),
        "w_ada": f(inp["w_ada"]), "b_ada": f(inp["b_ada"]),
        "b_ada_col": f(np.asarray(inp["b_ada"]).reshape(DEPTH, 48, 128).transpose(0, 2, 1)),
        "n1col": f(np.asarray(inp["norm1_g"]).reshape(DEPTH, 8, 128).transpose(0, 2, 1)),
        "n2col": f(np.asarray(inp["norm2_g"]).reshape(DEPTH, 8, 128).transpose(0, 2, 1)),
        "w_in": f(inp["w_in"]), "w_out": f(inp["w_out"]),
        "lamv": f(np.concatenate([np.asarray(inp["lambda_q1"]), np.asarray(inp["lambda_k1"]), np.asarray(inp["lambda_q2"]), np.asarray(inp["lambda_k2"])], axis=1)),
        "subln_g": f(inp["subln_g"]), "sgu_norm_g": f(inp["sgu_norm_g"]), "sgu_w": f(inp["sgu_w"]),
        "sgu_bT": f(np.asarray(inp["sgu_b"]).transpose(0, 2, 1)),
        "w_router": f(inp["w_router"]), "w_gate": f(inp["w_gate"]), "w_up": f(inp["w_up"]), "w_down": f(inp["w_down"]),
        "norm_f_g": f(np.asarray(inp["norm_f_g"]).reshape(1, D)),
    }
    return sh


def prep_core(inp, b):
    x = np.asarray(inp["x"], dtype=np.float32)[b]; cx = np.asarray(inp["ctx"], dtype=np.float32)[b]
    c = np.asarray(inp["c"], dtype=np.float32)[b]; cctx = np.asarray(inp["c_ctx"], dtype=np.float32)
    cc = np.stack([c.reshape(8, 128).T, cctx.reshape(8, 128).T], axis=-1).reshape(128, 16)
    return {"x": np.ascontiguousarray(np.concatenate([x, cx], axis=0)), "cc": np.ascontiguousarray(cc)}


def kernel(**inputs):
    sh = prep_shared(inputs)
    nc = build()
    in_maps = []
    for b in range(8):
        m = dict(sh); m.update(prep_core(inputs, b)); in_maps.append(m)
    res = run_bass_kernel_spmd(nc, in_maps, core_ids=list(range(8)))
    return np.stack([np.asarray(r["out"], dtype=np.float32) for r in res.results], axis=0)
```

```python
import math
import numpy as np
from contextlib import ExitStack
import concourse.bass as bass
import concourse.mybir as mybir
from concourse.bass_utils import run_bass_kernel_spmd

F32 = mybir.dt.float32; BF16 = mybir.dt.bfloat16; I32 = mybir.dt.int32; U32 = mybir.dt.uint32
AF = mybir.ActivationFunctionType; ALU = mybir.AluOpType; AX = mybir.AxisListType

D = 1024; SEQ = 4096; CTX = 256; T = SEQ + CTX; NT = T // 128; NLT = SEQ // 128
DEPTH = 4; NE = 16; CAP = 512; CCAP = 32; FF = 2048
EPS = 1e-6


class Sem:
    def __init__(self, h, name):
        self.h = h; self.name = name; self.val = 0


class Buf:
    def __init__(self, name, sem=None, excl=False):
        self.name = name; self.w = None; self.r = {}; self.sem = sem; self.excl = excl


class Call:
    def __init__(self, name, a, k):
        self.name = name; self.a = a; self.k = k

    def run(self, e):
        return getattr(e, self.name)(*self.a, **self.k)


class Rec:
    def __getattr__(self, name):
        return lambda *a, **k: Call(name, a, k)


REC = Rec()


class Eng:
    def __init__(self, prog, name, sem):
        self.prog = prog; self.name = name; self.sem = sem; self.q = []; self.waited = {}

    def wait_tok(self, sem, val):
        if self.name == 'pe' and sem is self.sem:
            return
        if self.waited.get(sem, 0) >= val:
            return
        self.waited[sem] = val
        h = sem.h
        self.q.append(lambda e, h=h, val=val: e.wait_ge(h, val))

    def deps(self, reads, writes, pe_accum=False, nowaw=False):
        for b in reads:
            if b.w is not None:
                self.wait_tok(*b.w)
            if b.excl:
                for s, v in b.r.items():
                    if s is not self.sem:
                        self.wait_tok(s, v)
        for b in writes:
            if pe_accum:
                continue
            if b.w is not None and not (nowaw and b.w[0] is self.sem):
                self.wait_tok(*b.w)
            for s, v in b.r.items():
                self.wait_tok(s, v)

    def mark(self, tok, reads, writes):
        s, v = tok
        for b in reads:
            if b.r.get(s, 0) < v:
                b.r[s] = v
        for b in writes:
            b.w = tok; b.r = {}

    def op(self, fn, reads=(), writes=(), pe_accum=False, nowaw=False):
        self.deps(reads, writes, pe_accum, nowaw)
        self.sem.val += 1
        tok = (self.sem, self.sem.val)
        h = self.sem.h
        call = fn(REC)
        self.q.append(lambda e, call=call, h=h: call.run(e).then_inc(h, 1))
        self.mark(tok, reads, writes)
        return tok

    def dma(self, fn, sem, reads=(), writes=()):
        fns = fn if isinstance(fn, (list, tuple)) else [fn]
        self.deps(reads, writes)
        h = sem.h
        for f in fns:
            sem.val += 16
            call = f(REC)
            self.q.append(lambda e, call=call, h=h: call.run(e).then_inc(h, 16))
        tok = (sem, sem.val)
        self.mark(tok, reads, writes)
        return tok


class Prog:
    def __init__(self, nc):
        self.nc = nc; self.es = ExitStack(); self.sems = []; self.semcache = {}
        self.sp = Eng(self, 'sp', self.new_sem('e_sp'))
        self.act = Eng(self, 'act', self.new_sem('e_act'))
        self.pool = Eng(self, 'pool', self.new_sem('e_pool'))
        self.dve = Eng(self, 'dve', self.new_sem('e_dve'))
        self.pe = Eng(self, 'pe', self.new_sem('e_pe'))
        self.engs = [self.sp, self.act, self.pool, self.dve, self.pe]

    def new_sem(self, name):
        s = Sem(self.es.enter_context(self.nc.semaphore(name)), name)
        self.sems.append(s)
        return s

    def sbuf(self, name, shape, dt):
        return self.es.enter_context(self.nc.sbuf_tensor(name, shape, dt))

    def psum(self, name, shape, dt):
        return self.es.enter_context(self.nc.psum_tensor(name, shape, dt))

    def buf(self, name, dma=False):
        if not dma:
            return Buf(name, None)
        if name not in self.semcache:
            self.semcache[name] = self.new_sem('b_' + name)
        return Buf(name, self.semcache[name])

    def barrier(self):
        for e in self.engs:
            for s in self.sems:
                if s.val > 0:
                    e.wait_tok(s, s.val)

    def finish(self):
        self.barrier()
        with self.nc.Block() as block:
            @block.sync
            def _(e):
                for f in self.sp.q: f(e)
            @block.scalar
            def _(e):
                for f in self.act.q: f(e)
            @block.gpsimd
            def _(e):
                for f in self.pool.q: f(e)
            @block.vector
            def _(e):
                for f in self.dve.q: f(e)
            @block.tensor
            def _(e):
                for f in self.pe.q: f(e)
        self.es.close()


class Arena:
    def __init__(self, P, name, ncols, dt):
        self.t = P.sbuf(name, [128, ncols], dt); self.off = 0; self.n = ncols; self.name = name

    def alloc(self, cols):
        a = self.off; self.off += cols
        assert self.off <= self.n, (self.name, self.off, self.n)
        return self.t[:, a:a + cols]

    def reset(self):
        self.off = 0


def v3(ap, a):
    return ap.rearrange("p (a b) -> p a b", a=a)


def build(n_layers=DEPTH, final_norm=True):
    nc = bass.Bass("TRN2", target_bir_lowering=False)
    dram = lambda name, shape, dt, kind="ExternalInput": nc.dram_tensor(name, shape, dt, kind=kind).ap()
    x_in = dram("x", [T, D], F32)
    cc_d = dram("cc", [128, 16], F32)
    rope_d = dram("rope", [T, 128], F32)
    ident_d = dram("ident", [128, 128], F32)
    w_ada = dram("w_ada", [DEPTH, D, 6 * D], F32)
    b_ada_col = dram("b_ada_col", [DEPTH, 128, 48], F32)
    b_ada = dram("b_ada", [DEPTH, 6 * D], F32)
    n1col = dram("n1col", [DEPTH, 128, 8], F32)
    n2col = dram("n2col", [DEPTH, 128, 8], F32)
    w_in = dram("w_in", [DEPTH, D, 2560], F32)
    w_out = dram("w_out", [DEPTH, D, D], F32)
    lamv = dram("lamv", [DEPTH, 256], F32)
    subln_g = dram("subln_g", [DEPTH, 128], F32)
    sgu_norm_g = dram("sgu_norm_g", [DEPTH, 512], F32)
    sgu_w = dram("sgu_w", [DEPTH, 8, 128, 128], F32)
    sgu_bT = dram("sgu_bT", [DEPTH, 128, 8], F32)
    w_router = dram("w_router", [DEPTH, D, NE], F32)
    w_gate = dram("w_gate", [DEPTH, NE, D, FF], F32)
    w_up = dram("w_up", [DEPTH, NE, D, FF], F32)
    w_down = dram("w_down", [DEPTH, NE, FF, D], F32)
    norm_f_g = dram("norm_f_g", [1, D], F32)
    out_d = dram("out", [SEQ, D], F32, kind="ExternalOutput")
    xr = dram("xr", [T, D], F32, kind="Internal")
    qd = dram("qd", [T, 512], BF16, kind="Internal")
    sgd = dram("sgd", [T, 512], BF16, kind="Internal")
    h2d = dram("h2d", [T, D], BF16, kind="Internal")

    P = Prog(nc)
    sp, act, pool, dve, pe = P.sp, P.act, P.pool, P.dve, P.pe

    ident = P.sbuf("ident_sb", [128, 128], F32); b_ident = P.buf("ident", True)
    identb = P.sbuf("identb_sb", [128, 128], BF16); b_identb = P.buf("identb")
    grow = P.sbuf("grow", [128, 4 * D], F32); b_grow = P.buf("grow")
    siluc = P.sbuf("siluc", [128, 16], F32); b_siluc = P.buf("siluc", True)
    modT = P.sbuf("modT", [128, 96], F32); b_modT = P.buf("modT")
    bcol = P.sbuf("bcol", [128, 48], F32); b_bcol = P.buf("bcol", True)
    ncol = P.sbuf("ncol", [128, 16], F32); b_ncol = P.buf("ncol", True)
    AB = P.sbuf("ABcols", [128, 32], F32); b_AB = P.buf("ABc")
    lamt = P.sbuf("lamt", [128, 8], F32); b_lamt = P.buf("lamt")
    lv = P.sbuf("lv", [128, 256], F32); b_lv = P.buf("lv", True)
    lprod = P.sbuf("lprod", [128, 128], F32); b_lprod = P.buf("lprod")
    gsub = P.sbuf("gsub", [128, 128], F32); b_gsub = P.buf("gsub", True)
    sgng = P.sbuf("sgng", [128, 512], F32); b_sgng = P.buf("sgng", True)
    sbT = P.sbuf("sbT", [128, 8], F32); b_sbT = P.buf("sbT", True)
    bfull = P.sbuf("bfull", [128, 512], F32); b_bfull = P.buf("bfull")
    wsT = P.sbuf("wsT", [128, 8 * 128], BF16); b_wsT = P.buf("wsT")
    wrt = P.sbuf("wrt", [128, 8 * NE], F32); b_wrt = P.buf("wrt", True)
    small = P.sbuf("small", [128, 64], F32)
    epsc = P.sbuf("epsc", [128, 1], F32); b_epsc = P.buf("epsc")
    dve.op(lambda e: e.memset(epsc[:], EPS), writes=[b_epsc])

    def rstd_op(dst, src, scale, bufs):
        act.op(lambda e: e.activation(out=dst, in_=src, func=AF.Ln, scale=float(scale), bias=epsc[:, 0:1]), reads=bufs + [b_epsc], writes=bufs)
        act.op(lambda e: e.activation(out=dst, in_=dst, func=AF.Exp, scale=-0.5), reads=bufs, writes=bufs)

    AFa = Arena(P, "arenaF", 12928, F32)
    ABa = Arena(P, "arenaB", 62464, BF16)

    pb = [P.psum("pb%d" % i, [128, 512], F32) for i in range(5)]
    pbhr = P.psum("pbhr", [128, 512], F32)
    pbh = pbhr[:, 0:256].bitcast(BF16)
    pbr = pbhr[:, 256:512]
    pbb = P.psum("pbb", [128, 1024], F32)
    b_pb = [Buf("pb%d" % i, excl=True) for i in range(5)]
    b_pbh = Buf("pbh", excl=True); b_pbb = Buf("pbb", excl=True)

    sp.dma(lambda e: e.dma_start(out=ident[:], in_=ident_d), b_ident.sem, writes=[b_ident])
    dve.op(lambda e: e.tensor_copy(out=identb[:], in_=ident[:]), reads=[b_ident], writes=[b_identb])
    sp.dma(lambda e: e.dma_start(out=siluc[:], in_=cc_d), b_siluc.sem, writes=[b_siluc])
    act.op(lambda e: e.activation(out=siluc[:], in_=siluc[:], func=AF.Silu), reads=[b_siluc], writes=[b_siluc])
    b_xr = P.buf("xr", True)
    sp.dma([lambda e, i=i: e.dma_start(out=xr[i * (T // 4):(i + 1) * (T // 4), :], in_=x_in[i * (T // 4):(i + 1) * (T // 4), :]) for i in range(4)],
           b_xr.sem, writes=[b_xr])
    P.barrier()

    for l in range(n_layers):
        last = (l == DEPTH - 1)
        lam_init = 0.8 - 0.6 * math.exp(-0.3 * l)
        NTq = NLT if last else NT
        AFa.reset(); ABa.reset()
        wa = [AFa.alloc(8 * 512) for _ in range(2)]; b_wa = [P.buf("wa_%d" % i, True) for i in range(2)]
        brow = AFa.alloc(512); b_brow = P.buf("brow", True)
        swl = AFa.alloc(8 * 128); b_swl = P.buf("swl", True)
        srep = AFa.alloc(16 * 128); b_srep = P.buf("srep%d" % l)
        for ks in range(16):
            dve.op(lambda e, ks=ks: e.tensor_copy(out=srep[:, ks * 128:(ks + 1) * 128], in_=siluc[:, ks:ks + 1].to_broadcast([128, 128])),
                   reads=[b_siluc], writes=[b_srep])
        sp.dma(lambda e: e.dma_start(out=bcol[:], in_=b_ada_col[l]), b_bcol.sem, writes=[b_bcol])
        sp.dma([lambda e: e.dma_start(out=ncol[:, 0:8], in_=n1col[l]), lambda e: e.dma_start(out=ncol[:, 8:16], in_=n2col[l])], b_ncol.sem, writes=[b_ncol])
        sp.dma(lambda e: e.dma_start(out=lv[:], in_=lamv[l:l + 1, :].to_broadcast([128, 256])), b_lv.sem, writes=[b_lv])
        sp.dma(lambda e: e.dma_start(out=gsub[:], in_=subln_g[l:l + 1, :].to_broadcast([128, 128])), b_gsub.sem, writes=[b_gsub])
        sp.dma(lambda e: e.dma_start(out=sgng[:], in_=sgu_norm_g[l:l + 1, :].to_broadcast([128, 512])), b_sgng.sem, writes=[b_sgng])
        sp.dma(lambda e: e.dma_start(out=sbT[:], in_=sgu_bT[l]), b_sbT.sem, writes=[b_sbT])
        sp.dma(lambda e: e.dma_start(out=wrt[:].rearrange("p (k n) -> p k n", k=8), in_=w_router[l].rearrange("(k p) n -> p k n", p=128)),
               b_wrt.sem, writes=[b_wrt])
        dve.op(lambda e: e.tensor_tensor(out=v3(lprod[:], 2), in0=v3(lv[:], 2)[:, :, 0:64], in1=v3(lv[:], 2)[:, :, 64:128], op=ALU.mult),
               reads=[b_lv], writes=[b_lprod])
        dve.op(lambda e: e.tensor_reduce(out=lamt[:, 0:2], in_=v3(lprod[:], 2), axis=AX.X, op=ALU.add), reads=[b_lprod], writes=[b_lamt])
        act.op(lambda e: e.activation(out=lamt[:, 0:2], in_=lamt[:, 0:2], func=AF.Exp), reads=[b_lamt], writes=[b_lamt])
        dve.op(lambda e: e.tensor_tensor(out=lamt[:, 2:3], in0=lamt[:, 0:1], in1=lamt[:, 1:2], op=ALU.subtract), reads=[b_lamt], writes=[b_lamt])
        dve.op(lambda e: e.tensor_scalar(out=lamt[:, 2:3], in0=lamt[:, 2:3], scalar1=float(lam_init), scalar2=None, op0=ALU.add), reads=[b_lamt], writes=[b_lamt])
        dve.op(lambda e: e.tensor_scalar(out=lamt[:, 3:4], in0=lamt[:, 2:3], scalar1=-1.0, scalar2=None, op0=ALU.mult), reads=[b_lamt], writes=[b_lamt])
        dve.op(lambda e: e.tensor_scalar(out=gsub[:], in0=gsub[:], scalar1=float(1.0 - lam_init), scalar2=None, op0=ALU.mult), reads=[b_gsub], writes=[b_gsub])
        dve.op(lambda e: e.tensor_copy(out=v3(bfull[:], 8), in_=sbT[:].unsqueeze(2).to_broadcast([128, 8, 64])), reads=[b_sbT], writes=[b_bfull])
        for hh in range(2):
            sp.dma(lambda e, hh=hh: e.dma_start(out=v3(swl, 8)[:, 0:4, :], in_=sgu_w[l, hh * 4:(hh + 1) * 4].rearrange("h p q -> p h q")),
                   b_swl.sem, writes=[b_swl])
            for h4 in range(4):
                pe.op(lambda e, h4=h4: e.transpose(out=pbb[:, h4 * 128:(h4 + 1) * 128], in_=v3(swl, 8)[:, h4, :], identity=ident[:]),
                      reads=[b_swl, b_ident], writes=[b_pbb], pe_accum=(h4 > 0))
            act.op(lambda e, hh=hh: e.activation(out=wsT[:, hh * 512:(hh + 1) * 512], in_=pbb[:, 0:512], func=AF.Copy), reads=[b_pbb], writes=[b_wsT])
        for hg in range(12):
            g = hg // 2; half = hg % 2
            wb_ = wa[hg % 2]; bw = b_wa[hg % 2]
            sp.dma(lambda e, wb_=wb_, hg=hg: e.dma_start(out=v3(wb_, 8), in_=w_ada[l].rearrange("(k p) n -> p k n", p=128)[:, :, hg * 512:(hg + 1) * 512]),
                   bw.sem, writes=[bw])
            for nn in range(4):
                n = hg * 4 + nn
                for k in range(8):
                    pe.op(lambda e, wb_=wb_, nn=nn, k=k, n=n: e.matmul(pb[3][:, 2 * n:2 * n + 2], lhsT=v3(wb_, 8)[:, k, nn * 128:(nn + 1) * 128],
                                                                      rhs=siluc[:, 2 * k:2 * k + 2], start=(k == 0), stop=(k == 7)),
                          reads=[bw, b_siluc], writes=[b_pb[3]], pe_accum=not (hg == 0 and nn == 0 and k == 0))
            if g in (2, 5):
                which = 0 if g == 2 else 1
                sp.dma(lambda e, g=g, half=half: e.dma_start(out=brow, in_=b_ada[l:l + 1, g * 1024 + half * 512: g * 1024 + (half + 1) * 512].to_broadcast([128, 512])),
                       b_brow.sem, writes=[b_brow])
                for s in range(2):
                    for k in range(8):
                        pe.op(lambda e, wb_=wb_, s=s, k=k: e.matmul(pb[s][:, :], lhsT=srep[:, (2 * k + s) * 128:(2 * k + s + 1) * 128], rhs=v3(wb_, 8)[:, k, :],
                                                                    start=(k == 0), stop=(k == 7)),
                              reads=[bw, b_srep], writes=[b_pb[s]], pe_accum=(k > 0))
                    c0 = (s * 2 + which) * D + half * 512
                    dve.op(lambda e, s=s, c0=c0: e.tensor_tensor(out=grow[:, c0:c0 + 512], in0=pb[s][:, :], in1=brow, op=ALU.add),
                           reads=[b_pb[s], b_brow], writes=[b_grow])
        dve.op(lambda e: e.tensor_tensor(out=v3(modT[:], 48), in0=v3(pb[3][:, 0:96], 48), in1=bcol[:].unsqueeze(2).to_broadcast([128, 48, 2]), op=ALU.add),
               reads=[b_pb[3], b_bcol], writes=[b_modT])
        mT = v3(modT[:], 48)
        for w_ in range(2):
            for s in range(2):
                sc = mT[:, (1 + 3 * w_) * 8:(2 + 3 * w_) * 8, s]
                o = AB[:, (w_ * 2 + s) * 8:(w_ * 2 + s + 1) * 8]
                dve.op(lambda e, sc=sc, o=o, w_=w_: e.scalar_tensor_tensor(out=o, in0=sc, scalar=1.0, in1=ncol[:, w_ * 8:(w_ + 1) * 8], op0=ALU.add, op1=ALU.mult),
                       reads=[b_modT, b_ncol], writes=[b_AB])

        def Acol(w_, s, k): return AB[:, (w_ * 2 + s) * 8 + k:(w_ * 2 + s) * 8 + k + 1]
        def Bcol(w_, s, k): return modT[:, ((3 * w_) * 8 + k) * 2 + s:((3 * w_) * 8 + k) * 2 + s + 1]
        def growv(s, which): return grow[:, (s * 2 + which) * D:(s * 2 + which + 1) * D]
        P.barrier()

        AFa.reset(); ABa.reset()
        kT = ABa.alloc(4 * T); b_kT = P.buf("kT%d" % l)
        vaug = ABa.alloc(NT * 4 * 130); b_vaug = P.buf("vaug%d" % l)
        ab_mark = ABa.off
        w_in_sb = ABa.alloc(8 * 2560); b_win = P.buf("win", True)
        hT = [ABa.alloc(8 * 128) for _ in range(2)]; b_hT = [P.buf("hT%d_%d" % (l, i)) for i in range(2)]
        qr = ABa.alloc(512); b_qr = P.buf("qr", True)
        kr = ABa.alloc(512); b_kr = P.buf("kr%d" % l)
        gvn = [ABa.alloc(512) for _ in range(2)]; b_gvn = [P.buf("gvn%d_%d" % (l, i)) for i in range(2)]
        sgo = [ABa.alloc(512) for _ in range(2)]; b_sgo = [P.buf("sgo_%d" % i, True) for i in range(2)]
        xt = [AFa.alloc(D) for _ in range(2)]; b_xt = [P.buf("xt_%d" % i, True) for i in range(2)]
        rp = [AFa.alloc(128) for _ in range(3)]; b_rp = [P.buf("rp_%d" % i, True) for i in range(3)]
        xn = AFa.alloc(D); b_xn = P.buf("xn%d" % l)
        junk = AFa.alloc(D); b_junk = P.buf("junk%d" % l)
        t1 = AFa.alloc(512); b_t1 = P.buf("t1%d" % l)
        t2 = AFa.alloc(512); b_t2 = P.buf("t2%d" % l)
        u_sb = [AFa.alloc(512) for _ in range(2)]; b_u = [P.buf("u%d_%d" % (l, i)) for i in range(2)]
        gvg = [AFa.alloc(512) for _ in range(2)]; b_gvg = [P.buf("gvg%d_%d" % (l, i)) for i in range(2)]
        sqA = AFa.alloc(512); b_sqA = P.buf("sqA%d" % l)
        sqB = AFa.alloc(512); b_sqB = P.buf("sqB%d" % l)
        sqC = AFa.alloc(512); b_sqC = P.buf("sqC%d" % l)
        st = small[:, 0:8]; b_stN = P.buf("stN%d" % l)
        stP = [small[:, 8 + 16 * i:24 + 16 * i] for i in range(2)]; b_stP = [P.buf("stP%d_%d" % (l, i)) for i in range(2)]
        pool.dma([lambda e, i=i: e.dma_start(out=v3(w_in_sb, 8)[:, 2 * i:2 * i + 2, :], in_=w_in[l].rearrange("(k p) n -> p k n", p=128)[:, 2 * i:2 * i + 2, :]) for i in range(4)],
                 b_win.sem, writes=[b_win])
        va4 = vaug.rearrange("p (t h c) -> p t h c", t=NT, h=4)
        pool.op(lambda e: e.memset(vaug, 1.0), writes=[b_vaug])

        def load_tile(t):
            i = t % 2; i3 = t % 3
            sp.dma(lambda e: e.dma_start(out=xt[i], in_=xr[t * 128:(t + 1) * 128, :]), b_xt[i].sem, writes=[b_xt[i]])
            sp.dma(lambda e: e.dma_start(out=rp[i3], in_=rope_d[t * 128:(t + 1) * 128, :]), b_rp[i3].sem, writes=[b_rp[i3]])

        def N1(t):
            i = t % 2
            pool.op(lambda e: e.memset(st[:, 0:1], 0.0), writes=[b_stN])
            act.op(lambda e: e.activation(out=junk, in_=xt[i], func=AF.Square, accum_out=st[:, 0:1]), reads=[b_xt[i], b_stN], writes=[b_junk, b_stN])
            rstd_op(st[:, 1:2], st[:, 0:1], 1.0 / D, [b_stN])

        def N2(t):
            i = t % 2
            dve.op(lambda e: e.tensor_scalar(out=xn, in0=xt[i], scalar1=st[:, 1:2], scalar2=None, op0=ALU.mult), reads=[b_xt[i], b_stN], writes=[b_xn])

        def N3(t):
            for k in range(8):
                pe.op(lambda e: e.transpose(out=pbb[:, k * 128:(k + 1) * 128], in_=xn[:, k * 128:(k + 1) * 128], identity=ident[:]),
                      reads=[b_xn, b_ident], writes=[b_pbb], pe_accum=(k > 0))

        def N4(t):
            s_ = 0 if t < NLT else 1
            dst = hT[t % 2]
            for k in range(8):
                act.op(lambda e: e.activation(out=dst[:, k * 128:(k + 1) * 128], in_=pbb[:, k * 128:(k + 1) * 128], func=AF.Identity,
                                              scale=Acol(0, s_, k), bias=Bcol(0, s_, k)),
                       reads=[b_pbb, b_AB, b_modT], writes=[b_hT[t % 2]], nowaw=(k > 0))

        def rope(src_ps, b_src, rp_, b_rp_, dst, b_dst):
            s5 = src_ps.rearrange("p (g a r f) -> p g a r f", g=8, a=2, r=2)
            t5 = t2.rearrange("p (g a r f) -> p g a r f", g=8, a=2, r=2)
            sn = rp_[:, 64:128].rearrange("p (a r f) -> p a r f", a=2, r=2)
            dve.op(lambda e: e.tensor_tensor(out=v3(t1, 8), in0=v3(src_ps, 8), in1=rp_[:, 0:64].unsqueeze(1).to_broadcast([128, 8, 64]), op=ALU.mult),
                   reads=[b_src, b_rp_], writes=[b_t1])
            for a in range(2):
                for r in range(2):
                    dve.op(lambda e: e.tensor_tensor(out=t5[:, :, a, r, :], in0=s5[:, :, a, 1 - r, :],
                                                     in1=sn[:, a, r, :].unsqueeze(1).to_broadcast([128, 8, 16]), op=ALU.mult),
                           reads=[b_src, b_rp_], writes=[b_t2], nowaw=(a + r > 0))
            dve.op(lambda e: e.tensor_tensor(out=dst, in0=t1, in1=t2, op=ALU.add), reads=[b_t1, b_t2], writes=[b_dst])

        prot = [0, 1, 3, 4]; pcnt = [0]

        def proj(t, g):
            bi = prot[pcnt[0] % 4]; pcnt[0] += 1
            hsrc = hT[t % 2]
            for k in range(8):
                pe.op(lambda e: e.matmul(pb[bi][:, :], lhsT=hsrc[:, k * 128:(k + 1) * 128], rhs=v3(w_in_sb, 8)[:, k, g * 512:(g + 1) * 512],
                                         start=(k == 0), stop=(k == 7)),
                      reads=[b_hT[t % 2], b_win], writes=[b_pb[bi]], pe_accum=(k > 0))
            return pb[bi], b_pb[bi]

        load_tile(0); load_tile(1)
        N1(0); N2(0); N3(0); N4(0)
        pend = []

        def pop_pend():
            if pend:
                pend.pop(0)()
        for t in range(NT):
            nxt = t + 1 < NT
            i3 = t % 3
            need_q = t < NTq
            if t + 2 < NT:
                load_tile(t + 2)
            if nxt: N1(t + 1)
            pop_pend()
            if need_q:
                pbk, bpbk = proj(t, 0)
                rope(pbk[:, :], bpbk, rp[i3], b_rp[i3], qr, b_qr)
                sp.dma(lambda e: e.dma_start(out=qd[t * 128:(t + 1) * 128, :], in_=qr), b_qr.sem, reads=[b_qr])
            pop_pend()
            if nxt: N2(t + 1)
            pbk, bpbk = proj(t, 1)
            pop_pend()
            rope(pbk[:, :], bpbk, rp[i3], b_rp[i3], kr, b_kr)
            pop_pend()
            for j in range(4):
                pe.op(lambda e: e.transpose(out=pbh[:, j * 128:(j + 1) * 128], in_=kr[:, j * 128:(j + 1) * 128], identity=identb[:]),
                      reads=[b_kr, b_identb], writes=[b_pbh], pe_accum=(j > 0))
            act.op(lambda e: e.activation(out=v3(kT, 4)[:, :, t * 128:(t + 1) * 128], in_=v3(pbh[:, 0:512], 4), func=AF.Copy),
                   reads=[b_pbh], writes=[b_kT])
            if nxt: N3(t + 1)
            pbk, bpbk = proj(t, 2)
            act.op(lambda e: e.activation(out=va4[:, t, :, 0:128], in_=v3(pbk[:, :], 4), func=AF.Copy), reads=[bpbk], writes=[b_vaug])
            if nxt: N4(t + 1)
            if need_q:
                pr = t % 2
                pbk, bpbk = proj(t, 3)
                act.op(lambda e: e.activation(out=u_sb[pr], in_=pbk[:, :], func=AF.Gelu), reads=[bpbk], writes=[b_u[pr]])
                pbk, bpbk = proj(t, 4)
                act.op(lambda e: e.activation(out=gvg[pr], in_=pbk[:, :], func=AF.Gelu), reads=[bpbk], writes=[b_gvg[pr]])
                dve.op(lambda e: e.tensor_tensor(out=sqA, in0=gvg[pr], in1=gvg[pr], op=ALU.mult), reads=[b_gvg[pr]], writes=[b_sqA])
                dve.op(lambda e: e.tensor_reduce(out=stP[pr][:, 0:8], in_=v3(sqA, 8), axis=AX.X, op=ALU.add), reads=[b_sqA], writes=[b_stP[pr]])

                def T1(t=t, pr=pr):
                    rstd_op(stP[pr][:, 8:16], stP[pr][:, 0:8], 1.0 / 64, [b_stP[pr]])

                def T2(t=t, pr=pr):
                    dve.op(lambda e: e.tensor_tensor(out=v3(sqB, 8), in0=v3(gvg[pr], 8), in1=stP[pr][:, 8:16].unsqueeze(2).to_broadcast([128, 8, 64]), op=ALU.mult),
                           reads=[b_gvg[pr], b_stP[pr]], writes=[b_sqB])
                    dve.op(lambda e: e.tensor_tensor(out=gvn[pr], in0=sqB, in1=sgng[:], op=ALU.mult), reads=[b_sqB, b_sgng], writes=[b_gvn[pr]])

                def T3(t=t, pr=pr):
                    for h in range(8):
                        pe.op(lambda e: e.matmul(pb[2][:, h * 64:(h + 1) * 64], lhsT=wsT[:, h * 128:(h + 1) * 128], rhs=gvn[pr][:, h * 64:(h + 1) * 64],
                                                 start=True, stop=True),
                              reads=[b_wsT, b_gvn[pr]], writes=[b_pb[2]], pe_accum=(h > 0))

                def T4(t=t, pr=pr):
                    dve.op(lambda e: e.tensor_tensor(out=sqC, in0=pb[2][:, :], in1=bfull[:], op=ALU.add), reads=[b_pb[2], b_bfull], writes=[b_sqC])
                    dve.op(lambda e: e.tensor_tensor(out=sgo[pr], in0=sqC, in1=u_sb[pr], op=ALU.mult), reads=[b_sqC, b_u[pr]], writes=[b_sgo[pr]])
                    sp.dma(lambda e: e.dma_start(out=sgd[t * 128:(t + 1) * 128, :], in_=sgo[pr]), b_sgo[pr].sem, reads=[b_sgo[pr]])
                pend.extend([T1, T2, T3, T4])
        while pend:
            pend.pop(0)()
        P.barrier()

        AFa.reset(); ABa.off = ab_mark
        w_out_sb = ABa.alloc(8 * D); b_wout = P.buf("wout", True)
        qg = ABa.alloc(4 * 512); b_qg = P.buf("qg", True)
        qTm = [ABa.alloc(8 * 512) for _ in range(2)]; b_qTm = [P.buf("qTm%d_%d" % (l, i)) for i in range(2)]
        pT = [ABa.alloc(512) for _ in range(3)]; b_pT = [P.buf("pT%d_%d" % (l, i)) for i in range(3)]
        mixo = [[ABa.alloc(512) for _ in range(4)] for _ in range(2)]
        b_mixo = [[P.buf("mixo%d_%d_%d" % (l, pp, i)) for i in range(4)] for pp in range(2)]
        mixT = ABa.alloc(8 * 128); b_mixT = P.buf("mixT%d" % l)
        sgi = [ABa.alloc(512) for _ in range(2)]; b_sgi = [P.buf("sgi_%d" % i, True) for i in range(2)]
        h2 = ABa.alloc(D); b_h2 = P.buf("h2", True)
        affT = AFa.alloc(T); b_affT = P.buf("affT%d" % l)
        af_mark = AFa.off
        xt = [AFa.alloc(D) for _ in range(2)]; b_xt = [P.buf("xt_%d" % i, True) for i in range(2)]
        x1 = AFa.alloc(D); b_x1 = P.buf("x1", True)
        xn = AFa.alloc(D); b_xn = P.buf("xnc%d" % l)
        junk = AFa.alloc(D); b_junk = P.buf("junkc%d" % l)
        h2T = AFa.alloc(8 * 128); b_h2T = P.buf("h2T%d" % l)
        accS = AFa.alloc(8 * 130); b_accS = P.buf("accS%d" % l)
        t1 = AFa.alloc(128); b_t1 = P.buf("t1c%d" % l)
        osb = [AFa.alloc(128) for _ in range(4)]; b_osb = [P.buf("osb%d_%d" % (l, i)) for i in range(4)]
        af = AFa.alloc(NE); b_af = P.buf("af%d" % l)
        st = small[:, 0:32]
        b_stT = P.buf("stT%d" % l); b_stE = P.buf("stE%d" % l); b_stE2 = P.buf("stE2%d" % l)
        b_pbR = b_pbh; b_pbAf = b_pbh
        pool.dma(lambda e: e.dma_start(out=v3(w_out_sb, 8), in_=w_out[l].rearrange("(k p) n -> p k n", p=128)), b_wout.sem, writes=[b_wout])
        for pp in range(2):
            pool.op(lambda e: e.memset(qTm[pp], 0.0), writes=[b_qTm[pp]])

        def acc_ap(a):
            bank = 2 + a // 3; off = (a % 3) * 160
            return pb[bank][:, off:off + 130], b_pb[bank]

        qgroups = [(G * 4, 4) for G in range(8)] + ([] if last else [(NLT, 2)])

        def build_q(gi):
            t0, nq = qgroups[gi]; par = gi % 2
            ths = []

            def ld():
                sp.dma(lambda e: e.dma_start(out=v3(qg, 4)[:, 0:nq, :], in_=qd[t0 * 128:(t0 + nq) * 128, :].rearrange("(a p) n -> p a n", p=128)),
                       b_qg.sem, writes=[b_qg])
            ths.append(ld)
            for qi in range(nq):
                def f(qi=qi):
                    for j in range(4):
                        pe.op(lambda e: e.transpose(out=pbh[:, j * 128:(j + 1) * 128], in_=v3(qg, 4)[:, qi, j * 128:(j + 1) * 128], identity=identb[:]),
                              reads=[b_qg, b_identb], writes=[b_pbh], pe_accum=(j > 0))
                    for hf in range(2):
                        dst = qTm[par].rearrange("p (j f q) -> p j f q", j=4, f=2)[hf * 64:(hf + 1) * 64, :, hf, qi * 128:(qi + 1) * 128]
                        src = v3(pbh[:, 0:512], 4)[hf * 64:(hf + 1) * 64, :, :]
                        act.op(lambda e: e.activation(out=dst, in_=src, func=AF.Copy), reads=[b_pbh], writes=[b_qTm[par]], nowaw=True)
                ths.append(f)
            return ths

        def tile_thunks(gi, qi):
            t0, nq = qgroups[gi]; par = gi % 2
            t = t0 + qi; ii = t % 2
            s_ = 0 if t0 < NLT else 1
            mo = mixo[par][qi]; bmo = b_mixo[par][qi]
            ths = []

            def s0():
                sp.dma(lambda e: e.dma_start(out=sgi[ii], in_=sgd[t * 128:(t + 1) * 128, :]), b_sgi[ii].sem, writes=[b_sgi[ii]])
                sp.dma(lambda e: e.dma_start(out=xt[ii], in_=xr[t * 128:(t + 1) * 128, :]), b_xt[ii].sem, writes=[b_xt[ii]])
            ths.append(s0)

            def s1(half):
                srcb, bsrc = [(mo, bmo), (sgi[ii], b_sgi[ii])][half]
                for j in range(4):
                    pe.op(lambda e: e.transpose(out=pbh[:, j * 128:(j + 1) * 128], in_=srcb[:, j * 128:(j + 1) * 128], identity=identb[:]),
                          reads=[bsrc, b_identb], writes=[b_pbh], pe_accum=(j > 0))
                dve.op(lambda e: e.tensor_copy(out=mixT[:, half * 512:(half + 1) * 512], in_=pbh[:, 0:512]), reads=[b_pbh], writes=[b_mixT], nowaw=(half > 0))
            ths.append(lambda: s1(0))
            ths.append(lambda: s1(1))

            def s2(hv, fh):
                for f in range(fh * 4, fh * 4 + 4):
                    pe.op(lambda e: e.matmul(pbb[:, hv * 512:(hv + 1) * 512], lhsT=mixT[:, f * 128:(f + 1) * 128],
                                             rhs=v3(w_out_sb, 8)[:, f, hv * 512:(hv + 1) * 512], start=(f == 0), stop=(f == 7)),
                          reads=[b_mixT, b_wout], writes=[b_pbb], pe_accum=not (hv == 0 and f == 0))
            for hv in range(2):
                for fh in range(2):
                    ths.append(lambda hv=hv, fh=fh: s2(hv, fh))

            def s3():
                dve.op(lambda e: e.tensor_tensor(out=x1, in0=pbb[:, :], in1=growv(s_, 0), op=ALU.mult), reads=[b_pbb, b_grow], writes=[b_x1])
                dve.op(lambda e: e.tensor_tensor(out=x1, in0=x1, in1=xt[ii], op=ALU.add), reads=[b_x1, b_xt[ii]], writes=[b_x1])
                sp.dma(lambda e: e.dma_start(out=xr[t * 128:(t + 1) * 128, :], in_=x1), b_x1.sem, reads=[b_x1])
                pool.op(lambda e: e.memset(st[:, 0:1], 0.0), writes=[b_stT])
                act.op(lambda e: e.activation(out=junk, in_=x1, func=AF.Square, accum_out=st[:, 0:1]), reads=[b_x1, b_stT], writes=[b_junk, b_stT])
            ths.append(s3)

            def s4():
                rstd_op(st[:, 1:2], st[:, 0:1], 1.0 / D, [b_stT])
            ths.append(s4)

            def s5(hh):
                if hh == 0:
                    dve.op(lambda e: e.tensor_scalar(out=xn, in0=x1, scalar1=st[:, 1:2], scalar2=None, op0=ALU.mult), reads=[b_x1, b_stT], writes=[b_xn])
                for k in range(hh * 4, hh * 4 + 4):
                    pe.op(lambda e: e.transpose(out=pbb[:, k * 128:(k + 1) * 128], in_=xn[:, k * 128:(k + 1) * 128], identity=ident[:]),
                          reads=[b_xn, b_ident], writes=[b_pbb], pe_accum=(k > 0))
            ths.append(lambda: s5(0))
            ths.append(lambda: s5(1))

            def s6():
                ia = (1 * 2 + s_) * 8
                Ab = AB[:, ia:ia + 8].unsqueeze(2).to_broadcast([128, 8, 128])
                Bb = v3(modT[:], 48)[:, 24:32, s_].unsqueeze(2).to_broadcast([128, 8, 128])
                dve.op(lambda e: e.tensor_tensor(out=v3(h2T, 8), in0=v3(pbb[:, :], 8), in1=Ab, op=ALU.mult), reads=[b_pbb, b_AB], writes=[b_h2T])
                dve.op(lambda e: e.tensor_tensor(out=v3(h2T, 8), in0=v3(h2T, 8), in1=Bb, op=ALU.add), reads=[b_h2T, b_modT], writes=[b_h2T])
            ths.append(s6)

            def s7a():
                for k in range(8):
                    pe.op(lambda e: e.matmul(pbr[:, 0:NE], lhsT=h2T[:, k * 128:(k + 1) * 128], rhs=wrt[:, k * NE:(k + 1) * NE], start=(k == 0), stop=(k == 7)),
                          reads=[b_h2T, b_wrt], writes=[b_pbR], pe_accum=(k > 0))
            ths.append(s7a)

            def s7(hh):
                for k in range(hh * 4, hh * 4 + 4):
                    pe.op(lambda e: e.transpose(out=pbb[:, k * 128:(k + 1) * 128], in_=h2T[:, k * 128:(k + 1) * 128], identity=ident[:]),
                          reads=[b_h2T, b_ident], writes=[b_pbb], pe_accum=(k > 0))
            ths.append(lambda: s7(0))
            ths.append(lambda: s7(1))

            def s8():
                dve.op(lambda e: e.tensor_copy(out=h2, in_=pbb[:, :]), reads=[b_pbb], writes=[b_h2])
                sp.dma(lambda e: e.dma_start(out=h2d[t * 128:(t + 1) * 128, :], in_=h2), b_h2.sem, reads=[b_h2])
                dve.op(lambda e: e.tensor_reduce(out=st[:, 5:6], in_=pbr[:, 0:NE], axis=AX.X, op=ALU.max), reads=[b_pbR], writes=[b_stT])
                dve.op(lambda e: e.tensor_scalar(out=st[:, 5:6], in0=st[:, 5:6], scalar1=-1.0, scalar2=None, op0=ALU.mult), reads=[b_stT], writes=[b_stT])
                pool.op(lambda e: e.memset(st[:, 6:7], 0.0), writes=[b_stT])
                act.op(lambda e: e.activation(out=af, in_=pbr[:, 0:NE], func=AF.Exp, bias=st[:, 5:6], scale=1.0, accum_out=st[:, 6:7]),
                       reads=[b_pbR, b_stT], writes=[b_af, b_stT])
            ths.append(s8)

            def s9():
                dve.op(lambda e: e.reciprocal(out=st[:, 6:7], in_=st[:, 6:7]), reads=[b_stT], writes=[b_stT])
                dve.op(lambda e: e.tensor_scalar(out=af, in0=af, scalar1=st[:, 6:7], scalar2=None, op0=ALU.mult), reads=[b_af, b_stT], writes=[b_af])
                pe.op(lambda e: e.transpose(out=pbr[0:NE, 128:256], in_=af, identity=ident[:]), reads=[b_af, b_ident], writes=[b_pbAf])
            ths.append(s9)

            def s10():
                dve.op(lambda e: e.tensor_copy(out=affT[0:NE, t * 128:(t + 1) * 128], in_=pbr[0:NE, 128:256]), reads=[b_pbAf], writes=[b_affT])
            ths.append(s10)
            return ths

        from collections import deque
        bg = deque()
        for th in build_q(0):
            th()
        a8 = v3(accS, 8)
        pti = 0
        for gi, (t0, nq) in enumerate(qgroups):
            par = gi % 2
            s = 0 if t0 < NLT else 1
            NQ = nq * 128
            kts = list(range(NT)) if s == 0 else [NLT, NLT + 1]
            if gi + 1 < len(qgroups):
                bg.extendleft(reversed(build_q(gi + 1)))
            iters = [(h, c, ki, kt) for h in range(4) for c in range(2) for ki, kt in enumerate(kts)]
            timed = []

            def emit_S(idx):
                h, c, ki, kt = iters[idx]; sb = idx % 2
                i8 = c * 4 + h; j = i8 // 2
                pe.op(lambda e: e.matmul(pb[sb][:, 0:NQ], lhsT=v3(kT, 4)[:, j, kt * 128:(kt + 1) * 128], rhs=v3(qTm[par], 8)[:, i8, 0:NQ], start=True, stop=True),
                      reads=[b_kT, b_qTm[par]], writes=[b_pb[sb]])

            def E1(h):
                for bank in range(3):
                    na = 3 if bank < 2 else 2
                    src = pb[2 + bank][:, 0:na * 160].rearrange("p (a c) -> p a c", a=na)[:, :, 0:130]
                    dve.op(lambda e: e.tensor_copy(out=a8[:, bank * 3:bank * 3 + na, :], in_=src), reads=[b_pb[2 + bank]], writes=[b_accS])
                for qi in range(nq):
                    a0 = a8[:, qi, :]; a1 = a8[:, 4 + qi, :]
                    dve.op(lambda e: e.reciprocal(out=st[:, 2:3], in_=a0[:, 128:129]), reads=[b_accS], writes=[b_stE])
                    dve.op(lambda e: e.reciprocal(out=st[:, 3:4], in_=a1[:, 128:129]), reads=[b_accS], writes=[b_stE])
                    dve.op(lambda e: e.tensor_tensor(out=st[:, 3:4], in0=st[:, 3:4], in1=lamt[:, 3:4], op=ALU.mult), reads=[b_stE, b_lamt], writes=[b_stE])
                    dve.op(lambda e: e.tensor_scalar(out=t1, in0=a1[:, 0:128], scalar1=st[:, 3:4], scalar2=None, op0=ALU.mult), reads=[b_accS, b_stE], writes=[b_t1])
                    dve.op(lambda e: e.scalar_tensor_tensor(out=osb[qi], in0=a0[:, 0:128], scalar=st[:, 2:3], in1=t1, op0=ALU.mult, op1=ALU.add),
                           reads=[b_accS, b_stE, b_t1], writes=[b_osb[qi]])
                    dve.op(lambda e: e.tensor_tensor(out=t1, in0=osb[qi], in1=osb[qi], op=ALU.mult), reads=[b_osb[qi]], writes=[b_t1])
                    dve.op(lambda e: e.tensor_reduce(out=st[:, 8 + qi:9 + qi], in_=t1, axis=AX.X, op=ALU.add), reads=[b_t1], writes=[b_stE2])

            def E2(h):
                rstd_op(st[:, 8:8 + nq], st[:, 8:8 + nq], 1.0 / 128, [b_stE2])

            def E3(h):
                for qi in range(nq):
                    dve.op(lambda e: e.scalar_tensor_tensor(out=mixo[par][qi][:, h * 128:(h + 1) * 128], in0=osb[qi], scalar=st[:, 8 + qi:9 + qi], in1=gsub[:], op0=ALU.mult, op1=ALU.mult),
                           reads=[b_osb[qi], b_stE2, b_gsub], writes=[b_mixo[par][qi]])

            import os as _os
            NOLOOK = _os.environ.get("KNOLOOK") == "1"; NOBG = _os.environ.get("KNOBG") == "1"
            if not NOLOOK:
                emit_S(0)
            for idx in range(len(iters)):
                if NOLOOK:
                    emit_S(idx)
                elif idx + 1 < len(iters):
                    emit_S(idx + 1)
                h, c, ki, kt = iters[idx]; sb = idx % 2
                pi = pti % 3; pti += 1
                act.op(lambda e: e.activation(out=pT[pi][:, 0:NQ], in_=pb[sb][:, 0:NQ], func=AF.Exp, scale=0.125),
                       reads=[b_pb[sb]], writes=[b_pT[pi]])
                banks_seen = set()
                for qi in range(nq):
                    a_ = c * 4 + qi
                    aap, bacc = acc_ap(a_)
                    first_in_bank = (a_ // 3) not in banks_seen
                    banks_seen.add(a_ // 3)
                    pe.op(lambda e: e.matmul(aap, lhsT=pT[pi][:, qi * 128:(qi + 1) * 128], rhs=va4[:, kt, h, :],
                                             start=(ki == 0 and first_in_bank), stop=(ki == len(kts) - 1), skip_group_check=True),
                          reads=[b_pT[pi], b_vaug], writes=[bacc], pe_accum=(ki > 0))
                if c == 1 and ki == len(kts) - 1:
                    while timed:
                        timed.pop(0)[1]()
                    E1(h)
                    timed.append((idx + 6, lambda h=h: E2(h)))
                    timed.append((idx + 12, lambda h=h: E3(h)))
                while timed and timed[0][0] <= idx:
                    timed.pop(0)[1]()
                if idx % 2 == 1 and bg and not NOBG:
                    bg.popleft()()
            while timed:
                timed.pop(0)[1]()
            while NOBG and bg:
                bg.popleft()()
            for qi in range(nq):
                bg.extend(tile_thunks(gi, qi))
        while bg:
            bg.popleft()()
        P.barrier()

        AFa.off = af_mark; ABa.reset()
        NTOK = CAP if last else CAP + CCAP
        NJ = 4 if last else 5
        work = affT[:, 0:SEQ]; b_work = b_affT
        workc = affT[:, SEQ:T]
        vals = AFa.alloc(NTOK); b_vals = P.buf("vals%d" % l)
        idxu = AFa.alloc(NTOK).bitcast(U32); b_idxu = P.buf("idxu%d" % l)
        idxf = AFa.alloc(NTOK); b_idxf = P.buf("idxf%d" % l)
        gT = AFa.alloc(5 * NE); b_gT = P.buf("gT%d" % l)
        idxT = AFa.alloc(5 * NE).bitcast(I32); b_idxT = P.buf("idxT%d" % l)
        sgs = [AFa.alloc(544) for _ in range(2)]; b_sgs = [P.buf("sgs%d_%d" % (l, i)) for i in range(2)]
        ysc = AFa.alloc(5 * D); b_ysc = P.buf("ysc%d" % l)
        ring = [ABa.alloc(8192) for _ in range(3)]; b_ring = [P.buf("ring_%d" % i, True) for i in range(3)]
        xs = [ABa.alloc(5 * D) for _ in range(2)]; b_xs = [P.buf("xs_%d" % i, True) for i in range(2)]
        xsT = ABa.alloc(8 * 544); b_xsT = P.buf("xsT%d" % l)
        hidT = ABa.alloc(16 * 544); b_hidT = P.buf("hidT%d" % l)
        for r in range(CAP // 8):
            dve.op(lambda e, r=r: e.max(out=vals[0:NE, r * 8:(r + 1) * 8], in_=work[0:NE, :]), reads=[b_work], writes=[b_vals])
            dve.op(lambda e, r=r: e.max_index(out=idxu[0:NE, r * 8:(r + 1) * 8], in_max=vals[0:NE, r * 8:(r + 1) * 8], in_values=work[0:NE, :]),
                   reads=[b_work, b_vals], writes=[b_idxu])
            if r < CAP // 8 - 1:
                dve.op(lambda e, r=r: e.match_replace(out=work[0:NE, :], in_to_replace=vals[0:NE, r * 8:(r + 1) * 8], in_values=work[0:NE, :], imm_value=-1.0),
                       reads=[b_vals, b_work], writes=[b_work])
        if not last:
            for r in range(CCAP // 8):
                c0 = CAP + r * 8
                dve.op(lambda e, c0=c0: e.max(out=vals[0:NE, c0:c0 + 8], in_=workc[0:NE, :]), reads=[b_work], writes=[b_vals])
                dve.op(lambda e, c0=c0: e.max_index(out=idxu[0:NE, c0:c0 + 8], in_max=vals[0:NE, c0:c0 + 8], in_values=workc[0:NE, :]),
                       reads=[b_work, b_vals], writes=[b_idxu])
                if r < CCAP // 8 - 1:
                    dve.op(lambda e, c0=c0: e.match_replace(out=workc[0:NE, :], in_to_replace=vals[0:NE, c0:c0 + 8], in_values=workc[0:NE, :], imm_value=-1.0),
                           reads=[b_vals, b_work], writes=[b_work])
        dve.op(lambda e: e.tensor_copy(out=idxf[0:NE, :], in_=idxu[0:NE, :]), reads=[b_idxu], writes=[b_idxf])
        if not last:
            dve.op(lambda e: e.tensor_scalar(out=idxf[0:NE, CAP:NTOK], in0=idxf[0:NE, CAP:NTOK], scalar1=float(SEQ), scalar2=None, op0=ALU.add),
                   reads=[b_idxf], writes=[b_idxf])
        for (srcv, bsrc, dstv, bdst) in ((idxf, b_idxf, idxT, b_idxT), (vals, b_vals, gT, b_gT)):
            for jc in range(NJ):
                nj = 128 if jc < 4 else CCAP
                pe.op(lambda e, jc=jc, nj=nj, srcv=srcv: e.transpose(out=pb[4][0:nj, jc * NE:(jc + 1) * NE], in_=srcv[0:NE, jc * 128:jc * 128 + nj], identity=ident[0:NE, 0:NE]),
                      reads=[bsrc, b_ident], writes=[b_pb[4]], pe_accum=(jc > 0))
            dve.op(lambda e, dstv=dstv: e.tensor_copy(out=dstv[:, 0:4 * NE], in_=pb[4][:, 0:4 * NE]), reads=[b_pb[4]], writes=[bdst])
            if not last:
                dve.op(lambda e, dstv=dstv: e.tensor_copy(out=dstv[0:CCAP, 4 * NE:5 * NE], in_=pb[4][0:CCAP, 4 * NE:5 * NE]), reads=[b_pb[4]], writes=[bdst])

        def gather(e_):
            xb = xs[e_ % 2]; bx = b_xs[e_ % 2]
            NJs = [(jc, 128 if jc < 4 else CCAP) for jc in range(NJ)]
            pool.dma([lambda e, jc=jc, nj=nj: e.indirect_dma_start(
                out=xb[0:nj, jc * D:(jc + 1) * D], out_offset=None, in_=h2d[:, :],
                in_offset=bass.IndirectOffsetOnAxis(ap=idxT[0:nj, jc * NE + e_: jc * NE + e_ + 1], axis=0)) for (jc, nj) in NJs],
                bx.sem, reads=[b_idxT], writes=[bx])

        pieces = [(e_, p_) for e_ in range(NE) for p_ in range(6)]

        def load_piece(pi_):
            e_, p_ = pieces[pi_]
            rb = ring[pi_ % 3]; brb = b_ring[pi_ % 3]
            if p_ < 4:
                pool.dma([lambda e: e.dma_start(out=v3(rb[:, 0:4096], 8), in_=w_gate[l, e_].rearrange("(k p) n -> p k n", p=128)[:, :, p_ * 512:(p_ + 1) * 512]),
                          lambda e: e.dma_start(out=v3(rb[:, 4096:8192], 8), in_=w_up[l, e_].rearrange("(k p) n -> p k n", p=128)[:, :, p_ * 512:(p_ + 1) * 512])],
                         brb.sem, writes=[brb])
            else:
                hv = p_ - 4
                pool.dma([lambda e, q_=q_: e.dma_start(
                    out=v3(rb[:, q_ * 4096:(q_ + 1) * 4096], 8),
                    in_=w_down[l, e_].rearrange("(f p) n -> p f n", p=128)[:, q_ * 8:(q_ + 1) * 8, hv * 512:(hv + 1) * 512]) for q_ in range(2)],
                    brb.sem, writes=[brb])

        b_xrs = P.buf("xrs", True)
        gather(0)
        load_piece(0); load_piece(1)
        sgi_ = [0]
        for e_ in range(NE):
            xb = xs[e_ % 2]; bx = b_xs[e_ % 2]
            if e_ + 1 < NE:
                gather(e_ + 1)
            for jc in range(NJ):
                nj = 128 if jc < 4 else CCAP
                for kh in range(2):
                    for k4 in range(4):
                        k = kh * 4 + k4
                        pe.op(lambda e: e.transpose(out=pbh[:, k4 * 128:k4 * 128 + nj], in_=xb[0:nj, jc * D + k * 128: jc * D + (k + 1) * 128],
                                                    identity=identb[0:nj, 0:nj]),
                              reads=[bx, b_identb], writes=[b_pbh], pe_accum=(k4 > 0))
                    act.op(lambda e: e.activation(out=v3(xsT, 8)[:, kh * 4:(kh + 1) * 4, jc * 128:jc * 128 + nj], in_=v3(pbh[:, :], 4)[:, :, 0:nj], func=AF.Copy),
                           reads=[b_pbh], writes=[b_xsT])
            for p_ in range(6):
                pi_ = e_ * 6 + p_
                if pi_ + 2 < len(pieces):
                    load_piece(pi_ + 2)
                rb = ring[pi_ % 3]; brb = b_ring[pi_ % 3]
                if p_ < 4:
                    for fi in range(4):
                        f = p_ * 4 + fi
                        gb = fi % 2
                        for wi, (woff, pbt, bpbt) in enumerate(((0, pb[gb], b_pb[gb]), (4096, pb[2 + gb], b_pb[2 + gb]))):
                            for k in range(8):
                                pe.op(lambda e, rb=rb, woff=woff, k=k, fi=fi, pbt=pbt: e.matmul(pbt[:, 0:CAP], lhsT=v3(rb[:, woff:woff + 4096], 8)[:, k, fi * 128:(fi + 1) * 128],
                                                                                              rhs=v3(xsT, 8)[:, k, 0:CAP], start=(k == 0), stop=(k == 7)),
                                      reads=[brb, b_xsT], writes=[bpbt], pe_accum=(k > 0))
                            if not last:
                                co = gb * 64 + wi * 32
                                for k in range(8):
                                    pe.op(lambda e, rb=rb, woff=woff, k=k, fi=fi, co=co: e.matmul(pb[4][:, co:co + CCAP], lhsT=v3(rb[:, woff:woff + 4096], 8)[:, k, fi * 128:(fi + 1) * 128],
                                                                                                rhs=v3(xsT, 8)[:, k, CAP:NTOK], start=(k == 0), stop=(k == 7)),
                                          reads=[brb, b_xsT], writes=[b_pb[4]], pe_accum=(k > 0))
                        sg_ = sgs[sgi_[0] % 2]; bsg = b_sgs[sgi_[0] % 2]; sgi_[0] += 1
                        act.op(lambda e, gb=gb, sg_=sg_: e.activation(out=sg_[:, 0:CAP], in_=pb[gb][:, 0:CAP], func=AF.Silu), reads=[b_pb[gb]], writes=[bsg])
                        dve.op(lambda e, gb=gb, sg_=sg_, f=f: e.tensor_tensor(out=v3(hidT, 16)[:, f, 0:CAP], in0=sg_[:, 0:CAP], in1=pb[2 + gb][:, 0:CAP], op=ALU.mult),
                               reads=[bsg, b_pb[2 + gb]], writes=[b_hidT])
                        if not last:
                            co = gb * 64
                            act.op(lambda e, co=co, sg_=sg_: e.activation(out=sg_[:, CAP:NTOK], in_=pb[4][:, co:co + CCAP], func=AF.Silu), reads=[b_pb[4]], writes=[bsg])
                            dve.op(lambda e, co=co, sg_=sg_, f=f: e.tensor_tensor(out=v3(hidT, 16)[:, f, CAP:NTOK], in0=sg_[:, CAP:NTOK], in1=pb[4][:, co + 32:co + 32 + CCAP], op=ALU.mult),
                                   reads=[bsg, b_pb[4]], writes=[b_hidT])
                else:
                    hv = p_ - 4
                    for jc in range(NJ):
                        nj = 128 if jc < 4 else CCAP
                        yb = jc % 2
                        for f in range(16):
                            pe.op(lambda e, rb=rb, f=f, jc=jc, nj=nj, yb=yb: e.matmul(pbb[0:nj, yb * 512:(yb + 1) * 512], lhsT=v3(hidT, 16)[:, f, jc * 128:jc * 128 + nj],
                                                                                    rhs=v3(rb[:, (f // 8) * 4096:(f // 8 + 1) * 4096], 8)[:, f % 8, :], start=(f == 0), stop=(f == 15)),
                                  reads=[b_hidT, brb], writes=[b_pbb], pe_accum=(f > 0))
                        s = 0 if jc < 4 else 1
                        dve.op(lambda e, jc=jc, nj=nj, yb=yb, hv=hv, s=s, e_=e_: e.scalar_tensor_tensor(
                            out=ysc[0:nj, jc * D + hv * 512: jc * D + (hv + 1) * 512], in0=pbb[0:nj, yb * 512:(yb + 1) * 512],
                            scalar=gT[0:nj, jc * NE + e_: jc * NE + e_ + 1], in1=growv(s, 1)[0:nj, hv * 512:(hv + 1) * 512], op0=ALU.mult, op1=ALU.mult),
                            reads=[b_pbb, b_gT, b_grow], writes=[b_ysc])
            NJs = [(jc, 128 if jc < 4 else CCAP) for jc in range(NJ)]
            pool.dma([lambda e, jc=jc, nj=nj: e.indirect_dma_start(
                out=xr[:, :], out_offset=bass.IndirectOffsetOnAxis(ap=idxT[0:nj, jc * NE + e_: jc * NE + e_ + 1], axis=0),
                in_=ysc[0:nj, jc * D:(jc + 1) * D], in_offset=None, compute_op=ALU.add) for (jc, nj) in NJs],
                b_xrs.sem, reads=[b_ysc, b_idxT, b_xrs], writes=[b_xrs])
        P.barrier()

    AFa.reset(); ABa.reset()
    gf = AFa.alloc(D); b_gf = P.buf("gf", True)
    xt = [AFa.alloc(D) for _ in range(2)]; b_xt = [P.buf("xt_%d" % i, True) for i in range(2)]
    yo = [AFa.alloc(D) for _ in range(2)]; b_yo = [P.buf("yo%d" % i, True) for i in range(2)]
    junk = AFa.alloc(D); b_junk = P.buf("junkf")
    st = small[:, 32:40]; b_st = P.buf("stf")
    sp.dma(lambda e: e.dma_start(out=gf, in_=norm_f_g[0:1, :].to_broadcast([128, D])), b_gf.sem, writes=[b_gf])
    for t in range(NLT):
        i = t % 2
        sp.dma(lambda e, t=t, i=i: e.dma_start(out=xt[i], in_=xr[t * 128:(t + 1) * 128, :]), b_xt[i].sem, writes=[b_xt[i]])
        if final_norm:
            pool.op(lambda e: e.memset(st[:, 0:1], 0.0), writes=[b_st])
            act.op(lambda e, i=i: e.activation(out=junk, in_=xt[i], func=AF.Square, accum_out=st[:, 0:1]), reads=[b_xt[i], b_st], writes=[b_junk, b_st])
            rstd_op(st[:, 1:2], st[:, 0:1], 1.0 / D, [b_st])
            dve.op(lambda e, i=i: e.scalar_tensor_tensor(out=yo[i], in0=xt[i], scalar=st[:, 1:2], in1=gf, op0=ALU.mult, op1=ALU.mult),
                   reads=[b_xt[i], b_st, b_gf], writes=[b_yo[i]])
        else:
            dve.op(lambda e, i=i: e.tensor_copy(out=yo[i], in_=xt[i]), reads=[b_xt[i]], writes=[b_yo[i]])
        sp.dma(lambda e, t=t, i=i: e.dma_start(out=out_d[t * 128:(t + 1) * 128, :], in_=yo[i]), b_yo[i].sem, reads=[b_yo[i]])
    P.finish()
    return nc


def _rope_table():
    rows = SEQ // 64
    row = np.repeat(np.arange(rows), 64).astype(np.float32)
    col = np.tile(np.arange(64), rows).astype(np.float32)
    n_freq = 16
    freqs = (np.float32(10000.0) ** (-np.arange(n_freq, dtype=np.float32) / np.float32(n_freq))).astype(np.float32)
    ang_r = (row[:, None] * freqs).astype(np.float32)
    ang_c = (col[:, None] * freqs).astype(np.float32)
    ang = np.concatenate([ang_r, ang_r, ang_c, ang_c], axis=-1)
    cos = np.cos(ang).astype(np.float32); sin = np.sin(ang).astype(np.float32)
    tab = np.zeros((T, 128), np.float32)
    tab[:SEQ, 0:64] = cos
    sgn = np.ones((2, 2, 16), np.float32); sgn[:, 0, :] = -1.0
    tab[:SEQ, 64:128] = sin * sgn.reshape(64)
    tab[SEQ:, 0:64] = 1.0
    return tab


def prep_shared(inp):
    f = lambda a: np.ascontiguousarray(np.asarray(a, dtype=np.float32))
    sh = {
        "rope": _rope_table(), "ident": np.eye(128, dtype=np.float32),
        "w_ada": f(inp["w_ada"]), "b_ada": f(inp["b_ada"]),
        "b_ada_col": f(np.asarray(inp["b_ada"]).reshape(DEPTH, 48, 128).transpose(0, 2, 1)),
        "n1col": f(np.asarray(inp["norm1_g"]).reshape(DEPTH, 8, 128).transpose(0, 2, 1)),
        "n2col": f(np.asarray(inp["norm2_g"]).reshape(DEPTH, 8, 128).transpose(0, 2, 1)),
        "w_in": f(inp["w_in"]), "w_out": f(inp["w_out"]),
        "lamv": f(np.concatenate([np.asarray(inp["lambda_q1"]), np.asarray(inp["lambda_k1"]), np.asarray(inp["lambda_q2"]), np.asarray(inp["lambda_k2"])], axis=1)),
        "subln_g": f(inp["subln_g"]), "sgu_norm_g": f(inp["sgu_norm_g"]), "sgu_w": f(inp["sgu_w"]),
        "sgu_bT": f(np.asarray(inp["sgu_b"]).transpose(0, 2, 1)),
        "w_router": f(inp["w_router"]), "w_gate": f(inp["w_gate"]), "w_up": f(inp["w_up"]), "w_down": f(inp["w_down"]),
        "norm_f_g": f(np.asarray(inp["norm_f_g"]).reshape(1, D)),
    }
    return sh


def prep_core(inp, b):
    x = np.asarray(inp["x"], dtype=np.float32)[b]; cx = np.asarray(inp["ctx"], dtype=np.float32)[b]
    c = np.asarray(inp["c"], dtype=np.float32)[b]; cctx = np.asarray(inp["c_ctx"], dtype=np.float32)
    cc = np.stack([c.reshape(8, 128).T, cctx.reshape(8, 128).T], axis=-1).reshape(128, 16)
    return {"x": np.ascontiguousarray(np.concatenate([x, cx], axis=0)), "cc": np.ascontiguousarray(cc)}


def kernel(**inputs):
    sh = prep_shared(inputs)
    nc = build()
    in_maps = []
    for b in range(8):
        m = dict(sh); m.update(prep_core(inputs, b)); in_maps.append(m)
    res = run_bass_kernel_spmd(nc, in_maps, core_ids=list(range(8)))
    return np.stack([np.asarray(r["out"], dtype=np.float32) for r in res.results], axis=0)
```

```python
import math
import numpy as np
from contextlib import ExitStack
import concourse.bass as bass
import concourse.mybir as mybir
from concourse.bass_utils import run_bass_kernel_spmd

F32 = mybir.dt.float32; BF16 = mybir.dt.bfloat16; I32 = mybir.dt.int32; U32 = mybir.dt.uint32
AF = mybir.ActivationFunctionType; ALU = mybir.AluOpType; AX = mybir.AxisListType

D = 1024; SEQ = 4096; CTX = 256; T = SEQ + CTX; NT = T // 128; NLT = SEQ // 128
DEPTH = 4; NE = 16; CAP = 512; CCAP = 32; FF = 2048
EPS = 1e-6


class Sem:
    def __init__(self, h, name):
        self.h = h; self.name = name; self.val = 0


class Buf:
    def __init__(self, name, sem=None, excl=False):
        self.name = name; self.w = None; self.r = {}; self.sem = sem; self.excl = excl


class Call:
    def __init__(self, name, a, k):
        self.name = name; self.a = a; self.k = k

    def run(self, e):
        return getattr(e, self.name)(*self.a, **self.k)


class Rec:
    def __getattr__(self, name):
        return lambda *a, **k: Call(name, a, k)


REC = Rec()


class Eng:
    def __init__(self, prog, name, sem):
        self.prog = prog; self.name = name; self.sem = sem; self.q = []; self.waited = {}

    def wait_tok(self, sem, val):
        if self.name == 'pe' and sem is self.sem:
            return
        if self.waited.get(sem, 0) >= val:
            return
        self.waited[sem] = val
        h = sem.h
        self.q.append(lambda e, h=h, val=val: e.wait_ge(h, val))

    def deps(self, reads, writes, pe_accum=False, nowaw=False):
        for b in reads:
            if b.w is not None:
                self.wait_tok(*b.w)
            if b.excl:
                for s, v in b.r.items():
                    if s is not self.sem:
                        self.wait_tok(s, v)
        for b in writes:
            if pe_accum:
                continue
            if b.w is not None and not (nowaw and b.w[0] is self.sem):
                self.wait_tok(*b.w)
            for s, v in b.r.items():
                self.wait_tok(s, v)

    def mark(self, tok, reads, writes):
        s, v = tok
        for b in reads:
            if b.r.get(s, 0) < v:
                b.r[s] = v
        for b in writes:
            b.w = tok; b.r = {}

    def op(self, fn, reads=(), writes=(), pe_accum=False, nowaw=False):
        self.deps(reads, writes, pe_accum, nowaw)
        self.sem.val += 1
        tok = (self.sem, self.sem.val)
        h = self.sem.h
        call = fn(REC)
        self.q.append(lambda e, call=call, h=h: call.run(e).then_inc(h, 1))
        self.mark(tok, reads, writes)
        return tok

    def dma(self, fn, sem, reads=(), writes=()):
        fns = fn if isinstance(fn, (list, tuple)) else [fn]
        self.deps(reads, writes)
        h = sem.h
        for f in fns:
            sem.val += 16
            call = f(REC)
            self.q.append(lambda e, call=call, h=h: call.run(e).then_inc(h, 16))
        tok = (sem, sem.val)
        self.mark(tok, reads, writes)
        return tok


class Prog:
    def __init__(self, nc):
        self.nc = nc; self.es = ExitStack(); self.sems = []; self.semcache = {}
        self.sp = Eng(self, 'sp', self.new_sem('e_sp'))
        self.act = Eng(self, 'act', self.new_sem('e_act'))
        self.pool = Eng(self, 'pool', self.new_sem('e_pool'))
        self.dve = Eng(self, 'dve', self.new_sem('e_dve'))
        self.pe = Eng(self, 'pe', self.new_sem('e_pe'))
        self.engs = [self.sp, self.act, self.pool, self.dve, self.pe]

    def new_sem(self, name):
        s = Sem(self.es.enter_context(self.nc.semaphore(name)), name)
        self.sems.append(s)
        return s

    def sbuf(self, name, shape, dt):
        return self.es.enter_context(self.nc.sbuf_tensor(name, shape, dt))

    def psum(self, name, shape, dt):
        return self.es.enter_context(self.nc.psum_tensor(name, shape, dt))

    def buf(self, name, dma=False):
        if not dma:
            return Buf(name, None)
        if name not in self.semcache:
            self.semcache[name] = self.new_sem('b_' + name)
        return Buf(name, self.semcache[name])

    def barrier(self):
        for e in self.engs:
            for s in self.sems:
                if s.val > 0:
                    e.wait_tok(s, s.val)

    def finish(self):
        self.barrier()
        with self.nc.Block() as block:
            @block.sync
            def _(e):
                for f in self.sp.q: f(e)
            @block.scalar
            def _(e):
                for f in self.act.q: f(e)
            @block.gpsimd
            def _(e):
                for f in self.pool.q: f(e)
            @block.vector
            def _(e):
                for f in self.dve.q: f(e)
            @block.tensor
            def _(e):
                for f in self.pe.q: f(e)
        self.es.close()


class Arena:
    def __init__(self, P, name, ncols, dt):
        self.t = P.sbuf(name, [128, ncols], dt); self.off = 0; self.n = ncols; self.name = name

    def alloc(self, cols):
        a = self.off; self.off += cols
        assert self.off <= self.n, (self.name, self.off, self.n)
        return self.t[:, a:a + cols]

    def reset(self):
        self.off = 0


def v3(ap, a):
    return ap.rearrange("p (a b) -> p a b", a=a)


def build(n_layers=DEPTH, final_norm=True):
    nc = bass.Bass("TRN2", target_bir_lowering=False)
    dram = lambda name, shape, dt, kind="ExternalInput": nc.dram_tensor(name, shape, dt, kind=kind).ap()
    x_in = dram("x", [T, D], F32)
    cc_d = dram("cc", [128, 16], F32)
    rope_d = dram("rope", [T, 128], F32)
    ident_d = dram("ident", [128, 128], F32)
    w_ada = dram("w_ada", [DEPTH, D, 6 * D], F32)
    b_ada_col = dram("b_ada_col", [DEPTH, 128, 48], F32)
    b_ada = dram("b_ada", [DEPTH, 6 * D], F32)
    n1col = dram("n1col", [DEPTH, 128, 8], F32)
    n2col = dram("n2col", [DEPTH, 128, 8], F32)
    w_in = dram("w_in", [DEPTH, D, 2560], F32)
    w_out = dram("w_out", [DEPTH, D, D], F32)
    lamv = dram("lamv", [DEPTH, 256], F32)
    subln_g = dram("subln_g", [DEPTH, 128], F32)
    sgu_norm_g = dram("sgu_norm_g", [DEPTH, 512], F32)
    sgu_w = dram("sgu_w", [DEPTH, 8, 128, 128], F32)
    sgu_bT = dram("sgu_bT", [DEPTH, 128, 8], F32)
    w_router = dram("w_router", [DEPTH, D, NE], F32)
    w_gate = dram("w_gate", [DEPTH, NE, D, FF], F32)
    w_up = dram("w_up", [DEPTH, NE, D, FF], F32)
    w_down = dram("w_down", [DEPTH, NE, FF, D], F32)
    norm_f_g = dram("norm_f_g", [1, D], F32)
    out_d = dram("out", [SEQ, D], F32, kind="ExternalOutput")
    xr = dram("xr", [T, D], F32, kind="Internal")
    qd = dram("qd", [T, 512], BF16, kind="Internal")
    sgd = dram("sgd", [T, 512], BF16, kind="Internal")
    h2d = dram("h2d", [T, D], BF16, kind="Internal")

    P = Prog(nc)
    sp, act, pool, dve, pe = P.sp, P.act, P.pool, P.dve, P.pe

    ident = P.sbuf("ident_sb", [128, 128], F32); b_ident = P.buf("ident", True)
    identb = P.sbuf("identb_sb", [128, 128], BF16); b_identb = P.buf("identb")
    grow = P.sbuf("grow", [128, 4 * D], F32); b_grow = P.buf("grow")
    siluc = P.sbuf("siluc", [128, 16], F32); b_siluc = P.buf("siluc", True)
    modT = P.sbuf("modT", [128, 96], F32); b_modT = P.buf("modT")
    bcol = P.sbuf("bcol", [128, 48], F32); b_bcol = P.buf("bcol", True)
    ncol = P.sbuf("ncol", [128, 16], F32); b_ncol = P.buf("ncol", True)
    AB = P.sbuf("ABcols", [128, 32], F32); b_AB = P.buf("ABc")
    lamt = P.sbuf("lamt", [128, 8], F32); b_lamt = P.buf("lamt")
    lv = P.sbuf("lv", [128, 256], F32); b_lv = P.buf("lv", True)
    lprod = P.sbuf("lprod", [128, 128], F32); b_lprod = P.buf("lprod")
    gsub = P.sbuf("gsub", [128, 128], F32); b_gsub = P.buf("gsub", True)
    sgng = P.sbuf("sgng", [128, 512], F32); b_sgng = P.buf("sgng", True)
    sbT = P.sbuf("sbT", [128, 8], F32); b_sbT = P.buf("sbT", True)
    bfull = P.sbuf("bfull", [128, 512], F32); b_bfull = P.buf("bfull")
    wsT = P.sbuf("wsT", [128, 8 * 128], BF16); b_wsT = P.buf("wsT")
    wrt = P.sbuf("wrt", [128, 8 * NE], F32); b_wrt = P.buf("wrt", True)
    small = P.sbuf("small", [128, 64], F32)
    epsc = P.sbuf("epsc", [128, 1], F32); b_epsc = P.buf("epsc")
    dve.op(lambda e: e.memset(epsc[:], EPS), writes=[b_epsc])

    def rstd_op(dst, src, scale, bufs):
        act.op(lambda e: e.activation(out=dst, in_=src, func=AF.Ln, scale=float(scale), bias=epsc[:, 0:1]), reads=bufs + [b_epsc], writes=bufs)
        act.op(lambda e: e.activation(out=dst, in_=dst, func=AF.Exp, scale=-0.5), reads=bufs, writes=bufs)

    AFa = Arena(P, "arenaF", 12928, F32)
    ABa = Arena(P, "arenaB", 62464, BF16)

    pb = [P.psum("pb%d" % i, [128, 512], F32) for i in range(5)]
    pbhr = P.psum("pbhr", [128, 512], F32)
    pbh = pbhr[:, 0:256].bitcast(BF16)
    pbr = pbhr[:, 256:512]
    pbb = P.psum("pbb", [128, 1024], F32)
    b_pb = [Buf("pb%d" % i, excl=True) for i in range(5)]
    b_pbh = Buf("pbh", excl=True); b_pbb = Buf("pbb", excl=True)

    sp.dma(lambda e: e.dma_start(out=ident[:], in_=ident_d), b_ident.sem, writes=[b_ident])
    dve.op(lambda e: e.tensor_copy(out=identb[:], in_=ident[:]), reads=[b_ident], writes=[b_identb])
    sp.dma(lambda e: e.dma_start(out=siluc[:], in_=cc_d), b_siluc.sem, writes=[b_siluc])
    act.op(lambda e: e.activation(out=siluc[:], in_=siluc[:], func=AF.Silu), reads=[b_siluc], writes=[b_siluc])
    b_xr = P.buf("xr", True)
    sp.dma([lambda e, i=i: e.dma_start(out=xr[i * (T // 4):(i + 1) * (T // 4), :], in_=x_in[i * (T // 4):(i + 1) * (T // 4), :]) for i in range(4)],
           b_xr.sem, writes=[b_xr])
    P.barrier()

    for l in range(n_layers):
        last = (l == DEPTH - 1)
        lam_init = 0.8 - 0.6 * math.exp(-0.3 * l)
        NTq = NLT if last else NT
        AFa.reset(); ABa.reset()
        wa = [AFa.alloc(8 * 512) for _ in range(2)]; b_wa = [P.buf("wa_%d" % i, True) for i in range(2)]
        brow = AFa.alloc(512); b_brow = P.buf("brow", True)
        swl = AFa.alloc(8 * 128); b_swl = P.buf("swl", True)
        srep = AFa.alloc(16 * 128); b_srep = P.buf("srep%d" % l)
        for ks in range(16):
            dve.op(lambda e, ks=ks: e.tensor_copy(out=srep[:, ks * 128:(ks + 1) * 128], in_=siluc[:, ks:ks + 1].to_broadcast([128, 128])),
                   reads=[b_siluc], writes=[b_srep])
        sp.dma(lambda e: e.dma_start(out=bcol[:], in_=b_ada_col[l]), b_bcol.sem, writes=[b_bcol])
        sp.dma([lambda e: e.dma_start(out=ncol[:, 0:8], in_=n1col[l]), lambda e: e.dma_start(out=ncol[:, 8:16], in_=n2col[l])], b_ncol.sem, writes=[b_ncol])
        sp.dma(lambda e: e.dma_start(out=lv[:], in_=lamv[l:l + 1, :].to_broadcast([128, 256])), b_lv.sem, writes=[b_lv])
        sp.dma(lambda e: e.dma_start(out=gsub[:], in_=subln_g[l:l + 1, :].to_broadcast([128, 128])), b_gsub.sem, writes=[b_gsub])
        sp.dma(lambda e: e.dma_start(out=sgng[:], in_=sgu_norm_g[l:l + 1, :].to_broadcast([128, 512])), b_sgng.sem, writes=[b_sgng])
        sp.dma(lambda e: e.dma_start(out=sbT[:], in_=sgu_bT[l]), b_sbT.sem, writes=[b_sbT])
        sp.dma(lambda e: e.dma_start(out=wrt[:].rearrange("p (k n) -> p k n", k=8), in_=w_router[l].rearrange("(k p) n -> p k n", p=128)),
               b_wrt.sem, writes=[b_wrt])
        dve.op(lambda e: e.tensor_tensor(out=v3(lprod[:], 2), in0=v3(lv[:], 2)[:, :, 0:64], in1=v3(lv[:], 2)[:, :, 64:128], op=ALU.mult),
               reads=[b_lv], writes=[b_lprod])
        dve.op(lambda e: e.tensor_reduce(out=lamt[:, 0:2], in_=v3(lprod[:], 2), axis=AX.X, op=ALU.add), reads=[b_lprod], writes=[b_lamt])
        act.op(lambda e: e.activation(out=lamt[:, 0:2], in_=lamt[:, 0:2], func=AF.Exp), reads=[b_lamt], writes=[b_lamt])
        dve.op(lambda e: e.tensor_tensor(out=lamt[:, 2:3], in0=lamt[:, 0:1], in1=lamt[:, 1:2], op=ALU.subtract), reads=[b_lamt], writes=[b_lamt])
        dve.op(lambda e: e.tensor_scalar(out=lamt[:, 2:3], in0=lamt[:, 2:3], scalar1=float(lam_init), scalar2=None, op0=ALU.add), reads=[b_lamt], writes=[b_lamt])
        dve.op(lambda e: e.tensor_scalar(out=lamt[:, 3:4], in0=lamt[:, 2:3], scalar1=-1.0, scalar2=None, op0=ALU.mult), reads=[b_lamt], writes=[b_lamt])
        dve.op(lambda e: e.tensor_scalar(out=gsub[:], in0=gsub[:], scalar1=float(1.0 - lam_init), scalar2=None, op0=ALU.mult), reads=[b_gsub], writes=[b_gsub])
        dve.op(lambda e: e.tensor_copy(out=v3(bfull[:], 8), in_=sbT[:].unsqueeze(2).to_broadcast([128, 8, 64])), reads=[b_sbT], writes=[b_bfull])
        for hh in range(2):
            sp.dma(lambda e, hh=hh: e.dma_start(out=v3(swl, 8)[:, 0:4, :], in_=sgu_w[l, hh * 4:(hh + 1) * 4].rearrange("h p q -> p h q")),
                   b_swl.sem, writes=[b_swl])
            for h4 in range(4):
                pe.op(lambda e, h4=h4: e.transpose(out=pbb[:, h4 * 128:(h4 + 1) * 128], in_=v3(swl, 8)[:, h4, :], identity=ident[:]),
                      reads=[b_swl, b_ident], writes=[b_pbb], pe_accum=(h4 > 0))
            act.op(lambda e, hh=hh: e.activation(out=wsT[:, hh * 512:(hh + 1) * 512], in_=pbb[:, 0:512], func=AF.Copy), reads=[b_pbb], writes=[b_wsT])
        for hg in range(12):
            g = hg // 2; half = hg % 2
            wb_ = wa[hg % 2]; bw = b_wa[hg % 2]
            sp.dma(lambda e, wb_=wb_, hg=hg: e.dma_start(out=v3(wb_, 8), in_=w_ada[l].rearrange("(k p) n -> p k n", p=128)[:, :, hg * 512:(hg + 1) * 512]),
                   bw.sem, writes=[bw])
            for nn in range(4):
                n = hg * 4 + nn
                for k in range(8):
                    pe.op(lambda e, wb_=wb_, nn=nn, k=k, n=n: e.matmul(pb[3][:, 2 * n:2 * n + 2], lhsT=v3(wb_, 8)[:, k, nn * 128:(nn + 1) * 128],
                                                                      rhs=siluc[:, 2 * k:2 * k + 2], start=(k == 0), stop=(k == 7)),
                          reads=[bw, b_siluc], writes=[b_pb[3]], pe_accum=not (hg == 0 and nn == 0 and k == 0))
            if g in (2, 5):
                which = 0 if g == 2 else 1
                sp.dma(lambda e, g=g, half=half: e.dma_start(out=brow, in_=b_ada[l:l + 1, g * 1024 + half * 512: g * 1024 + (half + 1) * 512].to_broadcast([128, 512])),
                       b_brow.sem, writes=[b_brow])
                for s in range(2):
                    for k in range(8):
                        pe.op(lambda e, wb_=wb_, s=s, k=k: e.matmul(pb[s][:, :], lhsT=srep[:, (2 * k + s) * 128:(2 * k + s + 1) * 128], rhs=v3(wb_, 8)[:, k, :],
                                                                    start=(k == 0), stop=(k == 7)),
                              reads=[bw, b_srep], writes=[b_pb[s]], pe_accum=(k > 0))
                    c0 = (s * 2 + which) * D + half * 512
                    dve.op(lambda e, s=s, c0=c0: e.tensor_tensor(out=grow[:, c0:c0 + 512], in0=pb[s][:, :], in1=brow, op=ALU.add),
                           reads=[b_pb[s], b_brow], writes=[b_grow])
        dve.op(lambda e: e.tensor_tensor(out=v3(modT[:], 48), in0=v3(pb[3][:, 0:96], 48), in1=bcol[:].unsqueeze(2).to_broadcast([128, 48, 2]), op=ALU.add),
               reads=[b_pb[3], b_bcol], writes=[b_modT])
        mT = v3(modT[:], 48)
        for w_ in range(2):
            for s in range(2):
                sc = mT[:, (1 + 3 * w_) * 8:(2 + 3 * w_) * 8, s]
                o = AB[:, (w_ * 2 + s) * 8:(w_ * 2 + s + 1) * 8]
                dve.op(lambda e, sc=sc, o=o, w_=w_: e.scalar_tensor_tensor(out=o, in0=sc, scalar=1.0, in1=ncol[:, w_ * 8:(w_ + 1) * 8], op0=ALU.add, op1=ALU.mult),
                       reads=[b_modT, b_ncol], writes=[b_AB])

        def Acol(w_, s, k): return AB[:, (w_ * 2 + s) * 8 + k:(w_ * 2 + s) * 8 + k + 1]
        def Bcol(w_, s, k): return modT[:, ((3 * w_) * 8 + k) * 2 + s:((3 * w_) * 8 + k) * 2 + s + 1]
        def growv(s, which): return grow[:, (s * 2 + which) * D:(s * 2 + which + 1) * D]
        P.barrier()

        AFa.reset(); ABa.reset()
        kT = ABa.alloc(4 * T); b_kT = P.buf("kT%d" % l)
        vaug = ABa.alloc(NT * 4 * 130); b_vaug = P.buf("vaug%d" % l)
        ab_mark = ABa.off
        w_in_sb = ABa.alloc(8 * 2560); b_win = P.buf("win", True)
        hT = [ABa.alloc(8 * 128) for _ in range(2)]; b_hT = [P.buf("hT%d_%d" % (l, i)) for i in range(2)]
        qr = ABa.alloc(512); b_qr = P.buf("qr", True)
        kr = ABa.alloc(512); b_kr = P.buf("kr%d" % l)
        gvn = [ABa.alloc(512) for _ in range(2)]; b_gvn = [P.buf("gvn%d_%d" % (l, i)) for i in range(2)]
        sgo = [ABa.alloc(512) for _ in range(2)]; b_sgo = [P.buf("sgo_%d" % i, True) for i in range(2)]
        xt = [AFa.alloc(D) for _ in range(2)]; b_xt = [P.buf("xt_%d" % i, True) for i in range(2)]
        rp = [AFa.alloc(128) for _ in range(3)]; b_rp = [P.buf("rp_%d" % i, True) for i in range(3)]
        xn = AFa.alloc(D); b_xn = P.buf("xn%d" % l)
        junk = AFa.alloc(D); b_junk = P.buf("junk%d" % l)
        t1 = AFa.alloc(512); b_t1 = P.buf("t1%d" % l)
        t2 = AFa.alloc(512); b_t2 = P.buf("t2%d" % l)
        u_sb = [AFa.alloc(512) for _ in range(2)]; b_u = [P.buf("u%d_%d" % (l, i)) for i in range(2)]
        gvg = [AFa.alloc(512) for _ in range(2)]; b_gvg = [P.buf("gvg%d_%d" % (l, i)) for i in range(2)]
        sqA = AFa.alloc(512); b_sqA = P.buf("sqA%d" % l)
        sqB = AFa.alloc(512); b_sqB = P.buf("sqB%d" % l)
        sqC = AFa.alloc(512); b_sqC = P.buf("sqC%d" % l)
        st = small[:, 0:8]; b_stN = P.buf("stN%d" % l)
        stP = [small[:, 8 + 16 * i:24 + 16 * i] for i in range(2)]; b_stP = [P.buf("stP%d_%d" % (l, i)) for i in range(2)]
        pool.dma([lambda e, i=i: e.dma_start(out=v3(w_in_sb, 8)[:, 2 * i:2 * i + 2, :], in_=w_in[l].rearrange("(k p) n -> p k n", p=128)[:, 2 * i:2 * i + 2, :]) for i in range(4)],
                 b_win.sem, writes=[b_win])
        va4 = vaug.rearrange("p (t h c) -> p t h c", t=NT, h=4)
        pool.op(lambda e: e.memset(vaug, 1.0), writes=[b_vaug])

        def load_tile(t):
            i = t % 2; i3 = t % 3
            sp.dma(lambda e: e.dma_start(out=xt[i], in_=xr[t * 128:(t + 1) * 128, :]), b_xt[i].sem, writes=[b_xt[i]])
            sp.dma(lambda e: e.dma_start(out=rp[i3], in_=rope_d[t * 128:(t + 1) * 128, :]), b_rp[i3].sem, writes=[b_rp[i3]])

        def N1(t):
            i = t % 2
            pool.op(lambda e: e.memset(st[:, 0:1], 0.0), writes=[b_stN])
            act.op(lambda e: e.activation(out=junk, in_=xt[i], func=AF.Square, accum_out=st[:, 0:1]), reads=[b_xt[i], b_stN], writes=[b_junk, b_stN])
            rstd_op(st[:, 1:2], st[:, 0:1], 1.0 / D, [b_stN])

        def N2(t):
            i = t % 2
            dve.op(lambda e: e.tensor_scalar(out=xn, in0=xt[i], scalar1=st[:, 1:2], scalar2=None, op0=ALU.mult), reads=[b_xt[i], b_stN], writes=[b_xn])

        def N3(t):
            for k in range(8):
                pe.op(lambda e: e.transpose(out=pbb[:, k * 128:(k + 1) * 128], in_=xn[:, k * 128:(k + 1) * 128], identity=ident[:]),
                      reads=[b_xn, b_ident], writes=[b_pbb], pe_accum=(k > 0))

        def N4(t):
            s_ = 0 if t < NLT else 1
            dst = hT[t % 2]
            for k in range(8):
                act.op(lambda e: e.activation(out=dst[:, k * 128:(k + 1) * 128], in_=pbb[:, k * 128:(k + 1) * 128], func=AF.Identity,
                                              scale=Acol(0, s_, k), bias=Bcol(0, s_, k)),
                       reads=[b_pbb, b_AB, b_modT], writes=[b_hT[t % 2]], nowaw=(k > 0))

        def rope(src_ps, b_src, rp_, b_rp_, dst, b_dst):
            s5 = src_ps.rearrange("p (g a r f) -> p g a r f", g=8, a=2, r=2)
            t5 = t2.rearrange("p (g a r f) -> p g a r f", g=8, a=2, r=2)
            sn = rp_[:, 64:128].rearrange("p (a r f) -> p a r f", a=2, r=2)
            dve.op(lambda e: e.tensor_tensor(out=v3(t1, 8), in0=v3(src_ps, 8), in1=rp_[:, 0:64].unsqueeze(1).to_broadcast([128, 8, 64]), op=ALU.mult),
                   reads=[b_src, b_rp_], writes=[b_t1])
            for a in range(2):
                for r in range(2):
                    dve.op(lambda e: e.tensor_tensor(out=t5[:, :, a, r, :], in0=s5[:, :, a, 1 - r, :],
                                                     in1=sn[:, a, r, :].unsqueeze(1).to_broadcast([128, 8, 16]), op=ALU.mult),
                           reads=[b_src, b_rp_], writes=[b_t2], nowaw=(a + r > 0))
            dve.op(lambda e: e.tensor_tensor(out=dst, in0=t1, in1=t2, op=ALU.add), reads=[b_t1, b_t2], writes=[b_dst])

        prot = [0, 1, 3, 4]; pcnt = [0]

        def proj(t, g):
            bi = prot[pcnt[0] % 4]; pcnt[0] += 1
            hsrc = hT[t % 2]
            for k in range(8):
                pe.op(lambda e: e.matmul(pb[bi][:, :], lhsT=hsrc[:, k * 128:(k + 1) * 128], rhs=v3(w_in_sb, 8)[:, k, g * 512:(g + 1) * 512],
                                         start=(k == 0), stop=(k == 7)),
                      reads=[b_hT[t % 2], b_win], writes=[b_pb[bi]], pe_accum=(k > 0))
            return pb[bi], b_pb[bi]

        load_tile(0); load_tile(1)
        N1(0); N2(0); N3(0); N4(0)
        pend = []

        def pop_pend():
            if pend:
                pend.pop(0)()
        for t in range(NT):
            nxt = t + 1 < NT
            i3 = t % 3
            need_q = t < NTq
            if t + 2 < NT:
                load_tile(t + 2)
            if nxt: N1(t + 1)
            pop_pend()
            if need_q:
                pbk, bpbk = proj(t, 0)
                rope(pbk[:, :], bpbk, rp[i3], b_rp[i3], qr, b_qr)
                sp.dma(lambda e: e.dma_start(out=qd[t * 128:(t + 1) * 128, :], in_=qr), b_qr.sem, reads=[b_qr])
            pop_pend()
            if nxt: N2(t + 1)
            pbk, bpbk = proj(t, 1)
            pop_pend()
            rope(pbk[:, :], bpbk, rp[i3], b_rp[i3], kr, b_kr)
            pop_pend()
            for j in range(4):
                pe.op(lambda e: e.transpose(out=pbh[:, j * 128:(j + 1) * 128], in_=kr[:, j * 128:(j + 1) * 128], identity=identb[:]),
                      reads=[b_kr, b_identb], writes=[b_pbh], pe_accum=(j > 0))
            act.op(lambda e: e.activation(out=v3(kT, 4)[:, :, t * 128:(t + 1) * 128], in_=v3(pbh[:, 0:512], 4), func=AF.Copy),
                   reads=[b_pbh], writes=[b_kT])
            if nxt: N3(t + 1)
            pbk, bpbk = proj(t, 2)
            act.op(lambda e: e.activation(out=va4[:, t, :, 0:128], in_=v3(pbk[:, :], 4), func=AF.Copy), reads=[bpbk], writes=[b_vaug])
            if nxt: N4(t + 1)
            if need_q:
                pr = t % 2
                pbk, bpbk = proj(t, 3)
                act.op(lambda e: e.activation(out=u_sb[pr], in_=pbk[:, :], func=AF.Gelu), reads=[bpbk], writes=[b_u[pr]])
                pbk, bpbk = proj(t, 4)
                act.op(lambda e: e.activation(out=gvg[pr], in_=pbk[:, :], func=AF.Gelu), reads=[bpbk], writes=[b_gvg[pr]])
                dve.op(lambda e: e.tensor_tensor(out=sqA, in0=gvg[pr], in1=gvg[pr], op=ALU.mult), reads=[b_gvg[pr]], writes=[b_sqA])
                dve.op(lambda e: e.tensor_reduce(out=stP[pr][:, 0:8], in_=v3(sqA, 8), axis=AX.X, op=ALU.add), reads=[b_sqA], writes=[b_stP[pr]])

                def T1(t=t, pr=pr):
                    rstd_op(stP[pr][:, 8:16], stP[pr][:, 0:8], 1.0 / 64, [b_stP[pr]])

                def T2(t=t, pr=pr):
                    dve.op(lambda e: e.tensor_tensor(out=v3(sqB, 8), in0=v3(gvg[pr], 8), in1=stP[pr][:, 8:16].unsqueeze(2).to_broadcast([128, 8, 64]), op=ALU.mult),
                           reads=[b_gvg[pr], b_stP[pr]], writes=[b_sqB])
                    dve.op(lambda e: e.tensor_tensor(out=gvn[pr], in0=sqB, in1=sgng[:], op=ALU.mult), reads=[b_sqB, b_sgng], writes=[b_gvn[pr]])

                def T3(t=t, pr=pr):
                    for h in range(8):
                        pe.op(lambda e: e.matmul(pb[2][:, h * 64:(h + 1) * 64], lhsT=wsT[:, h * 128:(h + 1) * 128], rhs=gvn[pr][:, h * 64:(h + 1) * 64],
                                                 start=True, stop=True),
                              reads=[b_wsT, b_gvn[pr]], writes=[b_pb[2]], pe_accum=(h > 0))

                def T4(t=t, pr=pr):
                    dve.op(lambda e: e.tensor_tensor(out=sqC, in0=pb[2][:, :], in1=bfull[:], op=ALU.add), reads=[b_pb[2], b_bfull], writes=[b_sqC])
                    dve.op(lambda e: e.tensor_tensor(out=sgo[pr], in0=sqC, in1=u_sb[pr], op=ALU.mult), reads=[b_sqC, b_u[pr]], writes=[b_sgo[pr]])
                    sp.dma(lambda e: e.dma_start(out=sgd[t * 128:(t + 1) * 128, :], in_=sgo[pr]), b_sgo[pr].sem, reads=[b_sgo[pr]])
                pend.extend([T1, T2, T3, T4])
        while pend:
            pend.pop(0)()
        P.barrier()

        AFa.reset(); ABa.off = ab_mark
        w_out_sb = ABa.alloc(8 * D); b_wout = P.buf("wout", True)
        qg = ABa.alloc(4 * 512); b_qg = P.buf("qg", True)
        qTm = [ABa.alloc(8 * 512) for _ in range(2)]; b_qTm = [P.buf("qTm%d_%d" % (l, i)) for i in range(2)]
        pT = [ABa.alloc(512) for _ in range(3)]; b_pT = [P.buf("pT%d_%d" % (l, i)) for i in range(3)]
        mixo = [[ABa.alloc(512) for _ in range(4)] for _ in range(2)]
        b_mixo = [[P.buf("mixo%d_%d_%d" % (l, pp, i)) for i in range(4)] for pp in range(2)]
        mixT = ABa.alloc(8 * 128); b_mixT = P.buf("mixT%d" % l)
        sgi = [ABa.alloc(512) for _ in range(2)]; b_sgi = [P.buf("sgi_%d" % i, True) for i in range(2)]
        h2 = ABa.alloc(D); b_h2 = P.buf("h2", True)
        affT = AFa.alloc(T); b_affT = P.buf("affT%d" % l)
        af_mark = AFa.off
        xt = [AFa.alloc(D) for _ in range(2)]; b_xt = [P.buf("xt_%d" % i, True) for i in range(2)]
        x1 = AFa.alloc(D); b_x1 = P.buf("x1", True)
        xn = AFa.alloc(D); b_xn = P.buf("xnc%d" % l)
        junk = AFa.alloc(D); b_junk = P.buf("junkc%d" % l)
        h2T = AFa.alloc(8 * 128); b_h2T = P.buf("h2T%d" % l)
        accS = AFa.alloc(8 * 130); b_accS = P.buf("accS%d" % l)
        t1 = AFa.alloc(128); b_t1 = P.buf("t1c%d" % l)
        osb = [AFa.alloc(128) for _ in range(4)]; b_osb = [P.buf("osb%d_%d" % (l, i)) for i in range(4)]
        af = AFa.alloc(NE); b_af = P.buf("af%d" % l)
        st = small[:, 0:32]
        b_stT = P.buf("stT%d" % l); b_stE = P.buf("stE%d" % l); b_stE2 = P.buf("stE2%d" % l)
        b_pbR = b_pbh; b_pbAf = b_pbh
        pbL = pbb[:, 0:512]; b_pbL = Buf("pbL%d" % l, excl=True)
        Sb = [(pb[0], b_pb[0]), (pb[1], b_pb[1]), (pbb[:, 512:1024], Buf("pbS2%d" % l, excl=True))]
        pool.dma(lambda e: e.dma_start(out=v3(w_out_sb, 8), in_=w_out[l].rearrange("(k p) n -> p k n", p=128)), b_wout.sem, writes=[b_wout])
        for pp in range(2):
            pool.op(lambda e: e.memset(qTm[pp], 0.0), writes=[b_qTm[pp]])

        def acc_ap(a):
            bank = 2 + a // 3; off = (a % 3) * 160
            return pb[bank][:, off:off + 130], b_pb[bank]

        qgroups = [(G * 4, 4) for G in range(8)] + ([] if last else [(NLT, 2)])

        def build_q(gi):
            t0, nq = qgroups[gi]; par = gi % 2
            ths = []

            def ld():
                sp.dma(lambda e: e.dma_start(out=v3(qg, 4)[:, 0:nq, :], in_=qd[t0 * 128:(t0 + nq) * 128, :].rearrange("(a p) n -> p a n", p=128)),
                       b_qg.sem, writes=[b_qg])
            ths.append(ld)
            for qi in range(nq):
                def f(qi=qi):
                    for j in range(4):
                        pe.op(lambda e: e.transpose(out=pbh[:, j * 128:(j + 1) * 128], in_=v3(qg, 4)[:, qi, j * 128:(j + 1) * 128], identity=identb[:]),
                              reads=[b_qg, b_identb], writes=[b_pbh], pe_accum=(j > 0))
                    for hf in range(2):
                        dst = qTm[par].rearrange("p (j f q) -> p j f q", j=4, f=2)[hf * 64:(hf + 1) * 64, :, hf, qi * 128:(qi + 1) * 128]
                        src = v3(pbh[:, 0:512], 4)[hf * 64:(hf + 1) * 64, :, :]
                        act.op(lambda e: e.activation(out=dst, in_=src, func=AF.Copy), reads=[b_pbh], writes=[b_qTm[par]], nowaw=True)
                ths.append(f)
            return ths

        def tile_thunks(gi, qi):
            t0, nq = qgroups[gi]; par = gi % 2
            t = t0 + qi; ii = t % 2
            s_ = 0 if t0 < NLT else 1
            mo = mixo[par][qi]; bmo = b_mixo[par][qi]
            ths = []

            def s0():
                sp.dma(lambda e: e.dma_start(out=sgi[ii], in_=sgd[t * 128:(t + 1) * 128, :]), b_sgi[ii].sem, writes=[b_sgi[ii]])
                sp.dma(lambda e: e.dma_start(out=xt[ii], in_=xr[t * 128:(t + 1) * 128, :]), b_xt[ii].sem, writes=[b_xt[ii]])
            ths.append(s0)

            def s1(half):
                srcb, bsrc = [(mo, bmo), (sgi[ii], b_sgi[ii])][half]
                for j in range(4):
                    pe.op(lambda e: e.transpose(out=pbh[:, j * 128:(j + 1) * 128], in_=srcb[:, j * 128:(j + 1) * 128], identity=identb[:]),
                          reads=[bsrc, b_identb], writes=[b_pbh], pe_accum=(j > 0))
                dve.op(lambda e: e.tensor_copy(out=mixT[:, half * 512:(half + 1) * 512], in_=pbh[:, 0:512]), reads=[b_pbh], writes=[b_mixT], nowaw=(half > 0))
            ths.append(lambda: s1(0))
            ths.append(lambda: s1(1))

            def s2(hv, fh):
                for f in range(fh * 4, fh * 4 + 4):
                    pe.op(lambda e: e.matmul(pbL[:, :], lhsT=mixT[:, f * 128:(f + 1) * 128],
                                             rhs=v3(w_out_sb, 8)[:, f, hv * 512:(hv + 1) * 512], start=(f == 0), stop=(f == 7)),
                          reads=[b_mixT, b_wout], writes=[b_pbL], pe_accum=(f > 0))

            def s3(hv):
                c0 = hv * 512
                dve.op(lambda e: e.tensor_tensor(out=x1[:, c0:c0 + 512], in0=pbL[:, :], in1=growv(s_, 0)[:, c0:c0 + 512], op=ALU.mult), reads=[b_pbL, b_grow], writes=[b_x1])
                dve.op(lambda e: e.tensor_tensor(out=x1[:, c0:c0 + 512], in0=x1[:, c0:c0 + 512], in1=xt[ii][:, c0:c0 + 512], op=ALU.add), reads=[b_x1, b_xt[ii]], writes=[b_x1])
                if hv == 1:
                    sp.dma(lambda e: e.dma_start(out=xr[t * 128:(t + 1) * 128, :], in_=x1), b_x1.sem, reads=[b_x1])
                    pool.op(lambda e: e.memset(st[:, 0:1], 0.0), writes=[b_stT])
                    act.op(lambda e: e.activation(out=junk, in_=x1, func=AF.Square, accum_out=st[:, 0:1]), reads=[b_x1, b_stT], writes=[b_junk, b_stT])
            for hv in range(2):
                ths.append(lambda hv=hv: s2(hv, 0))
                ths.append(lambda hv=hv: s2(hv, 1))
                ths.append(lambda hv=hv: s3(hv))

            def s4():
                rstd_op(st[:, 1:2], st[:, 0:1], 1.0 / D, [b_stT])
                dve.op(lambda e: e.tensor_scalar(out=xn, in0=x1, scalar1=st[:, 1:2], scalar2=None, op0=ALU.mult), reads=[b_x1, b_stT], writes=[b_xn])
            ths.append(s4)

            def s5(hh):
                for k4 in range(4):
                    k = hh * 4 + k4
                    pe.op(lambda e: e.transpose(out=pbL[:, k4 * 128:(k4 + 1) * 128], in_=xn[:, k * 128:(k + 1) * 128], identity=ident[:]),
                          reads=[b_xn, b_ident], writes=[b_pbL], pe_accum=(k4 > 0))

            def s6(hh):
                ia = (1 * 2 + s_) * 8 + hh * 4
                Ab = AB[:, ia:ia + 4].unsqueeze(2).to_broadcast([128, 4, 128])
                Bb = v3(modT[:], 48)[:, 24 + hh * 4:28 + hh * 4, s_].unsqueeze(2).to_broadcast([128, 4, 128])
                hv_ = v3(h2T, 8)[:, hh * 4:(hh + 1) * 4, :]
                dve.op(lambda e: e.tensor_tensor(out=hv_, in0=v3(pbL[:, :], 4), in1=Ab, op=ALU.mult), reads=[b_pbL, b_AB], writes=[b_h2T])
                dve.op(lambda e: e.tensor_tensor(out=hv_, in0=hv_, in1=Bb, op=ALU.add), reads=[b_h2T, b_modT], writes=[b_h2T])
            for hh in range(2):
                ths.append(lambda hh=hh: s5(hh))
                ths.append(lambda hh=hh: s6(hh))

            def s7a():
                for k in range(8):
                    pe.op(lambda e: e.matmul(pbr[:, 0:NE], lhsT=h2T[:, k * 128:(k + 1) * 128], rhs=wrt[:, k * NE:(k + 1) * NE], start=(k == 0), stop=(k == 7)),
                          reads=[b_h2T, b_wrt], writes=[b_pbR], pe_accum=(k > 0))
            ths.append(s7a)

            def s7(hh):
                for k4 in range(4):
                    k = hh * 4 + k4
                    pe.op(lambda e: e.transpose(out=pbL[:, k4 * 128:(k4 + 1) * 128], in_=h2T[:, k * 128:(k + 1) * 128], identity=ident[:]),
                          reads=[b_h2T, b_ident], writes=[b_pbL], pe_accum=(k4 > 0))

            def s7c(hh):
                dve.op(lambda e: e.tensor_copy(out=h2[:, hh * 512:(hh + 1) * 512], in_=pbL[:, :]), reads=[b_pbL], writes=[b_h2])
                if hh == 1:
                    sp.dma(lambda e: e.dma_start(out=h2d[t * 128:(t + 1) * 128, :], in_=h2), b_h2.sem, reads=[b_h2])
            for hh in range(2):
                ths.append(lambda hh=hh: s7(hh))
                ths.append(lambda hh=hh: s7c(hh))

            def s8():
                dve.op(lambda e: e.tensor_reduce(out=st[:, 5:6], in_=pbr[:, 0:NE], axis=AX.X, op=ALU.max), reads=[b_pbR], writes=[b_stT])
                dve.op(lambda e: e.tensor_scalar(out=st[:, 5:6], in0=st[:, 5:6], scalar1=-1.0, scalar2=None, op0=ALU.mult), reads=[b_stT], writes=[b_stT])
                pool.op(lambda e: e.memset(st[:, 6:7], 0.0), writes=[b_stT])
                act.op(lambda e: e.activation(out=af, in_=pbr[:, 0:NE], func=AF.Exp, bias=st[:, 5:6], scale=1.0, accum_out=st[:, 6:7]),
                       reads=[b_pbR, b_stT], writes=[b_af, b_stT])
            ths.append(s8)

            def s9():
                dve.op(lambda e: e.reciprocal(out=st[:, 6:7], in_=st[:, 6:7]), reads=[b_stT], writes=[b_stT])
                dve.op(lambda e: e.tensor_scalar(out=af, in0=af, scalar1=st[:, 6:7], scalar2=None, op0=ALU.mult), reads=[b_af, b_stT], writes=[b_af])
                pe.op(lambda e: e.transpose(out=pbr[0:NE, 128:256], in_=af, identity=ident[:]), reads=[b_af, b_ident], writes=[b_pbAf])
            ths.append(s9)

            def s10():
                dve.op(lambda e: e.tensor_copy(out=affT[0:NE, t * 128:(t + 1) * 128], in_=pbr[0:NE, 128:256]), reads=[b_pbAf], writes=[b_affT])
            ths.append(s10)
            return ths

        from collections import deque
        bg = deque()
        for th in build_q(0):
            th()
        a8 = v3(accS, 8)
        pti = 0
        for gi, (t0, nq) in enumerate(qgroups):
            par = gi % 2
            s = 0 if t0 < NLT else 1
            NQ = nq * 128
            kts = list(range(NT)) if s == 0 else [NLT, NLT + 1]
            if gi + 1 < len(qgroups):
                bg.extendleft(reversed(build_q(gi + 1)))
            iters = [(h, c, ki, kt) for h in range(4) for c in range(2) for ki, kt in enumerate(kts)]
            timed = []

            def emit_S(idx):
                h, c, ki, kt = iters[idx]; sbt, bsb = Sb[idx % 3]
                i8 = c * 4 + h; j = i8 // 2
                pe.op(lambda e: e.matmul(sbt[:, 0:NQ], lhsT=v3(kT, 4)[:, j, kt * 128:(kt + 1) * 128], rhs=v3(qTm[par], 8)[:, i8, 0:NQ], start=True, stop=True),
                      reads=[b_kT, b_qTm[par]], writes=[bsb])

            def E1(h):
                for bank in range(3):
                    na = 3 if bank < 2 else 2
                    src = pb[2 + bank][:, 0:na * 160].rearrange("p (a c) -> p a c", a=na)[:, :, 0:130]
                    dve.op(lambda e: e.tensor_copy(out=a8[:, bank * 3:bank * 3 + na, :], in_=src), reads=[b_pb[2 + bank]], writes=[b_accS])
                for qi in range(nq):
                    a0 = a8[:, qi, :]; a1 = a8[:, 4 + qi, :]
                    dve.op(lambda e: e.reciprocal(out=st[:, 2:3], in_=a0[:, 128:129]), reads=[b_accS], writes=[b_stE])
                    dve.op(lambda e: e.reciprocal(out=st[:, 3:4], in_=a1[:, 128:129]), reads=[b_accS], writes=[b_stE])
                    dve.op(lambda e: e.tensor_tensor(out=st[:, 3:4], in0=st[:, 3:4], in1=lamt[:, 3:4], op=ALU.mult), reads=[b_stE, b_lamt], writes=[b_stE])
                    dve.op(lambda e: e.tensor_scalar(out=t1, in0=a1[:, 0:128], scalar1=st[:, 3:4], scalar2=None, op0=ALU.mult), reads=[b_accS, b_stE], writes=[b_t1])
                    dve.op(lambda e: e.scalar_tensor_tensor(out=osb[qi], in0=a0[:, 0:128], scalar=st[:, 2:3], in1=t1, op0=ALU.mult, op1=ALU.add),
                           reads=[b_accS, b_stE, b_t1], writes=[b_osb[qi]])
                    dve.op(lambda e: e.tensor_tensor(out=t1, in0=osb[qi], in1=osb[qi], op=ALU.mult), reads=[b_osb[qi]], writes=[b_t1])
                    dve.op(lambda e: e.tensor_reduce(out=st[:, 8 + qi:9 + qi], in_=t1, axis=AX.X, op=ALU.add), reads=[b_t1], writes=[b_stE2])

            def E2(h):
                rstd_op(st[:, 8:8 + nq], st[:, 8:8 + nq], 1.0 / 128, [b_stE2])

            def E3(h):
                for qi in range(nq):
                    dve.op(lambda e: e.scalar_tensor_tensor(out=mixo[par][qi][:, h * 128:(h + 1) * 128], in0=osb[qi], scalar=st[:, 8 + qi:9 + qi], in1=gsub[:], op0=ALU.mult, op1=ALU.mult),
                           reads=[b_osb[qi], b_stE2, b_gsub], writes=[b_mixo[par][qi]])

            import os as _os
            NOLOOK = _os.environ.get("KNOLOOK") == "1"; NOBG = _os.environ.get("KNOBG") == "1"
            if not NOLOOK:
                emit_S(0); emit_S(1)
            for idx in range(len(iters)):
                if NOLOOK:
                    emit_S(idx)
                elif idx + 2 < len(iters):
                    emit_S(idx + 2)
                h, c, ki, kt = iters[idx]; sbt, bsb = Sb[idx % 3]
                pi = pti % 3; pti += 1
                act.op(lambda e: e.activation(out=pT[pi][:, 0:NQ], in_=sbt[:, 0:NQ], func=AF.Exp, scale=0.125),
                       reads=[bsb], writes=[b_pT[pi]])
                banks_seen = set()
                for qi in range(nq):
                    a_ = c * 4 + qi
                    aap, bacc = acc_ap(a_)
                    first_in_bank = (a_ // 3) not in banks_seen
                    banks_seen.add(a_ // 3)
                    pe.op(lambda e: e.matmul(aap, lhsT=pT[pi][:, qi * 128:(qi + 1) * 128], rhs=va4[:, kt, h, :],
                                             start=(ki == 0 and first_in_bank), stop=(ki == len(kts) - 1), skip_group_check=True),
                          reads=[b_pT[pi], b_vaug], writes=[bacc], pe_accum=(ki > 0))
                if c == 1 and ki == len(kts) - 1:
                    while timed:
                        timed.pop(0)[1]()
                    E1(h)
                    timed.append((idx + 6, lambda h=h: E2(h)))
                    timed.append((idx + 12, lambda h=h: E3(h)))
                while timed and timed[0][0] <= idx:
                    timed.pop(0)[1]()
                if idx % 2 == 1 and bg and not NOBG:
                    bg.popleft()()
            while timed:
                timed.pop(0)[1]()
            while NOBG and bg:
                bg.popleft()()
            for qi in range(nq):
                bg.extend(tile_thunks(gi, qi))
        while bg:
            bg.popleft()()
        P.barrier()

        AFa.off = af_mark; ABa.reset()
        NTOK = CAP if last else CAP + CCAP
        NJ = 4 if last else 5
        work = affT[:, 0:SEQ]; b_work = b_affT
        workc = affT[:, SEQ:T]
        vals = AFa.alloc(NTOK); b_vals = P.buf("vals%d" % l)
        idxu = AFa.alloc(NTOK).bitcast(U32); b_idxu = P.buf("idxu%d" % l)
        idxf = AFa.alloc(NTOK); b_idxf = P.buf("idxf%d" % l)
        gT = AFa.alloc(5 * NE); b_gT = P.buf("gT%d" % l)
        idxT = AFa.alloc(5 * NE).bitcast(I32); b_idxT = P.buf("idxT%d" % l)
        sgs = [AFa.alloc(544) for _ in range(2)]; b_sgs = [P.buf("sgs%d_%d" % (l, i)) for i in range(2)]
        ysc = AFa.alloc(5 * D); b_ysc = P.buf("ysc%d" % l)
        ring = [ABa.alloc(8192) for _ in range(3)]; b_ring = [P.buf("ring_%d" % i, True) for i in range(3)]
        xs = [ABa.alloc(5 * D) for _ in range(2)]; b_xs = [P.buf("xs_%d" % i, True) for i in range(2)]
        xsT = ABa.alloc(8 * 544); b_xsT = P.buf("xsT%d" % l)
        hidT = ABa.alloc(16 * 544); b_hidT = P.buf("hidT%d" % l)
        for r in range(CAP // 8):
            dve.op(lambda e, r=r: e.max(out=vals[0:NE, r * 8:(r + 1) * 8], in_=work[0:NE, :]), reads=[b_work], writes=[b_vals])
            dve.op(lambda e, r=r: e.max_index(out=idxu[0:NE, r * 8:(r + 1) * 8], in_max=vals[0:NE, r * 8:(r + 1) * 8], in_values=work[0:NE, :]),
                   reads=[b_work, b_vals], writes=[b_idxu])
            if r < CAP // 8 - 1:
                dve.op(lambda e, r=r: e.match_replace(out=work[0:NE, :], in_to_replace=vals[0:NE, r * 8:(r + 1) * 8], in_values=work[0:NE, :], imm_value=-1.0),
                       reads=[b_vals, b_work], writes=[b_work])
        if not last:
            for r in range(CCAP // 8):
                c0 = CAP + r * 8
                dve.op(lambda e, c0=c0: e.max(out=vals[0:NE, c0:c0 + 8], in_=workc[0:NE, :]), reads=[b_work], writes=[b_vals])
                dve.op(lambda e, c0=c0: e.max_index(out=idxu[0:NE, c0:c0 + 8], in_max=vals[0:NE, c0:c0 + 8], in_values=workc[0:NE, :]),
                       reads=[b_work, b_vals], writes=[b_idxu])
                if r < CCAP // 8 - 1:
                    dve.op(lambda e, c0=c0: e.match_replace(out=workc[0:NE, :], in_to_replace=vals[0:NE, c0:c0 + 8], in_values=workc[0:NE, :], imm_value=-1.0),
                           reads=[b_vals, b_work], writes=[b_work])
        dve.op(lambda e: e.tensor_copy(out=idxf[0:NE, :], in_=idxu[0:NE, :]), reads=[b_idxu], writes=[b_idxf])
        if not last:
            dve.op(lambda e: e.tensor_scalar(out=idxf[0:NE, CAP:NTOK], in0=idxf[0:NE, CAP:NTOK], scalar1=float(SEQ), scalar2=None, op0=ALU.add),
                   reads=[b_idxf], writes=[b_idxf])
        for (srcv, bsrc, dstv, bdst) in ((idxf, b_idxf, idxT, b_idxT), (vals, b_vals, gT, b_gT)):
            for jc in range(NJ):
                nj = 128 if jc < 4 else CCAP
                pe.op(lambda e, jc=jc, nj=nj, srcv=srcv: e.transpose(out=pb[4][0:nj, jc * NE:(jc + 1) * NE], in_=srcv[0:NE, jc * 128:jc * 128 + nj], identity=ident[0:NE, 0:NE]),
                      reads=[bsrc, b_ident], writes=[b_pb[4]], pe_accum=(jc > 0))
            dve.op(lambda e, dstv=dstv: e.tensor_copy(out=dstv[:, 0:4 * NE], in_=pb[4][:, 0:4 * NE]), reads=[b_pb[4]], writes=[bdst])
            if not last:
                dve.op(lambda e, dstv=dstv: e.tensor_copy(out=dstv[0:CCAP, 4 * NE:5 * NE], in_=pb[4][0:CCAP, 4 * NE:5 * NE]), reads=[b_pb[4]], writes=[bdst])

        def gather(e_):
            xb = xs[e_ % 2]; bx = b_xs[e_ % 2]
            NJs = [(jc, 128 if jc < 4 else CCAP) for jc in range(NJ)]
            pool.dma([lambda e, jc=jc, nj=nj: e.indirect_dma_start(
                out=xb[0:nj, jc * D:(jc + 1) * D], out_offset=None, in_=h2d[:, :],
                in_offset=bass.IndirectOffsetOnAxis(ap=idxT[0:nj, jc * NE + e_: jc * NE + e_ + 1], axis=0)) for (jc, nj) in NJs],
                bx.sem, reads=[b_idxT], writes=[bx])

        pieces = [(e_, p_) for e_ in range(NE) for p_ in range(6)]

        def load_piece(pi_):
            e_, p_ = pieces[pi_]
            rb = ring[pi_ % 3]; brb = b_ring[pi_ % 3]
            if p_ < 4:
                pool.dma([lambda e: e.dma_start(out=v3(rb[:, 0:4096], 8), in_=w_gate[l, e_].rearrange("(k p) n -> p k n", p=128)[:, :, p_ * 512:(p_ + 1) * 512]),
                          lambda e: e.dma_start(out=v3(rb[:, 4096:8192], 8), in_=w_up[l, e_].rearrange("(k p) n -> p k n", p=128)[:, :, p_ * 512:(p_ + 1) * 512])],
                         brb.sem, writes=[brb])
            else:
                hv = p_ - 4
                pool.dma([lambda e, q_=q_: e.dma_start(
                    out=v3(rb[:, q_ * 4096:(q_ + 1) * 4096], 8),
                    in_=w_down[l, e_].rearrange("(f p) n -> p f n", p=128)[:, q_ * 8:(q_ + 1) * 8, hv * 512:(hv + 1) * 512]) for q_ in range(2)],
                    brb.sem, writes=[brb])

        b_xrs = P.buf("xrs", True)
        gather(0)
        load_piece(0); load_piece(1)
        sgi_ = [0]
        for e_ in range(NE):
            xb = xs[e_ % 2]; bx = b_xs[e_ % 2]
            if e_ + 1 < NE:
                gather(e_ + 1)
            for jc in range(NJ):
                nj = 128 if jc < 4 else CCAP
                for kh in range(2):
                    for k4 in range(4):
                        k = kh * 4 + k4
                        pe.op(lambda e: e.transpose(out=pbh[:, k4 * 128:k4 * 128 + nj], in_=xb[0:nj, jc * D + k * 128: jc * D + (k + 1) * 128],
                                                    identity=identb[0:nj, 0:nj]),
                              reads=[bx, b_identb], writes=[b_pbh], pe_accum=(k4 > 0))
                    act.op(lambda e: e.activation(out=v3(xsT, 8)[:, kh * 4:(kh + 1) * 4, jc * 128:jc * 128 + nj], in_=v3(pbh[:, :], 4)[:, :, 0:nj], func=AF.Copy),
                           reads=[b_pbh], writes=[b_xsT])
            for p_ in range(6):
                pi_ = e_ * 6 + p_
                if pi_ + 2 < len(pieces):
                    load_piece(pi_ + 2)
                rb = ring[pi_ % 3]; brb = b_ring[pi_ % 3]
                if p_ < 4:
                    for fi in range(4):
                        f = p_ * 4 + fi
                        gb = fi % 2
                        for wi, (woff, pbt, bpbt) in enumerate(((0, pb[gb], b_pb[gb]), (4096, pb[2 + gb], b_pb[2 + gb]))):
                            for k in range(8):
                                pe.op(lambda e, rb=rb, woff=woff, k=k, fi=fi, pbt=pbt: e.matmul(pbt[:, 0:CAP], lhsT=v3(rb[:, woff:woff + 4096], 8)[:, k, fi * 128:(fi + 1) * 128],
                                                                                              rhs=v3(xsT, 8)[:, k, 0:CAP], start=(k == 0), stop=(k == 7)),
                                      reads=[brb, b_xsT], writes=[bpbt], pe_accum=(k > 0))
                            if not last:
                                co = gb * 64 + wi * 32
                                for k in range(8):
                                    pe.op(lambda e, rb=rb, woff=woff, k=k, fi=fi, co=co: e.matmul(pb[4][:, co:co + CCAP], lhsT=v3(rb[:, woff:woff + 4096], 8)[:, k, fi * 128:(fi + 1) * 128],
                                                                                                rhs=v3(xsT, 8)[:, k, CAP:NTOK], start=(k == 0), stop=(k == 7)),
                                          reads=[brb, b_xsT], writes=[b_pb[4]], pe_accum=(k > 0))
                        sg_ = sgs[sgi_[0] % 2]; bsg = b_sgs[sgi_[0] % 2]; sgi_[0] += 1
                        act.op(lambda e, gb=gb, sg_=sg_: e.activation(out=sg_[:, 0:CAP], in_=pb[gb][:, 0:CAP], func=AF.Silu), reads=[b_pb[gb]], writes=[bsg])
                        dve.op(lambda e, gb=gb, sg_=sg_, f=f: e.tensor_tensor(out=v3(hidT, 16)[:, f, 0:CAP], in0=sg_[:, 0:CAP], in1=pb[2 + gb][:, 0:CAP], op=ALU.mult),
                               reads=[bsg, b_pb[2 + gb]], writes=[b_hidT])
                        if not last:
                            co = gb * 64
                            act.op(lambda e, co=co, sg_=sg_: e.activation(out=sg_[:, CAP:NTOK], in_=pb[4][:, co:co + CCAP], func=AF.Silu), reads=[b_pb[4]], writes=[bsg])
                            dve.op(lambda e, co=co, sg_=sg_, f=f: e.tensor_tensor(out=v3(hidT, 16)[:, f, CAP:NTOK], in0=sg_[:, CAP:NTOK], in1=pb[4][:, co + 32:co + 32 + CCAP], op=ALU.mult),
                                   reads=[bsg, b_pb[4]], writes=[b_hidT])
                else:
                    hv = p_ - 4
                    for jc in range(NJ):
                        nj = 128 if jc < 4 else CCAP
                        yb = jc % 2
                        for f in range(16):
                            pe.op(lambda e, rb=rb, f=f, jc=jc, nj=nj, yb=yb: e.matmul(pbb[0:nj, yb * 512:(yb + 1) * 512], lhsT=v3(hidT, 16)[:, f, jc * 128:jc * 128 + nj],
                                                                                    rhs=v3(rb[:, (f // 8) * 4096:(f // 8 + 1) * 4096], 8)[:, f % 8, :], start=(f == 0), stop=(f == 15)),
                                  reads=[b_hidT, brb], writes=[b_pbb], pe_accum=(f > 0))
                        s = 0 if jc < 4 else 1
                        dve.op(lambda e, jc=jc, nj=nj, yb=yb, hv=hv, s=s, e_=e_: e.scalar_tensor_tensor(
                            out=ysc[0:nj, jc * D + hv * 512: jc * D + (hv + 1) * 512], in0=pbb[0:nj, yb * 512:(yb + 1) * 512],
                            scalar=gT[0:nj, jc * NE + e_: jc * NE + e_ + 1], in1=growv(s, 1)[0:nj, hv * 512:(hv + 1) * 512], op0=ALU.mult, op1=ALU.mult),
                            reads=[b_pbb, b_gT, b_grow], writes=[b_ysc])
            NJs = [(jc, 128 if jc < 4 else CCAP) for jc in range(NJ)]
            pool.dma([lambda e, jc=jc, nj=nj: e.indirect_dma_start(
                out=xr[:, :], out_offset=bass.IndirectOffsetOnAxis(ap=idxT[0:nj, jc * NE + e_: jc * NE + e_ + 1], axis=0),
                in_=ysc[0:nj, jc * D:(jc + 1) * D], in_offset=None, compute_op=ALU.add) for (jc, nj) in NJs],
                b_xrs.sem, reads=[b_ysc, b_idxT, b_xrs], writes=[b_xrs])
        P.barrier()

    AFa.reset(); ABa.reset()
    gf = AFa.alloc(D); b_gf = P.buf("gf", True)
    xt = [AFa.alloc(D) for _ in range(2)]; b_xt = [P.buf("xt_%d" % i, True) for i in range(2)]
    yo = [AFa.alloc(D) for _ in range(2)]; b_yo = [P.buf("yo%d" % i, True) for i in range(2)]
    junk = AFa.alloc(D); b_junk = P.buf("junkf")
    st = small[:, 32:40]; b_st = P.buf("stf")
    sp.dma(lambda e: e.dma_start(out=gf, in_=norm_f_g[0:1, :].to_broadcast([128, D])), b_gf.sem, writes=[b_gf])
    for t in range(NLT):
        i = t % 2
        sp.dma(lambda e, t=t, i=i: e.dma_start(out=xt[i], in_=xr[t * 128:(t + 1) * 128, :]), b_xt[i].sem, writes=[b_xt[i]])
        if final_norm:
            pool.op(lambda e: e.memset(st[:, 0:1], 0.0), writes=[b_st])
            act.op(lambda e, i=i: e.activation(out=junk, in_=xt[i], func=AF.Square, accum_out=st[:, 0:1]), reads=[b_xt[i], b_st], writes=[b_junk, b_st])
            rstd_op(st[:, 1:2], st[:, 0:1], 1.0 / D, [b_st])
            dve.op(lambda e, i=i: e.scalar_tensor_tensor(out=yo[i], in0=xt[i], scalar=st[:, 1:2], in1=gf, op0=ALU.mult, op1=ALU.mult),
                   reads=[b_xt[i], b_st, b_gf], writes=[b_yo[i]])
        else:
            dve.op(lambda e, i=i: e.tensor_copy(out=yo[i], in_=xt[i]), reads=[b_xt[i]], writes=[b_yo[i]])
        sp.dma(lambda e, t=t, i=i: e.dma_start(out=out_d[t * 128:(t + 1) * 128, :], in_=yo[i]), b_yo[i].sem, reads=[b_yo[i]])
    P.finish()
    return nc


def _rope_table():
    rows = SEQ // 64
    row = np.repeat(np.arange(rows), 64).astype(np.float32)
    col = np.tile(np.arange(64), rows).astype(np.float32)
    n_freq = 16
    freqs = (np.float32(10000.0) ** (-np.arange(n_freq, dtype=np.float32) / np.float32(n_freq))).astype(np.float32)
    ang_r = (row[:, None] * freqs).astype(np.float32)
    ang_c = (col[:, None] * freqs).astype(np.float32)
    ang = np.concatenate([ang_r, ang_r, ang_c, ang_c], axis=-1)
    cos = np.cos(ang).astype(np.float32); sin = np.sin(ang).astype(np.float32)
    tab = np.zeros((T, 128), np.float32)
    tab[:SEQ, 0:64] = cos
    sgn = np.ones((2, 2, 16), np.float32); sgn[:, 0, :] = -1.0
    tab[:SEQ, 64:128] = sin * sgn.reshape(64)
    tab[SEQ:, 0:64] = 1.0
    return tab


def prep_shared(inp):
    f = lambda a: np.ascontiguousarray(np.asarray(a, dtype=np.float32))
    sh = {
        "rope": _rope_table(), "ident": np.eye(128, dtype=np.float32),
        "w_ada": f(inp["w_ada"]), "b_ada": f(inp["b_ada"]),
        "b_ada_col": f(np.asarray(inp["b_ada"]).reshape(DEPTH, 48, 128).transpose(0, 2, 1)),
        "n1col": f(np.asarray(inp["norm1_g"]).reshape(DEPTH, 8, 128).transpose(0, 2, 1)),
        "n2col": f(np.asarray(inp["norm2_g"]).reshape(DEPTH, 8, 128).transpose(0, 2, 1)),
        "w_in": f(inp["w_in"]), "w_out": f(inp["w_out"]),
        "lamv": f(np.concatenate([np.asarray(inp["lambda_q1"]), np.asarray(inp["lambda_k1"]), np.asarray(inp["lambda_q2"]), np.asarray(inp["lambda_k2"])], axis=1)),
        "subln_g": f(inp["subln_g"]), "sgu_norm_g": f(inp["sgu_norm_g"]), "sgu_w": f(inp["sgu_w"]),
        "sgu_bT": f(np.asarray(inp["sgu_b"]).transpose(0, 2, 1)),
        "w_router": f(inp["w_router"]), "w_gate": f(inp["w_gate"]), "w_up": f(inp["w_up"]), "w_down": f(inp["w_down"]),
        "norm_f_g": f(np.asarray(inp["norm_f_g"]).reshape(1, D)),
    }
    return sh


def prep_core(inp, b):
    x = np.asarray(inp["x"], dtype=np.float32)[b]; cx = np.asarray(inp["ctx"], dtype=np.float32)[b]
    c = np.asarray(inp["c"], dtype=np.float32)[b]; cctx = np.asarray(inp["c_ctx"], dtype=np.float32)
    cc = np.stack([c.reshape(8, 128).T, cctx.reshape(8, 128).T], axis=-1).reshape(128, 16)
    return {"x": np.ascontiguousarray(np.concatenate([x, cx], axis=0)), "cc": np.ascontiguousarray(cc)}


def kernel(**inputs):
    sh = prep_shared(inputs)
    nc = build()
    in_maps = []
    for b in range(8):
        m = dict(sh); m.update(prep_core(inputs, b)); in_maps.append(m)
    res = run_bass_kernel_spmd(nc, in_maps, core_ids=list(range(8)))
    return np.stack([np.asarray(r["out"], dtype=np.float32) for r in res.results], axis=0)
```

```python
import math
import numpy as np
from contextlib import ExitStack
import concourse.bass as bass
import concourse.mybir as mybir
from concourse.bass_utils import run_bass_kernel_spmd

F32 = mybir.dt.float32; BF16 = mybir.dt.bfloat16; I32 = mybir.dt.int32; U32 = mybir.dt.uint32
AF = mybir.ActivationFunctionType; ALU = mybir.AluOpType; AX = mybir.AxisListType

D = 1024; SEQ = 4096; CTX = 256; T = SEQ + CTX; NT = T // 128; NLT = SEQ // 128
DEPTH = 4; NE = 16; CAP = 512; CCAP = 32; FF = 2048
EPS = 1e-6


class Sem:
    def __init__(self, h, name):
        self.h = h; self.name = name; self.val = 0


class Buf:
    def __init__(self, name, sem=None, excl=False):
        self.name = name; self.w = None; self.r = {}; self.sem = sem; self.excl = excl


class Call:
    def __init__(self, name, a, k):
        self.name = name; self.a = a; self.k = k

    def run(self, e):
        return getattr(e, self.name)(*self.a, **self.k)


class Rec:
    def __getattr__(self, name):
        return lambda *a, **k: Call(name, a, k)


REC = Rec()


class Eng:
    def __init__(self, prog, name, sem):
        self.prog = prog; self.name = name; self.sem = sem; self.q = []; self.waited = {}

    def wait_tok(self, sem, val):
        if self.name == 'pe' and sem is self.sem:
            return
        if self.waited.get(sem, 0) >= val:
            return
        self.waited[sem] = val
        h = sem.h
        self.q.append(lambda e, h=h, val=val: e.wait_ge(h, val))

    def deps(self, reads, writes, pe_accum=False, nowaw=False):
        for b in reads:
            if b.w is not None:
                self.wait_tok(*b.w)
            if b.excl:
                for s, v in b.r.items():
                    if s is not self.sem:
                        self.wait_tok(s, v)
        for b in writes:
            if pe_accum:
                continue
            if b.w is not None and not (nowaw and b.w[0] is self.sem):
                self.wait_tok(*b.w)
            for s, v in b.r.items():
                self.wait_tok(s, v)

    def mark(self, tok, reads, writes):
        s, v = tok
        for b in reads:
            if b.r.get(s, 0) < v:
                b.r[s] = v
        for b in writes:
            b.w = tok; b.r = {}

    def op(self, fn, reads=(), writes=(), pe_accum=False, nowaw=False):
        self.deps(reads, writes, pe_accum, nowaw)
        self.sem.val += 1
        tok = (self.sem, self.sem.val)
        h = self.sem.h
        call = fn(REC)
        self.q.append(lambda e, call=call, h=h: call.run(e).then_inc(h, 1))
        self.mark(tok, reads, writes)
        return tok

    def dma(self, fn, sem, reads=(), writes=()):
        fns = fn if isinstance(fn, (list, tuple)) else [fn]
        self.deps(reads, writes)
        h = sem.h
        for f in fns:
            sem.val += 16
            call = f(REC)
            self.q.append(lambda e, call=call, h=h: call.run(e).then_inc(h, 16))
        tok = (sem, sem.val)
        self.mark(tok, reads, writes)
        return tok


class Prog:
    def __init__(self, nc):
        self.nc = nc; self.es = ExitStack(); self.sems = []; self.semcache = {}
        self.sp = Eng(self, 'sp', self.new_sem('e_sp'))
        self.act = Eng(self, 'act', self.new_sem('e_act'))
        self.pool = Eng(self, 'pool', self.new_sem('e_pool'))
        self.dve = Eng(self, 'dve', self.new_sem('e_dve'))
        self.pe = Eng(self, 'pe', self.new_sem('e_pe'))
        self.engs = [self.sp, self.act, self.pool, self.dve, self.pe]

    def new_sem(self, name):
        s = Sem(self.es.enter_context(self.nc.semaphore(name)), name)
        self.sems.append(s)
        return s

    def sbuf(self, name, shape, dt):
        return self.es.enter_context(self.nc.sbuf_tensor(name, shape, dt))

    def psum(self, name, shape, dt):
        return self.es.enter_context(self.nc.psum_tensor(name, shape, dt))

    def buf(self, name, dma=False):
        if not dma:
            return Buf(name, None)
        if name not in self.semcache:
            self.semcache[name] = self.new_sem('b_' + name)
        return Buf(name, self.semcache[name])

    def barrier(self):
        for e in self.engs:
            for s in self.sems:
                if s.val > 0:
                    e.wait_tok(s, s.val)

    def finish(self):
        self.barrier()
        with self.nc.Block() as block:
            @block.sync
            def _(e):
                for f in self.sp.q: f(e)
            @block.scalar
            def _(e):
                for f in self.act.q: f(e)
            @block.gpsimd
            def _(e):
                for f in self.pool.q: f(e)
            @block.vector
            def _(e):
                for f in self.dve.q: f(e)
            @block.tensor
            def _(e):
                for f in self.pe.q: f(e)
        self.es.close()


class Arena:
    def __init__(self, P, name, ncols, dt):
        self.t = P.sbuf(name, [128, ncols], dt); self.off = 0; self.n = ncols; self.name = name

    def alloc(self, cols):
        a = self.off; self.off += cols
        assert self.off <= self.n, (self.name, self.off, self.n)
        return self.t[:, a:a + cols]

    def reset(self):
        self.off = 0


def v3(ap, a):
    return ap.rearrange("p (a b) -> p a b", a=a)


def build(n_layers=DEPTH, final_norm=True):
    nc = bass.Bass("TRN2", target_bir_lowering=False)
    dram = lambda name, shape, dt, kind="ExternalInput": nc.dram_tensor(name, shape, dt, kind=kind).ap()
    x_in = dram("x", [T, D], F32)
    cc_d = dram("cc", [128, 16], F32)
    rope_d = dram("rope", [T, 128], F32)
    ident_d = dram("ident", [128, 128], F32)
    w_ada = dram("w_ada", [DEPTH, D, 6 * D], F32)
    b_ada_col = dram("b_ada_col", [DEPTH, 128, 48], F32)
    b_ada = dram("b_ada", [DEPTH, 6 * D], F32)
    n1col = dram("n1col", [DEPTH, 128, 8], F32)
    n2col = dram("n2col", [DEPTH, 128, 8], F32)
    w_in = dram("w_in", [DEPTH, D, 2560], F32)
    w_out = dram("w_out", [DEPTH, D, D], F32)
    lamv = dram("lamv", [DEPTH, 256], F32)
    subln_g = dram("subln_g", [DEPTH, 128], F32)
    sgu_norm_g = dram("sgu_norm_g", [DEPTH, 512], F32)
    sgu_w = dram("sgu_w", [DEPTH, 8, 128, 128], F32)
    sgu_bT = dram("sgu_bT", [DEPTH, 128, 8], F32)
    w_router = dram("w_router", [DEPTH, D, NE], F32)
    w_gate = dram("w_gate", [DEPTH, NE, D, FF], F32)
    w_up = dram("w_up", [DEPTH, NE, D, FF], F32)
    w_down = dram("w_down", [DEPTH, NE, FF, D], F32)
    norm_f_g = dram("norm_f_g", [1, D], F32)
    out_d = dram("out", [SEQ, D], F32, kind="ExternalOutput")
    xr = dram("xr", [T, D], F32, kind="Internal")
    qd = dram("qd", [T, 512], BF16, kind="Internal")
    sgd = dram("sgd", [T, 512], BF16, kind="Internal")
    h2d = dram("h2d", [T, D], BF16, kind="Internal")

    P = Prog(nc)
    sp, act, pool, dve, pe = P.sp, P.act, P.pool, P.dve, P.pe

    ident = P.sbuf("ident_sb", [128, 128], F32); b_ident = P.buf("ident", True)
    identb = P.sbuf("identb_sb", [128, 128], BF16); b_identb = P.buf("identb")
    grow = P.sbuf("grow", [128, 4 * D], F32); b_grow = P.buf("grow")
    siluc = P.sbuf("siluc", [128, 16], F32); b_siluc = P.buf("siluc", True)
    modT = P.sbuf("modT", [128, 96], F32); b_modT = P.buf("modT")
    bcol = P.sbuf("bcol", [128, 48], F32); b_bcol = P.buf("bcol", True)
    ncol = P.sbuf("ncol", [128, 16], F32); b_ncol = P.buf("ncol", True)
    AB = P.sbuf("ABcols", [128, 32], F32); b_AB = P.buf("ABc")
    lamt = P.sbuf("lamt", [128, 8], F32); b_lamt = P.buf("lamt")
    lv = P.sbuf("lv", [128, 256], F32); b_lv = P.buf("lv", True)
    lprod = P.sbuf("lprod", [128, 128], F32); b_lprod = P.buf("lprod")
    gsub = P.sbuf("gsub", [128, 128], F32); b_gsub = P.buf("gsub", True)
    sgng = P.sbuf("sgng", [128, 512], F32); b_sgng = P.buf("sgng", True)
    sbT = P.sbuf("sbT", [128, 8], F32); b_sbT = P.buf("sbT", True)
    bfull = P.sbuf("bfull", [128, 512], F32); b_bfull = P.buf("bfull")
    wsT = P.sbuf("wsT", [128, 8 * 128], BF16); b_wsT = P.buf("wsT")
    wrt = P.sbuf("wrt", [128, 8 * NE], F32); b_wrt = P.buf("wrt", True)
    small = P.sbuf("small", [128, 64], F32)
    epsc = P.sbuf("epsc", [128, 1], F32); b_epsc = P.buf("epsc")
    dve.op(lambda e: e.memset(epsc[:], EPS), writes=[b_epsc])

    def rstd_op(dst, src, scale, bufs):
        act.op(lambda e: e.activation(out=dst, in_=src, func=AF.Ln, scale=float(scale), bias=epsc[:, 0:1]), reads=bufs + [b_epsc], writes=bufs)
        act.op(lambda e: e.activation(out=dst, in_=dst, func=AF.Exp, scale=-0.5), reads=bufs, writes=bufs)

    AFa = Arena(P, "arenaF", 12928, F32)
    ABa = Arena(P, "arenaB", 62464, BF16)

    pb = [P.psum("pb%d" % i, [128, 512], F32) for i in range(5)]
    pbhr = P.psum("pbhr", [128, 512], F32)
    pbh = pbhr[:, 0:256].bitcast(BF16)
    pbr = pbhr[:, 256:512]
    pbb = P.psum("pbb", [128, 1024], F32)
    b_pb = [Buf("pb%d" % i, excl=True) for i in range(5)]
    b_pbh = Buf("pbh", excl=True); b_pbb = Buf("pbb", excl=True)

    sp.dma(lambda e: e.dma_start(out=ident[:], in_=ident_d), b_ident.sem, writes=[b_ident])
    dve.op(lambda e: e.tensor_copy(out=identb[:], in_=ident[:]), reads=[b_ident], writes=[b_identb])
    sp.dma(lambda e: e.dma_start(out=siluc[:], in_=cc_d), b_siluc.sem, writes=[b_siluc])
    act.op(lambda e: e.activation(out=siluc[:], in_=siluc[:], func=AF.Silu), reads=[b_siluc], writes=[b_siluc])
    b_xr = P.buf("xr", True)
    sp.dma([lambda e, i=i: e.dma_start(out=xr[i * (T // 4):(i + 1) * (T // 4), :], in_=x_in[i * (T // 4):(i + 1) * (T // 4), :]) for i in range(4)],
           b_xr.sem, writes=[b_xr])
    P.barrier()

    for l in range(n_layers):
        last = (l == DEPTH - 1)
        lam_init = 0.8 - 0.6 * math.exp(-0.3 * l)
        NTq = NLT if last else NT
        AFa.reset(); ABa.reset()
        wa = [AFa.alloc(8 * 512) for _ in range(2)]; b_wa = [P.buf("wa_%d" % i, True) for i in range(2)]
        brow = AFa.alloc(512); b_brow = P.buf("brow", True)
        swl = AFa.alloc(8 * 128); b_swl = P.buf("swl", True)
        srep = AFa.alloc(16 * 128); b_srep = P.buf("srep%d" % l)
        rowbuf = ABa.alloc(2 * 6 * D).bitcast(F32); b_rowbuf = P.buf("rowbuf%d" % l)
        for ks in range(16):
            dve.op(lambda e, ks=ks: e.tensor_copy(out=srep[:, ks * 128:(ks + 1) * 128], in_=siluc[:, ks:ks + 1].to_broadcast([128, 128])),
                   reads=[b_siluc], writes=[b_srep])
        sp.dma(lambda e: e.dma_start(out=bcol[:], in_=b_ada_col[l]), b_bcol.sem, writes=[b_bcol])
        sp.dma([lambda e: e.dma_start(out=ncol[:, 0:8], in_=n1col[l]), lambda e: e.dma_start(out=ncol[:, 8:16], in_=n2col[l])], b_ncol.sem, writes=[b_ncol])
        sp.dma(lambda e: e.dma_start(out=lv[:], in_=lamv[l:l + 1, :].to_broadcast([128, 256])), b_lv.sem, writes=[b_lv])
        sp.dma(lambda e: e.dma_start(out=gsub[:], in_=subln_g[l:l + 1, :].to_broadcast([128, 128])), b_gsub.sem, writes=[b_gsub])
        sp.dma(lambda e: e.dma_start(out=sgng[:], in_=sgu_norm_g[l:l + 1, :].to_broadcast([128, 512])), b_sgng.sem, writes=[b_sgng])
        sp.dma(lambda e: e.dma_start(out=sbT[:], in_=sgu_bT[l]), b_sbT.sem, writes=[b_sbT])
        sp.dma(lambda e: e.dma_start(out=wrt[:].rearrange("p (k n) -> p k n", k=8), in_=w_router[l].rearrange("(k p) n -> p k n", p=128)),
               b_wrt.sem, writes=[b_wrt])
        dve.op(lambda e: e.tensor_tensor(out=v3(lprod[:], 2), in0=v3(lv[:], 2)[:, :, 0:64], in1=v3(lv[:], 2)[:, :, 64:128], op=ALU.mult),
               reads=[b_lv], writes=[b_lprod])
        dve.op(lambda e: e.tensor_reduce(out=lamt[:, 0:2], in_=v3(lprod[:], 2), axis=AX.X, op=ALU.add), reads=[b_lprod], writes=[b_lamt])
        act.op(lambda e: e.activation(out=lamt[:, 0:2], in_=lamt[:, 0:2], func=AF.Exp), reads=[b_lamt], writes=[b_lamt])
        dve.op(lambda e: e.tensor_tensor(out=lamt[:, 2:3], in0=lamt[:, 0:1], in1=lamt[:, 1:2], op=ALU.subtract), reads=[b_lamt], writes=[b_lamt])
        dve.op(lambda e: e.tensor_scalar(out=lamt[:, 2:3], in0=lamt[:, 2:3], scalar1=float(lam_init), scalar2=None, op0=ALU.add), reads=[b_lamt], writes=[b_lamt])
        dve.op(lambda e: e.tensor_scalar(out=lamt[:, 3:4], in0=lamt[:, 2:3], scalar1=-1.0, scalar2=None, op0=ALU.mult), reads=[b_lamt], writes=[b_lamt])
        dve.op(lambda e: e.tensor_scalar(out=gsub[:], in0=gsub[:], scalar1=float(1.0 - lam_init), scalar2=None, op0=ALU.mult), reads=[b_gsub], writes=[b_gsub])
        dve.op(lambda e: e.tensor_copy(out=v3(bfull[:], 8), in_=sbT[:].unsqueeze(2).to_broadcast([128, 8, 64])), reads=[b_sbT], writes=[b_bfull])
        for hh in range(2):
            sp.dma(lambda e, hh=hh: e.dma_start(out=v3(swl, 8)[:, 0:4, :], in_=sgu_w[l, hh * 4:(hh + 1) * 4].rearrange("h p q -> p h q")),
                   b_swl.sem, writes=[b_swl])
            for h4 in range(4):
                pe.op(lambda e, h4=h4: e.transpose(out=pbb[:, h4 * 128:(h4 + 1) * 128], in_=v3(swl, 8)[:, h4, :], identity=ident[:]),
                      reads=[b_swl, b_ident], writes=[b_pbb], pe_accum=(h4 > 0))
            act.op(lambda e, hh=hh: e.activation(out=wsT[:, hh * 512:(hh + 1) * 512], in_=pbb[:, 0:512], func=AF.Copy), reads=[b_pbb], writes=[b_wsT])
        for hg in range(12):
            g = hg // 2; half = hg % 2
            wb_ = wa[hg % 2]; bw = b_wa[hg % 2]
            sp.dma(lambda e, wb_=wb_, hg=hg: e.dma_start(out=v3(wb_, 8), in_=w_ada[l].rearrange("(k p) n -> p k n", p=128)[:, :, hg * 512:(hg + 1) * 512]),
                   bw.sem, writes=[bw])
            rb_ = 2 if hg % 2 == 0 else 4
            for k in range(8):
                pe.op(lambda e: e.matmul(pb[rb_][0:2, :], lhsT=siluc[:, 2 * k:2 * k + 2], rhs=v3(wb_, 8)[:, k, :], start=(k == 0), stop=(k == 7)),
                      reads=[bw, b_siluc], writes=[b_pb[rb_]], pe_accum=(k > 0))
            act.op(lambda e: e.activation(out=rowbuf[0:2, hg * 512:(hg + 1) * 512], in_=pb[rb_][0:2, :], func=AF.Copy), reads=[b_pb[rb_]], writes=[b_rowbuf], nowaw=True)
            if g in (2, 5):
                which = 0 if g == 2 else 1
                sp.dma(lambda e, g=g, half=half: e.dma_start(out=brow, in_=b_ada[l:l + 1, g * 1024 + half * 512: g * 1024 + (half + 1) * 512].to_broadcast([128, 512])),
                       b_brow.sem, writes=[b_brow])
                for s in range(2):
                    for k in range(8):
                        pe.op(lambda e, wb_=wb_, s=s, k=k: e.matmul(pb[s][:, :], lhsT=srep[:, (2 * k + s) * 128:(2 * k + s + 1) * 128], rhs=v3(wb_, 8)[:, k, :],
                                                                    start=(k == 0), stop=(k == 7)),
                              reads=[bw, b_srep], writes=[b_pb[s]], pe_accum=(k > 0))
                    c0 = (s * 2 + which) * D + half * 512
                    dve.op(lambda e, s=s, c0=c0: e.tensor_tensor(out=grow[:, c0:c0 + 512], in0=pb[s][:, :], in1=brow, op=ALU.add),
                           reads=[b_pb[s], b_brow], writes=[b_grow])
        for n in range(48):
            pe.op(lambda e: e.transpose(out=pb[3][:, 2 * n:2 * n + 2], in_=rowbuf[0:2, n * 128:(n + 1) * 128], identity=ident[0:2, 0:2]),
                  reads=[b_rowbuf, b_ident], writes=[b_pb[3]], pe_accum=(n > 0))
        dve.op(lambda e: e.tensor_tensor(out=v3(modT[:], 48), in0=v3(pb[3][:, 0:96], 48), in1=bcol[:].unsqueeze(2).to_broadcast([128, 48, 2]), op=ALU.add),
               reads=[b_pb[3], b_bcol], writes=[b_modT])
        mT = v3(modT[:], 48)
        for w_ in range(2):
            for s in range(2):
                sc = mT[:, (1 + 3 * w_) * 8:(2 + 3 * w_) * 8, s]
                o = AB[:, (w_ * 2 + s) * 8:(w_ * 2 + s + 1) * 8]
                dve.op(lambda e, sc=sc, o=o, w_=w_: e.scalar_tensor_tensor(out=o, in0=sc, scalar=1.0, in1=ncol[:, w_ * 8:(w_ + 1) * 8], op0=ALU.add, op1=ALU.mult),
                       reads=[b_modT, b_ncol], writes=[b_AB])

        def Acol(w_, s, k): return AB[:, (w_ * 2 + s) * 8 + k:(w_ * 2 + s) * 8 + k + 1]
        def Bcol(w_, s, k): return modT[:, ((3 * w_) * 8 + k) * 2 + s:((3 * w_) * 8 + k) * 2 + s + 1]
        def growv(s, which): return grow[:, (s * 2 + which) * D:(s * 2 + which + 1) * D]
        P.barrier()

        AFa.reset(); ABa.reset()
        kT = ABa.alloc(4 * T); b_kT = P.buf("kT%d" % l)
        vaug = ABa.alloc(NT * 4 * 130); b_vaug = P.buf("vaug%d" % l)
        ab_mark = ABa.off
        w_in_sb = ABa.alloc(8 * 2560); b_win = P.buf("win", True)
        hT = [ABa.alloc(8 * 128) for _ in range(2)]; b_hT = [P.buf("hT%d_%d" % (l, i)) for i in range(2)]
        qr = ABa.alloc(512); b_qr = P.buf("qr", True)
        kr = ABa.alloc(512); b_kr = P.buf("kr%d" % l)
        gvn = [ABa.alloc(512) for _ in range(2)]; b_gvn = [P.buf("gvn%d_%d" % (l, i)) for i in range(2)]
        sgo = [ABa.alloc(512) for _ in range(2)]; b_sgo = [P.buf("sgo_%d" % i, True) for i in range(2)]
        xt = [AFa.alloc(D) for _ in range(2)]; b_xt = [P.buf("xt_%d" % i, True) for i in range(2)]
        rp = [AFa.alloc(128) for _ in range(3)]; b_rp = [P.buf("rp_%d" % i, True) for i in range(3)]
        xn = AFa.alloc(D); b_xn = P.buf("xn%d" % l)
        junk = AFa.alloc(D); b_junk = P.buf("junk%d" % l)
        t1 = AFa.alloc(512); b_t1 = P.buf("t1%d" % l)
        t2 = AFa.alloc(512); b_t2 = P.buf("t2%d" % l)
        u_sb = [AFa.alloc(512) for _ in range(2)]; b_u = [P.buf("u%d_%d" % (l, i)) for i in range(2)]
        gvg = [AFa.alloc(512) for _ in range(2)]; b_gvg = [P.buf("gvg%d_%d" % (l, i)) for i in range(2)]
        sqA = AFa.alloc(512); b_sqA = P.buf("sqA%d" % l)
        sqB = AFa.alloc(512); b_sqB = P.buf("sqB%d" % l)
        sqC = AFa.alloc(512); b_sqC = P.buf("sqC%d" % l)
        st = small[:, 0:8]; b_stN = P.buf("stN%d" % l)
        stP = [small[:, 8 + 16 * i:24 + 16 * i] for i in range(2)]; b_stP = [P.buf("stP%d_%d" % (l, i)) for i in range(2)]
        pool.dma([lambda e, i=i: e.dma_start(out=v3(w_in_sb, 8)[:, 2 * i:2 * i + 2, :], in_=w_in[l].rearrange("(k p) n -> p k n", p=128)[:, 2 * i:2 * i + 2, :]) for i in range(4)],
                 b_win.sem, writes=[b_win])
        va4 = vaug.rearrange("p (t h c) -> p t h c", t=NT, h=4)
        pool.op(lambda e: e.memset(vaug, 1.0), writes=[b_vaug])

        def load_tile(t):
            i = t % 2; i3 = t % 3
            sp.dma(lambda e: e.dma_start(out=xt[i], in_=xr[t * 128:(t + 1) * 128, :]), b_xt[i].sem, writes=[b_xt[i]])
            sp.dma(lambda e: e.dma_start(out=rp[i3], in_=rope_d[t * 128:(t + 1) * 128, :]), b_rp[i3].sem, writes=[b_rp[i3]])

        def N1(t):
            i = t % 2
            pool.op(lambda e: e.memset(st[:, 0:1], 0.0), writes=[b_stN])
            act.op(lambda e: e.activation(out=junk, in_=xt[i], func=AF.Square, accum_out=st[:, 0:1]), reads=[b_xt[i], b_stN], writes=[b_junk, b_stN])
            rstd_op(st[:, 1:2], st[:, 0:1], 1.0 / D, [b_stN])

        def N2(t):
            i = t % 2
            dve.op(lambda e: e.tensor_scalar(out=xn, in0=xt[i], scalar1=st[:, 1:2], scalar2=None, op0=ALU.mult), reads=[b_xt[i], b_stN], writes=[b_xn])

        def N3(t):
            for k in range(8):
                pe.op(lambda e: e.transpose(out=pbb[:, k * 128:(k + 1) * 128], in_=xn[:, k * 128:(k + 1) * 128], identity=ident[:]),
                      reads=[b_xn, b_ident], writes=[b_pbb], pe_accum=(k > 0))

        def N4(t):
            s_ = 0 if t < NLT else 1
            dst = hT[t % 2]
            for k in range(8):
                act.op(lambda e: e.activation(out=dst[:, k * 128:(k + 1) * 128], in_=pbb[:, k * 128:(k + 1) * 128], func=AF.Identity,
                                              scale=Acol(0, s_, k), bias=Bcol(0, s_, k)),
                       reads=[b_pbb, b_AB, b_modT], writes=[b_hT[t % 2]], nowaw=(k > 0))

        def rope(src_ps, b_src, rp_, b_rp_, dst, b_dst):
            s5 = src_ps.rearrange("p (g a r f) -> p g a r f", g=8, a=2, r=2)
            t5 = t2.rearrange("p (g a r f) -> p g a r f", g=8, a=2, r=2)
            sn = rp_[:, 64:128].rearrange("p (a r f) -> p a r f", a=2, r=2)
            dve.op(lambda e: e.tensor_tensor(out=v3(t1, 8), in0=v3(src_ps, 8), in1=rp_[:, 0:64].unsqueeze(1).to_broadcast([128, 8, 64]), op=ALU.mult),
                   reads=[b_src, b_rp_], writes=[b_t1])
            for a in range(2):
                for r in range(2):
                    dve.op(lambda e: e.tensor_tensor(out=t5[:, :, a, r, :], in0=s5[:, :, a, 1 - r, :],
                                                     in1=sn[:, a, r, :].unsqueeze(1).to_broadcast([128, 8, 16]), op=ALU.mult),
                           reads=[b_src, b_rp_], writes=[b_t2], nowaw=(a + r > 0))
            dve.op(lambda e: e.tensor_tensor(out=dst, in0=t1, in1=t2, op=ALU.add), reads=[b_t1, b_t2], writes=[b_dst])

        prot = [0, 1, 3, 4]; pcnt = [0]

        def proj(t, g):
            bi = prot[pcnt[0] % 4]; pcnt[0] += 1
            hsrc = hT[t % 2]
            for k in range(8):
                pe.op(lambda e: e.matmul(pb[bi][:, :], lhsT=hsrc[:, k * 128:(k + 1) * 128], rhs=v3(w_in_sb, 8)[:, k, g * 512:(g + 1) * 512],
                                         start=(k == 0), stop=(k == 7)),
                      reads=[b_hT[t % 2], b_win], writes=[b_pb[bi]], pe_accum=(k > 0))
            return pb[bi], b_pb[bi]

        load_tile(0); load_tile(1)
        N1(0); N2(0); N3(0); N4(0)
        pend = []

        def pop_pend():
            if pend:
                pend.pop(0)()
        for t in range(NT):
            nxt = t + 1 < NT
            i3 = t % 3
            need_q = t < NTq
            if t + 2 < NT:
                load_tile(t + 2)
            if nxt: N1(t + 1)
            pop_pend()
            if need_q:
                pbk, bpbk = proj(t, 0)
                rope(pbk[:, :], bpbk, rp[i3], b_rp[i3], qr, b_qr)
                sp.dma(lambda e: e.dma_start(out=qd[t * 128:(t + 1) * 128, :], in_=qr), b_qr.sem, reads=[b_qr])
            pop_pend()
            if nxt: N2(t + 1)
            pbk, bpbk = proj(t, 1)
            pop_pend()
            rope(pbk[:, :], bpbk, rp[i3], b_rp[i3], kr, b_kr)
            pop_pend()
            for j in range(4):
                pe.op(lambda e: e.transpose(out=pbh[:, j * 128:(j + 1) * 128], in_=kr[:, j * 128:(j + 1) * 128], identity=identb[:]),
                      reads=[b_kr, b_identb], writes=[b_pbh], pe_accum=(j > 0))
            act.op(lambda e: e.activation(out=v3(kT, 4)[:, :, t * 128:(t + 1) * 128], in_=v3(pbh[:, 0:512], 4), func=AF.Copy),
                   reads=[b_pbh], writes=[b_kT])
            if nxt: N3(t + 1)
            pbk, bpbk = proj(t, 2)
            act.op(lambda e: e.activation(out=va4[:, t, :, 0:128], in_=v3(pbk[:, :], 4), func=AF.Copy), reads=[bpbk], writes=[b_vaug])
            if nxt: N4(t + 1)
            if need_q:
                pr = t % 2
                pbk, bpbk = proj(t, 3)
                act.op(lambda e: e.activation(out=u_sb[pr], in_=pbk[:, :], func=AF.Gelu), reads=[bpbk], writes=[b_u[pr]])
                pbk, bpbk = proj(t, 4)
                act.op(lambda e: e.activation(out=gvg[pr], in_=pbk[:, :], func=AF.Gelu), reads=[bpbk], writes=[b_gvg[pr]])
                dve.op(lambda e: e.tensor_tensor(out=sqA, in0=gvg[pr], in1=gvg[pr], op=ALU.mult), reads=[b_gvg[pr]], writes=[b_sqA])
                dve.op(lambda e: e.tensor_reduce(out=stP[pr][:, 0:8], in_=v3(sqA, 8), axis=AX.X, op=ALU.add), reads=[b_sqA], writes=[b_stP[pr]])

                def T1(t=t, pr=pr):
                    rstd_op(stP[pr][:, 8:16], stP[pr][:, 0:8], 1.0 / 64, [b_stP[pr]])

                def T2(t=t, pr=pr):
                    dve.op(lambda e: e.tensor_tensor(out=v3(sqB, 8), in0=v3(gvg[pr], 8), in1=stP[pr][:, 8:16].unsqueeze(2).to_broadcast([128, 8, 64]), op=ALU.mult),
                           reads=[b_gvg[pr], b_stP[pr]], writes=[b_sqB])
                    dve.op(lambda e: e.tensor_tensor(out=gvn[pr], in0=sqB, in1=sgng[:], op=ALU.mult), reads=[b_sqB, b_sgng], writes=[b_gvn[pr]])

                def T3(t=t, pr=pr):
                    for h in range(8):
                        pe.op(lambda e: e.matmul(pb[2][:, h * 64:(h + 1) * 64], lhsT=wsT[:, h * 128:(h + 1) * 128], rhs=gvn[pr][:, h * 64:(h + 1) * 64],
                                                 start=True, stop=True),
                              reads=[b_wsT, b_gvn[pr]], writes=[b_pb[2]], pe_accum=(h > 0))

                def T4(t=t, pr=pr):
                    dve.op(lambda e: e.tensor_tensor(out=sqC, in0=pb[2][:, :], in1=bfull[:], op=ALU.add), reads=[b_pb[2], b_bfull], writes=[b_sqC])
                    dve.op(lambda e: e.tensor_tensor(out=sgo[pr], in0=sqC, in1=u_sb[pr], op=ALU.mult), reads=[b_sqC, b_u[pr]], writes=[b_sgo[pr]])
                    sp.dma(lambda e: e.dma_start(out=sgd[t * 128:(t + 1) * 128, :], in_=sgo[pr]), b_sgo[pr].sem, reads=[b_sgo[pr]])
                pend.extend([T1, T2, T3, T4])
        while pend:
            pend.pop(0)()
        P.barrier()

        AFa.reset(); ABa.off = ab_mark
        w_out_sb = ABa.alloc(8 * D); b_wout = P.buf("wout", True)
        qg = ABa.alloc(4 * 512); b_qg = P.buf("qg", True)
        qTm = [ABa.alloc(8 * 512) for _ in range(2)]; b_qTm = [P.buf("qTm%d_%d" % (l, i)) for i in range(2)]
        pT = [ABa.alloc(512) for _ in range(3)]; b_pT = [P.buf("pT%d_%d" % (l, i)) for i in range(3)]
        mixo = [[ABa.alloc(512) for _ in range(4)] for _ in range(2)]
        b_mixo = [[P.buf("mixo%d_%d_%d" % (l, pp, i)) for i in range(4)] for pp in range(2)]
        mixT = ABa.alloc(8 * 128); b_mixT = P.buf("mixT%d" % l)
        sgi = [ABa.alloc(512) for _ in range(2)]; b_sgi = [P.buf("sgi_%d" % i, True) for i in range(2)]
        h2 = ABa.alloc(D); b_h2 = P.buf("h2", True)
        affT = AFa.alloc(T); b_affT = P.buf("affT%d" % l)
        af_mark = AFa.off
        xt = [AFa.alloc(D) for _ in range(2)]; b_xt = [P.buf("xt_%d" % i, True) for i in range(2)]
        x1 = AFa.alloc(D); b_x1 = P.buf("x1", True)
        xn = AFa.alloc(D); b_xn = P.buf("xnc%d" % l)
        junk = AFa.alloc(D); b_junk = P.buf("junkc%d" % l)
        h2T = AFa.alloc(8 * 128); b_h2T = P.buf("h2T%d" % l)
        accS = AFa.alloc(8 * 130); b_accS = P.buf("accS%d" % l)
        t1 = AFa.alloc(128); b_t1 = P.buf("t1c%d" % l)
        osb = [AFa.alloc(128) for _ in range(4)]; b_osb = [P.buf("osb%d_%d" % (l, i)) for i in range(4)]
        af = AFa.alloc(NE); b_af = P.buf("af%d" % l)
        st = small[:, 0:32]
        b_stT = P.buf("stT%d" % l); b_stE = P.buf("stE%d" % l); b_stE2 = P.buf("stE2%d" % l)
        b_pbR = b_pbh; b_pbAf = b_pbh
        pbL = pbb[:, 0:512]; b_pbL = Buf("pbL%d" % l, excl=True)
        Sb = [(pb[0], b_pb[0]), (pb[1], b_pb[1]), (pbb[:, 512:1024], Buf("pbS2%d" % l, excl=True))]
        pool.dma(lambda e: e.dma_start(out=v3(w_out_sb, 8), in_=w_out[l].rearrange("(k p) n -> p k n", p=128)), b_wout.sem, writes=[b_wout])
        for pp in range(2):
            pool.op(lambda e: e.memset(qTm[pp], 0.0), writes=[b_qTm[pp]])

        def acc_ap(a):
            bank = 2 + a // 3; off = (a % 3) * 160
            return pb[bank][:, off:off + 130], b_pb[bank]

        qgroups = [(G * 4, 4) for G in range(8)] + ([] if last else [(NLT, 2)])

        def build_q(gi):
            t0, nq = qgroups[gi]; par = gi % 2
            ths = []

            def ld():
                sp.dma(lambda e: e.dma_start(out=v3(qg, 4)[:, 0:nq, :], in_=qd[t0 * 128:(t0 + nq) * 128, :].rearrange("(a p) n -> p a n", p=128)),
                       b_qg.sem, writes=[b_qg])
            ths.append(ld)
            for qi in range(nq):
                def f(qi=qi):
                    for j in range(4):
                        pe.op(lambda e: e.transpose(out=pbh[:, j * 128:(j + 1) * 128], in_=v3(qg, 4)[:, qi, j * 128:(j + 1) * 128], identity=identb[:]),
                              reads=[b_qg, b_identb], writes=[b_pbh], pe_accum=(j > 0))
                    for hf in range(2):
                        dst = qTm[par].rearrange("p (j f q) -> p j f q", j=4, f=2)[hf * 64:(hf + 1) * 64, :, hf, qi * 128:(qi + 1) * 128]
                        src = v3(pbh[:, 0:512], 4)[hf * 64:(hf + 1) * 64, :, :]
                        act.op(lambda e: e.activation(out=dst, in_=src, func=AF.Copy), reads=[b_pbh], writes=[b_qTm[par]], nowaw=True)
                ths.append(f)
            return ths

        def tile_thunks(gi, qi):
            t0, nq = qgroups[gi]; par = gi % 2
            t = t0 + qi; ii = t % 2
            s_ = 0 if t0 < NLT else 1
            mo = mixo[par][qi]; bmo = b_mixo[par][qi]
            ths = []

            def s0():
                sp.dma(lambda e: e.dma_start(out=sgi[ii], in_=sgd[t * 128:(t + 1) * 128, :]), b_sgi[ii].sem, writes=[b_sgi[ii]])
                sp.dma(lambda e: e.dma_start(out=xt[ii], in_=xr[t * 128:(t + 1) * 128, :]), b_xt[ii].sem, writes=[b_xt[ii]])
            ths.append(s0)

            def s1(half):
                srcb, bsrc = [(mo, bmo), (sgi[ii], b_sgi[ii])][half]
                for j in range(4):
                    pe.op(lambda e: e.transpose(out=pbh[:, j * 128:(j + 1) * 128], in_=srcb[:, j * 128:(j + 1) * 128], identity=identb[:]),
                          reads=[bsrc, b_identb], writes=[b_pbh], pe_accum=(j > 0))
                dve.op(lambda e: e.tensor_copy(out=mixT[:, half * 512:(half + 1) * 512], in_=pbh[:, 0:512]), reads=[b_pbh], writes=[b_mixT], nowaw=(half > 0))
            ths.append(lambda: s1(0))
            ths.append(lambda: s1(1))

            def s2(hv, fh):
                for f in range(fh * 4, fh * 4 + 4):
                    pe.op(lambda e: e.matmul(pbL[:, :], lhsT=mixT[:, f * 128:(f + 1) * 128],
                                             rhs=v3(w_out_sb, 8)[:, f, hv * 512:(hv + 1) * 512], start=(f == 0), stop=(f == 7)),
                          reads=[b_mixT, b_wout], writes=[b_pbL], pe_accum=(f > 0))

            def s3(hv):
                c0 = hv * 512
                dve.op(lambda e: e.tensor_tensor(out=x1[:, c0:c0 + 512], in0=pbL[:, :], in1=growv(s_, 0)[:, c0:c0 + 512], op=ALU.mult), reads=[b_pbL, b_grow], writes=[b_x1])
                dve.op(lambda e: e.tensor_tensor(out=x1[:, c0:c0 + 512], in0=x1[:, c0:c0 + 512], in1=xt[ii][:, c0:c0 + 512], op=ALU.add), reads=[b_x1, b_xt[ii]], writes=[b_x1])
                if hv == 1:
                    sp.dma(lambda e: e.dma_start(out=xr[t * 128:(t + 1) * 128, :], in_=x1), b_x1.sem, reads=[b_x1])
                    pool.op(lambda e: e.memset(st[:, 0:1], 0.0), writes=[b_stT])
                    act.op(lambda e: e.activation(out=junk, in_=x1, func=AF.Square, accum_out=st[:, 0:1]), reads=[b_x1, b_stT], writes=[b_junk, b_stT])
            for hv in range(2):
                ths.append(lambda hv=hv: s2(hv, 0))
                ths.append(lambda hv=hv: s2(hv, 1))
                ths.append(lambda hv=hv: s3(hv))

            def s4():
                rstd_op(st[:, 1:2], st[:, 0:1], 1.0 / D, [b_stT])
                dve.op(lambda e: e.tensor_scalar(out=xn, in0=x1, scalar1=st[:, 1:2], scalar2=None, op0=ALU.mult), reads=[b_x1, b_stT], writes=[b_xn])
            ths.append(s4)

            def s5(hh):
                for k4 in range(4):
                    k = hh * 4 + k4
                    pe.op(lambda e: e.transpose(out=pbL[:, k4 * 128:(k4 + 1) * 128], in_=xn[:, k * 128:(k + 1) * 128], identity=ident[:]),
                          reads=[b_xn, b_ident], writes=[b_pbL], pe_accum=(k4 > 0))

            def s6(hh):
                ia = (1 * 2 + s_) * 8 + hh * 4
                Ab = AB[:, ia:ia + 4].unsqueeze(2).to_broadcast([128, 4, 128])
                Bb = v3(modT[:], 48)[:, 24 + hh * 4:28 + hh * 4, s_].unsqueeze(2).to_broadcast([128, 4, 128])
                hv_ = v3(h2T, 8)[:, hh * 4:(hh + 1) * 4, :]
                dve.op(lambda e: e.tensor_tensor(out=hv_, in0=v3(pbL[:, :], 4), in1=Ab, op=ALU.mult), reads=[b_pbL, b_AB], writes=[b_h2T])
                dve.op(lambda e: e.tensor_tensor(out=hv_, in0=hv_, in1=Bb, op=ALU.add), reads=[b_h2T, b_modT], writes=[b_h2T])
            for hh in range(2):
                ths.append(lambda hh=hh: s5(hh))
                ths.append(lambda hh=hh: s6(hh))

            def s7a():
                for k in range(8):
                    pe.op(lambda e: e.matmul(pbr[:, 0:NE], lhsT=h2T[:, k * 128:(k + 1) * 128], rhs=wrt[:, k * NE:(k + 1) * NE], start=(k == 0), stop=(k == 7)),
                          reads=[b_h2T, b_wrt], writes=[b_pbR], pe_accum=(k > 0))
            ths.append(s7a)

            def s7(hh):
                for k4 in range(4):
                    k = hh * 4 + k4
                    pe.op(lambda e: e.transpose(out=pbL[:, k4 * 128:(k4 + 1) * 128], in_=h2T[:, k * 128:(k + 1) * 128], identity=ident[:]),
                          reads=[b_h2T, b_ident], writes=[b_pbL], pe_accum=(k4 > 0))

            def s7c(hh):
                dve.op(lambda e: e.tensor_copy(out=h2[:, hh * 512:(hh + 1) * 512], in_=pbL[:, :]), reads=[b_pbL], writes=[b_h2])
                if hh == 1:
                    sp.dma(lambda e: e.dma_start(out=h2d[t * 128:(t + 1) * 128, :], in_=h2), b_h2.sem, reads=[b_h2])
            for hh in range(2):
                ths.append(lambda hh=hh: s7(hh))
                ths.append(lambda hh=hh: s7c(hh))

            def s8():
                dve.op(lambda e: e.tensor_reduce(out=st[:, 5:6], in_=pbr[:, 0:NE], axis=AX.X, op=ALU.max), reads=[b_pbR], writes=[b_stT])
                dve.op(lambda e: e.tensor_scalar(out=st[:, 5:6], in0=st[:, 5:6], scalar1=-1.0, scalar2=None, op0=ALU.mult), reads=[b_stT], writes=[b_stT])
                pool.op(lambda e: e.memset(st[:, 6:7], 0.0), writes=[b_stT])
                act.op(lambda e: e.activation(out=af, in_=pbr[:, 0:NE], func=AF.Exp, bias=st[:, 5:6], scale=1.0, accum_out=st[:, 6:7]),
                       reads=[b_pbR, b_stT], writes=[b_af, b_stT])
            ths.append(s8)

            def s9():
                dve.op(lambda e: e.reciprocal(out=st[:, 6:7], in_=st[:, 6:7]), reads=[b_stT], writes=[b_stT])
                dve.op(lambda e: e.tensor_scalar(out=af, in0=af, scalar1=st[:, 6:7], scalar2=None, op0=ALU.mult), reads=[b_af, b_stT], writes=[b_af])
                pe.op(lambda e: e.transpose(out=pbr[0:NE, 128:256], in_=af, identity=ident[:]), reads=[b_af, b_ident], writes=[b_pbAf])
            ths.append(s9)

            def s10():
                dve.op(lambda e: e.tensor_copy(out=affT[0:NE, t * 128:(t + 1) * 128], in_=pbr[0:NE, 128:256]), reads=[b_pbAf], writes=[b_affT])
            ths.append(s10)
            return ths

        from collections import deque
        bg = deque()
        for th in build_q(0):
            th()
        a8 = v3(accS, 8)
        pti = 0
        for gi, (t0, nq) in enumerate(qgroups):
            par = gi % 2
            s = 0 if t0 < NLT else 1
            NQ = nq * 128
            kts = list(range(NT)) if s == 0 else [NLT, NLT + 1]
            if gi + 1 < len(qgroups):
                bg.extendleft(reversed(build_q(gi + 1)))
            iters = [(h, c, ki, kt) for h in range(4) for c in range(2) for ki, kt in enumerate(kts)]
            timed = []

            def emit_S(idx):
                h, c, ki, kt = iters[idx]; sbt, bsb = Sb[idx % 3]
                i8 = c * 4 + h; j = i8 // 2
                pe.op(lambda e: e.matmul(sbt[:, 0:NQ], lhsT=v3(kT, 4)[:, j, kt * 128:(kt + 1) * 128], rhs=v3(qTm[par], 8)[:, i8, 0:NQ], start=True, stop=True),
                      reads=[b_kT, b_qTm[par]], writes=[bsb])

            def E1(h):
                for bank in range(3):
                    na = 3 if bank < 2 else 2
                    src = pb[2 + bank][:, 0:na * 160].rearrange("p (a c) -> p a c", a=na)[:, :, 0:130]
                    dve.op(lambda e: e.tensor_copy(out=a8[:, bank * 3:bank * 3 + na, :], in_=src), reads=[b_pb[2 + bank]], writes=[b_accS])
                for qi in range(nq):
                    a0 = a8[:, qi, :]; a1 = a8[:, 4 + qi, :]
                    dve.op(lambda e: e.reciprocal(out=st[:, 2:3], in_=a0[:, 128:129]), reads=[b_accS], writes=[b_stE])
                    dve.op(lambda e: e.reciprocal(out=st[:, 3:4], in_=a1[:, 128:129]), reads=[b_accS], writes=[b_stE])
                    dve.op(lambda e: e.tensor_tensor(out=st[:, 3:4], in0=st[:, 3:4], in1=lamt[:, 3:4], op=ALU.mult), reads=[b_stE, b_lamt], writes=[b_stE])
                    dve.op(lambda e: e.tensor_scalar(out=t1, in0=a1[:, 0:128], scalar1=st[:, 3:4], scalar2=None, op0=ALU.mult), reads=[b_accS, b_stE], writes=[b_t1])
                    dve.op(lambda e: e.scalar_tensor_tensor(out=osb[qi], in0=a0[:, 0:128], scalar=st[:, 2:3], in1=t1, op0=ALU.mult, op1=ALU.add),
                           reads=[b_accS, b_stE, b_t1], writes=[b_osb[qi]])
                    dve.op(lambda e: e.tensor_tensor(out=t1, in0=osb[qi], in1=osb[qi], op=ALU.mult), reads=[b_osb[qi]], writes=[b_t1])
                    dve.op(lambda e: e.tensor_reduce(out=st[:, 8 + qi:9 + qi], in_=t1, axis=AX.X, op=ALU.add), reads=[b_t1], writes=[b_stE2])

            def E2(h):
                rstd_op(st[:, 8:8 + nq], st[:, 8:8 + nq], 1.0 / 128, [b_stE2])

            def E3(h):
                for qi in range(nq):
                    dve.op(lambda e: e.scalar_tensor_tensor(out=mixo[par][qi][:, h * 128:(h + 1) * 128], in0=osb[qi], scalar=st[:, 8 + qi:9 + qi], in1=gsub[:], op0=ALU.mult, op1=ALU.mult),
                           reads=[b_osb[qi], b_stE2, b_gsub], writes=[b_mixo[par][qi]])

            import os as _os
            NOLOOK = _os.environ.get("KNOLOOK") == "1"; NOBG = _os.environ.get("KNOBG") == "1"
            if not NOLOOK:
                emit_S(0); emit_S(1)
            for idx in range(len(iters)):
                if NOLOOK:
                    emit_S(idx)
                elif idx + 2 < len(iters):
                    emit_S(idx + 2)
                h, c, ki, kt = iters[idx]; sbt, bsb = Sb[idx % 3]
                pi = pti % 3; pti += 1
                act.op(lambda e: e.activation(out=pT[pi][:, 0:NQ], in_=sbt[:, 0:NQ], func=AF.Exp, scale=0.125),
                       reads=[bsb], writes=[b_pT[pi]])
                banks_seen = set()
                for qi in range(nq):
                    a_ = c * 4 + qi
                    aap, bacc = acc_ap(a_)
                    first_in_bank = (a_ // 3) not in banks_seen
                    banks_seen.add(a_ // 3)
                    pe.op(lambda e: e.matmul(aap, lhsT=pT[pi][:, qi * 128:(qi + 1) * 128], rhs=va4[:, kt, h, :],
                                             start=(ki == 0 and first_in_bank), stop=(ki == len(kts) - 1), skip_group_check=True),
                          reads=[b_pT[pi], b_vaug], writes=[bacc], pe_accum=(ki > 0))
                if c == 1 and ki == len(kts) - 1:
                    while timed:
                        timed.pop(0)[1]()
                    E1(h)
                    timed.append((idx + 6, lambda h=h: E2(h)))
                    timed.append((idx + 12, lambda h=h: E3(h)))
                while timed and timed[0][0] <= idx:
                    timed.pop(0)[1]()
                if idx % 2 == 1 and bg and not NOBG:
                    bg.popleft()()
            while timed:
                timed.pop(0)[1]()
            while NOBG and bg:
                bg.popleft()()
            for qi in range(nq):
                bg.extend(tile_thunks(gi, qi))
        while bg:
            bg.popleft()()
        P.barrier()

        AFa.off = af_mark; ABa.reset()
        NTOK = CAP if last else CAP + CCAP
        NJ = 4 if last else 5
        work = affT[:, 0:SEQ]; b_work = b_affT
        workc = affT[:, SEQ:T]
        vals = AFa.alloc(NTOK); b_vals = P.buf("vals%d" % l)
        idxu = AFa.alloc(NTOK).bitcast(U32); b_idxu = P.buf("idxu%d" % l)
        idxf = AFa.alloc(NTOK); b_idxf = P.buf("idxf%d" % l)
        gT = AFa.alloc(5 * NE); b_gT = P.buf("gT%d" % l)
        idxT = AFa.alloc(5 * NE).bitcast(I32); b_idxT = P.buf("idxT%d" % l)
        sgs = [AFa.alloc(544) for _ in range(2)]; b_sgs = [P.buf("sgs%d_%d" % (l, i)) for i in range(2)]
        ysc = AFa.alloc(5 * D); b_ysc = P.buf("ysc%d" % l)
        ring = [ABa.alloc(8192) for _ in range(3)]; b_ring = [P.buf("ring_%d" % i, True) for i in range(3)]
        xs = [ABa.alloc(5 * D) for _ in range(2)]; b_xs = [P.buf("xs_%d" % i, True) for i in range(2)]
        xsT = ABa.alloc(8 * 544); b_xsT = P.buf("xsT%d" % l)
        hidT = ABa.alloc(16 * 544); b_hidT = P.buf("hidT%d" % l)
        for r in range(CAP // 8):
            dve.op(lambda e, r=r: e.max(out=vals[0:NE, r * 8:(r + 1) * 8], in_=work[0:NE, :]), reads=[b_work], writes=[b_vals])
            dve.op(lambda e, r=r: e.max_index(out=idxu[0:NE, r * 8:(r + 1) * 8], in_max=vals[0:NE, r * 8:(r + 1) * 8], in_values=work[0:NE, :]),
                   reads=[b_work, b_vals], writes=[b_idxu])
            if r < CAP // 8 - 1:
                dve.op(lambda e, r=r: e.match_replace(out=work[0:NE, :], in_to_replace=vals[0:NE, r * 8:(r + 1) * 8], in_values=work[0:NE, :], imm_value=-1.0),
                       reads=[b_vals, b_work], writes=[b_work])
        if not last:
            for r in range(CCAP // 8):
                c0 = CAP + r * 8
                dve.op(lambda e, c0=c0: e.max(out=vals[0:NE, c0:c0 + 8], in_=workc[0:NE, :]), reads=[b_work], writes=[b_vals])
                dve.op(lambda e, c0=c0: e.max_index(out=idxu[0:NE, c0:c0 + 8], in_max=vals[0:NE, c0:c0 + 8], in_values=workc[0:NE, :]),
                       reads=[b_work, b_vals], writes=[b_idxu])
                if r < CCAP // 8 - 1:
                    dve.op(lambda e, c0=c0: e.match_replace(out=workc[0:NE, :], in_to_replace=vals[0:NE, c0:c0 + 8], in_values=workc[0:NE, :], imm_value=-1.0),
                           reads=[b_vals, b_work], writes=[b_work])
        dve.op(lambda e: e.tensor_copy(out=idxf[0:NE, :], in_=idxu[0:NE, :]), reads=[b_idxu], writes=[b_idxf])
        if not last:
            dve.op(lambda e: e.tensor_scalar(out=idxf[0:NE, CAP:NTOK], in0=idxf[0:NE, CAP:NTOK], scalar1=float(SEQ), scalar2=None, op0=ALU.add),
                   reads=[b_idxf], writes=[b_idxf])
        for (srcv, bsrc, dstv, bdst) in ((idxf, b_idxf, idxT, b_idxT), (vals, b_vals, gT, b_gT)):
            for jc in range(NJ):
                nj = 128 if jc < 4 else CCAP
                pe.op(lambda e, jc=jc, nj=nj, srcv=srcv: e.transpose(out=pb[4][0:nj, jc * NE:(jc + 1) * NE], in_=srcv[0:NE, jc * 128:jc * 128 + nj], identity=ident[0:NE, 0:NE]),
                      reads=[bsrc, b_ident], writes=[b_pb[4]], pe_accum=(jc > 0))
            dve.op(lambda e, dstv=dstv: e.tensor_copy(out=dstv[:, 0:4 * NE], in_=pb[4][:, 0:4 * NE]), reads=[b_pb[4]], writes=[bdst])
            if not last:
                dve.op(lambda e, dstv=dstv: e.tensor_copy(out=dstv[0:CCAP, 4 * NE:5 * NE], in_=pb[4][0:CCAP, 4 * NE:5 * NE]), reads=[b_pb[4]], writes=[bdst])

        def gather(e_):
            xb = xs[e_ % 2]; bx = b_xs[e_ % 2]
            NJs = [(jc, 128 if jc < 4 else CCAP) for jc in range(NJ)]
            pool.dma([lambda e, jc=jc, nj=nj: e.indirect_dma_start(
                out=xb[0:nj, jc * D:(jc + 1) * D], out_offset=None, in_=h2d[:, :],
                in_offset=bass.IndirectOffsetOnAxis(ap=idxT[0:nj, jc * NE + e_: jc * NE + e_ + 1], axis=0)) for (jc, nj) in NJs],
                bx.sem, reads=[b_idxT], writes=[bx])

        pieces = [(e_, p_) for e_ in range(NE) for p_ in range(6)]

        def load_piece(pi_):
            e_, p_ = pieces[pi_]
            rb = ring[pi_ % 3]; brb = b_ring[pi_ % 3]
            if p_ < 4:
                pool.dma([lambda e: e.dma_start(out=v3(rb[:, 0:4096], 8), in_=w_gate[l, e_].rearrange("(k p) n -> p k n", p=128)[:, :, p_ * 512:(p_ + 1) * 512]),
                          lambda e: e.dma_start(out=v3(rb[:, 4096:8192], 8), in_=w_up[l, e_].rearrange("(k p) n -> p k n", p=128)[:, :, p_ * 512:(p_ + 1) * 512])],
                         brb.sem, writes=[brb])
            else:
                hv = p_ - 4
                pool.dma([lambda e, q_=q_: e.dma_start(
                    out=v3(rb[:, q_ * 4096:(q_ + 1) * 4096], 8),
                    in_=w_down[l, e_].rearrange("(f p) n -> p f n", p=128)[:, q_ * 8:(q_ + 1) * 8, hv * 512:(hv + 1) * 512]) for q_ in range(2)],
                    brb.sem, writes=[brb])

        b_xrs = P.buf("xrs", True)
        gather(0)
        load_piece(0); load_piece(1)
        sgi_ = [0]
        for e_ in range(NE):
            xb = xs[e_ % 2]; bx = b_xs[e_ % 2]
            if e_ + 1 < NE:
                gather(e_ + 1)
            for jc in range(NJ):
                nj = 128 if jc < 4 else CCAP
                for kh in range(2):
                    for k4 in range(4):
                        k = kh * 4 + k4
                        pe.op(lambda e: e.transpose(out=pbh[:, k4 * 128:k4 * 128 + nj], in_=xb[0:nj, jc * D + k * 128: jc * D + (k + 1) * 128],
                                                    identity=identb[0:nj, 0:nj]),
                              reads=[bx, b_identb], writes=[b_pbh], pe_accum=(k4 > 0))
                    act.op(lambda e: e.activation(out=v3(xsT, 8)[:, kh * 4:(kh + 1) * 4, jc * 128:jc * 128 + nj], in_=v3(pbh[:, :], 4)[:, :, 0:nj], func=AF.Copy),
                           reads=[b_pbh], writes=[b_xsT])
            for p_ in range(6):
                pi_ = e_ * 6 + p_
                if pi_ + 2 < len(pieces):
                    load_piece(pi_ + 2)
                rb = ring[pi_ % 3]; brb = b_ring[pi_ % 3]
                if p_ < 4:
                    for fi in range(4):
                        f = p_ * 4 + fi
                        gb = fi % 2
                        for wi, (woff, pbt, bpbt) in enumerate(((0, pb[gb], b_pb[gb]), (4096, pb[2 + gb], b_pb[2 + gb]))):
                            for k in range(8):
                                pe.op(lambda e, rb=rb, woff=woff, k=k, fi=fi, pbt=pbt: e.matmul(pbt[:, 0:CAP], lhsT=v3(rb[:, woff:woff + 4096], 8)[:, k, fi * 128:(fi + 1) * 128],
                                                                                              rhs=v3(xsT, 8)[:, k, 0:CAP], start=(k == 0), stop=(k == 7)),
                                      reads=[brb, b_xsT], writes=[bpbt], pe_accum=(k > 0))
                            if not last:
                                co = gb * 64 + wi * 32
                                for k in range(8):
                                    pe.op(lambda e, rb=rb, woff=woff, k=k, fi=fi, co=co: e.matmul(pb[4][:, co:co + CCAP], lhsT=v3(rb[:, woff:woff + 4096], 8)[:, k, fi * 128:(fi + 1) * 128],
                                                                                                rhs=v3(xsT, 8)[:, k, CAP:NTOK], start=(k == 0), stop=(k == 7)),
                                          reads=[brb, b_xsT], writes=[b_pb[4]], pe_accum=(k > 0))
                        sg_ = sgs[sgi_[0] % 2]; bsg = b_sgs[sgi_[0] % 2]; sgi_[0] += 1
                        act.op(lambda e, gb=gb, sg_=sg_: e.activation(out=sg_[:, 0:CAP], in_=pb[gb][:, 0:CAP], func=AF.Silu), reads=[b_pb[gb]], writes=[bsg])
                        dve.op(lambda e, gb=gb, sg_=sg_, f=f: e.tensor_tensor(out=v3(hidT, 16)[:, f, 0:CAP], in0=sg_[:, 0:CAP], in1=pb[2 + gb][:, 0:CAP], op=ALU.mult),
                               reads=[bsg, b_pb[2 + gb]], writes=[b_hidT])
                        if not last:
                            co = gb * 64
                            act.op(lambda e, co=co, sg_=sg_: e.activation(out=sg_[:, CAP:NTOK], in_=pb[4][:, co:co + CCAP], func=AF.Silu), reads=[b_pb[4]], writes=[bsg])
                            dve.op(lambda e, co=co, sg_=sg_, f=f: e.tensor_tensor(out=v3(hidT, 16)[:, f, CAP:NTOK], in0=sg_[:, CAP:NTOK], in1=pb[4][:, co + 32:co + 32 + CCAP], op=ALU.mult),
                                   reads=[bsg, b_pb[4]], writes=[b_hidT])
                else:
                    hv = p_ - 4
                    for jc in range(NJ):
                        nj = 128 if jc < 4 else CCAP
                        yb = jc % 2
                        for f in range(16):
                            pe.op(lambda e, rb=rb, f=f, jc=jc, nj=nj, yb=yb: e.matmul(pbb[0:nj, yb * 512:(yb + 1) * 512], lhsT=v3(hidT, 16)[:, f, jc * 128:jc * 128 + nj],
                                                                                    rhs=v3(rb[:, (f // 8) * 4096:(f // 8 + 1) * 4096], 8)[:, f % 8, :], start=(f == 0), stop=(f == 15)),
                                  reads=[b_hidT, brb], writes=[b_pbb], pe_accum=(f > 0))
                        s = 0 if jc < 4 else 1
                        dve.op(lambda e, jc=jc, nj=nj, yb=yb, hv=hv, s=s, e_=e_: e.scalar_tensor_tensor(
                            out=ysc[0:nj, jc * D + hv * 512: jc * D + (hv + 1) * 512], in0=pbb[0:nj, yb * 512:(yb + 1) * 512],
                            scalar=gT[0:nj, jc * NE + e_: jc * NE + e_ + 1], in1=growv(s, 1)[0:nj, hv * 512:(hv + 1) * 512], op0=ALU.mult, op1=ALU.mult),
                            reads=[b_pbb, b_gT, b_grow], writes=[b_ysc])
            NJs = [(jc, 128 if jc < 4 else CCAP) for jc in range(NJ)]
            pool.dma([lambda e, jc=jc, nj=nj: e.indirect_dma_start(
                out=xr[:, :], out_offset=bass.IndirectOffsetOnAxis(ap=idxT[0:nj, jc * NE + e_: jc * NE + e_ + 1], axis=0),
                in_=ysc[0:nj, jc * D:(jc + 1) * D], in_offset=None, compute_op=ALU.add) for (jc, nj) in NJs],
                b_xrs.sem, reads=[b_ysc, b_idxT, b_xrs], writes=[b_xrs])
        P.barrier()

    AFa.reset(); ABa.reset()
    gf = AFa.alloc(D); b_gf = P.buf("gf", True)
    xt = [AFa.alloc(D) for _ in range(2)]; b_xt = [P.buf("xt_%d" % i, True) for i in range(2)]
    yo = [AFa.alloc(D) for _ in range(2)]; b_yo = [P.buf("yo%d" % i, True) for i in range(2)]
    junk = AFa.alloc(D); b_junk = P.buf("junkf")
    st = small[:, 32:40]; b_st = P.buf("stf")
    sp.dma(lambda e: e.dma_start(out=gf, in_=norm_f_g[0:1, :].to_broadcast([128, D])), b_gf.sem, writes=[b_gf])
    for t in range(NLT):
        i = t % 2
        sp.dma(lambda e, t=t, i=i: e.dma_start(out=xt[i], in_=xr[t * 128:(t + 1) * 128, :]), b_xt[i].sem, writes=[b_xt[i]])
        if final_norm:
            pool.op(lambda e: e.memset(st[:, 0:1], 0.0), writes=[b_st])
            act.op(lambda e, i=i: e.activation(out=junk, in_=xt[i], func=AF.Square, accum_out=st[:, 0:1]), reads=[b_xt[i], b_st], writes=[b_junk, b_st])
            rstd_op(st[:, 1:2], st[:, 0:1], 1.0 / D, [b_st])
            dve.op(lambda e, i=i: e.scalar_tensor_tensor(out=yo[i], in0=xt[i], scalar=st[:, 1:2], in1=gf, op0=ALU.mult, op1=ALU.mult),
                   reads=[b_xt[i], b_st, b_gf], writes=[b_yo[i]])
        else:
            dve.op(lambda e, i=i: e.tensor_copy(out=yo[i], in_=xt[i]), reads=[b_xt[i]], writes=[b_yo[i]])
        sp.dma(lambda e, t=t, i=i: e.dma_start(out=out_d[t * 128:(t + 1) * 128, :], in_=yo[i]), b_yo[i].sem, reads=[b_yo[i]])
    P.finish()
    return nc


def _rope_table():
    rows = SEQ // 64
    row = np.repeat(np.arange(rows), 64).astype(np.float32)
    col = np.tile(np.arange(64), rows).astype(np.float32)
    n_freq = 16
    freqs = (np.float32(10000.0) ** (-np.arange(n_freq, dtype=np.float32) / np.float32(n_freq))).astype(np.float32)
    ang_r = (row[:, None] * freqs).astype(np.float32)
    ang_c = (col[:, None] * freqs).astype(np.float32)
    ang = np.concatenate([ang_r, ang_r, ang_c, ang_c], axis=-1)
    cos = np.cos(ang).astype(np.float32); sin = np.sin(ang).astype(np.float32)
    tab = np.zeros((T, 128), np.float32)
    tab[:SEQ, 0:64] = cos
    sgn = np.ones((2, 2, 16), np.float32); sgn[:, 0, :] = -1.0
    tab[:SEQ, 64:128] = sin * sgn.reshape(64)
    tab[SEQ:, 0:64] = 1.0
    return tab


def prep_shared(inp):
    f = lambda a: np.ascontiguousarray(np.asarray(a, dtype=np.float32))
    sh = {
        "rope": _rope_table(), "ident": np.eye(128, dtype=np.float32),
        "w_ada": f(inp["w_ada"]), "b_ada": f(inp["b_ada"]),
        "b_ada_col": f(np.asarray(inp["b_ada"]).reshape(DEPTH, 48, 128).transpose(0, 2, 1)),
        "n1col": f(np.asarray(inp["norm1_g"]).reshape(DEPTH, 8, 128).transpose(0, 2, 1)),
        "n2col": f(np.asarray(inp["norm2_g"]).reshape(DEPTH, 8, 128).transpose(0, 2, 1)),
        "w_in": f(inp["w_in"]), "w_out": f(inp["w_out"]),
        "lamv": f(np.concatenate([np.asarray(inp["lambda_q1"]), np.asarray(inp["lambda_k1"]), np.asarray(inp["lambda_q2"]), np.asarray(inp["lambda_k2"])], axis=1)),
        "subln_g": f(inp["subln_g"]), "sgu_norm_g": f(inp["sgu_norm_g"]), "sgu_w": f(inp["sgu_w"]),
        "sgu_bT": f(np.asarray(inp["sgu_b"]).transpose(0, 2, 1)),
        "w_router": f(inp["w_router"]), "w_gate": f(inp["w_gate"]), "w_up": f(inp["w_up"]), "w_down": f(inp["w_down"]),
        "norm_f_g": f(np.asarray(inp["norm_f_g"]).reshape(1, D)),
    }
    return sh


def prep_core(inp, b):
    x = np.asarray(inp["x"], dtype=np.float32)[b]; cx = np.asarray(inp["ctx"], dtype=np.float32)[b]
    c = np.asarray(inp["c"], dtype=np.float32)[b]; cctx = np.asarray(inp["c_ctx"], dtype=np.float32)
    cc = np.stack([c.reshape(8, 128).T, cctx.reshape(8, 128).T], axis=-1).reshape(128, 16)
    return {"x": np.ascontiguousarray(np.concatenate([x, cx], axis=0)), "cc": np.ascontiguousarray(cc)}


def kernel(**inputs):
    sh = prep_shared(inputs)
    nc = build()
    in_maps = []
    for b in range(8):
        m = dict(sh); m.update(prep_core(inputs, b)); in_maps.append(m)
    res = run_bass_kernel_spmd(nc, in_maps, core_ids=list(range(8)))
    return np.stack([np.asarray(r["out"], dtype=np.float32) for r in res.results], axis=0)
```

```python
import math
import numpy as np
from contextlib import ExitStack
import concourse.bass as bass
import concourse.mybir as mybir
from concourse.bass_utils import run_bass_kernel_spmd

F32 = mybir.dt.float32; BF16 = mybir.dt.bfloat16; I32 = mybir.dt.int32; U32 = mybir.dt.uint32
AF = mybir.ActivationFunctionType; ALU = mybir.AluOpType; AX = mybir.AxisListType

D = 1024; SEQ = 4096; CTX = 256; T = SEQ + CTX; NT = T // 128; NLT = SEQ // 128
DEPTH = 4; NE = 16; CAP = 512; CCAP = 32; FF = 2048
EPS = 1e-6


class Sem:
    def __init__(self, h, name):
        self.h = h; self.name = name; self.val = 0


class Buf:
    def __init__(self, name, sem=None, excl=False):
        self.name = name; self.w = None; self.r = {}; self.sem = sem; self.excl = excl


class Call:
    def __init__(self, name, a, k):
        self.name = name; self.a = a; self.k = k

    def run(self, e):
        return getattr(e, self.name)(*self.a, **self.k)


class Rec:
    def __getattr__(self, name):
        return lambda *a, **k: Call(name, a, k)


REC = Rec()


class Eng:
    def __init__(self, prog, name, sem):
        self.prog = prog; self.name = name; self.sem = sem; self.q = []; self.waited = {}

    def wait_tok(self, sem, val):
        if self.name == 'pe' and sem is self.sem:
            return
        if self.waited.get(sem, 0) >= val:
            return
        self.waited[sem] = val
        h = sem.h
        self.q.append(lambda e, h=h, val=val: e.wait_ge(h, val))

    def deps(self, reads, writes, pe_accum=False, nowaw=False):
        for b in reads:
            if b.w is not None:
                self.wait_tok(*b.w)
            if b.excl:
                for s, v in b.r.items():
                    if s is not self.sem:
                        self.wait_tok(s, v)
        for b in writes:
            if pe_accum:
                continue
            if b.w is not None and not (nowaw and b.w[0] is self.sem):
                self.wait_tok(*b.w)
            for s, v in b.r.items():
                self.wait_tok(s, v)

    def mark(self, tok, reads, writes):
        s, v = tok
        for b in reads:
            if b.r.get(s, 0) < v:
                b.r[s] = v
        for b in writes:
            b.w = tok; b.r = {}

    def op(self, fn, reads=(), writes=(), pe_accum=False, nowaw=False):
        self.deps(reads, writes, pe_accum, nowaw)
        self.sem.val += 1
        tok = (self.sem, self.sem.val)
        h = self.sem.h
        call = fn(REC)
        self.q.append(lambda e, call=call, h=h: call.run(e).then_inc(h, 1))
        self.mark(tok, reads, writes)
        return tok

    def dma(self, fn, sem, reads=(), writes=()):
        fns = fn if isinstance(fn, (list, tuple)) else [fn]
        self.deps(reads, writes)
        h = sem.h
        for f in fns:
            sem.val += 16
            call = f(REC)
            self.q.append(lambda e, call=call, h=h: call.run(e).then_inc(h, 16))
        tok = (sem, sem.val)
        self.mark(tok, reads, writes)
        return tok


class Prog:
    def __init__(self, nc):
        self.nc = nc; self.es = ExitStack(); self.sems = []; self.semcache = {}
        self.sp = Eng(self, 'sp', self.new_sem('e_sp'))
        self.act = Eng(self, 'act', self.new_sem('e_act'))
        self.pool = Eng(self, 'pool', self.new_sem('e_pool'))
        self.dve = Eng(self, 'dve', self.new_sem('e_dve'))
        self.pe = Eng(self, 'pe', self.new_sem('e_pe'))
        self.engs = [self.sp, self.act, self.pool, self.dve, self.pe]

    def new_sem(self, name):
        s = Sem(self.es.enter_context(self.nc.semaphore(name)), name)
        self.sems.append(s)
        return s

    def sbuf(self, name, shape, dt):
        return self.es.enter_context(self.nc.sbuf_tensor(name, shape, dt))

    def psum(self, name, shape, dt):
        return self.es.enter_context(self.nc.psum_tensor(name, shape, dt))

    def buf(self, name, dma=False):
        if not dma:
            return Buf(name, None)
        if name not in self.semcache:
            self.semcache[name] = self.new_sem('b_' + name)
        return Buf(name, self.semcache[name])

    def barrier(self):
        for e in self.engs:
            for s in self.sems:
                if s.val > 0:
                    e.wait_tok(s, s.val)

    def finish(self):
        self.barrier()
        with self.nc.Block() as block:
            @block.sync
            def _(e):
                for f in self.sp.q: f(e)
            @block.scalar
            def _(e):
                for f in self.act.q: f(e)
            @block.gpsimd
            def _(e):
                for f in self.pool.q: f(e)
            @block.vector
            def _(e):
                for f in self.dve.q: f(e)
            @block.tensor
            def _(e):
                for f in self.pe.q: f(e)
        self.es.close()


class Arena:
    def __init__(self, P, name, ncols, dt):
        self.t = P.sbuf(name, [128, ncols], dt); self.off = 0; self.n = ncols; self.name = name

    def alloc(self, cols):
        a = self.off; self.off += cols
        assert self.off <= self.n, (self.name, self.off, self.n)
        return self.t[:, a:a + cols]

    def reset(self):
        self.off = 0


def v3(ap, a):
    return ap.rearrange("p (a b) -> p a b", a=a)


def build(n_layers=DEPTH, final_norm=True):
    nc = bass.Bass("TRN2", target_bir_lowering=False)
    dram = lambda name, shape, dt, kind="ExternalInput": nc.dram_tensor(name, shape, dt, kind=kind).ap()
    x_in = dram("x", [T, D], F32)
    cc_d = dram("cc", [128, 16], F32)
    rope_d = dram("rope", [T, 128], F32)
    ident_d = dram("ident", [128, 128], F32)
    w_ada = dram("w_ada", [DEPTH, D, 6 * D], F32)
    b_ada_col = dram("b_ada_col", [DEPTH, 128, 48], F32)
    b_ada = dram("b_ada", [DEPTH, 6 * D], F32)
    n1col = dram("n1col", [DEPTH, 128, 8], F32)
    n2col = dram("n2col", [DEPTH, 128, 8], F32)
    w_in = dram("w_in", [DEPTH, D, 2560], F32)
    w_out = dram("w_out", [DEPTH, D, D], F32)
    lamv = dram("lamv", [DEPTH, 256], F32)
    subln_g = dram("subln_g", [DEPTH, 128], F32)
    sgu_norm_g = dram("sgu_norm_g", [DEPTH, 512], F32)
    sgu_w = dram("sgu_w", [DEPTH, 8, 128, 128], F32)
    sgu_bT = dram("sgu_bT", [DEPTH, 128, 8], F32)
    w_router = dram("w_router", [DEPTH, D, NE], F32)
    w_gate = dram("w_gate", [DEPTH, NE, D, FF], F32)
    w_up = dram("w_up", [DEPTH, NE, D, FF], F32)
    w_down = dram("w_down", [DEPTH, NE, FF, D], F32)
    norm_f_g = dram("norm_f_g", [1, D], F32)
    out_d = dram("out", [SEQ, D], F32, kind="ExternalOutput")
    xr = dram("xr", [T, D], F32, kind="Internal")
    qd = dram("qd", [T, 512], BF16, kind="Internal")
    sgd = dram("sgd", [T, 512], BF16, kind="Internal")
    h2d = dram("h2d", [T, D], BF16, kind="Internal")

    P = Prog(nc)
    sp, act, pool, dve, pe = P.sp, P.act, P.pool, P.dve, P.pe

    ident = P.sbuf("ident_sb", [128, 128], F32); b_ident = P.buf("ident", True)
    identb = P.sbuf("identb_sb", [128, 128], BF16); b_identb = P.buf("identb")
    grow = P.sbuf("grow", [128, 4 * D], F32); b_grow = P.buf("grow")
    siluc = P.sbuf("siluc", [128, 16], F32); b_siluc = P.buf("siluc", True)
    modT = P.sbuf("modT", [128, 96], F32); b_modT = P.buf("modT")
    bcol = P.sbuf("bcol", [128, 48], F32); b_bcol = P.buf("bcol", True)
    ncol = P.sbuf("ncol", [128, 16], F32); b_ncol = P.buf("ncol", True)
    AB = P.sbuf("ABcols", [128, 32], F32); b_AB = P.buf("ABc")
    lamt = P.sbuf("lamt", [128, 8], F32); b_lamt = P.buf("lamt")
    lv = P.sbuf("lv", [128, 256], F32); b_lv = P.buf("lv", True)
    lprod = P.sbuf("lprod", [128, 128], F32); b_lprod = P.buf("lprod")
    gsub = P.sbuf("gsub", [128, 128], F32); b_gsub = P.buf("gsub", True)
    sgng = P.sbuf("sgng", [128, 512], F32); b_sgng = P.buf("sgng", True)
    sbT = P.sbuf("sbT", [128, 8], F32); b_sbT = P.buf("sbT", True)
    bfull = P.sbuf("bfull", [128, 512], F32); b_bfull = P.buf("bfull")
    wsT = P.sbuf("wsT", [128, 8 * 128], BF16); b_wsT = P.buf("wsT")
    wrt = P.sbuf("wrt", [128, 8 * NE], F32); b_wrt = P.buf("wrt", True)
    small = P.sbuf("small", [128, 64], F32)
    epsc = P.sbuf("epsc", [128, 1], F32); b_epsc = P.buf("epsc")
    dve.op(lambda e: e.memset(epsc[:], EPS), writes=[b_epsc])

    def rstd_op(dst, src, scale, bufs):
        act.op(lambda e: e.activation(out=dst, in_=src, func=AF.Ln, scale=float(scale), bias=epsc[:, 0:1]), reads=bufs + [b_epsc], writes=bufs)
        act.op(lambda e: e.activation(out=dst, in_=dst, func=AF.Exp, scale=-0.5), reads=bufs, writes=bufs)

    AFa = Arena(P, "arenaF", 12928, F32)
    ABa = Arena(P, "arenaB", 62464, BF16)

    pb = [P.psum("pb%d" % i, [128, 512], F32) for i in range(5)]
    pbhr = P.psum("pbhr", [128, 512], F32)
    pbh = pbhr[:, 0:256].bitcast(BF16)
    pbr = pbhr[:, 256:512]
    pbb = P.psum("pbb", [128, 1024], F32)
    b_pb = [Buf("pb%d" % i, excl=True) for i in range(5)]
    b_pbh = Buf("pbh", excl=True); b_pbb = Buf("pbb", excl=True)

    sp.dma(lambda e: e.dma_start(out=ident[:], in_=ident_d), b_ident.sem, writes=[b_ident])
    dve.op(lambda e: e.tensor_copy(out=identb[:], in_=ident[:]), reads=[b_ident], writes=[b_identb])
    sp.dma(lambda e: e.dma_start(out=siluc[:], in_=cc_d), b_siluc.sem, writes=[b_siluc])
    act.op(lambda e: e.activation(out=siluc[:], in_=siluc[:], func=AF.Silu), reads=[b_siluc], writes=[b_siluc])
    b_xr = P.buf("xr", True)
    sp.dma([lambda e, i=i: e.dma_start(out=xr[i * (T // 4):(i + 1) * (T // 4), :], in_=x_in[i * (T // 4):(i + 1) * (T // 4), :]) for i in range(4)],
           b_xr.sem, writes=[b_xr])
    P.barrier()

    for l in range(n_layers):
        last = (l == DEPTH - 1)
        lam_init = 0.8 - 0.6 * math.exp(-0.3 * l)
        NTq = NLT if last else NT
        AFa.reset(); ABa.reset()
        wa = [AFa.alloc(8 * 512) for _ in range(2)]; b_wa = [P.buf("wa_%d" % i, True) for i in range(2)]
        brow = AFa.alloc(512); b_brow = P.buf("brow", True)
        swl = AFa.alloc(8 * 128); b_swl = P.buf("swl", True)
        srep = AFa.alloc(16 * 128); b_srep = P.buf("srep%d" % l)
        for ks in range(16):
            dve.op(lambda e, ks=ks: e.tensor_copy(out=srep[:, ks * 128:(ks + 1) * 128], in_=siluc[:, ks:ks + 1].to_broadcast([128, 128])),
                   reads=[b_siluc], writes=[b_srep])
        sp.dma(lambda e: e.dma_start(out=bcol[:], in_=b_ada_col[l]), b_bcol.sem, writes=[b_bcol])
        sp.dma([lambda e: e.dma_start(out=ncol[:, 0:8], in_=n1col[l]), lambda e: e.dma_start(out=ncol[:, 8:16], in_=n2col[l])], b_ncol.sem, writes=[b_ncol])
        sp.dma(lambda e: e.dma_start(out=lv[:], in_=lamv[l:l + 1, :].to_broadcast([128, 256])), b_lv.sem, writes=[b_lv])
        sp.dma(lambda e: e.dma_start(out=gsub[:], in_=subln_g[l:l + 1, :].to_broadcast([128, 128])), b_gsub.sem, writes=[b_gsub])
        sp.dma(lambda e: e.dma_start(out=sgng[:], in_=sgu_norm_g[l:l + 1, :].to_broadcast([128, 512])), b_sgng.sem, writes=[b_sgng])
        sp.dma(lambda e: e.dma_start(out=sbT[:], in_=sgu_bT[l]), b_sbT.sem, writes=[b_sbT])
        sp.dma(lambda e: e.dma_start(out=wrt[:].rearrange("p (k n) -> p k n", k=8), in_=w_router[l].rearrange("(k p) n -> p k n", p=128)),
               b_wrt.sem, writes=[b_wrt])
        dve.op(lambda e: e.tensor_tensor(out=v3(lprod[:], 2), in0=v3(lv[:], 2)[:, :, 0:64], in1=v3(lv[:], 2)[:, :, 64:128], op=ALU.mult),
               reads=[b_lv], writes=[b_lprod])
        dve.op(lambda e: e.tensor_reduce(out=lamt[:, 0:2], in_=v3(lprod[:], 2), axis=AX.X, op=ALU.add), reads=[b_lprod], writes=[b_lamt])
        act.op(lambda e: e.activation(out=lamt[:, 0:2], in_=lamt[:, 0:2], func=AF.Exp), reads=[b_lamt], writes=[b_lamt])
        dve.op(lambda e: e.tensor_tensor(out=lamt[:, 2:3], in0=lamt[:, 0:1], in1=lamt[:, 1:2], op=ALU.subtract), reads=[b_lamt], writes=[b_lamt])
        dve.op(lambda e: e.tensor_scalar(out=lamt[:, 2:3], in0=lamt[:, 2:3], scalar1=float(lam_init), scalar2=None, op0=ALU.add), reads=[b_lamt], writes=[b_lamt])
        dve.op(lambda e: e.tensor_scalar(out=lamt[:, 3:4], in0=lamt[:, 2:3], scalar1=-1.0, scalar2=None, op0=ALU.mult), reads=[b_lamt], writes=[b_lamt])
        dve.op(lambda e: e.tensor_scalar(out=gsub[:], in0=gsub[:], scalar1=float(1.0 - lam_init), scalar2=None, op0=ALU.mult), reads=[b_gsub], writes=[b_gsub])
        dve.op(lambda e: e.tensor_copy(out=v3(bfull[:], 8), in_=sbT[:].unsqueeze(2).to_broadcast([128, 8, 64])), reads=[b_sbT], writes=[b_bfull])
        for hh in range(2):
            sp.dma(lambda e, hh=hh: e.dma_start(out=v3(swl, 8)[:, 0:4, :], in_=sgu_w[l, hh * 4:(hh + 1) * 4].rearrange("h p q -> p h q")),
                   b_swl.sem, writes=[b_swl])
            for h4 in range(4):
                pe.op(lambda e, h4=h4: e.transpose(out=pbb[:, h4 * 128:(h4 + 1) * 128], in_=v3(swl, 8)[:, h4, :], identity=ident[:]),
                      reads=[b_swl, b_ident], writes=[b_pbb], pe_accum=(h4 > 0))
            act.op(lambda e, hh=hh: e.activation(out=wsT[:, hh * 512:(hh + 1) * 512], in_=pbb[:, 0:512], func=AF.Copy), reads=[b_pbb], writes=[b_wsT])
        for hg in range(12):
            g = hg // 2; half = hg % 2
            wb_ = wa[hg % 2]; bw = b_wa[hg % 2]
            sp.dma(lambda e, wb_=wb_, hg=hg: e.dma_start(out=v3(wb_, 8), in_=w_ada[l].rearrange("(k p) n -> p k n", p=128)[:, :, hg * 512:(hg + 1) * 512]),
                   bw.sem, writes=[bw])
            for nn in range(4):
                n = hg * 4 + nn
                for k in range(8):
                    pe.op(lambda e, wb_=wb_, nn=nn, k=k, n=n: e.matmul(pb[3][:, 2 * n:2 * n + 2], lhsT=v3(wb_, 8)[:, k, nn * 128:(nn + 1) * 128],
                                                                      rhs=siluc[:, 2 * k:2 * k + 2], start=(k == 0), stop=(k == 7)),
                          reads=[bw, b_siluc], writes=[b_pb[3]], pe_accum=not (hg == 0 and nn == 0 and k == 0))
            if g in (2, 5):
                which = 0 if g == 2 else 1
                sp.dma(lambda e, g=g, half=half: e.dma_start(out=brow, in_=b_ada[l:l + 1, g * 1024 + half * 512: g * 1024 + (half + 1) * 512].to_broadcast([128, 512])),
                       b_brow.sem, writes=[b_brow])
                for s in range(2):
                    for k in range(8):
                        pe.op(lambda e, wb_=wb_, s=s, k=k: e.matmul(pb[s][:, :], lhsT=srep[:, (2 * k + s) * 128:(2 * k + s + 1) * 128], rhs=v3(wb_, 8)[:, k, :],
                                                                    start=(k == 0), stop=(k == 7)),
                              reads=[bw, b_srep], writes=[b_pb[s]], pe_accum=(k > 0))
                    c0 = (s * 2 + which) * D + half * 512
                    dve.op(lambda e, s=s, c0=c0: e.tensor_tensor(out=grow[:, c0:c0 + 512], in0=pb[s][:, :], in1=brow, op=ALU.add),
                           reads=[b_pb[s], b_brow], writes=[b_grow])
        dve.op(lambda e: e.tensor_tensor(out=v3(modT[:], 48), in0=v3(pb[3][:, 0:96], 48), in1=bcol[:].unsqueeze(2).to_broadcast([128, 48, 2]), op=ALU.add),
               reads=[b_pb[3], b_bcol], writes=[b_modT])
        mT = v3(modT[:], 48)
        for w_ in range(2):
            for s in range(2):
                sc = mT[:, (1 + 3 * w_) * 8:(2 + 3 * w_) * 8, s]
                o = AB[:, (w_ * 2 + s) * 8:(w_ * 2 + s + 1) * 8]
                dve.op(lambda e, sc=sc, o=o, w_=w_: e.scalar_tensor_tensor(out=o, in0=sc, scalar=1.0, in1=ncol[:, w_ * 8:(w_ + 1) * 8], op0=ALU.add, op1=ALU.mult),
                       reads=[b_modT, b_ncol], writes=[b_AB])

        def Acol(w_, s, k): return AB[:, (w_ * 2 + s) * 8 + k:(w_ * 2 + s) * 8 + k + 1]
        def Bcol(w_, s, k): return modT[:, ((3 * w_) * 8 + k) * 2 + s:((3 * w_) * 8 + k) * 2 + s + 1]
        def growv(s, which): return grow[:, (s * 2 + which) * D:(s * 2 + which + 1) * D]
        P.barrier()

        AFa.reset(); ABa.reset()
        kT = ABa.alloc(4 * T); b_kT = P.buf("kT%d" % l)
        vaug = ABa.alloc(NT * 4 * 130); b_vaug = P.buf("vaug%d" % l)
        ab_mark = ABa.off
        w_in_sb = ABa.alloc(8 * 2560); b_win = P.buf("win", True)
        hT = [ABa.alloc(8 * 128) for _ in range(2)]; b_hT = [P.buf("hT%d_%d" % (l, i)) for i in range(2)]
        qr = ABa.alloc(512); b_qr = P.buf("qr", True)
        kr = ABa.alloc(512); b_kr = P.buf("kr%d" % l)
        gvn = [ABa.alloc(512) for _ in range(2)]; b_gvn = [P.buf("gvn%d_%d" % (l, i)) for i in range(2)]
        sgo = [ABa.alloc(512) for _ in range(2)]; b_sgo = [P.buf("sgo_%d" % i, True) for i in range(2)]
        xt = [AFa.alloc(D) for _ in range(2)]; b_xt = [P.buf("xt_%d" % i, True) for i in range(2)]
        rp = [AFa.alloc(128) for _ in range(3)]; b_rp = [P.buf("rp_%d" % i, True) for i in range(3)]
        xn = AFa.alloc(D); b_xn = P.buf("xn%d" % l)
        junk = AFa.alloc(D); b_junk = P.buf("junk%d" % l)
        t1 = AFa.alloc(512); b_t1 = P.buf("t1%d" % l)
        t2 = AFa.alloc(512); b_t2 = P.buf("t2%d" % l)
        u_sb = [AFa.alloc(512) for _ in range(2)]; b_u = [P.buf("u%d_%d" % (l, i)) for i in range(2)]
        gvg = [AFa.alloc(512) for _ in range(2)]; b_gvg = [P.buf("gvg%d_%d" % (l, i)) for i in range(2)]
        sqA = AFa.alloc(512); b_sqA = P.buf("sqA%d" % l)
        sqB = AFa.alloc(512); b_sqB = P.buf("sqB%d" % l)
        sqC = AFa.alloc(512); b_sqC = P.buf("sqC%d" % l)
        st = small[:, 0:8]; b_stN = P.buf("stN%d" % l)
        stP = [small[:, 8 + 16 * i:24 + 16 * i] for i in range(2)]; b_stP = [P.buf("stP%d_%d" % (l, i)) for i in range(2)]
        pool.dma([lambda e, i=i: e.dma_start(out=v3(w_in_sb, 8)[:, 2 * i:2 * i + 2, :], in_=w_in[l].rearrange("(k p) n -> p k n", p=128)[:, 2 * i:2 * i + 2, :]) for i in range(4)],
                 b_win.sem, writes=[b_win])
        va4 = vaug.rearrange("p (t h c) -> p t h c", t=NT, h=4)
        pool.op(lambda e: e.memset(vaug, 1.0), writes=[b_vaug])

        def load_tile(t):
            i = t % 2; i3 = t % 3
            sp.dma(lambda e: e.dma_start(out=xt[i], in_=xr[t * 128:(t + 1) * 128, :]), b_xt[i].sem, writes=[b_xt[i]])
            sp.dma(lambda e: e.dma_start(out=rp[i3], in_=rope_d[t * 128:(t + 1) * 128, :]), b_rp[i3].sem, writes=[b_rp[i3]])

        def N1(t):
            i = t % 2
            pool.op(lambda e: e.memset(st[:, 0:1], 0.0), writes=[b_stN])
            act.op(lambda e: e.activation(out=junk, in_=xt[i], func=AF.Square, accum_out=st[:, 0:1]), reads=[b_xt[i], b_stN], writes=[b_junk, b_stN])
            rstd_op(st[:, 1:2], st[:, 0:1], 1.0 / D, [b_stN])

        def N2(t):
            i = t % 2
            dve.op(lambda e: e.tensor_scalar(out=xn, in0=xt[i], scalar1=st[:, 1:2], scalar2=None, op0=ALU.mult), reads=[b_xt[i], b_stN], writes=[b_xn])

        def N3(t):
            for k in range(8):
                pe.op(lambda e: e.transpose(out=pbb[:, k * 128:(k + 1) * 128], in_=xn[:, k * 128:(k + 1) * 128], identity=ident[:]),
                      reads=[b_xn, b_ident], writes=[b_pbb], pe_accum=(k > 0))

        def N4(t):
            s_ = 0 if t < NLT else 1
            dst = hT[t % 2]
            for k in range(8):
                act.op(lambda e: e.activation(out=dst[:, k * 128:(k + 1) * 128], in_=pbb[:, k * 128:(k + 1) * 128], func=AF.Identity,
                                              scale=Acol(0, s_, k), bias=Bcol(0, s_, k)),
                       reads=[b_pbb, b_AB, b_modT], writes=[b_hT[t % 2]], nowaw=(k > 0))

        def rope(src_ps, b_src, rp_, b_rp_, dst, b_dst):
            s5 = src_ps.rearrange("p (g a r f) -> p g a r f", g=8, a=2, r=2)
            t5 = t2.rearrange("p (g a r f) -> p g a r f", g=8, a=2, r=2)
            sn = rp_[:, 64:128].rearrange("p (a r f) -> p a r f", a=2, r=2)
            dve.op(lambda e: e.tensor_tensor(out=v3(t1, 8), in0=v3(src_ps, 8), in1=rp_[:, 0:64].unsqueeze(1).to_broadcast([128, 8, 64]), op=ALU.mult),
                   reads=[b_src, b_rp_], writes=[b_t1])
            for a in range(2):
                for r in range(2):
                    dve.op(lambda e: e.tensor_tensor(out=t5[:, :, a, r, :], in0=s5[:, :, a, 1 - r, :],
                                                     in1=sn[:, a, r, :].unsqueeze(1).to_broadcast([128, 8, 16]), op=ALU.mult),
                           reads=[b_src, b_rp_], writes=[b_t2], nowaw=(a + r > 0))
            dve.op(lambda e: e.tensor_tensor(out=dst, in0=t1, in1=t2, op=ALU.add), reads=[b_t1, b_t2], writes=[b_dst])

        prot = [0, 1, 3, 4]; pcnt = [0]

        def proj(t, g):
            bi = prot[pcnt[0] % 4]; pcnt[0] += 1
            hsrc = hT[t % 2]
            for k in range(8):
                pe.op(lambda e: e.matmul(pb[bi][:, :], lhsT=hsrc[:, k * 128:(k + 1) * 128], rhs=v3(w_in_sb, 8)[:, k, g * 512:(g + 1) * 512],
                                         start=(k == 0), stop=(k == 7)),
                      reads=[b_hT[t % 2], b_win], writes=[b_pb[bi]], pe_accum=(k > 0))
            return pb[bi], b_pb[bi]

        load_tile(0); load_tile(1)
        N1(0); N2(0); N3(0); N4(0)
        pend = []

        def pop_pend():
            if pend:
                pend.pop(0)()
        for t in range(NT):
            nxt = t + 1 < NT
            i3 = t % 3
            need_q = t < NTq
            if t + 2 < NT:
                load_tile(t + 2)
            if nxt: N1(t + 1)
            pop_pend()
            if need_q:
                pbk, bpbk = proj(t, 0)
                rope(pbk[:, :], bpbk, rp[i3], b_rp[i3], qr, b_qr)
                sp.dma(lambda e: e.dma_start(out=qd[t * 128:(t + 1) * 128, :], in_=qr), b_qr.sem, reads=[b_qr])
            pop_pend()
            if nxt: N2(t + 1)
            pbk, bpbk = proj(t, 1)
            pop_pend()
            rope(pbk[:, :], bpbk, rp[i3], b_rp[i3], kr, b_kr)
            pop_pend()
            for j in range(4):
                pe.op(lambda e: e.transpose(out=pbh[:, j * 128:(j + 1) * 128], in_=kr[:, j * 128:(j + 1) * 128], identity=identb[:]),
                      reads=[b_kr, b_identb], writes=[b_pbh], pe_accum=(j > 0))
            act.op(lambda e: e.activation(out=v3(kT, 4)[:, :, t * 128:(t + 1) * 128], in_=v3(pbh[:, 0:512], 4), func=AF.Copy),
                   reads=[b_pbh], writes=[b_kT])
            if nxt: N3(t + 1)
            pbk, bpbk = proj(t, 2)
            act.op(lambda e: e.activation(out=va4[:, t, :, 0:128], in_=v3(pbk[:, :], 4), func=AF.Copy), reads=[bpbk], writes=[b_vaug])
            if nxt: N4(t + 1)
            if need_q:
                pr = t % 2
                pbk, bpbk = proj(t, 3)
                act.op(lambda e: e.activation(out=u_sb[pr], in_=pbk[:, :], func=AF.Gelu), reads=[bpbk], writes=[b_u[pr]])
                pbk, bpbk = proj(t, 4)
                act.op(lambda e: e.activation(out=gvg[pr], in_=pbk[:, :], func=AF.Gelu), reads=[bpbk], writes=[b_gvg[pr]])
                dve.op(lambda e: e.tensor_tensor(out=sqA, in0=gvg[pr], in1=gvg[pr], op=ALU.mult), reads=[b_gvg[pr]], writes=[b_sqA])
                dve.op(lambda e: e.tensor_reduce(out=stP[pr][:, 0:8], in_=v3(sqA, 8), axis=AX.X, op=ALU.add), reads=[b_sqA], writes=[b_stP[pr]])

                def T1(t=t, pr=pr):
                    rstd_op(stP[pr][:, 8:16], stP[pr][:, 0:8], 1.0 / 64, [b_stP[pr]])

                def T2(t=t, pr=pr):
                    dve.op(lambda e: e.tensor_tensor(out=v3(sqB, 8), in0=v3(gvg[pr], 8), in1=stP[pr][:, 8:16].unsqueeze(2).to_broadcast([128, 8, 64]), op=ALU.mult),
                           reads=[b_gvg[pr], b_stP[pr]], writes=[b_sqB])
                    dve.op(lambda e: e.tensor_tensor(out=gvn[pr], in0=sqB, in1=sgng[:], op=ALU.mult), reads=[b_sqB, b_sgng], writes=[b_gvn[pr]])

                def T3(t=t, pr=pr):
                    for h in range(8):
                        pe.op(lambda e: e.matmul(pb[2][:, h * 64:(h + 1) * 64], lhsT=wsT[:, h * 128:(h + 1) * 128], rhs=gvn[pr][:, h * 64:(h + 1) * 64],
                                                 start=True, stop=True),
                              reads=[b_wsT, b_gvn[pr]], writes=[b_pb[2]], pe_accum=(h > 0))

                def T4(t=t, pr=pr):
                    dve.op(lambda e: e.tensor_tensor(out=sqC, in0=pb[2][:, :], in1=bfull[:], op=ALU.add), reads=[b_pb[2], b_bfull], writes=[b_sqC])
                    dve.op(lambda e: e.tensor_tensor(out=sgo[pr], in0=sqC, in1=u_sb[pr], op=ALU.mult), reads=[b_sqC, b_u[pr]], writes=[b_sgo[pr]])
                    sp.dma(lambda e: e.dma_start(out=sgd[t * 128:(t + 1) * 128, :], in_=sgo[pr]), b_sgo[pr].sem, reads=[b_sgo[pr]])
                pend.extend([T1, T2, T3, T4])
        while pend:
            pend.pop(0)()
        P.barrier()

        AFa.reset(); ABa.off = ab_mark
        w_out_sb = ABa.alloc(8 * D); b_wout = P.buf("wout", True)
        qg = ABa.alloc(4 * 512); b_qg = P.buf("qg", True)
        qTm = [ABa.alloc(8 * 512) for _ in range(2)]; b_qTm = [P.buf("qTm%d_%d" % (l, i)) for i in range(2)]
        pT = [ABa.alloc(512) for _ in range(3)]; b_pT = [P.buf("pT%d_%d" % (l, i)) for i in range(3)]
        mixo = [[ABa.alloc(512) for _ in range(4)] for _ in range(2)]
        b_mixo = [[P.buf("mixo%d_%d_%d" % (l, pp, i)) for i in range(4)] for pp in range(2)]
        mixT = ABa.alloc(8 * 128); b_mixT = P.buf("mixT%d" % l)
        sgi = [ABa.alloc(512) for _ in range(2)]; b_sgi = [P.buf("sgi_%d" % i, True) for i in range(2)]
        h2 = ABa.alloc(D); b_h2 = P.buf("h2", True)
        affT = AFa.alloc(T); b_affT = P.buf("affT%d" % l)
        af_mark = AFa.off
        xt = [AFa.alloc(D) for _ in range(2)]; b_xt = [P.buf("xt_%d" % i, True) for i in range(2)]
        x1 = AFa.alloc(D); b_x1 = P.buf("x1", True)
        xn = AFa.alloc(D); b_xn = P.buf("xnc%d" % l)
        junk = AFa.alloc(D); b_junk = P.buf("junkc%d" % l)
        h2T = AFa.alloc(8 * 128); b_h2T = P.buf("h2T%d" % l)
        accS = AFa.alloc(8 * 130); b_accS = P.buf("accS%d" % l)
        t1 = AFa.alloc(128); b_t1 = P.buf("t1c%d" % l)
        osb = [AFa.alloc(128) for _ in range(4)]; b_osb = [P.buf("osb%d_%d" % (l, i)) for i in range(4)]
        af = AFa.alloc(NE); b_af = P.buf("af%d" % l)
        st = small[:, 0:32]
        b_stT = P.buf("stT%d" % l); b_stE = P.buf("stE%d" % l); b_stE2 = P.buf("stE2%d" % l)
        b_pbR = b_pbh; b_pbAf = b_pbh
        pbL = pbb[:, 0:512]; b_pbL = Buf("pbL%d" % l, excl=True)
        Sb = [(pb[0], b_pb[0]), (pb[1], b_pb[1]), (pbb[:, 512:1024], Buf("pbS2%d" % l, excl=True))]
        pool.dma(lambda e: e.dma_start(out=v3(w_out_sb, 8), in_=w_out[l].rearrange("(k p) n -> p k n", p=128)), b_wout.sem, writes=[b_wout])
        for pp in range(2):
            pool.op(lambda e: e.memset(qTm[pp], 0.0), writes=[b_qTm[pp]])

        def acc_ap(a):
            bank = 2 + a // 3; off = (a % 3) * 160
            return pb[bank][:, off:off + 130], b_pb[bank]

        qgroups = [(G * 4, 4) for G in range(8)] + ([] if last else [(NLT, 2)])

        def build_q(gi):
            t0, nq = qgroups[gi]; par = gi % 2
            ths = []

            def ld():
                sp.dma(lambda e: e.dma_start(out=v3(qg, 4)[:, 0:nq, :], in_=qd[t0 * 128:(t0 + nq) * 128, :].rearrange("(a p) n -> p a n", p=128)),
                       b_qg.sem, writes=[b_qg])
            ths.append(ld)
            for qi in range(nq):
                def f(qi=qi):
                    for j in range(4):
                        pe.op(lambda e: e.transpose(out=pbh[:, j * 128:(j + 1) * 128], in_=v3(qg, 4)[:, qi, j * 128:(j + 1) * 128], identity=identb[:]),
                              reads=[b_qg, b_identb], writes=[b_pbh], pe_accum=(j > 0))
                    for hf in range(2):
                        dst = qTm[par].rearrange("p (j f q) -> p j f q", j=4, f=2)[hf * 64:(hf + 1) * 64, :, hf, qi * 128:(qi + 1) * 128]
                        src = v3(pbh[:, 0:512], 4)[hf * 64:(hf + 1) * 64, :, :]
                        act.op(lambda e: e.activation(out=dst, in_=src, func=AF.Copy), reads=[b_pbh], writes=[b_qTm[par]], nowaw=True)
                ths.append(f)
            return ths

        def tile_thunks(gi, qi):
            t0, nq = qgroups[gi]; par = gi % 2
            t = t0 + qi; ii = t % 2
            s_ = 0 if t0 < NLT else 1
            mo = mixo[par][qi]; bmo = b_mixo[par][qi]
            ths = []

            def s0():
                sp.dma(lambda e: e.dma_start(out=sgi[ii], in_=sgd[t * 128:(t + 1) * 128, :]), b_sgi[ii].sem, writes=[b_sgi[ii]])
                sp.dma(lambda e: e.dma_start(out=xt[ii], in_=xr[t * 128:(t + 1) * 128, :]), b_xt[ii].sem, writes=[b_xt[ii]])
            ths.append(s0)

            def s1(half):
                srcb, bsrc = [(mo, bmo), (sgi[ii], b_sgi[ii])][half]
                for j in range(4):
                    pe.op(lambda e: e.transpose(out=pbh[:, j * 128:(j + 1) * 128], in_=srcb[:, j * 128:(j + 1) * 128], identity=identb[:]),
                          reads=[bsrc, b_identb], writes=[b_pbh], pe_accum=(j > 0))
                dve.op(lambda e: e.tensor_copy(out=mixT[:, half * 512:(half + 1) * 512], in_=pbh[:, 0:512]), reads=[b_pbh], writes=[b_mixT], nowaw=(half > 0))
            ths.append(lambda: s1(0))
            ths.append(lambda: s1(1))

            def s2(hv, fh):
                for f in range(fh * 4, fh * 4 + 4):
                    pe.op(lambda e: e.matmul(pbL[:, :], lhsT=mixT[:, f * 128:(f + 1) * 128],
                                             rhs=v3(w_out_sb, 8)[:, f, hv * 512:(hv + 1) * 512], start=(f == 0), stop=(f == 7)),
                          reads=[b_mixT, b_wout], writes=[b_pbL], pe_accum=(f > 0))

            def s3(hv):
                c0 = hv * 512
                dve.op(lambda e: e.tensor_tensor(out=x1[:, c0:c0 + 512], in0=pbL[:, :], in1=growv(s_, 0)[:, c0:c0 + 512], op=ALU.mult), reads=[b_pbL, b_grow], writes=[b_x1])
                dve.op(lambda e: e.tensor_tensor(out=x1[:, c0:c0 + 512], in0=x1[:, c0:c0 + 512], in1=xt[ii][:, c0:c0 + 512], op=ALU.add), reads=[b_x1, b_xt[ii]], writes=[b_x1])
                if hv == 1:
                    sp.dma(lambda e: e.dma_start(out=xr[t * 128:(t + 1) * 128, :], in_=x1), b_x1.sem, reads=[b_x1])
                    pool.op(lambda e: e.memset(st[:, 0:1], 0.0), writes=[b_stT])
            for hv in range(2):
                ths.append(lambda hv=hv: s2(hv, 0))
                ths.append(lambda hv=hv: s2(hv, 1))
                ths.append(lambda hv=hv: s3(hv))

            def s4a():
                act.op(lambda e: e.activation(out=junk, in_=x1, func=AF.Square, accum_out=st[:, 0:1]), reads=[b_x1, b_stT], writes=[b_junk, b_stT])
            ths.append(s4a)

            def s4():
                rstd_op(st[:, 1:2], st[:, 0:1], 1.0 / D, [b_stT])
                dve.op(lambda e: e.tensor_scalar(out=xn, in0=x1, scalar1=st[:, 1:2], scalar2=None, op0=ALU.mult), reads=[b_x1, b_stT], writes=[b_xn])
            ths.append(s4)

            def s5(hh):
                for k4 in range(4):
                    k = hh * 4 + k4
                    pe.op(lambda e: e.transpose(out=pbL[:, k4 * 128:(k4 + 1) * 128], in_=xn[:, k * 128:(k + 1) * 128], identity=ident[:]),
                          reads=[b_xn, b_ident], writes=[b_pbL], pe_accum=(k4 > 0))

            def s6(hh):
                ia = (1 * 2 + s_) * 8 + hh * 4
                Ab = AB[:, ia:ia + 4].unsqueeze(2).to_broadcast([128, 4, 128])
                Bb = v3(modT[:], 48)[:, 24 + hh * 4:28 + hh * 4, s_].unsqueeze(2).to_broadcast([128, 4, 128])
                hv_ = v3(h2T, 8)[:, hh * 4:(hh + 1) * 4, :]
                dve.op(lambda e: e.tensor_tensor(out=hv_, in0=v3(pbL[:, :], 4), in1=Ab, op=ALU.mult), reads=[b_pbL, b_AB], writes=[b_h2T])
                dve.op(lambda e: e.tensor_tensor(out=hv_, in0=hv_, in1=Bb, op=ALU.add), reads=[b_h2T, b_modT], writes=[b_h2T])
            for hh in range(2):
                ths.append(lambda hh=hh: s5(hh))
                ths.append(lambda hh=hh: s6(hh))

            def s7a():
                for k in range(8):
                    pe.op(lambda e: e.matmul(pbr[:, 0:NE], lhsT=h2T[:, k * 128:(k + 1) * 128], rhs=wrt[:, k * NE:(k + 1) * NE], start=(k == 0), stop=(k == 7)),
                          reads=[b_h2T, b_wrt], writes=[b_pbR], pe_accum=(k > 0))
            ths.append(s7a)

            def s7(hh):
                for k4 in range(4):
                    k = hh * 4 + k4
                    pe.op(lambda e: e.transpose(out=pbL[:, k4 * 128:(k4 + 1) * 128], in_=h2T[:, k * 128:(k + 1) * 128], identity=ident[:]),
                          reads=[b_h2T, b_ident], writes=[b_pbL], pe_accum=(k4 > 0))

            def s7c(hh):
                dve.op(lambda e: e.tensor_copy(out=h2[:, hh * 512:(hh + 1) * 512], in_=pbL[:, :]), reads=[b_pbL], writes=[b_h2])
                if hh == 1:
                    sp.dma(lambda e: e.dma_start(out=h2d[t * 128:(t + 1) * 128, :], in_=h2), b_h2.sem, reads=[b_h2])
            for hh in range(2):
                ths.append(lambda hh=hh: s7(hh))
                ths.append(lambda hh=hh: s7c(hh))

            def s8():
                dve.op(lambda e: e.tensor_reduce(out=st[:, 5:6], in_=pbr[:, 0:NE], axis=AX.X, op=ALU.max), reads=[b_pbR], writes=[b_stT])
                dve.op(lambda e: e.tensor_scalar(out=st[:, 5:6], in0=st[:, 5:6], scalar1=-1.0, scalar2=None, op0=ALU.mult), reads=[b_stT], writes=[b_stT])
                pool.op(lambda e: e.memset(st[:, 6:7], 0.0), writes=[b_stT])
                act.op(lambda e: e.activation(out=af, in_=pbr[:, 0:NE], func=AF.Exp, bias=st[:, 5:6], scale=1.0, accum_out=st[:, 6:7]),
                       reads=[b_pbR, b_stT], writes=[b_af, b_stT])
            ths.append(s8)

            def s9():
                dve.op(lambda e: e.reciprocal(out=st[:, 6:7], in_=st[:, 6:7]), reads=[b_stT], writes=[b_stT])
                dve.op(lambda e: e.tensor_scalar(out=af, in0=af, scalar1=st[:, 6:7], scalar2=None, op0=ALU.mult), reads=[b_af, b_stT], writes=[b_af])
                pe.op(lambda e: e.transpose(out=pbr[0:NE, 128:256], in_=af, identity=ident[:]), reads=[b_af, b_ident], writes=[b_pbAf])
            ths.append(s9)

            def s10():
                dve.op(lambda e: e.tensor_copy(out=affT[0:NE, t * 128:(t + 1) * 128], in_=pbr[0:NE, 128:256]), reads=[b_pbAf], writes=[b_affT])
            ths.append(s10)
            return ths

        from collections import deque
        bg = deque()
        for th in build_q(0):
            th()
        a8 = v3(accS, 8)
        pti = 0
        for gi, (t0, nq) in enumerate(qgroups):
            par = gi % 2
            s = 0 if t0 < NLT else 1
            NQ = nq * 128
            kts = list(range(NT)) if s == 0 else [NLT, NLT + 1]
            if gi + 1 < len(qgroups):
                bg.extendleft(reversed(build_q(gi + 1)))
            iters = [(h, c, ki, kt) for h in range(4) for c in range(2) for ki, kt in enumerate(kts)]
            timed = []

            def emit_S(idx):
                h, c, ki, kt = iters[idx]; sbt, bsb = Sb[idx % 3]
                i8 = c * 4 + h; j = i8 // 2
                pe.op(lambda e: e.matmul(sbt[:, 0:NQ], lhsT=v3(kT, 4)[:, j, kt * 128:(kt + 1) * 128], rhs=v3(qTm[par], 8)[:, i8, 0:NQ], start=True, stop=True),
                      reads=[b_kT, b_qTm[par]], writes=[bsb])

            def E1(h):
                for bank in range(3):
                    na = 3 if bank < 2 else 2
                    src = pb[2 + bank][:, 0:na * 160].rearrange("p (a c) -> p a c", a=na)[:, :, 0:130]
                    dve.op(lambda e: e.tensor_copy(out=a8[:, bank * 3:bank * 3 + na, :], in_=src), reads=[b_pb[2 + bank]], writes=[b_accS])
                for qi in range(nq):
                    a0 = a8[:, qi, :]; a1 = a8[:, 4 + qi, :]
                    dve.op(lambda e: e.reciprocal(out=st[:, 2:3], in_=a0[:, 128:129]), reads=[b_accS], writes=[b_stE])
                    dve.op(lambda e: e.reciprocal(out=st[:, 3:4], in_=a1[:, 128:129]), reads=[b_accS], writes=[b_stE])
                    dve.op(lambda e: e.tensor_tensor(out=st[:, 3:4], in0=st[:, 3:4], in1=lamt[:, 3:4], op=ALU.mult), reads=[b_stE, b_lamt], writes=[b_stE])
                    dve.op(lambda e: e.tensor_scalar(out=t1, in0=a1[:, 0:128], scalar1=st[:, 3:4], scalar2=None, op0=ALU.mult), reads=[b_accS, b_stE], writes=[b_t1])
                    dve.op(lambda e: e.scalar_tensor_tensor(out=osb[qi], in0=a0[:, 0:128], scalar=st[:, 2:3], in1=t1, op0=ALU.mult, op1=ALU.add),
                           reads=[b_accS, b_stE, b_t1], writes=[b_osb[qi]])
                    dve.op(lambda e: e.tensor_tensor(out=t1, in0=osb[qi], in1=osb[qi], op=ALU.mult), reads=[b_osb[qi]], writes=[b_t1])
                    dve.op(lambda e: e.tensor_reduce(out=st[:, 8 + qi:9 + qi], in_=t1, axis=AX.X, op=ALU.add), reads=[b_t1], writes=[b_stE2])

            def E2(h):
                rstd_op(st[:, 8:8 + nq], st[:, 8:8 + nq], 1.0 / 128, [b_stE2])

            def E3(h):
                for qi in range(nq):
                    dve.op(lambda e: e.scalar_tensor_tensor(out=mixo[par][qi][:, h * 128:(h + 1) * 128], in0=osb[qi], scalar=st[:, 8 + qi:9 + qi], in1=gsub[:], op0=ALU.mult, op1=ALU.mult),
                           reads=[b_osb[qi], b_stE2, b_gsub], writes=[b_mixo[par][qi]])

            import os as _os
            NOLOOK = _os.environ.get("KNOLOOK") == "1"; NOBG = _os.environ.get("KNOBG") == "1"
            if not NOLOOK:
                emit_S(0); emit_S(1)
            for idx in range(len(iters)):
                if NOLOOK:
                    emit_S(idx)
                elif idx + 2 < len(iters):
                    emit_S(idx + 2)
                h, c, ki, kt = iters[idx]; sbt, bsb = Sb[idx % 3]
                pi = pti % 3; pti += 1
                act.op(lambda e: e.activation(out=pT[pi][:, 0:NQ], in_=sbt[:, 0:NQ], func=AF.Exp, scale=0.125),
                       reads=[bsb], writes=[b_pT[pi]])
                banks_seen = set()
                for qi in range(nq):
                    a_ = c * 4 + qi
                    aap, bacc = acc_ap(a_)
                    first_in_bank = (a_ // 3) not in banks_seen
                    banks_seen.add(a_ // 3)
                    pe.op(lambda e: e.matmul(aap, lhsT=pT[pi][:, qi * 128:(qi + 1) * 128], rhs=va4[:, kt, h, :],
                                             start=(ki == 0 and first_in_bank), stop=(ki == len(kts) - 1), skip_group_check=True),
                          reads=[b_pT[pi], b_vaug], writes=[bacc], pe_accum=(ki > 0))
                if c == 1 and ki == len(kts) - 1:
                    while timed:
                        timed.pop(0)[1]()
                    E1(h)
                    timed.append((idx + 18, lambda h=h: E2(h)))
                    timed.append((idx + 26, lambda h=h: E3(h)))
                while timed and timed[0][0] <= idx:
                    timed.pop(0)[1]()
                if idx % 2 == 1 and bg and not NOBG:
                    bg.popleft()()
            while timed:
                timed.pop(0)[1]()
            while NOBG and bg:
                bg.popleft()()
            for qi in range(nq):
                bg.extend(tile_thunks(gi, qi))
        while bg:
            bg.popleft()()
        P.barrier()

        AFa.off = af_mark; ABa.reset()
        NTOK = CAP if last else CAP + CCAP
        NJ = 4 if last else 5
        work = affT[:, 0:SEQ]; b_work = b_affT
        workc = affT[:, SEQ:T]
        vals = AFa.alloc(NTOK); b_vals = P.buf("vals%d" % l)
        idxu = AFa.alloc(NTOK).bitcast(U32); b_idxu = P.buf("idxu%d" % l)
        idxf = AFa.alloc(NTOK); b_idxf = P.buf("idxf%d" % l)
        gT = AFa.alloc(5 * NE); b_gT = P.buf("gT%d" % l)
        idxT = AFa.alloc(5 * NE).bitcast(I32); b_idxT = P.buf("idxT%d" % l)
        sgs = [AFa.alloc(544) for _ in range(2)]; b_sgs = [P.buf("sgs%d_%d" % (l, i)) for i in range(2)]
        ysc = AFa.alloc(5 * D); b_ysc = P.buf("ysc%d" % l)
        ring = [ABa.alloc(8192) for _ in range(3)]; b_ring = [P.buf("ring_%d" % i, True) for i in range(3)]
        xs = [ABa.alloc(5 * D) for _ in range(2)]; b_xs = [P.buf("xs_%d" % i, True) for i in range(2)]
        xsT = ABa.alloc(8 * 544); b_xsT = P.buf("xsT%d" % l)
        hidT = ABa.alloc(16 * 544); b_hidT = P.buf("hidT%d" % l)
        for r in range(CAP // 8):
            dve.op(lambda e, r=r: e.max(out=vals[0:NE, r * 8:(r + 1) * 8], in_=work[0:NE, :]), reads=[b_work], writes=[b_vals])
            dve.op(lambda e, r=r: e.max_index(out=idxu[0:NE, r * 8:(r + 1) * 8], in_max=vals[0:NE, r * 8:(r + 1) * 8], in_values=work[0:NE, :]),
                   reads=[b_work, b_vals], writes=[b_idxu])
            if r < CAP // 8 - 1:
                dve.op(lambda e, r=r: e.match_replace(out=work[0:NE, :], in_to_replace=vals[0:NE, r * 8:(r + 1) * 8], in_values=work[0:NE, :], imm_value=-1.0),
                       reads=[b_vals, b_work], writes=[b_work])
        if not last:
            for r in range(CCAP // 8):
                c0 = CAP + r * 8
                dve.op(lambda e, c0=c0: e.max(out=vals[0:NE, c0:c0 + 8], in_=workc[0:NE, :]), reads=[b_work], writes=[b_vals])
                dve.op(lambda e, c0=c0: e.max_index(out=idxu[0:NE, c0:c0 + 8], in_max=vals[0:NE, c0:c0 + 8], in_values=workc[0:NE, :]),
                       reads=[b_work, b_vals], writes=[b_idxu])
                if r < CCAP // 8 - 1:
                    dve.op(lambda e, c0=c0: e.match_replace(out=workc[0:NE, :], in_to_replace=vals[0:NE, c0:c0 + 8], in_values=workc[0:NE, :], imm_value=-1.0),
                           reads=[b_vals, b_work], writes=[b_work])
        dve.op(lambda e: e.tensor_copy(out=idxf[0:NE, :], in_=idxu[0:NE, :]), reads=[b_idxu], writes=[b_idxf])
        if not last:
            dve.op(lambda e: e.tensor_scalar(out=idxf[0:NE, CAP:NTOK], in0=idxf[0:NE, CAP:NTOK], scalar1=float(SEQ), scalar2=None, op0=ALU.add),
                   reads=[b_idxf], writes=[b_idxf])
        for (srcv, bsrc, dstv, bdst) in ((idxf, b_idxf, idxT, b_idxT), (vals, b_vals, gT, b_gT)):
            for jc in range(NJ):
                nj = 128 if jc < 4 else CCAP
                pe.op(lambda e, jc=jc, nj=nj, srcv=srcv: e.transpose(out=pb[4][0:nj, jc * NE:(jc + 1) * NE], in_=srcv[0:NE, jc * 128:jc * 128 + nj], identity=ident[0:NE, 0:NE]),
                      reads=[bsrc, b_ident], writes=[b_pb[4]], pe_accum=(jc > 0))
            dve.op(lambda e, dstv=dstv: e.tensor_copy(out=dstv[:, 0:4 * NE], in_=pb[4][:, 0:4 * NE]), reads=[b_pb[4]], writes=[bdst])
            if not last:
                dve.op(lambda e, dstv=dstv: e.tensor_copy(out=dstv[0:CCAP, 4 * NE:5 * NE], in_=pb[4][0:CCAP, 4 * NE:5 * NE]), reads=[b_pb[4]], writes=[bdst])

        def gather(e_):
            xb = xs[e_ % 2]; bx = b_xs[e_ % 2]
            NJs = [(jc, 128 if jc < 4 else CCAP) for jc in range(NJ)]
            pool.dma([lambda e, jc=jc, nj=nj: e.indirect_dma_start(
                out=xb[0:nj, jc * D:(jc + 1) * D], out_offset=None, in_=h2d[:, :],
                in_offset=bass.IndirectOffsetOnAxis(ap=idxT[0:nj, jc * NE + e_: jc * NE + e_ + 1], axis=0)) for (jc, nj) in NJs],
                bx.sem, reads=[b_idxT], writes=[bx])

        pieces = [(e_, p_) for e_ in range(NE) for p_ in range(6)]

        def load_piece(pi_):
            e_, p_ = pieces[pi_]
            rb = ring[pi_ % 3]; brb = b_ring[pi_ % 3]
            if p_ < 4:
                pool.dma([lambda e: e.dma_start(out=v3(rb[:, 0:4096], 8), in_=w_gate[l, e_].rearrange("(k p) n -> p k n", p=128)[:, :, p_ * 512:(p_ + 1) * 512]),
                          lambda e: e.dma_start(out=v3(rb[:, 4096:8192], 8), in_=w_up[l, e_].rearrange("(k p) n -> p k n", p=128)[:, :, p_ * 512:(p_ + 1) * 512])],
                         brb.sem, writes=[brb])
            else:
                hv = p_ - 4
                pool.dma([lambda e, q_=q_: e.dma_start(
                    out=v3(rb[:, q_ * 4096:(q_ + 1) * 4096], 8),
                    in_=w_down[l, e_].rearrange("(f p) n -> p f n", p=128)[:, q_ * 8:(q_ + 1) * 8, hv * 512:(hv + 1) * 512]) for q_ in range(2)],
                    brb.sem, writes=[brb])

        b_xrs = P.buf("xrs", True)
        gather(0)
        load_piece(0); load_piece(1)
        sgi_ = [0]
        for e_ in range(NE):
            xb = xs[e_ % 2]; bx = b_xs[e_ % 2]
            if e_ + 1 < NE:
                gather(e_ + 1)
            for jc in range(NJ):
                nj = 128 if jc < 4 else CCAP
                for kh in range(2):
                    for k4 in range(4):
                        k = kh * 4 + k4
                        pe.op(lambda e: e.transpose(out=pbh[:, k4 * 128:k4 * 128 + nj], in_=xb[0:nj, jc * D + k * 128: jc * D + (k + 1) * 128],
                                                    identity=identb[0:nj, 0:nj]),
                              reads=[bx, b_identb], writes=[b_pbh], pe_accum=(k4 > 0))
                    act.op(lambda e: e.activation(out=v3(xsT, 8)[:, kh * 4:(kh + 1) * 4, jc * 128:jc * 128 + nj], in_=v3(pbh[:, :], 4)[:, :, 0:nj], func=AF.Copy),
                           reads=[b_pbh], writes=[b_xsT])
            for p_ in range(6):
                pi_ = e_ * 6 + p_
                if pi_ + 2 < len(pieces):
                    load_piece(pi_ + 2)
                rb = ring[pi_ % 3]; brb = b_ring[pi_ % 3]
                if p_ < 4:
                    for fi in range(4):
                        f = p_ * 4 + fi
                        gb = fi % 2
                        for wi, (woff, pbt, bpbt) in enumerate(((0, pb[gb], b_pb[gb]), (4096, pb[2 + gb], b_pb[2 + gb]))):
                            for k in range(8):
                                pe.op(lambda e, rb=rb, woff=woff, k=k, fi=fi, pbt=pbt: e.matmul(pbt[:, 0:CAP], lhsT=v3(rb[:, woff:woff + 4096], 8)[:, k, fi * 128:(fi + 1) * 128],
                                                                                              rhs=v3(xsT, 8)[:, k, 0:CAP], start=(k == 0), stop=(k == 7)),
                                      reads=[brb, b_xsT], writes=[bpbt], pe_accum=(k > 0))
                            if not last:
                                co = gb * 64 + wi * 32
                                for k in range(8):
                                    pe.op(lambda e, rb=rb, woff=woff, k=k, fi=fi, co=co: e.matmul(pb[4][:, co:co + CCAP], lhsT=v3(rb[:, woff:woff + 4096], 8)[:, k, fi * 128:(fi + 1) * 128],
                                                                                                rhs=v3(xsT, 8)[:, k, CAP:NTOK], start=(k == 0), stop=(k == 7)),
                                          reads=[brb, b_xsT], writes=[b_pb[4]], pe_accum=(k > 0))
                        sg_ = sgs[sgi_[0] % 2]; bsg = b_sgs[sgi_[0] % 2]; sgi_[0] += 1
                        act.op(lambda e, gb=gb, sg_=sg_: e.activation(out=sg_[:, 0:CAP], in_=pb[gb][:, 0:CAP], func=AF.Silu), reads=[b_pb[gb]], writes=[bsg])
                        dve.op(lambda e, gb=gb, sg_=sg_, f=f: e.tensor_tensor(out=v3(hidT, 16)[:, f, 0:CAP], in0=sg_[:, 0:CAP], in1=pb[2 + gb][:, 0:CAP], op=ALU.mult),
                               reads=[bsg, b_pb[2 + gb]], writes=[b_hidT])
                        if not last:
                            co = gb * 64
                            act.op(lambda e, co=co, sg_=sg_: e.activation(out=sg_[:, CAP:NTOK], in_=pb[4][:, co:co + CCAP], func=AF.Silu), reads=[b_pb[4]], writes=[bsg])
                            dve.op(lambda e, co=co, sg_=sg_, f=f: e.tensor_tensor(out=v3(hidT, 16)[:, f, CAP:NTOK], in0=sg_[:, CAP:NTOK], in1=pb[4][:, co + 32:co + 32 + CCAP], op=ALU.mult),
                                   reads=[bsg, b_pb[4]], writes=[b_hidT])
                else:
                    hv = p_ - 4
                    for jc in range(NJ):
                        nj = 128 if jc < 4 else CCAP
                        yb = jc % 2
                        for f in range(16):
                            pe.op(lambda e, rb=rb, f=f, jc=jc, nj=nj, yb=yb: e.matmul(pbb[0:nj, yb * 512:(yb + 1) * 512], lhsT=v3(hidT, 16)[:, f, jc * 128:jc * 128 + nj],
                                                                                    rhs=v3(rb[:, (f // 8) * 4096:(f // 8 + 1) * 4096], 8)[:, f % 8, :], start=(f == 0), stop=(f == 15)),
                                  reads=[b_hidT, brb], writes=[b_pbb], pe_accum=(f > 0))
                        s = 0 if jc < 4 else 1
                        dve.op(lambda e, jc=jc, nj=nj, yb=yb, hv=hv, s=s, e_=e_: e.scalar_tensor_tensor(
                            out=ysc[0:nj, jc * D + hv * 512: jc * D + (hv + 1) * 512], in0=pbb[0:nj, yb * 512:(yb + 1) * 512],
                            scalar=gT[0:nj, jc * NE + e_: jc * NE + e_ + 1], in1=growv(s, 1)[0:nj, hv * 512:(hv + 1) * 512], op0=ALU.mult, op1=ALU.mult),
                            reads=[b_pbb, b_gT, b_grow], writes=[b_ysc])
            NJs = [(jc, 128 if jc < 4 else CCAP) for jc in range(NJ)]
            pool.dma([lambda e, jc=jc, nj=nj: e.indirect_dma_start(
                out=xr[:, :], out_offset=bass.IndirectOffsetOnAxis(ap=idxT[0:nj, jc * NE + e_: jc * NE + e_ + 1], axis=0),
                in_=ysc[0:nj, jc * D:(jc + 1) * D], in_offset=None, compute_op=ALU.add) for (jc, nj) in NJs],
                b_xrs.sem, reads=[b_ysc, b_idxT, b_xrs], writes=[b_xrs])
        P.barrier()

    AFa.reset(); ABa.reset()
    gf = AFa.alloc(D); b_gf = P.buf("gf", True)
    xt = [AFa.alloc(D) for _ in range(2)]; b_xt = [P.buf("xt_%d" % i, True) for i in range(2)]
    yo = [AFa.alloc(D) for _ in range(2)]; b_yo = [P.buf("yo%d" % i, True) for i in range(2)]
    junk = AFa.alloc(D); b_junk = P.buf("junkf")
    st = small[:, 32:40]; b_st = P.buf("stf")
    sp.dma(lambda e: e.dma_start(out=gf, in_=norm_f_g[0:1, :].to_broadcast([128, D])), b_gf.sem, writes=[b_gf])
    for t in range(NLT):
        i = t % 2
        sp.dma(lambda e, t=t, i=i: e.dma_start(out=xt[i], in_=xr[t * 128:(t + 1) * 128, :]), b_xt[i].sem, writes=[b_xt[i]])
        if final_norm:
            pool.op(lambda e: e.memset(st[:, 0:1], 0.0), writes=[b_st])
            act.op(lambda e, i=i: e.activation(out=junk, in_=xt[i], func=AF.Square, accum_out=st[:, 0:1]), reads=[b_xt[i], b_st], writes=[b_junk, b_st])
            rstd_op(st[:, 1:2], st[:, 0:1], 1.0 / D, [b_st])
            dve.op(lambda e, i=i: e.scalar_tensor_tensor(out=yo[i], in0=xt[i], scalar=st[:, 1:2], in1=gf, op0=ALU.mult, op1=ALU.mult),
                   reads=[b_xt[i], b_st, b_gf], writes=[b_yo[i]])
        else:
            dve.op(lambda e, i=i: e.tensor_copy(out=yo[i], in_=xt[i]), reads=[b_xt[i]], writes=[b_yo[i]])
        sp.dma(lambda e, t=t, i=i: e.dma_start(out=out_d[t * 128:(t + 1) * 128, :], in_=yo[i]), b_yo[i].sem, reads=[b_yo[i]])
    P.finish()
    return nc


def _rope_table():
    rows = SEQ // 64
    row = np.repeat(np.arange(rows), 64).astype(np.float32)
    col = np.tile(np.arange(64), rows).astype(np.float32)
    n_freq = 16
    freqs = (np.float32(10000.0) ** (-np.arange(n_freq, dtype=np.float32) / np.float32(n_freq))).astype(np.float32)
    ang_r = (row[:, None] * freqs).astype(np.float32)
    ang_c = (col[:, None] * freqs).astype(np.float32)
    ang = np.concatenate([ang_r, ang_r, ang_c, ang_c], axis=-1)
    cos = np.cos(ang).astype(np.float32); sin = np.sin(ang).astype(np.float32)
    tab = np.zeros((T, 128), np.float32)
    tab[:SEQ, 0:64] = cos
    sgn = np.ones((2, 2, 16), np.float32); sgn[:, 0, :] = -1.0
    tab[:SEQ, 64:128] = sin * sgn.reshape(64)
    tab[SEQ:, 0:64] = 1.0
    return tab


def prep_shared(inp):
    f = lambda a: np.ascontiguousarray(np.asarray(a, dtype=np.float32))
    sh = {
        "rope": _rope_table(), "ident": np.eye(128, dtype=np.float32),
        "w_ada": f(inp["w_ada"]), "b_ada": f(inp["b_ada"]),
        "b_ada_col": f(np.asarray(inp["b_ada"]).reshape(DEPTH, 48, 128).transpose(0, 2, 1)),
        "n1col": f(np.asarray(inp["norm1_g"]).reshape(DEPTH, 8, 128).transpose(0, 2, 1)),
        "n2col": f(np.asarray(inp["norm2_g"]).reshape(DEPTH, 8, 128).transpose(0, 2, 1)),
        "w_in": f(inp["w_in"]), "w_out": f(inp["w_out"]),
        "lamv": f(np.concatenate([np.asarray(inp["lambda_q1"]), np.asarray(inp["lambda_k1"]), np.asarray(inp["lambda_q2"]), np.asarray(inp["lambda_k2"])], axis=1)),
        "subln_g": f(inp["subln_g"]), "sgu_norm_g": f(inp["sgu_norm_g"]), "sgu_w": f(inp["sgu_w"]),
        "sgu_bT": f(np.asarray(inp["sgu_b"]).transpose(0, 2, 1)),
        "w_router": f(inp["w_router"]), "w_gate": f(inp["w_gate"]), "w_up": f(inp["w_up"]), "w_down": f(inp["w_down"]),
        "norm_f_g": f(np.asarray(inp["norm_f_g"]).reshape(1, D)),
    }
    return sh


def prep_core(inp, b):
    x = np.asarray(inp["x"], dtype=np.float32)[b]; cx = np.asarray(inp["ctx"], dtype=np.float32)[b]
    c = np.asarray(inp["c"], dtype=np.float32)[b]; cctx = np.asarray(inp["c_ctx"], dtype=np.float32)
    cc = np.stack([c.reshape(8, 128).T, cctx.reshape(8, 128).T], axis=-1).reshape(128, 16)
    return {"x": np.ascontiguousarray(np.concatenate([x, cx], axis=0)), "cc": np.ascontiguousarray(cc)}


def kernel(**inputs):
    sh = prep_shared(inputs)
    nc = build()
    in_maps = []
    for b in range(8):
        m = dict(sh); m.update(prep_core(inputs, b)); in_maps.append(m)
    res = run_bass_kernel_spmd(nc, in_maps, core_ids=list(range(8)))
    return np.stack([np.asarray(r["out"], dtype=np.float32) for r in res.results], axis=0)
```
